# Optimizing a Trainium2 kernel written in Bass

```python
import math
import jax
import jax.numpy as jnp
from jax import lax
import numpy as np

D_MODEL = 1024
BATCH = 2
SEQ = 8192
DEPTH = 2

N_EVEN = (DEPTH + 1) // 2
N_ODD = DEPTH // 2

HEAD_DIM = 64
NEG = -1e30
BIG = 1e30
NORM_EPS = 1e-6

N_BUCKETS = 32
MAX_DISTANCE = 1024
N_BIAS_HEADS = 8

NSA_HEADS = 8
NSA_KV_HEADS = 2
NSA_GROUP = NSA_HEADS // NSA_KV_HEADS
CMP_LEN = 32
CMP_STRIDE = 16
CMP_HIDDEN = 256
SLC_BLOCK = 64
SLC_TOPN = 16
NSA_WINDOW = 512
NSA_QBLOCK = 128

MOBA_HEADS = 8
MOBA_BLOCK = 256
MOBA_TOPK = 3
MOBA_QBLOCK = 32

MLA_HEADS = 4
MLA_Q_RANK = 256
MLA_KV_RANK = 128
MLA_NOPE = 128
MLA_ROPE = 64
MLA_V = 128
ROPE_THETA = 10000.0
DENSE_QBLOCK = 128

SWA_HEADS = 8
SWA_KV_HEADS = 2
SWA_GROUP = SWA_HEADS // SWA_KV_HEADS
SWA_WINDOW = 128
SWA_QBLOCK = 128

N_EXPERTS = 16
N_GROUPS = 4
EXPERTS_PER_GROUP = N_EXPERTS // N_GROUPS
TOP_K = 2
D_EXPERT = 512
MOE_BLOCK = 256

ADA_INIT = 0.5

EVEN_WIDTHS = (NSA_HEADS * HEAD_DIM,) + (NSA_KV_HEADS * HEAD_DIM,) * 6 + (3 * NSA_HEADS,) + (MOBA_HEADS * HEAD_DIM,) * 3
EVEN_IN = sum(EVEN_WIDTHS)
EVEN_OUT = NSA_HEADS * HEAD_DIM + MOBA_HEADS * HEAD_DIM
ODD_WIDTHS = (MLA_Q_RANK, MLA_KV_RANK, MLA_ROPE, SWA_HEADS * HEAD_DIM, SWA_KV_HEADS * HEAD_DIM, SWA_KV_HEADS * HEAD_DIM)
ODD_IN = sum(ODD_WIDTHS)
ODD_OUT = MLA_HEADS * MLA_V + SWA_HEADS * HEAD_DIM

kernel_name = "hybrid_nsa_moba_mla_swa_grouped_moe"


def rmsnorm(x, g):
    xf = x.astype(jnp.float32)
    y = xf * lax.rsqrt(jnp.mean(xf * xf, axis=-1, keepdims=True) + NORM_EPS)
    return (y * g.astype(jnp.float32)).astype(x.dtype)


def split_cols(x, widths):
    idx, acc = [], 0
    for w in widths[:-1]:
        acc += w
        idx.append(acc)
    return jnp.split(x, idx, axis=-1)


def masked_softmax(s, mask):
    s = jnp.where(mask, s.astype(jnp.float32), NEG)
    p = jax.nn.softmax(s, axis=-1)
    return jnp.where(mask, p, 0.0)


def rel_bucket(dist):
    exact = N_BUCKETS // 2
    d = jnp.maximum(dist, 0)
    log_part = exact + (jnp.log(jnp.maximum(d, 1).astype(jnp.float32) / exact)
                        / math.log(MAX_DISTANCE / exact) * (N_BUCKETS - exact)).astype(jnp.int32)
    return jnp.where(d < exact, d, jnp.minimum(log_part, N_BUCKETS - 1))


def shared_bias(rel_table, dist, n_kv):
    b = jnp.moveaxis(rel_table[rel_bucket(dist)], -1, 0).astype(jnp.float32)
    return b.reshape(n_kv, -1, *dist.shape)


def gathered_bias(rel_table, dist, n_kv):
    table = rel_table.reshape(N_BUCKETS, n_kv, -1)
    kv = jnp.arange(n_kv)[None, :, None, None]
    return jnp.moveaxis(table[rel_bucket(dist), kv], -1, 2).astype(jnp.float32)


def rope_tables(S, dim):
    inv = ROPE_THETA ** (-jnp.arange(0, dim, 2, dtype=jnp.float32) / dim)
    ang = jnp.arange(S, dtype=jnp.float32)[:, None] * inv[None, :]
    return jnp.cos(ang), jnp.sin(ang)


def apply_rope(x, cos, sin):
    half = x.shape[-1] // 2
    x1 = x[..., :half].astype(jnp.float32)
    x2 = x[..., half:].astype(jnp.float32)
    return jnp.concatenate([x1 * cos - x2 * sin, x1 * sin + x2 * cos], axis=-1).astype(x.dtype)


def adaln(c, w, b):
    m = jax.nn.silu(c) @ w + b
    return jnp.split(m[:, None, :], 6, axis=-1)


def nsa_attention(q, kc, vc, ks, vs, kw, vw, gates, pos_k, pos_v, ck_w1, ck_w2, cv_w1, cv_w2, rel_table):
    B, S, _ = q.shape
    Hk, G, dh, QB = NSA_KV_HEADS, NSA_GROUP, HEAD_DIM, NSA_QBLOCK
    scale = dh ** -0.5
    q = q.reshape(B, S, Hk, G, dh).transpose(0, 2, 3, 1, 4)
    to_kv = lambda t: t.reshape(B, S, Hk, dh).transpose(0, 2, 1, 3)
    kc, vc, ks, vs, kw, vw = (to_kv(t) for t in (kc, vc, ks, vs, kw, vw))
    g = jax.nn.sigmoid(gates.astype(jnp.float32)).reshape(B, S, Hk, G, 3).transpose(0, 2, 3, 1, 4)

    n_cmp = (S - CMP_LEN) // CMP_STRIDE + 1
    cmp_start = np.arange(n_cmp, dtype=np.int32) * CMP_STRIDE
    win_idx = cmp_start[:, None] + np.arange(CMP_LEN, dtype=np.int32)[None, :]
    cmp_end = cmp_start + CMP_LEN - 1

    def compress(t, pos, w1, w2):
        blocks = t[:, :, win_idx, :] + pos
        flat = blocks.reshape(B, Hk, n_cmp, CMP_LEN * dh)
        return jax.nn.gelu(flat @ w1) @ w2

    k_cmp = compress(kc, pos_k, ck_w1, ck_w2)
    v_cmp = compress(vc, pos_v, cv_w1, cv_w2)

    n_slc = S // SLC_BLOCK
    slc_lo = np.arange(n_slc, dtype=np.int32) * SLC_BLOCK
    overlap = ((cmp_start[:, None] <= slc_lo[None, :] + SLC_BLOCK - 1)
               & (cmp_end[:, None] >= slc_lo[None, :])).astype(np.float32)
    n_sel = min(SLC_TOPN, n_slc)
    ks_blocks = ks.reshape(B, Hk, n_slc, SLC_BLOCK, dh)
    vs_blocks = vs.reshape(B, Hk, n_slc, SLC_BLOCK, dh)

    pad = ((0, 0), (0, 0), (NSA_WINDOW, 0), (0, 0))
    kw_pad, vw_pad = jnp.pad(kw, pad), jnp.pad(vw, pad)

    nq = S // QB
    q_blocks = jnp.moveaxis(q.reshape(B, Hk, G, nq, QB, dh), 3, 0)
    g_blocks = jnp.moveaxis(g.reshape(B, Hk, G, nq, QB, 3), 3, 0)
    b_idx = jnp.arange(B)[:, None, None, None]
    kv_idx = jnp.arange(Hk)[None, :, None, None]

    def block(args):
        qb, gb, i = args
        start = i * QB
        t = start + jnp.arange(QB)
        s_c = jnp.einsum('bkgqd,bknd->bkgqn', qb, k_cmp).astype(jnp.float32) * scale \
            + shared_bias(rel_table, t[:, None] - cmp_end[None, :], Hk)
        p_c = masked_softmax(s_c, cmp_end[None, :] <= t[:, None])
        o_c = jnp.einsum('bkgqn,bknd->bkgqd', p_c.astype(v_cmp.dtype), v_cmp)
        imp = jnp.einsum('bkgqn,nm->bkqm', p_c, overlap)
        blk_t = (t // SLC_BLOCK)[:, None]
        j = jnp.arange(n_slc)[None, :]
        forced = (j == 0) | (j == blk_t) | (j == blk_t - 1)
        score = jnp.where(forced, BIG, jnp.where(j <= blk_t, imp, NEG))
        sel = lax.top_k(score, n_sel)[1]
        k_sel = ks_blocks[b_idx, kv_idx, sel].reshape(B, Hk, QB, n_sel * SLC_BLOCK, dh)
        v_sel = vs_blocks[b_idx, kv_idx, sel].reshape(B, Hk, QB, n_sel * SLC_BLOCK, dh)
        pos_sel = (sel[..., None] * SLC_BLOCK + jnp.arange(SLC_BLOCK)).reshape(B, Hk, QB, -1)
        dist_s = t[:, None] - pos_sel
        s_s = jnp.einsum('bkgqd,bkqsd->bkgqs', qb, k_sel).astype(jnp.float32) * scale \
            + gathered_bias(rel_table, dist_s, Hk)
        p_s = masked_softmax(s_s, (dist_s >= 0)[:, :, None])
        o_s = jnp.einsum('bkgqs,bkqsd->bkgqd', p_s.astype(v_sel.dtype), v_sel)
        k_w = lax.dynamic_slice_in_dim(kw_pad, start, QB + NSA_WINDOW, axis=2)
        v_w = lax.dynamic_slice_in_dim(vw_pad, start, QB + NSA_WINDOW, axis=2)
        dist_w = t[:, None] - (start - NSA_WINDOW + jnp.arange(QB + NSA_WINDOW))[None, :]
        s_w = jnp.einsum('bkgqd,bksd->bkgqs', qb, k_w).astype(jnp.float32) * scale \
            + shared_bias(rel_table, dist_w, Hk)
        p_w = masked_softmax(s_w, (dist_w >= 0) & (dist_w < NSA_WINDOW))
        o_w = jnp.einsum('bkgqs,bksd->bkgqd', p_w.astype(v_w.dtype), v_w)
        return (gb[..., 0:1] * o_c + gb[..., 1:2] * o_s + gb[..., 2:3] * o_w).astype(qb.dtype)

    o = lax.map(block, (q_blocks, g_blocks, jnp.arange(nq)))
    return o.transpose(1, 0, 4, 2, 3, 5).reshape(B, S, NSA_HEADS * dh)


def moba_attention(q, k, v, rel_table):
    B, S, _ = q.shape
    H, dh, QB = MOBA_HEADS, HEAD_DIM, MOBA_QBLOCK
    scale = dh ** -0.5
    q, k, v = (t.reshape(B, S, H, dh).transpose(0, 2, 1, 3) for t in (q, k, v))
    n_blk = -(-S // MOBA_BLOCK)
    pad = ((0, 0), (0, 0), (0, n_blk * MOBA_BLOCK - S), (0, 0))
    k_pad, v_pad = jnp.pad(k, pad), jnp.pad(v, pad)
    k_blocks = k_pad.reshape(B, H, n_blk, MOBA_BLOCK, dh)
    v_blocks = v_pad.reshape(B, H, n_blk, MOBA_BLOCK, dh)
    k_mean = jnp.mean(k_blocks.astype(jnp.float32), axis=3)
    n_top = min(MOBA_TOPK, n_blk)
    n_s = n_top * MOBA_BLOCK
    nq = S // QB
    q_blocks = jnp.moveaxis(q.reshape(B, H, nq, QB, dh), 2, 0)
    b_idx = jnp.arange(B)[:, None, None, None]
    h_idx = jnp.arange(H)[None, :, None, None]

    def block(args):
        qb, i = args
        start = i * QB
        t = start + jnp.arange(QB)
        own = start // MOBA_BLOCK
        gate = jnp.einsum('bhqd,bhnd->bhqn', qb.astype(jnp.float32), k_mean)
        gate = jnp.where(jnp.arange(n_blk) < own, gate, NEG)
        sel = lax.top_k(gate, n_top)[1]
        k_sel = k_blocks[b_idx, h_idx, sel].reshape(B, H, QB, n_s, dh)
        v_sel = v_blocks[b_idx, h_idx, sel].reshape(B, H, QB, n_s, dh)
        pos_sel = (sel[..., None] * MOBA_BLOCK + jnp.arange(MOBA_BLOCK)).reshape(B, H, QB, n_s)
        mask_sel = pos_sel < own * MOBA_BLOCK
        k_own = lax.dynamic_slice_in_dim(k_pad, own * MOBA_BLOCK, MOBA_BLOCK, axis=2)
        v_own = lax.dynamic_slice_in_dim(v_pad, own * MOBA_BLOCK, MOBA_BLOCK, axis=2)
        dist_own = t[:, None] - (own * MOBA_BLOCK + jnp.arange(MOBA_BLOCK))[None, :]
        s_sel = jnp.einsum('bhqd,bhqsd->bhqs', qb, k_sel).astype(jnp.float32) * scale \
            + gathered_bias(rel_table, t[:, None] - pos_sel, H)[:, :, 0]
        s_own = jnp.einsum('bhqd,bhsd->bhqs', qb, k_own).astype(jnp.float32) * scale \
            + shared_bias(rel_table, dist_own, H)[:, 0]
        s = jnp.concatenate([s_sel, s_own], axis=-1)
        mask = jnp.concatenate([mask_sel, jnp.broadcast_to(dist_own >= 0, s_own.shape)], axis=-1)
        p = masked_softmax(s, mask).astype(v.dtype)
        return jnp.einsum('bhqs,bhqsd->bhqd', p[..., :n_s], v_sel) \
            + jnp.einsum('bhqs,bhsd->bhqd', p[..., n_s:], v_own)

    o = lax.map(block, (q_blocks, jnp.arange(nq)))
    return o.transpose(1, 0, 3, 2, 4).reshape(B, S, H * dh)


def mla_attention(c_q, c_kv, k_rope, q_norm, kv_norm, w_q_up, w_kv_up):
    B, S, _ = c_q.shape
    H, QB = MLA_HEADS, DENSE_QBLOCK
    q = (rmsnorm(c_q, q_norm) @ w_q_up).reshape(B, S, H, MLA_NOPE + MLA_ROPE)
    kv = (rmsnorm(c_kv, kv_norm) @ w_kv_up).reshape(B, S, H, MLA_NOPE + MLA_V)
    cos, sin = rope_tables(S, MLA_ROPE)
    q_nope = q[..., :MLA_NOPE]
    q_rope = apply_rope(q[..., MLA_NOPE:], cos[:, None, :], sin[:, None, :])
    k_rope = apply_rope(k_rope, cos, sin)
    k_nope = kv[..., :MLA_NOPE].transpose(0, 2, 1, 3)
    v = kv[..., MLA_NOPE:].transpose(0, 2, 1, 3)
    scale = (MLA_NOPE + MLA_ROPE) ** -0.5
    nq = S // QB
    qn_b = q_nope.reshape(B, nq, QB, H, MLA_NOPE).transpose(1, 0, 3, 2, 4)
    qr_b = q_rope.reshape(B, nq, QB, H, MLA_ROPE).transpose(1, 0, 3, 2, 4)
    kpos = jnp.arange(S)

    def block(args):
        qn, qr, i = args
        t = i * QB + jnp.arange(QB)
        s = (jnp.einsum('bhqd,bhkd->bhqk', qn, k_nope)
             + jnp.einsum('bhqd,bkd->bhqk', qr, k_rope)).astype(jnp.float32) * scale
        s = jnp.where(kpos[None, :] <= t[:, None], s, NEG)
        p = jax.nn.softmax(s, axis=-1).astype(v.dtype)
        return jnp.einsum('bhqk,bhkd->bhqd', p, v)

    o = lax.map(block, (qn_b, qr_b, jnp.arange(nq)))
    return o.transpose(1, 0, 3, 2, 4).reshape(B, S, H * MLA_V)


def swa_sink_attention(q, k, v, sinks, rel_table):
    B, S, _ = q.shape
    Hk, G, dh, QB, W = SWA_KV_HEADS, SWA_GROUP, HEAD_DIM, SWA_QBLOCK, SWA_WINDOW
    scale = dh ** -0.5
    q = q.reshape(B, S, Hk, G, dh).transpose(0, 2, 3, 1, 4)
    k = k.reshape(B, S, Hk, dh).transpose(0, 2, 1, 3)
    v = v.reshape(B, S, Hk, dh).transpose(0, 2, 1, 3)
    pad = ((0, 0), (0, 0), (W, 0), (0, 0))
    k_pad, v_pad = jnp.pad(k, pad), jnp.pad(v, pad)
    nq = S // QB
    q_blocks = jnp.moveaxis(q.reshape(B, Hk, G, nq, QB, dh), 3, 0)
    sink = sinks.astype(jnp.float32).reshape(Hk, G, 1, 1)

    def block(args):
        qb, i = args
        start = i * QB
        t = start + jnp.arange(QB)
        k_w = lax.dynamic_slice_in_dim(k_pad, start, QB + W, axis=2)
        v_w = lax.dynamic_slice_in_dim(v_pad, start, QB + W, axis=2)
        dist = t[:, None] - (start - W + jnp.arange(QB + W))[None, :]
        mask = (dist >= 0) & (dist < W)
        s = jnp.einsum('bkgqd,bksd->bkgqs', qb, k_w).astype(jnp.float32) * scale \
            + shared_bias(rel_table, dist, Hk)
        s = jnp.where(mask, s, NEG)
        m = jnp.maximum(jnp.max(s, axis=-1, keepdims=True), sink)
        e = jnp.exp(s - m)
        p = e / (jnp.sum(e, axis=-1, keepdims=True) + jnp.exp(sink - m))
        return jnp.einsum('bkgqs,bksd->bkgqd', p.astype(v_w.dtype), v_w)

    o = lax.map(block, (q_blocks, jnp.arange(nq)))
    return o.transpose(1, 0, 4, 2, 3, 5).reshape(B, S, SWA_HEADS * dh)


def even_mixer(h, w_in, w_out, pos_k, pos_v, ck_w1, ck_w2, cv_w1, cv_w2, rel_table):
    q_a, kc, vc, ks, vs, kw, vw, gates, q_b, k_b, v_b = split_cols(h @ w_in, EVEN_WIDTHS)
    o_a = nsa_attention(q_a, kc, vc, ks, vs, kw, vw, gates, pos_k, pos_v, ck_w1, ck_w2, cv_w1, cv_w2, rel_table)
    o_b = moba_attention(q_b, k_b, v_b, rel_table)
    return jnp.concatenate([o_a, o_b.astype(o_a.dtype)], axis=-1) @ w_out


def odd_mixer(h, w_in, w_out, q_norm, kv_norm, w_q_up, w_kv_up, sinks, rel_table):
    c_q, c_kv, k_rope, q_d, k_d, v_d = split_cols(h @ w_in, ODD_WIDTHS)
    o_c = mla_attention(c_q, c_kv, k_rope, q_norm, kv_norm, w_q_up, w_kv_up)
    o_d = swa_sink_attention(q_d, k_d, v_d, sinks, rel_table)
    return jnp.concatenate([o_c, o_d.astype(o_c.dtype)], axis=-1) @ w_out


def grouped_moe(h, router_w, router_b, w_gate, w_up, w_down):
    B, S, D = h.shape
    T = B * S
    E = N_EXPERTS
    xf = h.reshape(T, D)
    aff = jax.nn.sigmoid((xf @ router_w).astype(jnp.float32))
    biased = (aff + router_b.astype(jnp.float32)).reshape(T, N_GROUPS, EXPERTS_PER_GROUP)
    grp_score = jnp.sum(lax.top_k(biased, TOP_K)[0], axis=-1)
    g_sel = jnp.argmax(grp_score, axis=-1)
    tok = jnp.arange(T)
    local = lax.top_k(biased[tok, g_sel], TOP_K)[1]
    expert = g_sel[:, None] * EXPERTS_PER_GROUP + local
    w = aff[tok[:, None], expert]
    w = w / jnp.sum(w, axis=-1, keepdims=True)

    A = T * TOP_K
    flat_e = expert.reshape(A)
    flat_tok = jnp.repeat(tok, TOP_K)
    flat_w = w.reshape(A)
    order = jnp.argsort(flat_e)
    se, stok, sw = flat_e[order], flat_tok[order], flat_w[order]
    counts = jnp.zeros((E,), jnp.int32).at[flat_e].add(1)
    padded = (counts + MOE_BLOCK - 1) // MOE_BLOCK * MOE_BLOCK
    start = jnp.cumsum(counts) - counts
    pend = jnp.cumsum(padded)
    pstart = pend - padded
    dest = pstart[se] + jnp.arange(A) - start[se]
    n_blocks = (A + E * (MOE_BLOCK - 1) + MOE_BLOCK - 1) // MOE_BLOCK
    P = n_blocks * MOE_BLOCK
    tok_buf = jnp.zeros((P,), jnp.int32).at[dest].set(stok)
    w_buf = jnp.zeros((P,), jnp.float32).at[dest].set(sw)
    blk_expert = jnp.minimum(jnp.searchsorted(pend, jnp.arange(n_blocks) * MOE_BLOCK, side='right'), E - 1)
    xs = xf[tok_buf].reshape(n_blocks, MOE_BLOCK, D)

    def expert_block(args):
        xb, e = args
        hid = jax.nn.silu(xb @ w_gate[e]) * (xb @ w_up[e])
        return hid @ w_down[e]

    ys = lax.map(expert_block, (xs, blk_expert)).reshape(P, D)
    out = jnp.zeros((T, D), ys.dtype).at[tok_buf].add(ys * w_buf[:, None].astype(ys.dtype))
    return out.reshape(B, S, D).astype(h.dtype)


def setup_inputs(seed: int = 0) -> dict:
    key = jax.random.key(seed)
    ks = iter(jax.random.split(key, 40))
    D = D_MODEL

    def nrm(shape, std):
        return jax.random.normal(next(ks), shape, jnp.float32) * std

    def gain(shape):
        return 1.0 + nrm(shape, 0.02)

    return {
        "x": nrm((BATCH, SEQ, D), 1.0),
        "c": nrm((BATCH, D), 1.0),
        "rel_table": nrm((N_BUCKETS, N_BIAS_HEADS), 0.5),
        "router_w": nrm((D, N_EXPERTS), D ** -0.5),
        "router_b": nrm((N_EXPERTS,), 0.01),
        "final_norm": gain((D,)),
        "norm_mix": gain((DEPTH, D)),
        "norm_ffn": gain((DEPTH, D)),
        "ada_w": nrm((DEPTH, D, 6 * D), ADA_INIT * D ** -0.5),
        "ada_b": nrm((DEPTH, 6 * D), 0.02),
        "moe_w_gate": nrm((DEPTH, N_EXPERTS, D, D_EXPERT), D ** -0.5),
        "moe_w_up": nrm((DEPTH, N_EXPERTS, D, D_EXPERT), D ** -0.5),
        "moe_w_down": nrm((DEPTH, N_EXPERTS, D_EXPERT, D), D_EXPERT ** -0.5),
        "ev_w_in": nrm((N_EVEN, D, EVEN_IN), D ** -0.5),
        "ev_w_out": nrm((N_EVEN, EVEN_OUT, D), EVEN_OUT ** -0.5),
        "nsa_pos_k": nrm((N_EVEN, CMP_LEN, HEAD_DIM), 0.1),
        "nsa_pos_v": nrm((N_EVEN, CMP_LEN, HEAD_DIM), 0.1),
        "nsa_ck_w1": nrm((N_EVEN, CMP_LEN * HEAD_DIM, CMP_HIDDEN), (CMP_LEN * HEAD_DIM) ** -0.5),
        "nsa_ck_w2": nrm((N_EVEN, CMP_HIDDEN, HEAD_DIM), CMP_HIDDEN ** -0.5),
        "nsa_cv_w1": nrm((N_EVEN, CMP_LEN * HEAD_DIM, CMP_HIDDEN), (CMP_LEN * HEAD_DIM) ** -0.5),
        "nsa_cv_w2": nrm((N_EVEN, CMP_HIDDEN, HEAD_DIM), CMP_HIDDEN ** -0.5),
        "od_w_in": nrm((N_ODD, D, ODD_IN), D ** -0.5),
        "od_w_out": nrm((N_ODD, ODD_OUT, D), ODD_OUT ** -0.5),
        "mla_q_norm": gain((N_ODD, MLA_Q_RANK)),
        "mla_kv_norm": gain((N_ODD, MLA_KV_RANK)),
        "mla_w_q_up": nrm((N_ODD, MLA_Q_RANK, MLA_HEADS * (MLA_NOPE + MLA_ROPE)), MLA_Q_RANK ** -0.5),
        "mla_w_kv_up": nrm((N_ODD, MLA_KV_RANK, MLA_HEADS * (MLA_NOPE + MLA_V)), MLA_KV_RANK ** -0.5),
        "swa_sinks": nrm((N_ODD, SWA_HEADS), 1.0),
    }


def reference(x, c, rel_table, router_w, router_b, final_norm, norm_mix, norm_ffn, ada_w, ada_b,
              moe_w_gate, moe_w_up, moe_w_down, ev_w_in, ev_w_out, nsa_pos_k, nsa_pos_v,
              nsa_ck_w1, nsa_ck_w2, nsa_cv_w1, nsa_cv_w2, od_w_in, od_w_out, mla_q_norm,
              mla_kv_norm, mla_w_q_up, mla_w_kv_up, swa_sinks):
    for layer in range(DEPTH):
        shift_m, scale_m, gate_m, shift_f, scale_f, gate_f = adaln(c, ada_w[layer], ada_b[layer])
        h = rmsnorm(x, norm_mix[layer]) * (1.0 + scale_m) + shift_m
        i = layer // 2
        if layer % 2 == 0:
            mix = even_mixer(h, ev_w_in[i], ev_w_out[i], nsa_pos_k[i], nsa_pos_v[i], nsa_ck_w1[i],
                             nsa_ck_w2[i], nsa_cv_w1[i], nsa_cv_w2[i], rel_table)
        else:
            mix = odd_mixer(h, od_w_in[i], od_w_out[i], mla_q_norm[i], mla_kv_norm[i],
                            mla_w_q_up[i], mla_w_kv_up[i], swa_sinks[i], rel_table)
        x = x + gate_m * mix.astype(x.dtype)
        h = rmsnorm(x, norm_ffn[layer]) * (1.0 + scale_f) + shift_f
        x = x + gate_f * grouped_moe(h, router_w, router_b, moe_w_gate[layer], moe_w_up[layer],
                                     moe_w_down[layer]).astype(x.dtype)
    return rmsnorm(x, final_norm)
```

```python
import numpy as np
import concourse.bass as bass
import concourse.mybir as mybir

F32 = mybir.dt.float32
BF16 = mybir.dt.bfloat16
I32 = mybir.dt.int32
U32 = mybir.dt.uint32
AF = mybir.ActivationFunctionType
ALU = mybir.AluOpType
AX = mybir.AxisListType


class Buf:
    __slots__ = ("name", "w", "r")

    def __init__(self, name=""):
        self.name = name
        self.w = None
        self.r = {}


class Prog:
    COMPUTE = ("pe", "act", "dve", "pool")
    DMAQ = ("sp", "pool")

    def __init__(self, nc, n_dma_sems=24, same_engine_sync=True):
        self.nc = nc
        self.q = {e: [] for e in ("pe", "act", "dve", "pool", "sp")}
        self.eng_obj = {"pe": nc.tensor, "act": nc.scalar, "dve": nc.vector,
                        "pool": nc.gpsimd, "sp": nc.sync}
        self.sems = {}
        self.cnt = {}
        self.seen = {e: {} for e in self.q}
        self.same_engine_sync = same_engine_sync
        self._ctx = []
        for e in self.COMPUTE:
            self.sems[e] = self._sem("s_" + e)
            self.cnt[e] = 0
        self.dma_pool = {}
        for e in ("sp", "pool"):
            self.dma_pool[e] = [[self._sem(f"d_{e}{i}"), 0, None] for i in range(n_dma_sems)]
        self.dma_rr = {"sp": 0, "pool": 0}
        self.pending_noinc = {e: False for e in self.COMPUTE}

    def _sem(self, name):
        g = self.nc.semaphore(name)
        s = g.__enter__()
        self._ctx.append(g)
        return s

    def sbuf(self, name, shape, dt):
        g = self.nc.sbuf_tensor("sb_" + name, list(shape), dt)
        t = g.__enter__()
        self._ctx.append(g)
        return t

    def psum(self, name, shape, dt):
        g = self.nc.psum_tensor("ps_" + name, list(shape), dt)
        t = g.__enter__()
        self._ctx.append(g)
        return t

    def _collect(self, eng, reads, writes):
        deps = {}

        def add(tok):
            if tok is None:
                return
            s, v, owner = tok
            k = id(s)
            if k not in deps or deps[k][1] < v:
                deps[k] = (s, v, owner)

        for b in reads:
            add(b.w)
        for b in writes:
            add(b.w)
            for t in b.r.values():
                add(t)
        waits = []
        for k, (s, v, owner) in deps.items():
            if owner == eng and owner in self.COMPUTE:
                if eng == "pe" or not self.same_engine_sync:
                    continue
                if v > self.cnt[eng]:
                    continue
            if self.seen[eng].get(k, -1) >= v:
                continue
            self.seen[eng][k] = v
            waits.append((s, v))
        return waits

    def _mark(self, tok, reads, writes):
        k = id(tok[0])
        for b in reads:
            old = b.r.get(k)
            if old is None or old[1] < tok[1]:
                b.r[k] = tok
        for b in writes:
            b.w = tok
            b.r = {}

    def op(self, eng, fn, reads=(), writes=(), inc=True):
        assert eng in self.COMPUTE
        waits = self._collect(eng, reads, writes)
        if inc:
            self.cnt[eng] += 1
            tok = (self.sems[eng], self.cnt[eng], eng)
            self.pending_noinc[eng] = False
        else:
            tok = (self.sems[eng], self.cnt[eng] + 1, eng)
            self.pending_noinc[eng] = True
        self.q[eng].append((waits, fn, (self.sems[eng], 1) if inc else None))
        self._mark(tok, reads, writes)
        return tok

    def dma(self, eng, fn, reads=(), writes=()):
        pool = self.dma_pool[eng]
        i = self.dma_rr[eng]
        self.dma_rr[eng] = (i + 1) % len(pool)
        ent = pool[i]
        waits = self._collect(eng, reads, writes)
        if ent[2] is not None:
            s, v, _ = ent[2]
            k = id(s)
            if self.seen[eng].get(k, -1) < v:
                self.seen[eng][k] = v
                waits.append((s, v))
        ent[1] += 16
        tok = (ent[0], ent[1], "dma_" + eng)
        ent[2] = tok
        self.q[eng].append((waits, fn, (ent[0], 16)))
        self._mark(tok, reads, writes)
        return tok

    def wait_all(self, eng, toks):
        waits = []
        for tok in toks:
            s, v, _ = tok
            waits.append((s, v))
        self.q[eng].append((waits, None, None))

    def finish(self):
        nc = self.nc
        for e in self.COMPUTE:
            assert not self.pending_noinc[e], f"engine {e} ends with non-inc instruction"
        with nc.Block() as block:
            def run(engname):
                def body(e):
                    for waits, fn, inc in self.q[engname]:
                        for s, v in waits:
                            e.wait_ge(s, v)
                        if fn is not None:
                            ins = fn(e)
                            if inc is not None:
                                ins.then_inc(inc[0], inc[1])
                return body
            if self.q["sp"]:
                block.sync(run("sp"))
            if self.q["pe"]:
                block.tensor(run("pe"))
            if self.q["act"]:
                block.scalar(run("act"))
            if self.q["dve"]:
                block.vector(run("dve"))
            if self.q["pool"]:
                block.gpsimd(run("pool"))
        for g in reversed(self._ctx):
            g.__exit__(None, None, None)
        self._ctx = []


import numpy as np
import ml_dtypes

NPBF = ml_dtypes.bfloat16
D = 1024
S = 8192
NEGM = -30000.0


class RR:
    def __init__(self, engs=("act", "dve")):
        self.engs = engs
        self.i = 0

    def next(self):
        e = self.engs[self.i % len(self.engs)]
        self.i += 1
        return e


def evac(P, eng, out, in_, reads, writes):
    if eng == "act":
        return P.op("act", lambda e: e.copy(out=out, in_=in_), reads=reads, writes=writes)
    return P.op(eng, lambda e: e.tensor_copy(out=out, in_=in_), reads=reads, writes=writes)


class Consts:
    def __init__(self, P, nc, ident_ap):
        self.idf = P.sbuf("c_idf", [128, 128], F32)
        self.idb = P.sbuf("c_idb", [128, 128], BF16)
        self.b_idf = Buf("idf")
        self.b_idb = Buf("idb")
        P.dma("sp", lambda e: e.dma_start(out=self.idf[:], in_=ident_ap), writes=[self.b_idf])
        P.op("dve", lambda e: e.tensor_copy(out=self.idb[:], in_=self.idf[:]),
             reads=[self.b_idf], writes=[self.b_idb])
        self.antib = P.sbuf("c_antib", [128, 128], BF16)
        self.b_antib = Buf("antib")
        P.op("pool", lambda e: e.memset(self.antib[:], 0.0), writes=[self.b_antib])
        P.op("pool", lambda e: e.affine_select(out=self.antib[:], in_=self.antib[:], pattern=[[1, 128]],
                                               compare_op=ALU.not_equal, fill=1.0, base=-127, channel_multiplier=1),
             reads=[self.b_antib], writes=[self.b_antib])
        self.ones_f = P.sbuf("c_ones_f", [128, 128], F32)
        self.b_ones_f = Buf("ones_f")
        P.op("dve", lambda e: e.memset(self.ones_f[:], 1.0), writes=[self.b_ones_f])


def emit_adaln(P, nc, C, c_cols_ap, ada_w_ap, ada_b_ap, tag, psum_row, b_psum_row, psum_bc, b_psum_bc):
    GW = 256
    NG = 6144 // GW
    cc = P.sbuf(f"ada_c{tag}", [128, 8], F32); b_cc = Buf()
    sc = P.sbuf(f"ada_sc{tag}", [128, 8], F32); b_sc = Buf()
    row = [P.sbuf(f"ada_row{tag}{i}", [1, GW], F32) for i in range(2)]; b_row = [Buf(), Buf()]
    mod = P.sbuf(f"ada_mod{tag}", [128, 6, 1024], F32); b_mod = Buf()
    modf = mod[:].rearrange("p a n -> p (a n)")
    wst = [P.sbuf(f"ada_w{tag}_{i}", [128, 8, GW], F32) for i in range(2)]
    b_wst = [Buf(), Buf()]
    P.dma("sp", lambda e: e.dma_start(out=cc[:], in_=c_cols_ap), writes=[b_cc])
    P.dma("sp", lambda e: e.dma_start(out=modf, in_=ada_b_ap.to_broadcast([128, 6144])), writes=[b_mod])
    P.op("act", lambda e: e.activation(out=sc[:], in_=cc[:], func=AF.Silu), reads=[b_cc], writes=[b_sc])
    wv = ada_w_ap.rearrange("(k p) n -> p k n", p=128)
    for g in range(NG):
        w = wst[g % 2]; bw = b_wst[g % 2]
        r = row[g % 2]; br = b_row[g % 2]
        P.dma("sp", lambda e, w=w, g=g: e.dma_start(out=w[:], in_=wv[:, :, g * GW:(g + 1) * GW]), writes=[bw])
        for k in range(8):
            P.op("pe", lambda e, w=w, k=k: e.matmul(out=psum_row[0:1, 0:GW], lhsT=sc[:, k:k + 1], rhs=w[:, k, :],
                                                    start=(k == 0), stop=(k == 7)),
                 reads=[b_sc, bw], writes=[b_psum_row], inc=(k == 7))
        P.op("act", lambda e, r=r: e.copy(out=r[0:1, :], in_=psum_row[0:1, 0:GW]),
             reads=[b_psum_row], writes=[br])
        P.op("pe", lambda e, r=r: e.matmul(out=psum_bc[:, 0:GW], lhsT=C.ones_f[0:1, :], rhs=r[0:1, :],
                                           start=True, stop=True),
             reads=[C.b_ones_f, br], writes=[b_psum_bc])
        P.op("dve", lambda e, g=g: e.tensor_tensor(out=modf[:, g * GW:(g + 1) * GW], in0=psum_bc[:, 0:GW],
                                                   in1=modf[:, g * GW:(g + 1) * GW], op=ALU.add),
             reads=[b_psum_bc, b_mod], writes=[b_mod])
    return mod, b_mod


def emit_norm_tile(P, C, x_t, b_x, A, b_A, Bt, b_B, hT_out, b_hT, scratch, psum_tr, b_psum_tr, tag=""):
    sq, ss, rstd, h32, hb = scratch["sq"], scratch["ss"], scratch["rstd"], scratch["h32"], scratch["hb"]
    b_sq, b_ss, b_rstd, b_h32, b_hb = scratch["b"]
    P.op("act", lambda e: e.activation(out=sq[:], in_=x_t, func=AF.Square, accum_out=ss[:]),
         reads=[b_x], writes=[b_sq, b_ss])
    P.op("dve", lambda e: e.tensor_scalar(out=rstd[:], in0=ss[:], scalar1=1.0 / D, scalar2=1e-6,
                                          op0=ALU.mult, op1=ALU.add), reads=[b_ss], writes=[b_rstd])
    P.op("act", lambda e: e.activation(out=rstd[:], in_=rstd[:], func=AF.Sqrt), reads=[b_rstd], writes=[b_rstd])
    P.op("dve", lambda e: e.reciprocal(out=rstd[:], in_=rstd[:]), reads=[b_rstd], writes=[b_rstd])
    P.op("dve", lambda e: e.scalar_tensor_tensor(out=h32[:], in0=x_t, scalar=rstd[:, 0:1], in1=A,
                                                 op0=ALU.mult, op1=ALU.mult),
         reads=[b_x, b_rstd, b_A], writes=[b_h32])
    P.op("pool", lambda e: e.tensor_tensor(out=hb[:], in0=h32[:], in1=Bt, op=ALU.add),
         reads=[b_h32, b_B], writes=[b_hb])
    for k in range(8):
        P.op("pe", lambda e, k=k: e.transpose(out=psum_tr[:, k, :], in_=hb[:, k * 128:(k + 1) * 128],
                                              identity=C.idb[:]),
             reads=[b_hb, C.b_idb], writes=[b_psum_tr], inc=(k == 7))
    P.op("act", lambda e: e.copy(out=hT_out, in_=psum_tr[:]), reads=[b_psum_tr], writes=[b_hT])


EV = dict(q_a=(0, 512), kc=(512, 640), vc=(640, 768), ks=(768, 896), vs=(896, 1024), kw=(1024, 1152),
          vw=(1152, 1280), gates=(1280, 1304), q_b=(1304, 1816), k_b=(1816, 2328), v_b=(2328, 2840))


def host_w_in_even(w):
    sl = lambda n: w[:, EV[n][0]:EV[n][1]]
    units = []
    for nm in ("q_a", "q_b"):
        for h in range(8):
            u = np.zeros((1024, 128), np.float32)
            u[:, (h % 2) * 64:(h % 2 + 1) * 64] = sl(nm)[:, h * 64:(h + 1) * 64]
            units.append(u)
    for cc in range(4):
        units.append(sl("k_b")[:, cc * 128:(cc + 1) * 128])
    for nm in ("ks", "kw"):
        for kv in range(2):
            c = sl(nm)[:, kv * 64:(kv + 1) * 64]
            units.append(np.concatenate([c, c], axis=1))
    WF = np.concatenate(units, axis=1)
    WP = np.zeros((1024, 4, 2, 128), np.float32)
    for X, (nm, kv) in enumerate([("kc", 0), ("kc", 1), ("vc", 0), ("vc", 1)]):
        cols = sl(nm)[:, kv * 64:(kv + 1) * 64]
        WP[:, X, 0, 0:64] = cols
        WP[:, X, 1, 64:128] = cols
    WT = np.concatenate([sl("vs"), sl("vw"), sl("gates"), sl("v_b")], axis=1)
    return np.ascontiguousarray(WF), np.ascontiguousarray(WP.reshape(1024, 1024)), np.ascontiguousarray(WT)


NU0 = 24
def load_w_bf16(P, nc, name, ap2d, ncols, rows=1024):
    kc = rows // 128
    t = P.sbuf(name, [128, kc, ncols], BF16)
    b = Buf(name)
    v = ap2d.rearrange("(k p) n -> p k n", p=128)
    for k in range(kc):
        P.dma("pool", lambda e, k=k: e.dma_start(out=t[:, k, :], in_=v[:, k, :]), writes=[b])
    return t, b


def norm_scratch(P, tag):
    sc = dict(sq=P.sbuf(f"n_sq{tag}", [128, 1024], F32), ss=P.sbuf(f"n_ss{tag}", [128, 1], F32),
              rstd=P.sbuf(f"n_rstd{tag}", [128, 1], F32), h32=P.sbuf(f"n_h32{tag}", [128, 1024], F32),
              hb=P.sbuf(f"n_hb{tag}", [128, 1024], BF16))
    sc["b"] = [Buf() for _ in range(5)]
    return sc


def build_L1(NT=16):
    nc = bass.Bass("TRN2", target_bir_lowering=False)
    NTOK = NT * 128
    dt = lambda n, s, d, k: nc.dram_tensor(n, s, d, kind=k).ap()
    x = dt("x", [NTOK, D], F32, "ExternalInput")
    c_cols = dt("c_cols", [128, 8], F32, "ExternalInput")
    ada_w = dt("ada_w", [D, 6 * D], F32, "ExternalInput")
    ada_b = dt("ada_b", [1, 6 * D], F32, "ExternalInput")
    g_mix = dt("g_mix", [1, D], F32, "ExternalInput")
    wf_d = dt("wf", [D, NU0 * 128], F32, "ExternalInput")
    wp_d = dt("wp", [D, 1024], F32, "ExternalInput")
    wt_d = dt("wt", [D, 792], F32, "ExternalInput")
    ident = dt("ident", [128, 128], F32, "ExternalInput")
    o_fm = dt("o_fm", [128, NU0, NTOK], BF16, "ExternalOutput")
    o_kc2 = dt("o_kc2", [128, 4, NTOK // 2], BF16, "ExternalOutput")
    o_vtok = dt("o_vtok", [NTOK, 768], BF16, "ExternalOutput")
    o_gates = dt("o_gates", [NTOK, 24], F32, "ExternalOutput")
    o_mod = dt("o_mod", [6, D], F32, "ExternalOutput")

    P = Prog(nc)
    C = Consts(P, nc, ident[:, :])
    ps_row = P.psum("ps_row", [1, 512], F32); b_ps_row = Buf()
    ps_bc = P.psum("ps_bc", [128, 512], F32); b_ps_bc = Buf()
    ps_tr = P.psum("ps_tr", [128, 8, 128], BF16); b_ps_tr = Buf()
    ps_mm = [P.psum(f"ps_mm{i}", [128, 512], F32) for i in range(4)]
    b_ps_mm = [Buf() for _ in range(4)]

    mod, b_mod = emit_adaln(P, nc, C, c_cols[:, :], ada_w, ada_b[:, :], "0", ps_row, b_ps_row, ps_bc, b_ps_bc)
    outs = []
    outs.append(P.dma("sp", lambda e: e.dma_start(out=o_mod[:, :], in_=mod[0:1, :, :]), reads=[b_mod]))
    gm = P.sbuf("gm", [128, 1024], F32); b_gm = Buf()
    A = P.sbuf("A_m", [128, 1024], F32); b_A = Buf()
    P.dma("sp", lambda e: e.dma_start(out=gm[:], in_=g_mix[0:1, :].to_broadcast([128, 1024])), writes=[b_gm])
    P.op("dve", lambda e: e.scalar_tensor_tensor(out=A[:], in0=mod[:, 1, :], scalar=1.0, in1=gm[:],
                                                 op0=ALU.add, op1=ALU.mult), reads=[b_mod, b_gm], writes=[b_A])
    Bt = mod[:, 0, :]
    wf, b_wf = load_w_bf16(P, nc, "wf_sb", wf_d, NU0 * 128)
    wp, b_wp = load_w_bf16(P, nc, "wp_sb", wp_d, 1024)
    wt, b_wt = load_w_bf16(P, nc, "wt_sb", wt_d, 792)
    xt = [P.sbuf(f"xt{i}", [128, 1024], F32) for i in range(2)]
    b_xt = [Buf(), Buf()]
    hT = [P.sbuf(f"hT{i}", [128, 8, 512], BF16) for i in range(2)]
    b_hT = [Buf(), Buf()]
    nsc = norm_scratch(P, "a")
    stg = [P.sbuf(f"stg{i}", [128, 512], BF16) for i in range(4)]
    b_stg = [Buf() for _ in range(4)]
    gst = [P.sbuf(f"gst{i}", [128, 24], F32) for i in range(2)]
    b_gst = [Buf(), Buf()]
    rr = RR()
    si = 0
    mi = 0
    for tg in range(NT // 4):
        h = hT[tg % 2]; bh = b_hT[tg % 2]
        for tt in range(4):
            t = tg * 4 + tt
            xb = xt[t % 2]; bx = b_xt[t % 2]
            P.dma("sp", lambda e, xb=xb, t=t: e.dma_start(out=xb[:], in_=x[t * 128:(t + 1) * 128, :]), writes=[bx])
            emit_norm_tile(P, C, xb[:], bx, A[:], b_A, Bt, b_mod, h[:, :, tt * 128:(tt + 1) * 128], bh,
                           nsc, ps_tr, b_ps_tr)
        for u in range(NU0):
            ps = ps_mm[mi % 4]; bps = b_ps_mm[mi % 4]; mi += 1
            for k in range(8):
                P.op("pe", lambda e, ps=ps, u=u, k=k, h=h: e.matmul(out=ps[:], lhsT=wf[:, k, u * 128:(u + 1) * 128],
                                                                  rhs=h[:, k, :], start=(k == 0), stop=(k == 7)),
                     reads=[b_wf, bh], writes=[bps], inc=(k == 7))
            st = stg[si % 4]; bst = b_stg[si % 4]; si += 1
            evac(P, rr.next(), st[:], ps[:], [bps], [bst])
            outs.append(P.dma("sp", lambda e, st=st, u=u, tg=tg: e.dma_start(
                out=o_fm[:, u, tg * 512:(tg + 1) * 512], in_=st[:]), reads=[bst]))
        for X in range(4):
            ps = ps_mm[mi % 4]; bps = b_ps_mm[mi % 4]; mi += 1
            n = 0
            for lo in range(2):
                for k in range(8):
                    P.op("pe", lambda e, ps=ps, X=X, lo=lo, k=k, h=h, n=n: e.matmul(
                        out=ps[:, 0:256], lhsT=wp[:, k, (X * 2 + lo) * 128:(X * 2 + lo + 1) * 128],
                        rhs=h[:, k, lo:512:2], start=(n == 0), stop=(n == 15)),
                        reads=[b_wp, bh], writes=[bps], inc=(n == 15))
                    n += 1
            st = stg[si % 4]; bst = b_stg[si % 4]; si += 1
            evac(P, rr.next(), st[:, 0:256], ps[:, 0:256], [bps], [bst])
            outs.append(P.dma("sp", lambda e, st=st, X=X, tg=tg: e.dma_start(
                out=o_kc2[:, X, tg * 256:(tg + 1) * 256], in_=st[:, 0:256]), reads=[bst]))
        for tt in range(4):
            t = tg * 4 + tt
            for grp, (c0, c1) in enumerate([(0, 280), (280, 792)]):
                ps = ps_mm[mi % 4]; bps = b_ps_mm[mi % 4]; mi += 1
                w_ = c1 - c0
                for k in range(8):
                    P.op("pe", lambda e, ps=ps, k=k, h=h, tt=tt, c0=c0, c1=c1, w_=w_: e.matmul(
                        out=ps[:, 0:w_], lhsT=h[:, k, tt * 128:(tt + 1) * 128], rhs=wt[:, k, c0:c1],
                        start=(k == 0), stop=(k == 7)), reads=[b_wt, bh], writes=[bps], inc=(k == 7))
                st = stg[si % 4]; bst = b_stg[si % 4]; si += 1
                if grp == 0:
                    evac(P, rr.next(), st[:, 0:256], ps[:, 0:256], [bps], [bst])
                    outs.append(P.dma("sp", lambda e, st=st, t=t: e.dma_start(
                        out=o_vtok[t * 128:(t + 1) * 128, 0:256], in_=st[:, 0:256]), reads=[bst]))
                    g = gst[t % 2]; bg = b_gst[t % 2]
                    P.op("act", lambda e, g=g, ps=ps: e.activation(out=g[:], in_=ps[:, 256:280], func=AF.Sigmoid),
                         reads=[bps], writes=[bg])
                    outs.append(P.dma("sp", lambda e, g=g, t=t: e.dma_start(
                        out=o_gates[t * 128:(t + 1) * 128, :], in_=g[:]), reads=[bg]))
                else:
                    evac(P, rr.next(), st[:], ps[:], [bps], [bst])
                    outs.append(P.dma("sp", lambda e, st=st, t=t: e.dma_start(
                        out=o_vtok[t * 128:(t + 1) * 128, 256:768], in_=st[:]), reads=[bst]))
    P.wait_all("sp", outs)
    P.finish()
    return nc


def rel_bucket_np(d):
    d = np.maximum(d, 0)
    lp = 16 + (np.log(np.maximum(d, 1).astype(np.float32) / 16) / np.float32(np.log(1024 / 16)) * 16).astype(np.int32)
    return np.where(d < 16, d, np.minimum(lp, 31))


def onehot_table(dvals, mode, win=None):
    L = len(dvals)
    oh = np.zeros((33, L), np.float32)
    ok = dvals >= 0
    if win is not None:
        ok &= dvals < win
    b = rel_bucket_np(dvals)
    idx = np.nonzero(ok)[0]
    oh[b[idx], idx] += 8.0
    if mode == "rel":
        oh[31, idx] -= 8.0
    oh[32, ~ok] = NEGM
    return oh


def build_ctab(P, nc, C, tab33, b_tab33, oh_dram, L, name, scratch_dram, ps, b_ps):
    b_scr = Buf(name + "_scr")
    oh = P.sbuf(name + "_oh", [33, 512], F32); b_oh = Buf()
    cb = P.sbuf(name + "_cb", [8, 512], BF16); b_cb = Buf()
    for c0 in range(0, L, 512):
        w = min(512, L - c0)
        P.dma("sp", lambda e, c0=c0, w=w: e.dma_start(out=oh[:, 0:w], in_=oh_dram[:, c0:c0 + w]), writes=[b_oh])
        P.op("pe", lambda e, w=w: e.matmul(out=ps[0:8, 0:w], lhsT=tab33[:, :], rhs=oh[:, 0:w], start=True, stop=True),
             reads=[b_tab33, b_oh], writes=[b_ps])
        P.op("act", lambda e, w=w: e.copy(out=cb[:, 0:w], in_=ps[0:8, 0:w]), reads=[b_ps], writes=[b_cb])
        P.dma("sp", lambda e, c0=c0, w=w: e.dma_start(out=scratch_dram[:, c0:c0 + w], in_=cb[:, 0:w]),
              reads=[b_cb], writes=[b_scr])
    return b_scr


def toeplitz_load(P, Wt, b_W, scratch_dram, b_scr, L, h0, nR, rstride=128, pstride=1, base=0, nh=4, width=128):
    from concourse.bass_types import AP
    for hh in range(nh):
        src = AP(scratch_dram.tensor, scratch_dram.offset + (h0 + hh) * L + base,
                 [[pstride, 128], [rstride, nR], [1, width]])
        P.dma("sp", lambda e, hh=hh, src=src: e.dma_start(out=Wt[:, :, hh, :], in_=src),
              reads=[b_scr], writes=[b_W])


class AttnRes:
    def __init__(self, P, nS=2, nP=3):
        self.S = [(P.psum(f"at_S{i}", [128, 512], F32), Buf()) for i in range(nS)]
        self.Pt = [(P.sbuf(f"at_P{i}", [128, 512], BF16), Buf()) for i in range(nP)]
        self.si = 0
        self.pi = 0

    def nextS(self):
        r = self.S[self.si % len(self.S)]; self.si += 1
        return r

    def nextP(self):
        r = self.Pt[self.pi % len(self.Pt)]; self.pi += 1
        return r


def attn_steps(P, R, kts, qk_fn, extra_fn, v_fn, O, b_O, scale, nv=65, post_fn=None, bias_ap=None, b_bias=None, nh=4):
    n = len(kts)
    for ii, kt in enumerate(kts):
        S, b_S = R.nextS()
        ex = extra_fn(kt) if extra_fn else []
        qk = qk_fn(kt)
        for qi, (c0, c1, mms, reads) in enumerate(qk):
            for mi, (lhsT, rhs) in enumerate(mms):
                last = (not ex) and qi == len(qk) - 1 and mi == len(mms) - 1
                P.op("pe", lambda e, S=S, c0=c0, c1=c1, lhsT=lhsT, rhs=rhs, mi=mi, qi=qi, last=last: e.matmul(
                    out=S[:, c0:c1], lhsT=lhsT, rhs=rhs, start=(mi == 0 and qi == 0), stop=last,
                    skip_group_check=True),
                    reads=reads, writes=[b_S], inc=last)
        for xi, (lhsT, rhs, reads) in enumerate(ex):
            P.op("pe", lambda e, S=S, lhsT=lhsT, rhs=rhs, xi=xi, nx=len(ex): e.matmul(
                out=S[:, 0:nh * 128], lhsT=lhsT, rhs=rhs, start=False, stop=(xi == nx - 1), skip_group_check=True),
                reads=reads, writes=[b_S], inc=(xi == len(ex) - 1))
        Pt, b_P = R.nextP()
        if bias_ap is None:
            P.op("act", lambda e, S=S, Pt=Pt: e.activation(out=Pt[:, 0:nh * 128], in_=S[:, 0:nh * 128], func=AF.Exp, scale=scale),
                 reads=[b_S], writes=[b_P])
        else:
            P.op("act", lambda e, S=S, Pt=Pt: e.activation(out=Pt[:], in_=S[:], func=AF.Exp, scale=scale, bias=bias_ap),
                 reads=[b_S, b_bias], writes=[b_P])
        for hh in range(nh):
            rhs, reads = v_fn(kt, hh)
            P.op("pe", lambda e, Pt=Pt, hh=hh, rhs=rhs, ii=ii: e.matmul(
                out=O[:, hh, 0:nv], lhsT=Pt[:, hh * 128:(hh + 1) * 128], rhs=rhs, start=(ii == 0 and hh == 0),
                stop=(ii == n - 1 and hh == nh - 1), skip_group_check=True),
                reads=[b_P] + reads, writes=[b_O], inc=(hh == nh - 1 and post_fn is None))
        if post_fn is not None:
            post_fn(kt, ii, n, Pt, b_P)


def moba_static(NT, STRIDE, j):
    LC = 11 * 128 + 127
    y = np.arange(LC)
    d = y - 127 - 384 + 128 * j
    oh_rel = onehot_table(d, "rel")
    ohsel = np.zeros((32, 32, 128), np.float32)
    for n in range(32):
        ohsel[n, n, :] = 1.0
    negvalid = np.zeros((NT, 32), np.float32)
    own1h = np.zeros((NT, 32), np.float32)
    for lt in range(NT):
        own = (STRIDE * lt + j) // 2
        negvalid[lt, own:] = -1e30
        own1h[lt, own] = 1.0
    return dict(oh_rel=oh_rel, ohsel=ohsel.reshape(32, 4096).astype(NPBF), negvalid=negvalid.reshape(1, -1),
                own1h=own1h.reshape(1, -1))


def gelu_tanh_ops(P, u, b_u, t, b_t, out_bf, b_out, width):
    P.op("dve", lambda e: e.tensor_tensor(out=t[:, 0:width], in0=u[:, 0:width], in1=u[:, 0:width], op=ALU.mult),
         reads=[b_u], writes=[b_t])
    P.op("dve", lambda e: e.tensor_scalar(out=t[:, 0:width], in0=t[:, 0:width], scalar1=0.044715, scalar2=1.0,
                                          op0=ALU.mult, op1=ALU.add), reads=[b_t], writes=[b_t])
    P.op("dve", lambda e: e.tensor_tensor(out=t[:, 0:width], in0=t[:, 0:width], in1=u[:, 0:width], op=ALU.mult),
         reads=[b_t, b_u], writes=[b_t])
    P.op("act", lambda e: e.activation(out=t[:, 0:width], in_=t[:, 0:width], func=AF.Sigmoid, scale=1.5957691216057308),
         reads=[b_t], writes=[b_t])
    P.op("dve", lambda e: e.tensor_tensor(out=out_bf, in0=t[:, 0:width], in1=u[:, 0:width], op=ALU.mult),
         reads=[b_t, b_u], writes=[b_out])


def attn0_static(NT, STRIDE, j, NKT=64):
    st = moba_static(NT, STRIDE, j)
    LW = 8 * 128 + 127
    y = np.arange(LW)
    st["oh_win"] = onehot_table(y - 127 - 384 + 128 * j, "abs", win=512)
    NRC = -(-2853 // (128 * STRIDE))
    LCM = 128 * STRIDE * (NRC - 1) + 127 + 16 * 127 + 1
    y = np.arange(LCM)
    st["oh_cmp"] = onehot_table(y + 128 * j - 2063, "abs")
    n = np.arange(512)
    cs = n * 16
    ce = cs + 31
    m = np.arange(128) * 64
    ov = ((cs[:, None] <= m[None, :] + 63) & (ce[:, None] >= m[None, :])).astype(np.float32)
    ov[511] = 0
    st["overlap"] = ov.reshape(4, 128, 128).transpose(1, 0, 2).reshape(128, 512).astype(NPBF)
    E = np.zeros((128, NKT * 128), np.float32)
    keys = np.arange(NKT * 128)
    E[keys // 64, keys] = 1.0
    st["E"] = E.astype(NPBF)
    OFF = 2 * STRIDE * (NT - 1)
    width = OFF + 128
    Fw = np.zeros((128, width), np.float32)
    q = np.arange(128)[:, None]
    xx = np.arange(width)[None, :]
    rel = xx - OFF - 2 * j
    hq = (q >= 64).astype(np.int64)
    Fw[np.broadcast_to(rel > hq, Fw.shape)] = -1e30
    Fw[np.broadcast_to((rel == hq) | (rel == hq - 1), Fw.shape)] = 1e30
    st["fwide"] = Fw
    st["cnt_win"] = pad_counts(NT, STRIDE, j, 512)
    return st


def n_aff(STRIDE, win):
    return -(-(win // 128) // STRIDE)


def pad_counts(NT, STRIDE, j, win):
    na = n_aff(STRIDE, win)
    cnt = np.zeros((32, na * 128), np.float32)
    for a in range(na):
        for q in range(128):
            t = 128 * (STRIDE * a + j) + q
            if t + 1 <= win - 1:
                d = np.arange(t + 1, win)
                bb = rel_bucket_np(d)
                cnt[:, a * 128 + q] = np.bincount(bb, minlength=32)
    return cnt


def build_attn0(NT=16, STRIDE=4, NKT=64, do_nsa=True, do_moba=True):
    nc = bass.Bass("TRN2", target_bir_lowering=False)
    NTOK = NT * 128
    NK = NKT * 128
    LC = 11 * 128 + 127
    LW = 8 * 128 + 127
    NRC = -(-2853 // (128 * STRIDE))
    LCM = 128 * STRIDE * (NRC - 1) + 127 + 16 * 127 + 1
    OFFW = 2 * STRIDE * (NT - 1)
    dt = lambda n, s, d, k: nc.dram_tensor(n, s, d, kind=k).ap()
    qT_d = dt("qT", [128, 16, NTOK], BF16, "ExternalInput")
    gates_d = dt("gates", [NTOK, 24], F32, "ExternalInput")
    kbT = dt("kbT", [128, 4, NK], BF16, "ExternalInput")
    ksT = dt("ksT", [128, 2, NK], BF16, "ExternalInput")
    kwT = dt("kwT", [128, 2, NK], BF16, "ExternalInput")
    kc2 = dt("kc2", [128, 4, NK // 2], BF16, "ExternalInput")
    vtok = dt("vtok", [NK, 768], BF16, "ExternalInput")
    tab33_d = dt("tab33", [33, 8], F32, "ExternalInput")
    oh_rel = dt("oh_rel", [33, LC], F32, "ExternalInput")
    oh_win = dt("oh_win", [33, LW], F32, "ExternalInput")
    oh_cmp = dt("oh_cmp", [33, LCM], F32, "ExternalInput")
    ohsel_d = dt("ohsel", [32, 32 * 128], BF16, "ExternalInput")
    negvalid_d = dt("negvalid", [1, NT * 32], F32, "ExternalInput")
    own1h_d = dt("own1h", [1, NT * 32], F32, "ExternalInput")
    overlap_d = dt("overlap", [128, 512], BF16, "ExternalInput")
    E_d = dt("E", [128, NK], BF16, "ExternalInput")
    fwide_d = dt("fwide", [128, OFFW + 128], F32, "ExternalInput")
    NAFF = n_aff(STRIDE, 512)
    cnt_win_d = dt("cnt_win", [32, NAFF * 128], F32, "ExternalInput")
    posc_d = dt("posc", [128, 2, 16], F32, "ExternalInput")
    w1_d = dt("cw1", [2, 2048, 256], F32, "ExternalInput")
    w2k_d = dt("cw2k", [256, 128], F32, "ExternalInput")
    w2v_d = dt("cw2v", [256, 64], F32, "ExternalInput")
    ident = dt("ident", [128, 128], F32, "ExternalInput")
    o_out = dt("o_attn", [NTOK, 1024], BF16, "ExternalOutput")
    crel_scr = dt("crel_scr", [8, LC], BF16, "Internal")
    cwin_scr = dt("cwin_scr", [8, LW], BF16, "Internal")
    ccmp_scr = dt("ccmp_scr", [8, LCM], BF16, "Internal")

    P = Prog(nc)
    C = Consts(P, nc, ident[:, :])
    R = AttnRes(P)
    ps_misc = P.psum("ps_misc", [128, 512], F32); b_ps_misc = Buf()
    ps_trf = P.psum("ps_trb", [128, 8, 128], BF16); b_ps_tr = Buf()
    Ops = [(P.psum(f"O{i}", [128, 4, 128], F32), Buf()) for i in range(3)]
    IMP = P.psum("IMP", [128, 4, 128], F32); b_IMP = Buf()
    tab33 = P.sbuf("tab33", [33, 8], F32); b_tab33 = Buf()
    P.dma("sp", lambda e: e.dma_start(out=tab33[:], in_=tab33_d[:, :]), writes=[b_tab33])
    b_scr_rel = build_ctab(P, nc, C, tab33, b_tab33, oh_rel, LC, "crel", crel_scr, ps_misc, b_ps_misc)
    b31 = P.sbuf("b31", [128, 8], F32); b_b31 = Buf()
    P.dma("sp", lambda e: e.dma_start(out=b31[:], in_=tab33_d[31:32, :].to_broadcast([128, 8])), writes=[b_b31])
    P.op("dve", lambda e: e.tensor_scalar(out=b31[:], in0=b31[:], scalar1=8.0, scalar2=None, op0=ALU.mult),
         reads=[b_b31], writes=[b_b31])
    QT = P.sbuf("QT", [128, 8, NTOK], BF16); b_QT = Buf()
    KV = P.sbuf("KV", [128, 33280], BF16); b_KV = Buf()
    o_sb = P.sbuf("o_sb", [128, NT, 1024], BF16); b_osb = Buf()
    W = P.sbuf("W", [128, 11, 4, 128], BF16); b_W = Buf()
    rz = P.sbuf("rz", [128, 4, 1], F32); b_rz = Buf()
    outs = []
    Esb = P.sbuf("Esb", [128, NK], BF16); b_E = Buf()
    u32 = P.sbuf("u32", [128, 512], F32); b_u32 = Buf()
    t32 = P.sbuf("t32", [128, 512], F32); b_t32 = Buf()
    Wwin = P.sbuf("Wwin", [128, 8, 4, 128], BF16); b_Wwin = Buf()
    Wc = P.sbuf("Wc", [128, NRC, 4, 128], BF16); b_Wc = Buf()

    if do_nsa:
        b_scr_win = build_ctab(P, nc, C, tab33, b_tab33, oh_win, LW, "cwin", cwin_scr, ps_misc, b_ps_misc)
        b_scr_cmp = build_ctab(P, nc, C, tab33, b_tab33, oh_cmp, LCM, "ccmp", ccmp_scr, ps_misc, b_ps_misc)
        P.dma("sp", lambda e: e.dma_start(out=Esb[:], in_=E_d[:, :]), writes=[b_E])
        ovl = P.sbuf("ovl", [128, 4, 128], BF16); b_ovl = Buf()
        P.dma("sp", lambda e: e.dma_start(out=ovl[:].rearrange("p a m -> p (a m)"), in_=overlap_d[:, :]), writes=[b_ovl])
        fw = P.sbuf("fw", [128, OFFW + 128], F32); b_fw = Buf()
        P.dma("sp", lambda e: e.dma_start(out=fw[:], in_=fwide_d[:, :]), writes=[b_fw])
        exptab = P.sbuf("exptab", [32, 8], F32); b_exptab = Buf()
        P.op("act", lambda e: e.activation(out=exptab[:], in_=tab33[0:32, :], func=AF.Exp), reads=[b_tab33], writes=[b_exptab])
        cntw = P.sbuf("cntw", [32, NAFF * 128], F32); b_cntw = Buf()
        P.dma("sp", lambda e: e.dma_start(out=cntw[:], in_=cnt_win_d[:, :]), writes=[b_cntw])
        zpad = P.sbuf("zpad", [128, NAFF, 8], F32); b_zpad = Buf()
        for a in range(NAFF):
            P.op("pe", lambda e, a=a: e.matmul(out=ps_misc[:, 0:8], lhsT=cntw[:, a * 128:(a + 1) * 128], rhs=exptab[:, :],
                                               start=True, stop=True), reads=[b_cntw, b_exptab], writes=[b_ps_misc])
            P.op("act", lambda e, a=a: e.copy(out=zpad[:, a, :], in_=ps_misc[:, 0:8]), reads=[b_ps_misc], writes=[b_zpad])
        gts = P.sbuf("gts", [128, NT, 24], F32); b_gts = Buf()
        P.dma("sp", lambda e: e.dma_start(out=gts[:], in_=gates_d.rearrange("(t p) n -> p t n", p=128)), writes=[b_gts])
        P.dma("sp", lambda e: e.dma_start(out=QT[:], in_=qT_d[:, 0:8, :]), writes=[b_QT])
        posc = P.sbuf("posc", [128, 2, 16], F32); b_posc = Buf()
        poscb = P.sbuf("poscb", [128, 2, 16], BF16); b_poscb = Buf()
        P.dma("sp", lambda e: e.dma_start(out=posc[:], in_=posc_d[:, :, :]), writes=[b_posc])
        P.op("dve", lambda e: e.tensor_copy(out=poscb[:], in_=posc[:]), reads=[b_posc], writes=[b_poscb])
        w2k = P.sbuf("w2k", [128, 2, 128], BF16); b_w2k = Buf()
        w2v = P.sbuf("w2v", [128, 2, 64], BF16); b_w2v = Buf()
        P.dma("pool", lambda e: e.dma_start(out=w2k[:], in_=w2k_d.rearrange("(k p) n -> p k n", p=128)), writes=[b_w2k])
        P.dma("pool", lambda e: e.dma_start(out=w2v[:], in_=w2v_d.rearrange("(k p) n -> p k n", p=128)), writes=[b_w2v])
        w1 = KV[:, 8192:8192 + 4096].rearrange("p (k n) -> p k n", n=256); b_w1 = Buf()
        kc2sb = KV[:, 0:NK // 2]; b_kc2 = Buf()
        bias1 = P.sbuf("bias1", [128, 2], F32); b_bias1 = Buf()
        GT = P.sbuf("GT", [128, 2, 512], BF16); b_GT = Buf()
        P.op("pool", lambda e: e.memset(GT[:], 0.0), writes=[b_GT])
        KcT = P.sbuf("KcT", [128, 2, 512], BF16); b_KcT = Buf()
        Vc = P.sbuf("Vc", [128, 2, 4, 65], BF16); b_Vc = Buf()
        P.op("pool", lambda e: e.memset(KcT[:], 0.0), writes=[b_KcT])
        P.op("pool", lambda e: e.memset(Vc[:], 1.0), writes=[b_Vc])
        ncmp = NK // 16 - 1
        for kvt in range(2):
            for k in range(16):
                P.dma("pool", lambda e, k=k, kvt=kvt: e.dma_start(out=w1[:, k, :], in_=w1_d[kvt, k * 128:(k + 1) * 128, :]),
                      writes=[b_w1])
            for hc in range(2):
                for k in range(16):
                    P.op("pe", lambda e, hc=hc, k=k, kvt=kvt: e.matmul(
                        out=ps_misc[:, 0:1], lhsT=w1[:, k, hc * 128:(hc + 1) * 128], rhs=poscb[:, kvt, k:k + 1],
                        start=(k == 0), stop=(k == 15)), reads=[b_w1, b_poscb], writes=[b_ps_misc], inc=(k == 15))
                P.op("act", lambda e, hc=hc: e.copy(out=bias1[:, hc:hc + 1], in_=ps_misc[:, 0:1]),
                     reads=[b_ps_misc], writes=[b_bias1])
            for kv in range(2):
                X = kvt * 2 + kv
                P.dma("sp", lambda e, X=X: e.dma_start(out=kc2sb, in_=kc2[:, X, :]), writes=[b_kc2])
                for hc in range(2):
                    for k in range(16):
                        P.op("pe", lambda e, hc=hc, k=k: e.matmul(
                            out=ps_misc[:, 0:ncmp], lhsT=w1[:, k, hc * 128:(hc + 1) * 128],
                            rhs=kc2sb[:, k:k + 8 * (ncmp - 1) + 1:8], start=(k == 0), stop=(k == 15)),
                            reads=[b_w1, b_kc2], writes=[b_ps_misc], inc=(k == 15))
                    P.op("act", lambda e, hc=hc: e.activation(out=u32[:, 0:ncmp], in_=ps_misc[:, 0:ncmp], func=AF.Identity,
                                                              bias=bias1[:, hc:hc + 1]),
                         reads=[b_ps_misc, b_bias1], writes=[b_u32])
                    gelu_tanh_ops(P, u32, b_u32, t32, b_t32, GT[:, hc, 0:ncmp], b_GT, ncmp)
                if kvt == 0:
                    for hc in range(2):
                        P.op("pe", lambda e, hc=hc: e.matmul(out=ps_misc[:, 0:512], lhsT=w2k[:, hc, :], rhs=GT[:, hc, :],
                                                             start=(hc == 0), stop=(hc == 1)),
                             reads=[b_w2k, b_GT], writes=[b_ps_misc], inc=(hc == 1))
                    P.op("act", lambda e, kv=kv: e.copy(out=KcT[:, kv, :], in_=ps_misc[:, 0:512]),
                         reads=[b_ps_misc], writes=[b_KcT])
                else:
                    for nc_ in range(4):
                        for hc in range(2):
                            P.op("pe", lambda e, hc=hc, nc_=nc_: e.matmul(
                                out=ps_misc[:, nc_ * 64:(nc_ + 1) * 64], lhsT=GT[:, hc, nc_ * 128:(nc_ + 1) * 128],
                                rhs=w2v[:, hc, :], start=(hc == 0 and nc_ == 0), stop=(hc == 1 and nc_ == 3),
                                skip_group_check=True),
                                reads=[b_w2v, b_GT], writes=[b_ps_misc], inc=(hc == 1 and nc_ == 3))
                    P.op("act", lambda e, kv=kv: e.copy(out=Vc[:, kv, :, 0:64],
                                                        in_=ps_misc[:, 0:256].rearrange("p (a d) -> p a d", d=64)),
                         reads=[b_ps_misc], writes=[b_Vc])
        KsT = KV[:, 0:NK]
        KwT = KV[:, NK:2 * NK]
        Vs = KV[:, 2 * NK:2 * NK + NKT * 65].rearrange("p (k d) -> p k d", d=65)
        Vw = KV[:, 2 * NK + NKT * 65:2 * NK + 2 * NKT * 65].rearrange("p (k d) -> p k d", d=65)
        imp = P.sbuf("imp", [128, 128], F32); b_imp = Buf()
        sc2 = P.sbuf("sc2", [128, 128], F32); b_sc2 = Buf()
        m8 = P.sbuf("m8", [128, 2, 8], F32); b_m8 = Buf()
        negm4 = P.sbuf("negm4", [128, 4, 128], BF16); b_negm4 = Buf()
        nT4 = [(P.sbuf(f"nT4_{i}", [128, 4, 128], BF16), Buf()) for i in range(2)]
        b31row = P.sbuf("b31row", [1, 2, 4, 128], BF16); b_b31row = Buf()
        for kv in range(2):
            P.op("dve", lambda e, kv=kv: e.tensor_copy(
                out=b31row[0:1, kv, :, :], in_=b31[0:1, 4 * kv:4 * kv + 4].unsqueeze(2).to_broadcast([1, 4, 128])),
                reads=[b_b31], writes=[b_b31row])
        ones_b = P.sbuf("ones_b", [1, 128], BF16); b_ones_b = Buf()
        P.op("dve", lambda e: e.memset(ones_b[:], 1.0), writes=[b_ones_b])
        rzg = P.sbuf("rzg", [128, 4, 1], F32); b_rzg = Buf()
        oacc = P.sbuf("oacc", [128, 4, 64], F32); b_oacc = Buf()
        otmp = P.sbuf("otmp", [128, 4, 64], F32); b_otmp = Buf()
        for kv in range(2):
            P.dma("sp", lambda e, kv=kv: e.dma_start(out=KsT, in_=ksT[:, kv, :]), writes=[b_KV, b_w1, b_kc2])
            P.dma("sp", lambda e, kv=kv: e.dma_start(out=KwT, in_=kwT[:, kv, :]), writes=[b_KV])
            P.op("pool", lambda e: e.memset(Vs[:, :, 64:65], 1.0), writes=[b_KV])
            P.op("pool", lambda e: e.memset(Vw[:, :, 64:65], 1.0), writes=[b_KV])
            for k0 in range(0, NKT, 8):
                P.dma("sp", lambda e, kv=kv, k0=k0: e.dma_start(
                    out=Vs[:, k0:k0 + 8, 0:64], in_=vtok[k0 * 128:(k0 + 8) * 128, kv * 64:(kv + 1) * 64].rearrange(
                        "(kt p) d -> p kt d", p=128)), writes=[b_KV])
                P.dma("sp", lambda e, kv=kv, k0=k0: e.dma_start(
                    out=Vw[:, k0:k0 + 8, 0:64], in_=vtok[k0 * 128:(k0 + 8) * 128, 128 + kv * 64:128 + (kv + 1) * 64].rearrange(
                        "(kt p) d -> p kt d", p=128)), writes=[b_KV])
            toeplitz_load(P, W, b_W, crel_scr, b_scr_rel, LC, 4 * kv, 11)
            toeplitz_load(P, Wwin, b_Wwin, cwin_scr, b_scr_win, LW, 4 * kv, 8)
            toeplitz_load(P, Wc, b_Wc, ccmp_scr, b_scr_cmp, LCM, 4 * kv, NRC, rstride=128 * STRIDE, pstride=16)
            for lt in range(NT):
                Oc, b_Oc = Ops[0]; Os, b_Os = Ops[1]; Ow, b_Ow = Ops[2]

                def qk_gen(Ksrc, bK, lt=lt, kv=kv):
                    def qk_fn(kt):
                        return [(hh * 128, (hh + 1) * 128,
                                 [(Ksrc(kt), QT[:, 4 * kv + hh, lt * 128:(lt + 1) * 128])], [bK, b_QT])
                                for hh in range(4)]
                    return qk_fn
                ncs = list(range(0, min(4, (STRIDE * lt + STRIDE - 1) // 16 + 1)))

                def extra_c(nc_, lt=lt, kv=kv):
                    r = (STRIDE * lt - 16 * nc_) // STRIDE
                    if r < NRC:
                        return [(C.antib[:], Wc[:, r, :, :].rearrange("p h q -> p (h q)"), [C.b_antib, b_Wc])]
                    return [(ones_b[0:1, :], b31row[0:1, kv, :, :].rearrange("p h q -> p (h q)"), [b_ones_b, b_b31row])]

                def post_c(nc_, ii, n, Pt, b_P):
                    for g in range(4):
                        P.op("pe", lambda e, g=g, nc_=nc_, ii=ii, n=n, Pt=Pt: e.matmul(
                            out=IMP[:, g, :], lhsT=Pt[:, g * 128:(g + 1) * 128], rhs=ovl[:, nc_, :],
                            start=(ii == 0 and g == 0), stop=(ii == n - 1 and g == 3), skip_group_check=True),
                            reads=[b_P, b_ovl], writes=[b_IMP], inc=(g == 3))
                attn_steps(P, R, ncs, qk_gen(lambda nc_, kv=kv: KcT[:, kv, nc_ * 128:(nc_ + 1) * 128], b_KcT), extra_c,
                           lambda nc_, hh, kv=kv: (Vc[:, kv, nc_, :], [b_Vc]), Oc, b_Oc, 0.125, post_fn=post_c)
                P.op("dve", lambda e, Oc=Oc: e.tensor_scalar(out=rz[:], in0=Oc[:, :, 64:65], scalar1=1e-30, scalar2=None,
                                                             op0=ALU.max), reads=[b_Oc], writes=[b_rz])
                P.op("dve", lambda e: e.reciprocal(out=rz[:], in_=rz[:]), reads=[b_rz], writes=[b_rz])
                P.op("dve", lambda e: e.tensor_scalar(out=imp[:], in0=IMP[:, 0, :], scalar1=rz[:, 0, :], scalar2=None,
                                                      op0=ALU.mult), reads=[b_IMP, b_rz], writes=[b_imp])
                for g in range(1, 4):
                    P.op("dve", lambda e, g=g: e.scalar_tensor_tensor(out=imp[:], in0=IMP[:, g, :], scalar=rz[:, g, :],
                                                                      in1=imp[:], op0=ALU.mult, op1=ALU.add),
                         reads=[b_IMP, b_rz, b_imp], writes=[b_imp])
                f0 = OFFW - 2 * STRIDE * lt
                P.op("dve", lambda e, f0=f0: e.tensor_tensor(out=imp[:], in0=imp[:], in1=fw[:, f0:f0 + 128], op=ALU.add),
                     reads=[b_imp, b_fw], writes=[b_imp])
                P.op("dve", lambda e: e.memset(imp[:, 0:1], 1e30), reads=[b_imp], writes=[b_imp])
                P.op("dve", lambda e: e.max(out=m8[:, 0, :], in_=imp[:]), reads=[b_imp], writes=[b_m8])
                P.op("dve", lambda e: e.match_replace(out=sc2[:], in_to_replace=m8[:, 0, :], in_values=imp[:],
                                                      imm_value=-3.0e38), reads=[b_imp, b_m8], writes=[b_sc2])
                P.op("dve", lambda e: e.max(out=m8[:, 1, :], in_=sc2[:]), reads=[b_sc2], writes=[b_m8])
                P.op("dve", lambda e: e.tensor_scalar(out=sc2[:], in0=imp[:], scalar1=m8[:, 1, 7:8], scalar2=None,
                                                      op0=ALU.is_ge), reads=[b_imp, b_m8], writes=[b_sc2])
                P.op("dve", lambda e: e.tensor_scalar(out=sc2[:], in0=sc2[:], scalar1=-1.0, scalar2=-NEGM,
                                                      op0=ALU.add, op1=ALU.mult), reads=[b_sc2], writes=[b_sc2])
                for hh in range(4):
                    P.op("dve", lambda e, hh=hh, kv=kv: e.tensor_scalar(
                        out=negm4[:, hh, :], in0=sc2[:], scalar1=b31[:, 4 * kv + hh:4 * kv + hh + 1], scalar2=None,
                        op0=ALU.add), reads=[b_sc2, b_b31], writes=[b_negm4], inc=(hh == 3))
                for hh in range(4):
                    P.op("pe", lambda e, hh=hh: e.transpose(out=ps_trf[:, hh, :], in_=negm4[:, hh, :], identity=C.idb[:]),
                         reads=[b_negm4, C.b_idb], writes=[b_ps_tr], inc=(hh == 3))
                nT, b_nT = nT4[lt % 2]
                P.op("act", lambda e, nT=nT: e.copy(out=nT[:], in_=ps_trf[:, 0:4, :]), reads=[b_ps_tr], writes=[b_nT])
                kts = list(range(0, min(STRIDE * lt + STRIDE, NKT)))

                def extra_s(kt, lt=lt, nT=nT, b_nT=b_nT):
                    ex = [(Esb[:, kt * 128:(kt + 1) * 128], nT[:].rearrange("m h q -> m (h q)"), [b_E, b_nT])]
                    dl = STRIDE * lt - kt
                    if dl <= 7:
                        ex.append((C.antib[:], W[:, dl + 3, :, :].rearrange("p h q -> p (h q)"), [C.b_antib, b_W]))
                    return ex
                attn_steps(P, R, kts, qk_gen(lambda kt: KsT[:, kt * 128:(kt + 1) * 128], b_KV), extra_s,
                           lambda kt, hh: (Vs[:, kt, :], [b_KV]), Os, b_Os, 0.125)
                ktw = list(range(max(0, STRIDE * lt - 4), min(STRIDE * lt + STRIDE, NKT)))

                def extra_w(kt, lt=lt):
                    dl = STRIDE * lt - kt
                    return [(C.antib[:], Wwin[:, dl + 3, :, :].rearrange("p h q -> p (h q)"), [C.b_antib, b_Wwin])]
                attn_steps(P, R, ktw, qk_gen(lambda kt: KwT[:, kt * 128:(kt + 1) * 128], b_KV), extra_w,
                           lambda kt, hh: (Vw[:, kt, :], [b_KV]), Ow, b_Ow, 0.125)
                for br, (O, b_O) in enumerate([(Oc, b_Oc), (Os, b_Os), (Ow, b_Ow)]):
                    if br == 2 and lt < NAFF:
                        P.op("dve", lambda e, O=O, lt=lt, kv=kv: e.tensor_tensor(
                            out=rz[:], in0=O[:, :, 64:65], in1=zpad[:, lt, 4 * kv:4 * kv + 4].unsqueeze(2), op=ALU.add),
                            reads=[b_O, b_zpad], writes=[b_rz])
                    else:
                        P.op("dve", lambda e, O=O: e.tensor_scalar(out=rz[:], in0=O[:, :, 64:65], scalar1=1e-30, scalar2=None,
                                                                   op0=ALU.max), reads=[b_O], writes=[b_rz])
                    P.op("dve", lambda e: e.reciprocal(out=rz[:], in_=rz[:]), reads=[b_rz], writes=[b_rz])
                    gsl = gts[:, lt, 12 * kv:12 * kv + 12].rearrange("p (h b) -> p h b", b=3)[:, :, br:br + 1]
                    P.op("dve", lambda e, gsl=gsl: e.tensor_tensor(out=rzg[:], in0=rz[:], in1=gsl, op=ALU.mult),
                         reads=[b_rz, b_gts], writes=[b_rzg])
                    if br == 0:
                        P.op("dve", lambda e, O=O: e.tensor_tensor(out=oacc[:], in0=O[:, :, 0:64],
                                                                   in1=rzg[:].to_broadcast([128, 4, 64]), op=ALU.mult),
                             reads=[b_O, b_rzg], writes=[b_oacc])
                    else:
                        P.op("dve", lambda e, O=O: e.tensor_tensor(out=otmp[:], in0=O[:, :, 0:64],
                                                                   in1=rzg[:].to_broadcast([128, 4, 64]), op=ALU.mult),
                             reads=[b_O, b_rzg], writes=[b_otmp])
                        if br == 1:
                            P.op("pool", lambda e: e.tensor_tensor(out=oacc[:], in0=oacc[:], in1=otmp[:], op=ALU.add),
                                 reads=[b_oacc, b_otmp], writes=[b_oacc])
                        else:
                            P.op("pool", lambda e, lt=lt, kv=kv: e.tensor_tensor(
                                out=o_sb[:, lt, kv * 256:(kv + 1) * 256].rearrange("p (h d) -> p h d", d=64),
                                in0=oacc[:], in1=otmp[:], op=ALU.add), reads=[b_oacc, b_otmp], writes=[b_osb])

    if do_moba:
        ohsel = Esb[0:32, 0:32 * 128]; b_ohsel = b_E
        P.dma("sp", lambda e: e.dma_start(out=ohsel, in_=ohsel_d[:, :]), writes=[b_ohsel])
        assert NT * 32 <= 512
        negvalid = u32[:, 0:NT * 32].rearrange("p (a n) -> p a n", n=32); b_nv = b_u32
        own1h = t32[:, 0:NT * 32].rearrange("p (a n) -> p a n", n=32); b_own = b_t32
        P.dma("sp", lambda e: e.dma_start(out=u32[:, 0:NT * 32],
                                          in_=negvalid_d[0:1, :].to_broadcast([128, NT * 32])), writes=[b_nv])
        P.dma("sp", lambda e: e.dma_start(out=t32[:, 0:NT * 32],
                                          in_=own1h_d[0:1, :].to_broadcast([128, NT * 32])), writes=[b_own])
        P.dma("sp", lambda e: e.dma_start(out=QT[:], in_=qT_d[:, 8:16, :]), writes=[b_QT])
        KT = KV[:, 0:2 * NK].rearrange("p (c k) -> p c k", c=2)
        V = KV[:, 2 * NK:2 * NK + NKT * 4 * 65].rearrange("p (k h d) -> p k h d", h=4, d=65)
        kmT = P.sbuf("kmT", [128, 2, 32], BF16); b_kmT = Buf()
        kms = P.sbuf("kms", [128, 32], F32); b_kms = Buf()
        gate = P.sbuf("gate", [128, 4, 32], F32); b_gate = Buf()
        mx8 = P.sbuf("mx8", [128, 4, 8], F32); b_mx8 = Buf()
        sel = P.sbuf("sel", [128, 4, 32], F32); b_sel = Buf()
        negm = P.sbuf("negm", [128, 4, 32], BF16); b_negm = Buf()
        negmT = [(Wwin[0:32, i, :, :], b_Wwin) for i in range(2)]
        for g in range(2):
            for cc in range(2):
                P.dma("sp", lambda e, cc=cc, g=g: e.dma_start(out=KT[:, cc, :], in_=kbT[:, 2 * g + cc, :]), writes=[b_KV])
            P.op("pool", lambda e: e.memset(V[:, :, :, 64:65], 1.0), writes=[b_KV])
            for hh in range(4):
                for k0 in range(0, NKT, 8):
                    P.dma("sp", lambda e, hh=hh, g=g, k0=k0: e.dma_start(
                        out=V[:, k0:k0 + 8, hh, 0:64],
                        in_=vtok[k0 * 128:(k0 + 8) * 128, 256 + (4 * g + hh) * 64:256 + (4 * g + hh + 1) * 64].rearrange(
                            "(kt p) d -> p kt d", p=128)), writes=[b_KV])
            toeplitz_load(P, W, b_W, crel_scr, b_scr_rel, LC, 4 * g, 11)
            for cc in range(2):
                P.op("dve", lambda e, cc=cc: e.tensor_reduce(out=kms[:], in_=KT[:, cc, :].rearrange("p (n k) -> p n k", k=256),
                                                             axis=AX.X, op=ALU.add), reads=[b_KV], writes=[b_kms])
                P.op("dve", lambda e, cc=cc: e.tensor_scalar(out=kmT[:, cc, :], in0=kms[:], scalar1=1.0 / 256, scalar2=None,
                                                             op0=ALU.mult), reads=[b_kms], writes=[b_kmT])
            for lt in range(NT):
                for hh in range(4):
                    cc = hh // 2
                    P.op("pe", lambda e, hh=hh, cc=cc, lt=lt, g=g: e.matmul(
                        out=ps_misc[:, hh * 32:(hh + 1) * 32], lhsT=QT[:, 4 * g + hh, lt * 128:(lt + 1) * 128],
                        rhs=kmT[:, cc, :], start=(hh == 0), stop=(hh == 3), skip_group_check=True),
                        reads=[b_QT, b_kmT], writes=[b_ps_misc], inc=(hh == 3))
                P.op("dve", lambda e, lt=lt: e.tensor_tensor(
                    out=gate[:], in0=ps_misc[:, 0:128].rearrange("p (h n) -> p h n", n=32),
                    in1=negvalid[:, lt:lt + 1, :].to_broadcast([128, 4, 32]), op=ALU.add),
                    reads=[b_ps_misc, b_nv], writes=[b_gate])
                for hh in range(4):
                    P.op("dve", lambda e, hh=hh: e.max(out=mx8[:, hh, :], in_=gate[:, hh, :]),
                         reads=[b_gate], writes=[b_mx8], inc=(hh == 3))
                P.op("dve", lambda e: e.tensor_tensor(out=sel[:], in0=gate[:], in1=mx8[:, :, 2:3].to_broadcast([128, 4, 32]),
                                                      op=ALU.is_ge), reads=[b_gate, b_mx8], writes=[b_sel])
                P.op("dve", lambda e, lt=lt: e.tensor_tensor(out=sel[:], in0=sel[:],
                                                             in1=own1h[:, lt:lt + 1, :].to_broadcast([128, 4, 32]),
                                                             op=ALU.max), reads=[b_sel, b_own], writes=[b_sel])
                P.op("dve", lambda e: e.tensor_scalar(out=sel[:], in0=sel[:], scalar1=-1.0, scalar2=-NEGM,
                                                      op0=ALU.add, op1=ALU.mult), reads=[b_sel], writes=[b_sel])
                P.op("dve", lambda e, g=g: e.tensor_tensor(
                    out=negm[:], in0=sel[:], in1=b31[:, 4 * g:4 * g + 4].unsqueeze(2).to_broadcast([128, 4, 32]),
                    op=ALU.add), reads=[b_sel, b_b31], writes=[b_negm])
                for hh in range(4):
                    P.op("pe", lambda e, hh=hh: e.transpose(out=ps_trf[0:32, hh, :], in_=negm[:, hh, :], identity=C.idb[:]),
                         reads=[b_negm, C.b_idb], writes=[b_ps_tr], inc=(hh == 3))
                nT, b_nT = negmT[lt % 2]
                P.op("act", lambda e, nT=nT: e.copy(out=nT, in_=ps_trf[0:32, 0:4, :]), reads=[b_ps_tr], writes=[b_nT])
                O, b_O = Ops[lt % 2]
                kts = list(range(0, min(STRIDE * lt + STRIDE, NKT)))

                def qk_fn(kt, lt=lt, g=g):
                    return [(hh * 128, (hh + 1) * 128,
                             [(KT[:, hh // 2, kt * 128:(kt + 1) * 128], QT[:, 4 * g + hh, lt * 128:(lt + 1) * 128])],
                             [b_KV, b_QT]) for hh in range(4)]

                def extra_fn(kt, lt=lt, nT=nT, b_nT=b_nT):
                    ex = [(ohsel[:, (kt // 2) * 128:(kt // 2 + 1) * 128], nT.rearrange("n h q -> n (h q)"),
                           [b_ohsel, b_nT])]
                    dl = STRIDE * lt - kt
                    if dl <= 7:
                        ex.append((C.antib[:], W[:, dl + 3, :, :].rearrange("p h q -> p (h q)"), [C.b_antib, b_W]))
                    return ex
                attn_steps(P, R, kts, qk_fn, extra_fn, lambda kt, hh: (V[:, kt, hh, :], [b_KV]), O, b_O, 0.125)
                P.op("dve", lambda e, O=O: e.tensor_scalar(out=rz[:], in0=O[:, :, 64:65], scalar1=1e-30, scalar2=None,
                                                           op0=ALU.max), reads=[b_O], writes=[b_rz])
                P.op("dve", lambda e: e.reciprocal(out=rz[:], in_=rz[:]), reads=[b_rz], writes=[b_rz])
                P.op("dve", lambda e, O=O, lt=lt, g=g: e.tensor_tensor(
                    out=o_sb[:, lt, 512 + g * 256:512 + (g + 1) * 256].rearrange("p (h d) -> p h d", d=64),
                    in0=O[:, :, 0:64], in1=rz[:].to_broadcast([128, 4, 64]), op=ALU.mult),
                    reads=[b_O, b_rz], writes=[b_osb])
    for t in range(NT):
        outs.append(P.dma("sp", lambda e, t=t: e.dma_start(out=o_out[t * 128:(t + 1) * 128, :], in_=o_sb[:, t, :]),
                          reads=[b_osb]))
    P.wait_all("sp", outs)
    P.finish()
    return nc


def build_post(NT=16, final=False, NEXP=16):
    nc = bass.Bass("TRN2", target_bir_lowering=False)
    NTOK = NT * 128
    dt = lambda n, s, d, k: nc.dram_tensor(n, s, d, kind=k).ap()
    o_attn = dt("o_attn", [NTOK, 1024], BF16, "ExternalInput")
    x_d = dt("x", [NTOK, D], F32, "ExternalInput")
    mod_d = dt("mod", [6, D], F32, "ExternalInput")
    w_out_d = dt("w_out", [D, D], F32, "ExternalInput")
    g_ffn_d = dt("g_ffn", [1, D], F32, "ExternalInput")
    g_fin_d = dt("g_fin", [1, D], F32, "ExternalInput")
    rw_d = dt("router_w", [D, 16], F32, "ExternalInput")
    rb_d = dt("router_b", [1, 16], F32, "ExternalInput")
    wg_d = dt("wg", [16, D, 512], F32, "ExternalInput")
    wu_d = dt("wu", [16, D, 512], F32, "ExternalInput")
    wd_d = dt("wd", [16, 512, D], F32, "ExternalInput")
    oh16_d = dt("oh16", [16, 16 * 128], F32, "ExternalInput")
    ident = dt("ident", [128, 128], F32, "ExternalInput")
    x_out = dt("x_out", [NTOK, D], F32, "ExternalOutput")

    P = Prog(nc)
    C = Consts(P, nc, ident[:, :])
    ps_a = [(P.psum(f"pa{i}", [128, 512], F32), Buf()) for i in range(4)]
    ps_y = [(P.psum(f"py{i}", [128, 512], F32), Buf()) for i in range(2)]
    ps_w = (P.psum("pw", [128, 512], F32), Buf())
    ps_tr = P.psum("ptr", [128, 8, 128], BF16); b_ps_tr = Buf()
    x_sb = P.sbuf("x_sb", [128, NT, D], F32); b_x = [Buf() for _ in range(NT)]
    hfT = P.sbuf("hfT", [128, 8, NTOK], BF16); b_hfT = Buf()
    WB = [P.sbuf(f"WB{i}", [128, 12288], BF16) for i in range(2)]; b_WB = [Buf(), Buf()]
    bc = P.sbuf("bc", [128, 4, D], F32); b_bc = Buf()
    nsc = norm_scratch(P, "p")
    gf = nsc["h32"]; b_gf = nsc["b"][3]
    P.dma("sp", lambda e: e.dma_start(out=bc[:, 0, :], in_=mod_d[2:3, :].to_broadcast([128, D])), writes=[b_bc])
    P.dma("sp", lambda e: e.dma_start(out=bc[:, 1, :], in_=mod_d[4:5, :].to_broadcast([128, D])), writes=[b_bc])
    P.dma("sp", lambda e: e.dma_start(out=bc[:, 2, :], in_=mod_d[3:4, :].to_broadcast([128, D])), writes=[b_bc])
    P.dma("sp", lambda e: e.dma_start(out=bc[:, 3, :], in_=mod_d[5:6, :].to_broadcast([128, D])), writes=[b_bc])
    P.dma("sp", lambda e: e.dma_start(out=gf[:], in_=g_ffn_d[0:1, :].to_broadcast([128, D])), writes=[b_gf])
    P.op("dve", lambda e: e.scalar_tensor_tensor(out=bc[:, 1, :], in0=bc[:, 1, :], scalar=1.0, in1=gf[:],
                                                 op0=ALU.add, op1=ALU.mult), reads=[b_bc, b_gf], writes=[b_bc])
    wo = WB[1][:, 0:8192].rearrange("p (k n) -> p k n", n=1024)
    wov = w_out_d.rearrange("(k p) n -> p k n", p=128)
    for k in range(8):
        P.dma("pool", lambda e, k=k: e.dma_start(out=wo[:, k, :], in_=wov[:, k, :]), writes=[b_WB[1]])
    for k in range(8):
        P.op("dve", lambda e, k=k: e.tensor_tensor(out=wo[:, k, :], in0=wo[:, k, :], in1=bc[:, 0, :], op=ALU.mult),
             reads=[b_WB[1], b_bc], writes=[b_WB[1]])
    rw = P.sbuf("rw", [128, 8, 16], F32); b_rw = Buf()
    P.dma("sp", lambda e: e.dma_start(out=rw[:], in_=rw_d.rearrange("(k p) n -> p k n", p=128)), writes=[b_rw])
    rb = P.sbuf("rb", [128, 16], F32); b_rb = Buf()
    P.dma("sp", lambda e: e.dma_start(out=rb[:], in_=rb_d[0:1, :].to_broadcast([128, 16])), writes=[b_rb])
    oh16 = P.sbuf("oh16", [16, 16 * 128], F32); b_oh16 = Buf()
    P.dma("sp", lambda e: e.dma_start(out=oh16[:], in_=oh16_d[:, :]), writes=[b_oh16])
    wT = P.sbuf("wT", [16, NTOK], F32); b_wT = Buf()
    ob = [P.sbuf("ob0", [128, 1024], BF16)] * 2; b_ob = [Buf()] * 2
    oT = P.sbuf("oT", [128, 8, 128], BF16); b_oT = Buf()
    h32T = nsc["sq"][:].rearrange("p (k n) -> p k n", n=128); b_h32T = nsc["b"][0]
    r_aff = P.sbuf("r_aff", [128, 16], F32); b_aff = Buf()
    r_b = P.sbuf("r_b", [128, 4, 4], F32); b_rbias = Buf()
    r_t = P.sbuf("r_t", [128, 8, 4], F32); b_rt = Buf()
    r_w = P.sbuf("r_w", [128, 4, 4], F32); b_rw2 = Buf()
    r_s = P.sbuf("r_s", [128, 2], F32); b_rs = Buf()
    for t in range(NT):
        o_t = ob[t % 2]; bo = b_ob[t % 2]
        P.dma("sp", lambda e, o_t=o_t, t=t: e.dma_start(out=o_t[:], in_=o_attn[t * 128:(t + 1) * 128, :]), writes=[bo])
        P.dma("sp", lambda e, t=t: e.dma_start(out=x_sb[:, t, :], in_=x_d[t * 128:(t + 1) * 128, :]), writes=[b_x[t]])
        for k in range(8):
            P.op("pe", lambda e, k=k, o_t=o_t: e.transpose(out=ps_tr[:, k, :], in_=o_t[:, k * 128:(k + 1) * 128],
                                                           identity=C.idb[:]),
                 reads=[bo, C.b_idb], writes=[b_ps_tr], inc=(k == 7))
        P.op("act", lambda e: e.copy(out=oT[:], in_=ps_tr[:]), reads=[b_ps_tr], writes=[b_oT])
        for half in range(2):
            py, b_py = ps_y[half]
            for k in range(8):
                P.op("pe", lambda e, k=k, half=half, py=py: e.matmul(out=py[:], lhsT=oT[:, k, :],
                                                                   rhs=wo[:, k, half * 512:(half + 1) * 512],
                                                                   start=(k == 0), stop=(k == 7)),
                     reads=[b_oT, b_WB[1]], writes=[b_py], inc=(k == 7))
            P.op("dve", lambda e, half=half, py=py, t=t: e.tensor_tensor(
                out=x_sb[:, t, half * 512:(half + 1) * 512], in0=py[:], in1=x_sb[:, t, half * 512:(half + 1) * 512],
                op=ALU.add), reads=[b_py, b_x[t]], writes=[b_x[t]])
        emit_norm_tile(P, C, x_sb[:, t, :], b_x[t], bc[:, 1, :], b_bc, bc[:, 2, :], b_bc,
                       hfT[:, :, t * 128:(t + 1) * 128], b_hfT, nsc, ps_tr, b_ps_tr)
        h32 = nsc["h32"]; b_h32 = nsc["b"][3]
        P.op("dve", lambda e: e.tensor_tensor(out=h32[:], in0=h32[:], in1=bc[:, 2, :], op=ALU.add),
             reads=[b_h32, b_bc], writes=[b_h32])
        for hf_ in range(2):
            pa, b_pa = ps_a[hf_]
            for k in range(4):
                kk = hf_ * 4 + k
                P.op("pe", lambda e, k=k, kk=kk, pa=pa: e.transpose(out=pa[:, k * 128:(k + 1) * 128],
                                                                  in_=h32[:, kk * 128:(kk + 1) * 128], identity=C.idf[:]),
                     reads=[b_h32, C.b_idf], writes=[b_pa], inc=(k == 3))
            P.op("act", lambda e, hf_=hf_, pa=pa: e.copy(out=h32T[:, hf_ * 4:(hf_ + 1) * 4, :],
                                                         in_=pa[:].rearrange("p (k n) -> p k n", n=128)),
                 reads=[b_pa], writes=[b_h32T])
        pw, b_pw = ps_w
        for k in range(8):
            P.op("pe", lambda e, k=k: e.matmul(out=pw[:, 0:16], lhsT=h32T[:, k, :], rhs=rw[:, k, :],
                                               start=(k == 0), stop=(k == 7)),
                 reads=[b_h32T, b_rw], writes=[b_pw], inc=(k == 7))
        P.op("act", lambda e: e.activation(out=r_aff[:], in_=pw[:, 0:16], func=AF.Sigmoid), reads=[b_pw], writes=[b_aff])
        r_bf = r_b[:].rearrange("p g e -> p (g e)")
        P.op("dve", lambda e: e.tensor_tensor(out=r_bf, in0=r_aff[:], in1=rb[:], op=ALU.add),
             reads=[b_aff, b_rb], writes=[b_rbias])
        a_, b_, c_, d_ = (r_b[:, :, i] for i in range(4))
        T = lambda i: r_t[:, i, :]
        seq = [(T(0), a_, b_, ALU.max), (T(1), a_, b_, ALU.min), (T(2), c_, d_, ALU.max), (T(3), c_, d_, ALU.min),
               (T(4), T(0), T(2), ALU.max), (T(5), T(0), T(2), ALU.min), (T(6), T(1), T(3), ALU.max),
               (T(7), T(5), T(6), ALU.max),
               (T(0), T(4), T(7), ALU.add)]
        for (o_, i0, i1, op_) in seq:
            P.op("dve", lambda e, o_=o_, i0=i0, i1=i1, op_=op_: e.tensor_tensor(out=o_, in0=i0, in1=i1, op=op_),
                 reads=[b_rbias, b_rt], writes=[b_rt])
        P.op("dve", lambda e: e.tensor_reduce(out=r_s[:, 0:1], in_=r_t[:, 0, :], axis=AX.X, op=ALU.max),
             reads=[b_rt], writes=[b_rs])
        P.op("dve", lambda e: e.tensor_scalar(out=r_t[:, 1, :], in0=r_t[:, 0, :], scalar1=r_s[:, 0:1], scalar2=None,
                                              op0=ALU.is_ge), reads=[b_rt, b_rs], writes=[b_rt])
        P.op("dve", lambda e: e.tensor_tensor(out=r_w[:], in0=r_b[:], in1=r_t[:, 7, :].unsqueeze(2).to_broadcast([128, 4, 4]),
                                              op=ALU.is_ge), reads=[b_rbias, b_rt], writes=[b_rw2])
        P.op("dve", lambda e: e.tensor_tensor(out=r_w[:], in0=r_w[:], in1=r_t[:, 1, :].unsqueeze(2).to_broadcast([128, 4, 4]),
                                              op=ALU.mult), reads=[b_rw2, b_rt], writes=[b_rw2])
        r_wf = r_w[:].rearrange("p g e -> p (g e)")
        P.op("dve", lambda e: e.tensor_tensor(out=r_wf, in0=r_wf, in1=r_aff[:], op=ALU.mult),
             reads=[b_rw2, b_aff], writes=[b_rw2])
        P.op("dve", lambda e: e.tensor_reduce(out=r_s[:, 1:2], in_=r_wf, axis=AX.X, op=ALU.add),
             reads=[b_rw2], writes=[b_rs])
        P.op("dve", lambda e: e.reciprocal(out=r_s[:, 1:2], in_=r_s[:, 1:2]), reads=[b_rs], writes=[b_rs])
        P.op("dve", lambda e: e.tensor_scalar(out=r_wf, in0=r_wf, scalar1=r_s[:, 1:2], scalar2=None, op0=ALU.mult),
             reads=[b_rw2, b_rs], writes=[b_rw2])
        pa, b_pa = ps_a[2]
        P.op("pe", lambda e, pa=pa: e.transpose(out=pa[0:16, 0:128], in_=r_wf, identity=C.idf[:]),
             reads=[b_rw2, C.b_idf], writes=[b_pa])
        P.op("act", lambda e, pa=pa, t=t: e.copy(out=wT[:, t * 128:(t + 1) * 128], in_=pa[0:16, 0:128]),
             reads=[b_pa], writes=[b_wT])
    wbc = [P.sbuf("wbc0", [128, 512], F32)] * 2; b_wbc = [Buf()] * 2
    sg = [P.sbuf(f"sg{i}", [128, 512], BF16) for i in range(2)]; b_sg = [Buf(), Buf()]
    uw = [P.sbuf(f"uw{i}", [128, 512], BF16) for i in range(2)]; b_uw = [Buf(), Buf()]
    hid = [P.sbuf("hid0", [128, 4, 512], BF16)] * 2; b_hid = [Buf()] * 2
    NTG = NT // 4
    it = 0
    pai = 0
    for ex in range(NEXP):
        Wb = WB[ex % 2]; bW = b_WB[ex % 2]
        wg = Wb[:, 0:4096].rearrange("p (k n) -> p k n", n=512)
        wu = Wb[:, 4096:8192].rearrange("p (k n) -> p k n", n=512)
        wd = Wb[:, 8192:12288].rearrange("p (k n) -> p k n", n=1024)
        wgv = wg_d[ex].rearrange("(k p) n -> p k n", p=128)
        wuv = wu_d[ex].rearrange("(k p) n -> p k n", p=128)
        wdv = wd_d[ex].rearrange("(k p) n -> p k n", p=128)
        for k in range(8):
            P.dma("pool", lambda e, k=k, wg=wg, wgv=wgv: e.dma_start(out=wg[:, k, :], in_=wgv[:, k, :]), writes=[bW])
            P.dma("pool", lambda e, k=k, wu=wu, wuv=wuv: e.dma_start(out=wu[:, k, :], in_=wuv[:, k, :]), writes=[bW])
        for k in range(4):
            P.dma("pool", lambda e, k=k, wd=wd, wdv=wdv: e.dma_start(out=wd[:, k, :], in_=wdv[:, k, :]), writes=[bW])
        for k in range(4):
            P.op("pool", lambda e, k=k, wd=wd: e.tensor_tensor(out=wd[:, k, :], in0=wd[:, k, :], in1=bc[:, 3, :], op=ALU.mult),
                 reads=[bW, b_bc], writes=[bW])
        for tg in range(NTG):
            wb_ = wbc[it % 2]; bwb = b_wbc[it % 2]
            hd = hid[it % 2]; bhd = b_hid[it % 2]
            it += 1
            pw, b_pw = ps_w
            P.op("pe", lambda e, ex=ex, tg=tg: e.matmul(out=pw[:], lhsT=oh16[:, ex * 128:(ex + 1) * 128],
                                                        rhs=wT[:, tg * 512:(tg + 1) * 512], start=True, stop=True),
                 reads=[b_oh16, b_wT], writes=[b_pw])
            P.op("act", lambda e, wb_=wb_: e.copy(out=wb_[:], in_=pw[:]), reads=[b_pw], writes=[bwb])
            for fc in range(4):
                pg, b_pg = ps_a[pai % 4]; pai += 1
                pu, b_pu = ps_a[pai % 4]; pai += 1
                for k in range(8):
                    P.op("pe", lambda e, k=k, fc=fc, pg=pg, wg=wg, tg=tg: e.matmul(
                        out=pg[:], lhsT=wg[:, k, fc * 128:(fc + 1) * 128], rhs=hfT[:, k, tg * 512:(tg + 1) * 512],
                        start=(k == 0), stop=(k == 7)), reads=[bW, b_hfT], writes=[b_pg], inc=(k == 7))
                for k in range(8):
                    P.op("pe", lambda e, k=k, fc=fc, pu=pu, wu=wu, tg=tg: e.matmul(
                        out=pu[:], lhsT=wu[:, k, fc * 128:(fc + 1) * 128], rhs=hfT[:, k, tg * 512:(tg + 1) * 512],
                        start=(k == 0), stop=(k == 7)), reads=[bW, b_hfT], writes=[b_pu], inc=(k == 7))
                s_ = sg[fc % 2]; bs_ = b_sg[fc % 2]
                u_ = uw[fc % 2]; bu_ = b_uw[fc % 2]
                P.op("act", lambda e, pg=pg, s_=s_: e.activation(out=s_[:], in_=pg[:], func=AF.Silu), reads=[b_pg], writes=[bs_])
                P.op("dve", lambda e, pu=pu, u_=u_, wb_=wb_: e.tensor_tensor(out=u_[:], in0=pu[:], in1=wb_[:], op=ALU.mult),
                     reads=[b_pu, bwb], writes=[bu_])
                P.op("pool", lambda e, fc=fc, hd=hd, s_=s_, u_=u_: e.tensor_tensor(out=hd[:, fc, :], in0=s_[:], in1=u_[:],
                                                                                  op=ALU.mult),
                     reads=[bs_, bu_], writes=[bhd])
            for tt in range(4):
                t = tg * 4 + tt
                for half in range(2):
                    py, b_py = ps_y[half]
                    for fc in range(4):
                        P.op("pe", lambda e, fc=fc, tt=tt, half=half, py=py, hd=hd, wd=wd: e.matmul(
                            out=py[:], lhsT=hd[:, fc, tt * 128:(tt + 1) * 128], rhs=wd[:, fc, half * 512:(half + 1) * 512],
                            start=(fc == 0), stop=(fc == 3)), reads=[bhd, bW], writes=[b_py], inc=(fc == 3))
                    P.op("dve", lambda e, half=half, py=py, t=t: e.tensor_tensor(
                        out=x_sb[:, t, half * 512:(half + 1) * 512], in0=py[:], in1=x_sb[:, t, half * 512:(half + 1) * 512],
                        op=ALU.add), reads=[b_py, b_x[t]], writes=[b_x[t]])
    outs = []
    if final:
        gfin = bc[:, 0, :]
        b_gf = b_bc
        P.dma("sp", lambda e: e.dma_start(out=gfin, in_=g_fin_d[0:1, :].to_broadcast([128, D])), writes=[b_gf])
        sq, ss, rstd = nsc["sq"], nsc["ss"], nsc["rstd"]
        b_sq, b_ss, b_rstd = nsc["b"][0:3]
        for t in range(NT):
            P.op("act", lambda e, t=t: e.activation(out=sq[:], in_=x_sb[:, t, :], func=AF.Square, accum_out=ss[:]),
                 reads=[b_x[t]], writes=[b_sq, b_ss])
            P.op("dve", lambda e: e.tensor_scalar(out=rstd[:], in0=ss[:], scalar1=1.0 / D, scalar2=1e-6,
                                                  op0=ALU.mult, op1=ALU.add), reads=[b_ss], writes=[b_rstd])
            P.op("act", lambda e: e.activation(out=rstd[:], in_=rstd[:], func=AF.Sqrt), reads=[b_rstd], writes=[b_rstd])
            P.op("dve", lambda e: e.reciprocal(out=rstd[:], in_=rstd[:]), reads=[b_rstd], writes=[b_rstd])
            P.op("dve", lambda e, t=t: e.scalar_tensor_tensor(out=x_sb[:, t, :], in0=x_sb[:, t, :], scalar=rstd[:, 0:1],
                                                              in1=gfin, op0=ALU.mult, op1=ALU.mult),
                 reads=[b_x[t], b_rstd, b_gf], writes=[b_x[t]])
    for t in range(NT):
        outs.append(P.dma("sp", lambda e, t=t: e.dma_start(out=x_out[t * 128:(t + 1) * 128, :], in_=x_sb[:, t, :]),
                          reads=[b_x[t]]))
    P.wait_all("sp", outs)
    P.finish()
    return nc


def oh16_static():
    oh = np.zeros((16, 16, 128), np.float32)
    for e in range(16):
        oh[e, e, :] = 1.0
    return oh.reshape(16, 2048)


OD = dict(c_q=(0, 256), c_kv=(256, 384), k_rope=(384, 448), q_d=(448, 960), k_d=(960, 1088), v_d=(1088, 1216))
NU1 = 10


def host_w_in_odd(w, wq_up, wkv_up):
    sl = lambda n: w[:, OD[n][0]:OD[n][1]]
    units = []
    for h in range(8):
        u = np.zeros((1024, 128), np.float32)
        u[:, (h % 2) * 64:(h % 2 + 1) * 64] = sl("q_d")[:, h * 64:(h + 1) * 64]
        units.append(u)
    for kv in range(2):
        c = sl("k_d")[:, kv * 64:(kv + 1) * 64]
        units.append(np.concatenate([c, c], axis=1))
    WF = np.concatenate(units, axis=1)
    WT = np.concatenate([sl("c_q"), sl("c_kv"), sl("k_rope"), sl("v_d")], axis=1)
    wq = wq_up.reshape(256, 4, 192)
    wq_nope = np.ascontiguousarray(wq[:, :, 0:128].reshape(256, 512))
    wq_rope = np.ascontiguousarray(wq[:, :, 128:192].reshape(256, 256))
    wkv = wkv_up.reshape(128, 4, 256)
    wk_nope = np.ascontiguousarray(wkv[:, :, 0:128].reshape(128, 512))
    wv = np.ascontiguousarray(wkv[:, :, 128:256].reshape(128, 512))
    return dict(wf=np.ascontiguousarray(WF), wt=np.ascontiguousarray(WT), wq_nope=wq_nope, wq_rope=wq_rope,
                wk_nope=wk_nope, wv=wv)


def rope_static(positions):
    inv = (10000.0 ** (-np.arange(0, 64, 2, dtype=np.float32) / 64)).astype(np.float32)
    ang = positions.astype(np.float32)[:, None] * inv[None, :]
    return np.cos(ang).astype(np.float32), np.sin(ang).astype(np.float32)


def build_L1odd(NT=16):
    nc = bass.Bass("TRN2", target_bir_lowering=False)
    NTOK = NT * 128
    dt = lambda n, s, d, k: nc.dram_tensor(n, s, d, kind=k).ap()
    x = dt("x", [NTOK, D], F32, "ExternalInput")
    c_cols = dt("c_cols", [128, 8], F32, "ExternalInput")
    ada_w = dt("ada_w", [D, 6 * D], F32, "ExternalInput")
    ada_b = dt("ada_b", [1, 6 * D], F32, "ExternalInput")
    g_mix = dt("g_mix", [1, D], F32, "ExternalInput")
    wf_d = dt("wf", [D, NU1 * 128], F32, "ExternalInput")
    wt_d = dt("wt", [D, 576], F32, "ExternalInput")
    wqn_d = dt("wq_nope", [256, 512], F32, "ExternalInput")
    wqr_d = dt("wq_rope", [256, 256], F32, "ExternalInput")
    wkn_d = dt("wk_nope", [128, 512], F32, "ExternalInput")
    wv_d = dt("wv", [128, 512], F32, "ExternalInput")
    qn_g = dt("q_norm", [1, 256], F32, "ExternalInput")
    kvn_g = dt("kv_norm", [1, 128], F32, "ExternalInput")
    cos_d = dt("cos", [NTOK, 32], F32, "ExternalInput")
    sin_d = dt("sin", [NTOK, 32], F32, "ExternalInput")
    ident = dt("ident", [128, 128], F32, "ExternalInput")
    o_fm = dt("o_fm", [128, NU1, NTOK], BF16, "ExternalOutput")
    o_qn = dt("o_qn", [128, 4, NTOK], BF16, "ExternalOutput")
    o_qr = dt("o_qr", [64, 4, NTOK], BF16, "ExternalOutput")
    o_kn = dt("o_kn", [128, 4, NTOK], BF16, "ExternalOutput")
    o_kr = dt("o_kr", [64, NTOK], BF16, "ExternalOutput")
    o_vtok = dt("o_vtok", [NTOK, 640], BF16, "ExternalOutput")
    o_mod = dt("o_mod", [6, D], F32, "ExternalOutput")

    P = Prog(nc)
    C = Consts(P, nc, ident[:, :])
    ps_row = P.psum("ps_row", [128, 512], F32); b_ps_row = Buf()
    ps_bc = P.psum("ps_bc", [128, 512], F32); b_ps_bc = Buf()
    ps_tr = P.psum("ps_tr", [128, 8, 128], BF16); b_ps_tr = Buf()
    ps_mm = [P.psum(f"ps_mm{i}", [128, 512], F32) for i in range(4)]
    b_ps_mm = [Buf() for _ in range(4)]
    mod, b_mod = emit_adaln(P, nc, C, c_cols[:, :], ada_w, ada_b[:, :], "1", ps_row, b_ps_row, ps_bc, b_ps_bc)
    outs = []
    outs.append(P.dma("sp", lambda e: e.dma_start(out=o_mod[:, :], in_=mod[0:1, :, :]), reads=[b_mod]))
    gm = P.sbuf("gm", [128, 1024], F32); b_gm = Buf()
    A = P.sbuf("A_m", [128, 1024], F32); b_A = Buf()
    P.dma("sp", lambda e: e.dma_start(out=gm[:], in_=g_mix[0:1, :].to_broadcast([128, 1024])), writes=[b_gm])
    P.op("dve", lambda e: e.scalar_tensor_tensor(out=A[:], in0=mod[:, 1, :], scalar=1.0, in1=gm[:],
                                                 op0=ALU.add, op1=ALU.mult), reads=[b_mod, b_gm], writes=[b_A])
    Bt = mod[:, 0, :]
    wf, b_wf = load_w_bf16(P, nc, "wf_sb", wf_d, NU1 * 128)
    wt, b_wt = load_w_bf16(P, nc, "wt_sb", wt_d, 576)
    wqn, b_wqn = load_w_bf16(P, nc, "wqn_sb", wqn_d, 512, rows=256)
    wqr, b_wqr = load_w_bf16(P, nc, "wqr_sb", wqr_d, 256, rows=256)
    wkn, b_wkn = load_w_bf16(P, nc, "wkn_sb", wkn_d, 512, rows=128)
    wv, b_wv = load_w_bf16(P, nc, "wv_sb", wv_d, 512, rows=128)
    qng = P.sbuf("qng", [128, 256], F32); b_qng = Buf()
    kvng = P.sbuf("kvng", [128, 128], F32); b_kvng = Buf()
    P.dma("sp", lambda e: e.dma_start(out=qng[:], in_=qn_g[0:1, :].to_broadcast([128, 256])), writes=[b_qng])
    P.dma("sp", lambda e: e.dma_start(out=kvng[:], in_=kvn_g[0:1, :].to_broadcast([128, 128])), writes=[b_kvng])
    cs = P.sbuf("cs", [128, NT, 2, 32], F32); b_cs = Buf()
    P.dma("sp", lambda e: e.dma_start(out=cs[:, :, 0, :], in_=cos_d.rearrange("(t p) n -> p t n", p=128)), writes=[b_cs])
    P.dma("sp", lambda e: e.dma_start(out=cs[:, :, 1, :], in_=sin_d.rearrange("(t p) n -> p t n", p=128)), writes=[b_cs])
    xt = [P.sbuf(f"xt{i}", [128, 1024], F32) for i in range(2)]
    b_xt = [Buf(), Buf()]
    hT = [P.sbuf(f"hT{i}", [128, 8, 512], BF16) for i in range(2)]
    b_hT = [Buf(), Buf()]
    nsc = norm_scratch(P, "a")
    stg = [P.sbuf(f"stg{i}", [128, 512], BF16) for i in range(4)]
    b_stg = [Buf() for _ in range(4)]
    cq = P.sbuf("cq", [128, 384], F32); b_cq = Buf()
    cqn = P.sbuf("cqn", [128, 384], BF16); b_cqn = Buf()
    cT = P.sbuf("cT", [128, 3, 128], BF16); b_cT = Buf()
    mss = P.sbuf("mss", [128, 4], F32); b_mss = Buf()
    junk = P.sbuf("junk", [128, 256], F32); b_junk = Buf()
    rp = P.sbuf("rp", [128, 5, 64], F32); b_rp = Buf()
    rt = P.sbuf("rt", [128, 4, 5, 32], F32); b_rt = Buf()
    rpb = P.sbuf("rpb", [128, 5, 64], BF16); b_rpb = Buf()
    rT = P.sbuf("rT", [64, 5, 128], BF16); b_rT = Buf()
    rr = RR()
    si = 0
    mi = 0

    def next_ps():
        nonlocal mi
        r = (ps_mm[mi % 4], b_ps_mm[mi % 4]); mi += 1
        return r

    def next_stg():
        nonlocal si
        r = (stg[si % 4], b_stg[si % 4]); si += 1
        return r
    for tg in range(NT // 4):
        h = hT[tg % 2]; bh = b_hT[tg % 2]
        for tt in range(4):
            t = tg * 4 + tt
            xb = xt[t % 2]; bx = b_xt[t % 2]
            P.dma("sp", lambda e, xb=xb, t=t: e.dma_start(out=xb[:], in_=x[t * 128:(t + 1) * 128, :]), writes=[bx])
            emit_norm_tile(P, C, xb[:], bx, A[:], b_A, Bt, b_mod, h[:, :, tt * 128:(tt + 1) * 128], bh,
                           nsc, ps_tr, b_ps_tr)
        for u in range(NU1):
            ps, bps = next_ps()
            for k in range(8):
                P.op("pe", lambda e, ps=ps, u=u, k=k, h=h: e.matmul(out=ps[:], lhsT=wf[:, k, u * 128:(u + 1) * 128],
                                                                  rhs=h[:, k, :], start=(k == 0), stop=(k == 7)),
                     reads=[b_wf, bh], writes=[bps], inc=(k == 7))
            st, bst = next_stg()
            evac(P, rr.next(), st[:], ps[:], [bps], [bst])
            outs.append(P.dma("sp", lambda e, st=st, u=u, tg=tg: e.dma_start(
                out=o_fm[:, u, tg * 512:(tg + 1) * 512], in_=st[:]), reads=[bst]))
        for tt in range(4):
            t = tg * 4 + tt
            tsl = slice(tt * 128, (tt + 1) * 128)
            ps, bps = next_ps()
            for k in range(8):
                P.op("pe", lambda e, ps=ps, k=k, h=h, tsl=tsl: e.matmul(out=ps[:, 0:384], lhsT=h[:, k, tsl], rhs=wt[:, k, 0:384],
                                                                      start=(k == 0), stop=(k == 7)),
                     reads=[b_wt, bh], writes=[bps], inc=(k == 7))
            P.op("act", lambda e, ps=ps: e.copy(out=cq[:], in_=ps[:, 0:384]), reads=[bps], writes=[b_cq])
            psB, bpsB = next_ps()
            for k in range(8):
                P.op("pe", lambda e, psB=psB, k=k, h=h, tsl=tsl: e.matmul(out=psB[:, 0:192], lhsT=h[:, k, tsl], rhs=wt[:, k, 384:576],
                                                                        start=(k == 0), stop=(k == 7)),
                     reads=[b_wt, bh], writes=[bpsB], inc=(k == 7))
            st, bst = next_stg()
            P.op("act", lambda e, st=st, psB=psB: e.copy(out=st[:, 0:128], in_=psB[:, 64:192]), reads=[bpsB], writes=[bst])
            outs.append(P.dma("sp", lambda e, st=st, t=t: e.dma_start(out=o_vtok[t * 128:(t + 1) * 128, 512:640], in_=st[:, 0:128]),
                              reads=[bst]))
            P.op("act", lambda e, psB=psB: e.copy(out=rp[:, 4, :], in_=psB[:, 0:64]), reads=[bpsB], writes=[b_rp])
            P.op("act", lambda e: e.activation(out=junk[:, 0:256], in_=cq[:, 0:256], func=AF.Square, accum_out=mss[:, 0:1]),
                 reads=[b_cq], writes=[b_junk, b_mss])
            P.op("act", lambda e: e.activation(out=junk[:, 0:128], in_=cq[:, 256:384], func=AF.Square, accum_out=mss[:, 1:2]),
                 reads=[b_cq], writes=[b_junk, b_mss])
            P.op("dve", lambda e: e.tensor_scalar(out=mss[:, 2:3], in0=mss[:, 0:1], scalar1=1.0 / 256, scalar2=1e-6,
                                                  op0=ALU.mult, op1=ALU.add), reads=[b_mss], writes=[b_mss])
            P.op("dve", lambda e: e.tensor_scalar(out=mss[:, 3:4], in0=mss[:, 1:2], scalar1=1.0 / 128, scalar2=1e-6,
                                                  op0=ALU.mult, op1=ALU.add), reads=[b_mss], writes=[b_mss])
            P.op("act", lambda e: e.activation(out=mss[:, 2:4], in_=mss[:, 2:4], func=AF.Sqrt), reads=[b_mss], writes=[b_mss])
            P.op("dve", lambda e: e.reciprocal(out=mss[:, 2:4], in_=mss[:, 2:4]), reads=[b_mss], writes=[b_mss])
            P.op("dve", lambda e: e.scalar_tensor_tensor(out=cqn[:, 0:256], in0=cq[:, 0:256], scalar=mss[:, 2:3], in1=qng[:],
                                                         op0=ALU.mult, op1=ALU.mult), reads=[b_cq, b_mss, b_qng], writes=[b_cqn])
            P.op("dve", lambda e: e.scalar_tensor_tensor(out=cqn[:, 256:384], in0=cq[:, 256:384], scalar=mss[:, 3:4], in1=kvng[:],
                                                         op0=ALU.mult, op1=ALU.mult), reads=[b_cq, b_mss, b_kvng], writes=[b_cqn])
            for k in range(3):
                P.op("pe", lambda e, k=k: e.transpose(out=ps_tr[:, k, :], in_=cqn[:, k * 128:(k + 1) * 128], identity=C.idb[:]),
                     reads=[b_cqn, C.b_idb], writes=[b_ps_tr], inc=(k == 2))
            P.op("act", lambda e: e.copy(out=cT[:], in_=ps_tr[:, 0:3, :]), reads=[b_ps_tr], writes=[b_cT])
            ps, bps = next_ps()
            for hh in range(4):
                for k in range(2):
                    P.op("pe", lambda e, ps=ps, hh=hh, k=k: e.matmul(
                        out=ps[:, hh * 128:(hh + 1) * 128], lhsT=wqn[:, k, hh * 128:(hh + 1) * 128], rhs=cT[:, k, :],
                        start=(hh == 0 and k == 0), stop=(hh == 3 and k == 1), skip_group_check=True),
                        reads=[b_wqn, b_cT], writes=[bps], inc=(hh == 3 and k == 1))
            st, bst = next_stg()
            evac(P, rr.next(), st[:], ps[:], [bps], [bst])
            outs.append(P.dma("sp", lambda e, st=st, t=t: e.dma_start(
                out=o_qn[:, :, t * 128:(t + 1) * 128], in_=st[:].rearrange("p (h q) -> p h q", q=128)), reads=[bst]))
            ps, bps = next_ps()
            for hh in range(4):
                P.op("pe", lambda e, ps=ps, hh=hh: e.matmul(
                    out=ps[:, hh * 128:(hh + 1) * 128], lhsT=wkn[:, 0, hh * 128:(hh + 1) * 128], rhs=cT[:, 2, :],
                    start=(hh == 0), stop=(hh == 3), skip_group_check=True),
                    reads=[b_wkn, b_cT], writes=[bps], inc=(hh == 3))
            st, bst = next_stg()
            evac(P, rr.next(), st[:], ps[:], [bps], [bst])
            outs.append(P.dma("sp", lambda e, st=st, t=t: e.dma_start(
                out=o_kn[:, :, t * 128:(t + 1) * 128], in_=st[:].rearrange("p (h q) -> p h q", q=128)), reads=[bst]))
            ps, bps = next_ps()
            P.op("pe", lambda e, ps=ps: e.matmul(out=ps[:], lhsT=cT[:, 2, :], rhs=wv[:, 0, :], start=True, stop=True),
                 reads=[b_wv, b_cT], writes=[bps])
            st, bst = next_stg()
            evac(P, rr.next(), st[:], ps[:], [bps], [bst])
            outs.append(P.dma("sp", lambda e, st=st, t=t: e.dma_start(out=o_vtok[t * 128:(t + 1) * 128, 0:512], in_=st[:]),
                              reads=[bst]))
            ps, bps = next_ps()
            for k in range(2):
                P.op("pe", lambda e, ps=ps, k=k: e.matmul(out=ps[:, 0:256], lhsT=cT[:, k, :], rhs=wqr[:, k, :],
                                                          start=(k == 0), stop=(k == 1)),
                     reads=[b_wqr, b_cT], writes=[bps], inc=(k == 1))
            P.op("act", lambda e, ps=ps: e.copy(out=rp[:, 0:4, :], in_=ps[:, 0:256].rearrange("p (h d) -> p h d", d=64)),
                 reads=[bps], writes=[b_rp])
            cosb = cs[:, t, 0, :].unsqueeze(1).to_broadcast([128, 5, 32])
            sinb = cs[:, t, 1, :].unsqueeze(1).to_broadcast([128, 5, 32])
            x1 = rp[:, :, 0:32]; x2 = rp[:, :, 32:64]
            for i_, (a_, b_) in enumerate([(x1, cosb), (x2, sinb), (x1, sinb), (x2, cosb)]):
                P.op("dve", lambda e, i_=i_, a_=a_, b_=b_: e.tensor_tensor(out=rt[:, i_, :, :], in0=a_, in1=b_, op=ALU.mult),
                     reads=[b_rp, b_cs], writes=[b_rt])
            P.op("dve", lambda e: e.tensor_tensor(out=rpb[:, :, 0:32], in0=rt[:, 0, :, :], in1=rt[:, 1, :, :], op=ALU.subtract),
                 reads=[b_rt], writes=[b_rpb])
            P.op("dve", lambda e: e.tensor_tensor(out=rpb[:, :, 32:64], in0=rt[:, 2, :, :], in1=rt[:, 3, :, :], op=ALU.add),
                 reads=[b_rt], writes=[b_rpb])
            for v_ in range(5):
                P.op("pe", lambda e, v_=v_: e.transpose(out=ps_tr[0:64, v_, :], in_=rpb[:, v_, :], identity=C.idb[:]),
                     reads=[b_rpb, C.b_idb], writes=[b_ps_tr], inc=(v_ == 4))
            P.op("act", lambda e: e.copy(out=rT[:], in_=ps_tr[0:64, 0:5, :]), reads=[b_ps_tr], writes=[b_rT])
            outs.append(P.dma("sp", lambda e, t=t: e.dma_start(out=o_qr[:, :, t * 128:(t + 1) * 128], in_=rT[:, 0:4, :]),
                              reads=[b_rT]))
            outs.append(P.dma("sp", lambda e, t=t: e.dma_start(out=o_kr[:, t * 128:(t + 1) * 128], in_=rT[:, 4, :]),
                              reads=[b_rT]))
    P.wait_all("sp", outs)
    P.finish()
    return nc


def attn1_static(NT, STRIDE, j):
    S_ = STRIDE
    st = {}
    pp = np.arange(128)[:, None, None]
    r = np.arange(S_)[None, :, None]
    x = np.arange(128)[None, None, :]
    d = 128 * (r - (S_ - 1)) + 128 * j + x - (127 - pp)
    m = np.where(d >= 0, 0.0, NEGM).astype(np.float32)
    st["wm"] = np.ascontiguousarray(np.stack([m, m], axis=2)).astype(NPBF)
    L = 128 * S_ + 127 + 128
    y = np.arange(L)
    st["oh_swa"] = onehot_table(y - 127 - 128 * (S_ - 1) + 128 * j, "abs", win=128)
    st["cnt_swa"] = pad_counts(NT, STRIDE, j, 128)
    return st


def build_attn1(NT=16, STRIDE=4, NKT=64):
    nc = bass.Bass("TRN2", target_bir_lowering=False)
    NTOK = NT * 128
    NK = NKT * 128
    S_ = STRIDE
    LS = 128 * S_ + 127 + 128
    NAFF = n_aff(STRIDE, 128)
    dt = lambda n, s, d, k: nc.dram_tensor(n, s, d, kind=k).ap()
    qn_d = dt("qn", [128, 4, NTOK], BF16, "ExternalInput")
    qr_d = dt("qr", [64, 4, NTOK], BF16, "ExternalInput")
    qd_d = dt("qd", [128, 8, NTOK], BF16, "ExternalInput")
    kn_d = dt("kn", [128, 4, NK], BF16, "ExternalInput")
    kr_d = dt("kr", [64, NK], BF16, "ExternalInput")
    kd_d = dt("kd", [128, 2, NK], BF16, "ExternalInput")
    vtok = dt("vtok", [NK, 640], BF16, "ExternalInput")
    tab33_d = dt("tab33", [33, 8], F32, "ExternalInput")
    oh_swa = dt("oh_swa", [33, LS], F32, "ExternalInput")
    cnt_swa_d = dt("cnt_swa", [32, NAFF * 128], F32, "ExternalInput")
    sinks_d = dt("sinks", [1, 8], F32, "ExternalInput")
    wm_d = dt("wm", [128, S_, 2, 128], BF16, "ExternalInput")
    ident = dt("ident", [128, 128], F32, "ExternalInput")
    o_out = dt("o_attn", [NTOK, 1024], BF16, "ExternalOutput")
    cswa_scr = dt("cswa_scr", [8, LS], BF16, "Internal")

    P = Prog(nc)
    C = Consts(P, nc, ident[:, :])
    R = AttnRes(P)
    ps_misc = P.psum("ps_misc", [128, 512], F32); b_ps_misc = Buf()
    Ops = [(P.psum(f"O{i}", [128, 512], F32), Buf()) for i in range(2)]
    tab33 = P.sbuf("tab33", [33, 8], F32); b_tab33 = Buf()
    P.dma("sp", lambda e: e.dma_start(out=tab33[:], in_=tab33_d[:, :]), writes=[b_tab33])
    b_scr = build_ctab(P, nc, C, tab33, b_tab33, oh_swa, LS, "cswa", cswa_scr, ps_misc, b_ps_misc)
    exptab = P.sbuf("exptab", [32, 8], F32); b_exptab = Buf()
    P.op("act", lambda e: e.activation(out=exptab[:], in_=tab33[0:32, :], func=AF.Exp), reads=[b_tab33], writes=[b_exptab])
    cnts = P.sbuf("cnts", [32, NAFF * 128], F32); b_cnts = Buf()
    P.dma("sp", lambda e: e.dma_start(out=cnts[:], in_=cnt_swa_d[:, :]), writes=[b_cnts])
    zpad = P.sbuf("zpad", [128, NAFF, 8], F32); b_zpad = Buf()
    for a in range(NAFF):
        P.op("pe", lambda e, a=a: e.matmul(out=ps_misc[:, 0:8], lhsT=cnts[:, a * 128:(a + 1) * 128], rhs=exptab[:, :],
                                           start=True, stop=True), reads=[b_cnts, b_exptab], writes=[b_ps_misc])
        P.op("act", lambda e, a=a: e.copy(out=zpad[:, a, :], in_=ps_misc[:, 0:8]), reads=[b_ps_misc], writes=[b_zpad])
    esink = P.sbuf("esink", [128, 8], F32); b_esink = Buf()
    P.dma("sp", lambda e: e.dma_start(out=esink[:], in_=sinks_d[0:1, :].to_broadcast([128, 8])), writes=[b_esink])
    P.op("act", lambda e: e.activation(out=esink[:], in_=esink[:], func=AF.Exp), reads=[b_esink], writes=[b_esink])
    wm = P.sbuf("wm", [128, S_, 2, 128], BF16); b_wm = Buf()
    P.dma("sp", lambda e: e.dma_start(out=wm[:], in_=wm_d[:, :, :, :]), writes=[b_wm])
    Wswa = P.sbuf("Wswa", [128, S_ + 1, 4, 128], BF16); b_Wswa = Buf()
    QT = P.sbuf("QT", [128, 8, NTOK], BF16); b_QT = Buf()
    KV = P.sbuf("KV", [128, 33280], BF16); b_KV = Buf()
    KrT = P.sbuf("KrT", [64, NK], BF16); b_KrT = Buf()
    o_sb = P.sbuf("o_sb", [128, NT, 1024], BF16); b_osb = Buf()
    rz = P.sbuf("rz", [128, 4, 1], F32); b_rz = Buf()
    P.dma("sp", lambda e: e.dma_start(out=QT[:, 0:4, :], in_=qn_d[:, :, :]), writes=[b_QT])
    P.dma("sp", lambda e: e.dma_start(out=QT[0:64, 4:8, :], in_=qr_d[:, :, :]), writes=[b_QT])
    P.dma("sp", lambda e: e.dma_start(out=KrT[:], in_=kr_d[:, :]), writes=[b_KrT])
    KnT = KV[:, 0:2 * NK].rearrange("p (c k) -> p c k", c=2)
    Vm = KV[:, 2 * NK:2 * NK + NKT * 2 * 129].rearrange("p (k h d) -> p k h d", h=2, d=129)
    sc_mla = float(192 ** -0.5)
    for pp in range(2):
        for hh in range(2):
            P.dma("sp", lambda e, hh=hh, pp=pp: e.dma_start(out=KnT[:, hh, :], in_=kn_d[:, 2 * pp + hh, :]), writes=[b_KV])
        P.op("pool", lambda e: e.memset(Vm[:, :, :, 128:129], 1.0), writes=[b_KV])
        for hh in range(2):
            for k0 in range(0, NKT, 8):
                P.dma("sp", lambda e, hh=hh, pp=pp, k0=k0: e.dma_start(
                    out=Vm[:, k0:k0 + 8, hh, 0:128],
                    in_=vtok[k0 * 128:(k0 + 8) * 128, (2 * pp + hh) * 128:(2 * pp + hh + 1) * 128].rearrange(
                        "(kt p) d -> p kt d", p=128)), writes=[b_KV])
        for lt in range(NT):
            O, b_O = Ops[lt % 2]
            Ov = O[:].rearrange("p (h d) -> p h d", d=256)
            kts = list(range(0, min(S_ * lt + S_, NKT)))

            def qk_fn(kt, lt=lt, pp=pp):
                return [(hh * 128, (hh + 1) * 128,
                         [(KnT[:, hh, kt * 128:(kt + 1) * 128], QT[:, 2 * pp + hh, lt * 128:(lt + 1) * 128]),
                          (KrT[:, kt * 128:(kt + 1) * 128], QT[0:64, 4 + 2 * pp + hh, lt * 128:(lt + 1) * 128])],
                         [b_KV, b_QT, b_KrT]) for hh in range(2)]

            def extra_fn(kt, lt=lt):
                dl = S_ * lt - kt
                if dl <= 0:
                    return [(C.antib[:], wm[:, dl + S_ - 1, :, :].rearrange("p h q -> p (h q)"), [C.b_antib, b_wm])]
                return []
            attn_steps(P, R, kts, qk_fn, extra_fn, lambda kt, hh: (Vm[:, kt, hh, :], [b_KV]), Ov, b_O, sc_mla,
                       nv=129, nh=2)
            P.op("dve", lambda e, Ov=Ov: e.tensor_scalar(out=rz[:, 0:2, :], in0=Ov[:, :, 128:129], scalar1=1e-30, scalar2=None,
                                                         op0=ALU.max), reads=[b_O], writes=[b_rz])
            P.op("dve", lambda e: e.reciprocal(out=rz[:, 0:2, :], in_=rz[:, 0:2, :]), reads=[b_rz], writes=[b_rz])
            P.op("dve", lambda e, Ov=Ov, lt=lt, pp=pp: e.tensor_tensor(
                out=o_sb[:, lt, pp * 256:(pp + 1) * 256].rearrange("p (h d) -> p h d", d=128), in0=Ov[:, :, 0:128],
                in1=rz[:, 0:2, :].to_broadcast([128, 2, 128]), op=ALU.mult), reads=[b_O, b_rz], writes=[b_osb])
    P.dma("sp", lambda e: e.dma_start(out=QT[:], in_=qd_d[:, :, :]), writes=[b_QT])
    KdT = KV[:, 0:NK]
    Vd = KV[:, NK:NK + NKT * 65].rearrange("p (k d) -> p k d", d=65)
    for kv in range(2):
        P.dma("sp", lambda e, kv=kv: e.dma_start(out=KdT, in_=kd_d[:, kv, :]), writes=[b_KV])
        P.op("pool", lambda e: e.memset(Vd[:, :, 64:65], 1.0), writes=[b_KV])
        for k0 in range(0, NKT, 8):
            P.dma("sp", lambda e, kv=kv, k0=k0: e.dma_start(
                out=Vd[:, k0:k0 + 8, 0:64], in_=vtok[k0 * 128:(k0 + 8) * 128, 512 + kv * 64:512 + (kv + 1) * 64].rearrange(
                    "(kt p) d -> p kt d", p=128)), writes=[b_KV])
        toeplitz_load(P, Wswa, b_Wswa, cswa_scr, b_scr, LS, 4 * kv, S_ + 1)
        for lt in range(NT):
            O, b_O = Ops[lt % 2]
            Ov = O[:].rearrange("p (h d) -> p h d", d=128)
            kts = list(range(max(0, S_ * lt - 1), min(S_ * lt + S_, NKT)))

            def qk_fn(kt, lt=lt, kv=kv):
                return [(hh * 128, (hh + 1) * 128,
                         [(KdT[:, kt * 128:(kt + 1) * 128], QT[:, 4 * kv + hh, lt * 128:(lt + 1) * 128])],
                         [b_KV, b_QT]) for hh in range(4)]

            def extra_fn(kt, lt=lt):
                dl = S_ * lt - kt
                return [(C.antib[:], Wswa[:, dl + S_ - 1, :, :].rearrange("p h q -> p (h q)"), [C.b_antib, b_Wswa])]
            attn_steps(P, R, kts, qk_fn, extra_fn, lambda kt, hh: (Vd[:, kt, :], [b_KV]), Ov, b_O, 0.125)
            P.op("dve", lambda e, Ov=Ov, kv=kv: e.tensor_tensor(out=rz[:], in0=Ov[:, :, 64:65],
                                                                in1=esink[:, 4 * kv:4 * kv + 4].unsqueeze(2), op=ALU.add),
                 reads=[b_O, b_esink], writes=[b_rz])
            if lt < NAFF:
                P.op("dve", lambda e, lt=lt, kv=kv: e.tensor_tensor(out=rz[:], in0=rz[:],
                                                                    in1=zpad[:, lt, 4 * kv:4 * kv + 4].unsqueeze(2), op=ALU.add),
                     reads=[b_rz, b_zpad], writes=[b_rz])
            P.op("dve", lambda e: e.reciprocal(out=rz[:], in_=rz[:]), reads=[b_rz], writes=[b_rz])
            P.op("dve", lambda e, Ov=Ov, lt=lt, kv=kv: e.tensor_tensor(
                out=o_sb[:, lt, 512 + kv * 256:512 + (kv + 1) * 256].rearrange("p (h d) -> p h d", d=64),
                in0=Ov[:, :, 0:64], in1=rz[:].to_broadcast([128, 4, 64]), op=ALU.mult),
                reads=[b_O, b_rz], writes=[b_osb])
    outs = []
    for t in range(NT):
        outs.append(P.dma("sp", lambda e, t=t: e.dma_start(out=o_out[t * 128:(t + 1) * 128, :], in_=o_sb[:, t, :]),
                          reads=[b_osb]))
    P.wait_all("sp", outs)
    P.finish()
    return nc


from concourse.bass_utils import run_bass_kernel_spmd

_NT = 16
_STRIDE = 4
_PROGS = {}


def _prog(name, fn):
    if name not in _PROGS:
        _PROGS[name] = fn()
    return _PROGS[name]


def _own(a, j):
    sh = a.shape
    return np.ascontiguousarray(a.reshape((16, 4, 128) + sh[1:])[:, j].reshape((2048,) + sh[1:]))


def _gather_last(parts, blk=128):
    sh = parts[0].shape
    out = np.zeros(sh[:-1] + (64 * blk,), parts[0].dtype)
    o5 = out.reshape(sh[:-1] + (16, 4, blk))
    for r in range(4):
        o5[..., r, :] = parts[r].reshape(sh[:-1] + (16, blk))
    return out


def _gather_rows(parts):
    sh = parts[0].shape
    out = np.zeros((8192,) + sh[1:], parts[0].dtype)
    o5 = out.reshape((16, 4, 128) + sh[1:])
    for r in range(4):
        o5[:, r] = parts[r].reshape((16, 128) + sh[1:])
    return out


def _run(nc, ins):
    res = run_bass_kernel_spmd(nc, ins, core_ids=list(range(8)))
    return res.results


def kernel(x, c, rel_table, router_w, router_b, final_norm, norm_mix, norm_ffn, ada_w, ada_b,
           moe_w_gate, moe_w_up, moe_w_down, ev_w_in, ev_w_out, nsa_pos_k, nsa_pos_v,
           nsa_ck_w1, nsa_ck_w2, nsa_cv_w1, nsa_cv_w2, od_w_in, od_w_out, mla_q_norm,
           mla_kv_norm, mla_w_q_up, mla_w_kv_up, swa_sinks):
    f32 = lambda a: np.ascontiguousarray(np.asarray(a, dtype=np.float32))
    x = f32(x); c = f32(c)
    ident = np.eye(128, dtype=np.float32)
    tab33 = np.concatenate([f32(rel_table), np.ones((1, 8), np.float32)], 0)
    NT, ST = _NT, _STRIDE
    cores = [(cc // 4, cc % 4) for cc in range(8)]
    c_cols = [np.ascontiguousarray(c[b].reshape(8, 128).T) for b in range(2)]

    WF, WP, WT = host_w_in_even(f32(ev_w_in[0]))
    ins = [dict(x=_own(x[b], j), c_cols=c_cols[b], ada_w=f32(ada_w[0]), ada_b=f32(ada_b[0])[None],
                g_mix=f32(norm_mix[0])[None], wf=WF, wp=WP, wt=WT, ident=ident) for (b, j) in cores]
    r1 = _run(_prog("L1", lambda: build_L1(NT)), ins)
    posc = np.ascontiguousarray(np.stack([f32(nsa_pos_k[0]).reshape(16, 128).T, f32(nsa_pos_v[0]).reshape(16, 128).T], 1))
    cw1 = np.ascontiguousarray(np.stack([f32(nsa_ck_w1[0]), f32(nsa_cv_w1[0])], 0))
    cw2k = np.ascontiguousarray(np.concatenate([f32(nsa_ck_w2[0]), f32(nsa_ck_w2[0])], 1))
    G = {}
    for b in range(2):
        fm = [np.asarray(r1[4 * b + r]['o_fm']) for r in range(4)]
        G[b] = dict(kbT=_gather_last([f[:, 16:20] for f in fm]), ksT=_gather_last([f[:, 20:22] for f in fm]),
                    kwT=_gather_last([f[:, 22:24] for f in fm]),
                    kc2=_gather_last([np.asarray(r1[4 * b + r]['o_kc2']) for r in range(4)], blk=64),
                    vtok=_gather_rows([np.asarray(r1[4 * b + r]['o_vtok']) for r in range(4)]))
    ins = []
    for cc, (b, j) in enumerate(cores):
        st = attn0_static(NT, ST, j)
        ins.append(dict(qT=np.ascontiguousarray(np.asarray(r1[cc]['o_fm'])[:, 0:16]), gates=np.asarray(r1[cc]['o_gates']),
                        tab33=tab33, posc=posc, cw1=cw1, cw2k=cw2k, cw2v=f32(nsa_cv_w2[0]), ident=ident, **G[b], **st))
    ra = _run(_prog("A0", lambda: build_attn0(NT, ST, 64)), ins)
    oh16 = oh16_static()
    ins = [dict(o_attn=np.asarray(ra[cc]['o_attn']), x=_own(x[b], j), mod=np.asarray(r1[cc]['o_mod']),
                w_out=f32(ev_w_out[0]), g_ffn=f32(norm_ffn[0])[None], g_fin=f32(final_norm)[None],
                router_w=f32(router_w), router_b=f32(router_b)[None], wg=f32(moe_w_gate[0]), wu=f32(moe_w_up[0]),
                wd=f32(moe_w_down[0]), oh16=oh16, ident=ident) for cc, (b, j) in enumerate(cores)]
    rp0 = _run(_prog("P0", lambda: build_post(NT, final=False)), ins)
    hw = host_w_in_odd(f32(od_w_in[0]), f32(mla_w_q_up[0]), f32(mla_w_kv_up[0]))
    ins = []
    for cc, (b, j) in enumerate(cores):
        pos = (np.arange(64).reshape(16, 4)[:, j][:, None] * 128 + np.arange(128)[None, :]).reshape(-1)
        cos, sin = rope_static(pos)
        ins.append(dict(x=np.asarray(rp0[cc]['x_out']), c_cols=c_cols[b], ada_w=f32(ada_w[1]), ada_b=f32(ada_b[1])[None],
                        g_mix=f32(norm_mix[1])[None], q_norm=f32(mla_q_norm), kv_norm=f32(mla_kv_norm), cos=cos, sin=sin,
                        ident=ident, **hw))
    r2 = _run(_prog("L1o", lambda: build_L1odd(NT)), ins)
    G = {}
    for b in range(2):
        G[b] = dict(kn=_gather_last([np.asarray(r2[4 * b + r]['o_kn']) for r in range(4)]),
                    kr=_gather_last([np.asarray(r2[4 * b + r]['o_kr']) for r in range(4)]),
                    kd=_gather_last([np.asarray(r2[4 * b + r]['o_fm'])[:, 8:10] for r in range(4)]),
                    vtok=_gather_rows([np.asarray(r2[4 * b + r]['o_vtok']) for r in range(4)]))
    ins = []
    for cc, (b, j) in enumerate(cores):
        st = attn1_static(NT, ST, j)
        ins.append(dict(qn=np.asarray(r2[cc]['o_qn']), qr=np.asarray(r2[cc]['o_qr']),
                        qd=np.ascontiguousarray(np.asarray(r2[cc]['o_fm'])[:, 0:8]), tab33=tab33, sinks=f32(swa_sinks),
                        ident=ident, **G[b], **st))
    rb = _run(_prog("A1", lambda: build_attn1(NT, ST, 64)), ins)
    ins = [dict(o_attn=np.asarray(rb[cc]['o_attn']), x=np.asarray(rp0[cc]['x_out']), mod=np.asarray(r2[cc]['o_mod']),
                w_out=f32(od_w_out[0]), g_ffn=f32(norm_ffn[1])[None], g_fin=f32(final_norm)[None],
                router_w=f32(router_w), router_b=f32(router_b)[None], wg=f32(moe_w_gate[1]), wu=f32(moe_w_up[1]),
                wd=f32(moe_w_down[1]), oh16=oh16, ident=ident) for cc, (b, j) in enumerate(cores)]
    rp1 = _run(_prog("P1", lambda: build_post(NT, final=True)), ins)
    out = np.zeros((2, 8192, 1024), np.float32)
    o6 = out.reshape(2, 16, 4, 128, 1024)
    for cc, (b, j) in enumerate(cores):
        o6[b, :, j] = np.asarray(rp1[cc]['x_out']).reshape(16, 128, 1024)
    return out
```

```python
import numpy as np
import concourse.bass as bass
import concourse.mybir as mybir

F32 = mybir.dt.float32
BF16 = mybir.dt.bfloat16
I32 = mybir.dt.int32
U32 = mybir.dt.uint32
AF = mybir.ActivationFunctionType
ALU = mybir.AluOpType
AX = mybir.AxisListType


class Buf:
    __slots__ = ("name", "w", "r")

    def __init__(self, name=""):
        self.name = name
        self.w = None
        self.r = {}


class Prog:
    COMPUTE = ("pe", "act", "dve", "pool")
    DMAQ = ("sp", "pool")

    def __init__(self, nc, n_dma_sems=24, same_engine_sync=True):
        import os
        same_engine_sync = bool(int(os.environ.get('SES', '1' if same_engine_sync else '0')))
        self.nc = nc
        self.q = {e: [] for e in ("pe", "act", "dve", "pool", "sp")}
        self.eng_obj = {"pe": nc.tensor, "act": nc.scalar, "dve": nc.vector,
                        "pool": nc.gpsimd, "sp": nc.sync}
        self.sems = {}
        self.cnt = {}
        self.seen = {e: {} for e in self.q}
        self.same_engine_sync = same_engine_sync
        self._ctx = []
        for e in self.COMPUTE:
            self.sems[e] = self._sem("s_" + e)
            self.cnt[e] = 0
        self.dma_pool = {}
        for e in ("sp", "pool"):
            self.dma_pool[e] = [[self._sem(f"d_{e}{i}"), 0, None] for i in range(n_dma_sems)]
        self.dma_rr = {"sp": 0, "pool": 0}
        self.pending_noinc = {e: False for e in self.COMPUTE}

    def _sem(self, name):
        g = self.nc.semaphore(name)
        s = g.__enter__()
        self._ctx.append(g)
        return s

    def sbuf(self, name, shape, dt):
        g = self.nc.sbuf_tensor("sb_" + name, list(shape), dt)
        t = g.__enter__()
        self._ctx.append(g)
        return t

    def psum(self, name, shape, dt):
        g = self.nc.psum_tensor("ps_" + name, list(shape), dt)
        t = g.__enter__()
        self._ctx.append(g)
        return t

    def _collect(self, eng, reads, writes):
        deps = {}

        def add(tok):
            if tok is None:
                return
            s, v, owner = tok
            k = id(s)
            if k not in deps or deps[k][1] < v:
                deps[k] = (s, v, owner)

        for b in reads:
            add(b.w)
        for b in writes:
            add(b.w)
            for t in b.r.values():
                add(t)
        waits = []
        for k, (s, v, owner) in deps.items():
            if owner == eng and owner in self.COMPUTE:
                if eng == "pe" or not self.same_engine_sync:
                    continue
                if v > self.cnt[eng]:
                    continue
            if self.seen[eng].get(k, -1) >= v:
                continue
            self.seen[eng][k] = v
            waits.append((s, v))
        return waits

    def _mark(self, tok, reads, writes):
        k = id(tok[0])
        for b in reads:
            old = b.r.get(k)
            if old is None or old[1] < tok[1]:
                b.r[k] = tok
        for b in writes:
            b.w = tok
            b.r = {}

    def op(self, eng, fn, reads=(), writes=(), inc=True):
        assert eng in self.COMPUTE
        waits = self._collect(eng, reads, writes)
        if inc:
            self.cnt[eng] += 1
            tok = (self.sems[eng], self.cnt[eng], eng)
            self.pending_noinc[eng] = False
        else:
            tok = (self.sems[eng], self.cnt[eng] + 1, eng)
            self.pending_noinc[eng] = True
        self.q[eng].append((waits, fn, (self.sems[eng], 1) if inc else None))
        self._mark(tok, reads, writes)
        return tok

    def dma(self, eng, fn, reads=(), writes=()):
        pool = self.dma_pool[eng]
        i = self.dma_rr[eng]
        self.dma_rr[eng] = (i + 1) % len(pool)
        ent = pool[i]
        waits = self._collect(eng, reads, writes)
        if ent[2] is not None:
            s, v, _ = ent[2]
            k = id(s)
            if self.seen[eng].get(k, -1) < v:
                self.seen[eng][k] = v
                waits.append((s, v))
        ent[1] += 16
        tok = (ent[0], ent[1], "dma_" + eng)
        ent[2] = tok
        self.q[eng].append((waits, fn, (ent[0], 16)))
        self._mark(tok, reads, writes)
        return tok

    def wait_all(self, eng, toks):
        waits = []
        for tok in toks:
            s, v, _ = tok
            waits.append((s, v))
        self.q[eng].append((waits, None, None))

    def finish(self):
        nc = self.nc
        for e in self.COMPUTE:
            assert not self.pending_noinc[e], f"engine {e} ends with non-inc instruction"
        with nc.Block() as block:
            def run(engname):
                def body(e):
                    for waits, fn, inc in self.q[engname]:
                        for s, v in waits:
                            e.wait_ge(s, v)
                        if fn is not None:
                            ins = fn(e)
                            if inc is not None:
                                ins.then_inc(inc[0], inc[1])
                return body
            if self.q["sp"]:
                block.sync(run("sp"))
            if self.q["pe"]:
                block.tensor(run("pe"))
            if self.q["act"]:
                block.scalar(run("act"))
            if self.q["dve"]:
                block.vector(run("dve"))
            if self.q["pool"]:
                block.gpsimd(run("pool"))
        for g in reversed(self._ctx):
            g.__exit__(None, None, None)
        self._ctx = []


import numpy as np
import ml_dtypes

NPBF = ml_dtypes.bfloat16
D = 1024
S = 8192
NEGM = -30000.0


class RR:
    def __init__(self, engs=("act", "dve")):
        self.engs = engs
        self.i = 0

    def next(self):
        e = self.engs[self.i % len(self.engs)]
        self.i += 1
        return e


def evac(P, eng, out, in_, reads, writes):
    if eng == "act":
        return P.op("act", lambda e: e.copy(out=out, in_=in_), reads=reads, writes=writes)
    return P.op(eng, lambda e: e.tensor_copy(out=out, in_=in_), reads=reads, writes=writes)


class Consts:
    def __init__(self, P, nc, ident_ap):
        self.idf = P.sbuf("c_idf", [128, 128], F32)
        self.idb = P.sbuf("c_idb", [128, 128], BF16)
        self.b_idf = Buf("idf")
        self.b_idb = Buf("idb")
        P.dma("sp", lambda e: e.dma_start(out=self.idf[:], in_=ident_ap), writes=[self.b_idf])
        P.op("dve", lambda e: e.tensor_copy(out=self.idb[:], in_=self.idf[:]),
             reads=[self.b_idf], writes=[self.b_idb])
        self.antib = P.sbuf("c_antib", [128, 128], BF16)
        self.b_antib = Buf("antib")
        P.op("pool", lambda e: e.memset(self.antib[:], 0.0), writes=[self.b_antib])
        P.op("pool", lambda e: e.affine_select(out=self.antib[:], in_=self.antib[:], pattern=[[1, 128]],
                                               compare_op=ALU.not_equal, fill=1.0, base=-127, channel_multiplier=1),
             reads=[self.b_antib], writes=[self.b_antib])
        self.ones_f = P.sbuf("c_ones_f", [128, 128], F32)
        self.b_ones_f = Buf("ones_f")
        P.op("dve", lambda e: e.memset(self.ones_f[:], 1.0), writes=[self.b_ones_f])


def emit_adaln(P, nc, C, c_cols_ap, ada_w_ap, ada_b_ap, tag, psum_row, b_psum_row, psum_bc, b_psum_bc):
    GW = 256
    NG = 6144 // GW
    cc = P.sbuf(f"ada_c{tag}", [128, 8], F32); b_cc = Buf()
    sc = P.sbuf(f"ada_sc{tag}", [128, 8], F32); b_sc = Buf()
    row = [P.sbuf(f"ada_row{tag}{i}", [1, GW], F32) for i in range(2)]; b_row = [Buf(), Buf()]
    mod = P.sbuf(f"ada_mod{tag}", [128, 6, 1024], F32); b_mod = Buf()
    modf = mod[:].rearrange("p a n -> p (a n)")
    wst = [P.sbuf(f"ada_w{tag}_{i}", [128, 8, GW], F32) for i in range(2)]
    b_wst = [Buf(), Buf()]
    P.dma("sp", lambda e: e.dma_start(out=cc[:], in_=c_cols_ap), writes=[b_cc])
    P.dma("sp", lambda e: e.dma_start(out=modf, in_=ada_b_ap.to_broadcast([128, 6144])), writes=[b_mod])
    P.op("act", lambda e: e.activation(out=sc[:], in_=cc[:], func=AF.Silu), reads=[b_cc], writes=[b_sc])
    wv = ada_w_ap.rearrange("(k p) n -> p k n", p=128)
    for g in range(NG):
        w = wst[g % 2]; bw = b_wst[g % 2]
        r = row[g % 2]; br = b_row[g % 2]
        P.dma("sp", lambda e, w=w, g=g: e.dma_start(out=w[:], in_=wv[:, :, g * GW:(g + 1) * GW]), writes=[bw])
        for k in range(8):
            P.op("pe", lambda e, w=w, k=k: e.matmul(out=psum_row[0:1, 0:GW], lhsT=sc[:, k:k + 1], rhs=w[:, k, :],
                                                    start=(k == 0), stop=(k == 7)),
                 reads=[b_sc, bw], writes=[b_psum_row], inc=(k == 7))
        P.op("act", lambda e, r=r: e.copy(out=r[0:1, :], in_=psum_row[0:1, 0:GW]),
             reads=[b_psum_row], writes=[br])
        P.op("pe", lambda e, r=r: e.matmul(out=psum_bc[:, 0:GW], lhsT=C.ones_f[0:1, :], rhs=r[0:1, :],
                                           start=True, stop=True),
             reads=[C.b_ones_f, br], writes=[b_psum_bc])
        P.op("dve", lambda e, g=g: e.tensor_tensor(out=modf[:, g * GW:(g + 1) * GW], in0=psum_bc[:, 0:GW],
                                                   in1=modf[:, g * GW:(g + 1) * GW], op=ALU.add),
             reads=[b_psum_bc, b_mod], writes=[b_mod])
    return mod, b_mod


def emit_norm_tile(P, C, x_t, b_x, A, b_A, Bt, b_B, hT_out, b_hT, scratch, psum_tr, b_psum_tr, tag=""):
    sq, ss, rstd, h32, hb = scratch["sq"], scratch["ss"], scratch["rstd"], scratch["h32"], scratch["hb"]
    b_sq, b_ss, b_rstd, b_h32, b_hb = scratch["b"]
    P.op("act", lambda e: e.activation(out=sq[:], in_=x_t, func=AF.Square, accum_out=ss[:]),
         reads=[b_x], writes=[b_sq, b_ss])
    P.op("dve", lambda e: e.tensor_scalar(out=rstd[:], in0=ss[:], scalar1=1.0 / D, scalar2=1e-6,
                                          op0=ALU.mult, op1=ALU.add), reads=[b_ss], writes=[b_rstd])
    P.op("act", lambda e: e.activation(out=rstd[:], in_=rstd[:], func=AF.Sqrt), reads=[b_rstd], writes=[b_rstd])
    P.op("dve", lambda e: e.reciprocal(out=rstd[:], in_=rstd[:]), reads=[b_rstd], writes=[b_rstd])
    P.op("dve", lambda e: e.scalar_tensor_tensor(out=h32[:], in0=x_t, scalar=rstd[:, 0:1], in1=A,
                                                 op0=ALU.mult, op1=ALU.mult),
         reads=[b_x, b_rstd, b_A], writes=[b_h32])
    P.op("pool", lambda e: e.tensor_tensor(out=hb[:], in0=h32[:], in1=Bt, op=ALU.add),
         reads=[b_h32, b_B], writes=[b_hb])
    for k in range(8):
        P.op("pe", lambda e, k=k: e.transpose(out=psum_tr[:, k, :], in_=hb[:, k * 128:(k + 1) * 128],
                                              identity=C.idb[:]),
             reads=[b_hb, C.b_idb], writes=[b_psum_tr], inc=(k == 7))
    P.op("act", lambda e: e.copy(out=hT_out, in_=psum_tr[:]), reads=[b_psum_tr], writes=[b_hT])


EV = dict(q_a=(0, 512), kc=(512, 640), vc=(640, 768), ks=(768, 896), vs=(896, 1024), kw=(1024, 1152),
          vw=(1152, 1280), gates=(1280, 1304), q_b=(1304, 1816), k_b=(1816, 2328), v_b=(2328, 2840))


def host_w_in_even(w):
    sl = lambda n: w[:, EV[n][0]:EV[n][1]]
    units = []
    for nm in ("q_a", "q_b"):
        for h in range(8):
            u = np.zeros((1024, 128), np.float32)
            u[:, (h % 2) * 64:(h % 2 + 1) * 64] = sl(nm)[:, h * 64:(h + 1) * 64]
            units.append(u)
    for cc in range(4):
        units.append(sl("k_b")[:, cc * 128:(cc + 1) * 128])
    for nm in ("ks", "kw"):
        for kv in range(2):
            c = sl(nm)[:, kv * 64:(kv + 1) * 64]
            units.append(np.concatenate([c, c], axis=1))
    WF = np.concatenate(units, axis=1)
    WP = np.zeros((1024, 4, 2, 128), np.float32)
    for X, (nm, kv) in enumerate([("kc", 0), ("kc", 1), ("vc", 0), ("vc", 1)]):
        cols = sl(nm)[:, kv * 64:(kv + 1) * 64]
        WP[:, X, 0, 0:64] = cols
        WP[:, X, 1, 64:128] = cols
    WT = np.concatenate([sl("vs"), sl("vw"), sl("gates"), sl("v_b")], axis=1)
    return np.ascontiguousarray(WF), np.ascontiguousarray(WP.reshape(1024, 1024)), np.ascontiguousarray(WT)


NU0 = 24
def load_w_bf16(P, nc, name, ap2d, ncols, rows=1024):
    kc = rows // 128
    t = P.sbuf(name, [128, kc, ncols], BF16)
    b = Buf(name)
    v = ap2d.rearrange("(k p) n -> p k n", p=128)
    for k in range(kc):
        P.dma("pool", lambda e, k=k: e.dma_start(out=t[:, k, :], in_=v[:, k, :]), writes=[b])
    return t, b


def norm_scratch(P, tag):
    sc = dict(sq=P.sbuf(f"n_sq{tag}", [128, 1024], F32), ss=P.sbuf(f"n_ss{tag}", [128, 1], F32),
              rstd=P.sbuf(f"n_rstd{tag}", [128, 1], F32), h32=P.sbuf(f"n_h32{tag}", [128, 1024], F32),
              hb=P.sbuf(f"n_hb{tag}", [128, 1024], BF16))
    sc["b"] = [Buf() for _ in range(5)]
    return sc


def build_L1(NT=16):
    nc = bass.Bass("TRN2", target_bir_lowering=False)
    NTOK = NT * 128
    dt = lambda n, s, d, k: nc.dram_tensor(n, s, d, kind=k).ap()
    x = dt("x", [NTOK, D], F32, "ExternalInput")
    c_cols = dt("c_cols", [128, 8], F32, "ExternalInput")
    ada_w = dt("ada_w", [D, 6 * D], F32, "ExternalInput")
    ada_b = dt("ada_b", [1, 6 * D], F32, "ExternalInput")
    g_mix = dt("g_mix", [1, D], F32, "ExternalInput")
    wf_d = dt("wf", [D, NU0 * 128], F32, "ExternalInput")
    wp_d = dt("wp", [D, 1024], F32, "ExternalInput")
    wt_d = dt("wt", [D, 792], F32, "ExternalInput")
    ident = dt("ident", [128, 128], F32, "ExternalInput")
    o_fm = dt("o_fm", [128, NU0, NTOK], BF16, "ExternalOutput")
    o_kc2 = dt("o_kc2", [128, 4, NTOK // 2], BF16, "ExternalOutput")
    o_vtok = dt("o_vtok", [NTOK, 768], BF16, "ExternalOutput")
    o_gates = dt("o_gates", [NTOK, 24], F32, "ExternalOutput")
    o_mod = dt("o_mod", [6, D], F32, "ExternalOutput")

    P = Prog(nc)
    C = Consts(P, nc, ident[:, :])
    ps_row = P.psum("ps_row", [1, 512], F32); b_ps_row = Buf()
    ps_bc = P.psum("ps_bc", [128, 512], F32); b_ps_bc = Buf()
    ps_tr = P.psum("ps_tr", [128, 8, 128], BF16); b_ps_tr = Buf()
    ps_mm = [P.psum(f"ps_mm{i}", [128, 512], F32) for i in range(4)]
    b_ps_mm = [Buf() for _ in range(4)]

    mod, b_mod = emit_adaln(P, nc, C, c_cols[:, :], ada_w, ada_b[:, :], "0", ps_row, b_ps_row, ps_bc, b_ps_bc)
    outs = []
    outs.append(P.dma("sp", lambda e: e.dma_start(out=o_mod[:, :], in_=mod[0:1, :, :]), reads=[b_mod]))
    gm = P.sbuf("gm", [128, 1024], F32); b_gm = Buf()
    A = P.sbuf("A_m", [128, 1024], F32); b_A = Buf()
    P.dma("sp", lambda e: e.dma_start(out=gm[:], in_=g_mix[0:1, :].to_broadcast([128, 1024])), writes=[b_gm])
    P.op("dve", lambda e: e.scalar_tensor_tensor(out=A[:], in0=mod[:, 1, :], scalar=1.0, in1=gm[:],
                                                 op0=ALU.add, op1=ALU.mult), reads=[b_mod, b_gm], writes=[b_A])
    Bt = mod[:, 0, :]
    wf, b_wf = load_w_bf16(P, nc, "wf_sb", wf_d, NU0 * 128)
    wp, b_wp = load_w_bf16(P, nc, "wp_sb", wp_d, 1024)
    wt, b_wt = load_w_bf16(P, nc, "wt_sb", wt_d, 792)
    xt = [P.sbuf(f"xt{i}", [128, 1024], F32) for i in range(2)]
    b_xt = [Buf(), Buf()]
    hT = [P.sbuf(f"hT{i}", [128, 8, 512], BF16) for i in range(2)]
    b_hT = [Buf(), Buf()]
    nsc = norm_scratch(P, "a")
    stg = [P.sbuf(f"stg{i}", [128, 512], BF16) for i in range(4)]
    b_stg = [Buf() for _ in range(4)]
    gst = [P.sbuf(f"gst{i}", [128, 24], F32) for i in range(2)]
    b_gst = [Buf(), Buf()]
    rr = RR()
    si = 0
    mi = 0
    for tg in range(NT // 4):
        h = hT[tg % 2]; bh = b_hT[tg % 2]
        for tt in range(4):
            t = tg * 4 + tt
            xb = xt[t % 2]; bx = b_xt[t % 2]
            P.dma("sp", lambda e, xb=xb, t=t: e.dma_start(out=xb[:], in_=x[t * 128:(t + 1) * 128, :]), writes=[bx])
            emit_norm_tile(P, C, xb[:], bx, A[:], b_A, Bt, b_mod, h[:, :, tt * 128:(tt + 1) * 128], bh,
                           nsc, ps_tr, b_ps_tr)
        for u in range(NU0):
            ps = ps_mm[mi % 4]; bps = b_ps_mm[mi % 4]; mi += 1
            for k in range(8):
                P.op("pe", lambda e, ps=ps, u=u, k=k, h=h: e.matmul(out=ps[:], lhsT=wf[:, k, u * 128:(u + 1) * 128],
                                                                  rhs=h[:, k, :], start=(k == 0), stop=(k == 7)),
                     reads=[b_wf, bh], writes=[bps], inc=(k == 7))
            st = stg[si % 4]; bst = b_stg[si % 4]; si += 1
            evac(P, rr.next(), st[:], ps[:], [bps], [bst])
            outs.append(P.dma("sp", lambda e, st=st, u=u, tg=tg: e.dma_start(
                out=o_fm[:, u, tg * 512:(tg + 1) * 512], in_=st[:]), reads=[bst]))
        for X in range(4):
            ps = ps_mm[mi % 4]; bps = b_ps_mm[mi % 4]; mi += 1
            n = 0
            for lo in range(2):
                for k in range(8):
                    P.op("pe", lambda e, ps=ps, X=X, lo=lo, k=k, h=h, n=n: e.matmul(
                        out=ps[:, 0:256], lhsT=wp[:, k, (X * 2 + lo) * 128:(X * 2 + lo + 1) * 128],
                        rhs=h[:, k, lo:512:2], start=(n == 0), stop=(n == 15)),
                        reads=[b_wp, bh], writes=[bps], inc=(n == 15))
                    n += 1
            st = stg[si % 4]; bst = b_stg[si % 4]; si += 1
            evac(P, rr.next(), st[:, 0:256], ps[:, 0:256], [bps], [bst])
            outs.append(P.dma("sp", lambda e, st=st, X=X, tg=tg: e.dma_start(
                out=o_kc2[:, X, tg * 256:(tg + 1) * 256], in_=st[:, 0:256]), reads=[bst]))
        for tt in range(4):
            t = tg * 4 + tt
            for grp, (c0, c1) in enumerate([(0, 280), (280, 792)]):
                ps = ps_mm[mi % 4]; bps = b_ps_mm[mi % 4]; mi += 1
                w_ = c1 - c0
                for k in range(8):
                    P.op("pe", lambda e, ps=ps, k=k, h=h, tt=tt, c0=c0, c1=c1, w_=w_: e.matmul(
                        out=ps[:, 0:w_], lhsT=h[:, k, tt * 128:(tt + 1) * 128], rhs=wt[:, k, c0:c1],
                        start=(k == 0), stop=(k == 7)), reads=[b_wt, bh], writes=[bps], inc=(k == 7))
                st = stg[si % 4]; bst = b_stg[si % 4]; si += 1
                if grp == 0:
                    evac(P, rr.next(), st[:, 0:256], ps[:, 0:256], [bps], [bst])
                    outs.append(P.dma("sp", lambda e, st=st, t=t: e.dma_start(
                        out=o_vtok[t * 128:(t + 1) * 128, 0:256], in_=st[:, 0:256]), reads=[bst]))
                    g = gst[t % 2]; bg = b_gst[t % 2]
                    P.op("act", lambda e, g=g, ps=ps: e.activation(out=g[:], in_=ps[:, 256:280], func=AF.Sigmoid),
                         reads=[bps], writes=[bg])
                    outs.append(P.dma("sp", lambda e, g=g, t=t: e.dma_start(
                        out=o_gates[t * 128:(t + 1) * 128, :], in_=g[:]), reads=[bg]))
                else:
                    evac(P, rr.next(), st[:], ps[:], [bps], [bst])
                    outs.append(P.dma("sp", lambda e, st=st, t=t: e.dma_start(
                        out=o_vtok[t * 128:(t + 1) * 128, 256:768], in_=st[:]), reads=[bst]))
    P.wait_all("sp", outs)
    P.finish()
    return nc


def rel_bucket_np(d):
    d = np.maximum(d, 0)
    lp = 16 + (np.log(np.maximum(d, 1).astype(np.float32) / 16) / np.float32(np.log(1024 / 16)) * 16).astype(np.int32)
    return np.where(d < 16, d, np.minimum(lp, 31))


def onehot_table(dvals, mode, win=None):
    L = len(dvals)
    oh = np.zeros((33, L), np.float32)
    ok = dvals >= 0
    if win is not None:
        ok &= dvals < win
    b = rel_bucket_np(dvals)
    idx = np.nonzero(ok)[0]
    oh[b[idx], idx] += 8.0
    if mode == "rel":
        oh[31, idx] -= 8.0
    oh[32, ~ok] = NEGM
    return oh


def build_ctab(P, nc, C, tab33, b_tab33, oh_dram, L, name, scratch_dram, ps, b_ps):
    b_scr = Buf(name + "_scr")
    oh = P.sbuf(name + "_oh", [33, 512], F32); b_oh = Buf()
    cb = P.sbuf(name + "_cb", [8, 512], BF16); b_cb = Buf()
    for c0 in range(0, L, 512):
        w = min(512, L - c0)
        P.dma("sp", lambda e, c0=c0, w=w: e.dma_start(out=oh[:, 0:w], in_=oh_dram[:, c0:c0 + w]), writes=[b_oh])
        P.op("pe", lambda e, w=w: e.matmul(out=ps[0:8, 0:w], lhsT=tab33[:, :], rhs=oh[:, 0:w], start=True, stop=True),
             reads=[b_tab33, b_oh], writes=[b_ps])
        P.op("act", lambda e, w=w: e.copy(out=cb[:, 0:w], in_=ps[0:8, 0:w]), reads=[b_ps], writes=[b_cb])
        P.dma("sp", lambda e, c0=c0, w=w: e.dma_start(out=scratch_dram[:, c0:c0 + w], in_=cb[:, 0:w]),
              reads=[b_cb], writes=[b_scr])
    return b_scr


def toeplitz_load(P, Wt, b_W, scratch_dram, b_scr, L, h0, nR, rstride=128, pstride=1, base=0, nh=4, width=128):
    from concourse.bass_types import AP
    for hh in range(nh):
        src = AP(scratch_dram.tensor, scratch_dram.offset + (h0 + hh) * L + base,
                 [[pstride, 128], [rstride, nR], [1, width]])
        P.dma("sp", lambda e, hh=hh, src=src: e.dma_start(out=Wt[:, :, hh, :], in_=src),
              reads=[b_scr], writes=[b_W])


class AttnRes:
    def __init__(self, P, nS=2, nP=3):
        self.S = [(P.psum(f"at_S{i}", [128, 512], F32), Buf()) for i in range(nS)]
        self.Pt = [(P.sbuf(f"at_P{i}", [128, 512], BF16), Buf()) for i in range(nP)]
        self.si = 0
        self.pi = 0

    def nextS(self):
        r = self.S[self.si % len(self.S)]; self.si += 1
        return r

    def nextP(self):
        r = self.Pt[self.pi % len(self.Pt)]; self.pi += 1
        return r


def attn_steps(P, R, kts, qk_fn, extra_fn, v_fn, O, b_O, scale, nv=65, post_fn=None, bias_ap=None, b_bias=None, nh=4):
    n = len(kts)

    def emit_qk(kt):
        S, b_S = R.nextS()
        ex = extra_fn(kt) if extra_fn else []
        qk = qk_fn(kt)
        for qi, (c0, c1, mms, reads) in enumerate(qk):
            for mi, (lhsT, rhs) in enumerate(mms):
                last = (not ex) and qi == len(qk) - 1 and mi == len(mms) - 1
                P.op("pe", lambda e, S=S, c0=c0, c1=c1, lhsT=lhsT, rhs=rhs, mi=mi, qi=qi, last=last: e.matmul(
                    out=S[:, c0:c1], lhsT=lhsT, rhs=rhs, start=(mi == 0 and qi == 0), stop=last,
                    skip_group_check=True),
                    reads=reads, writes=[b_S], inc=last)
        for xi, (lhsT, rhs, reads) in enumerate(ex):
            P.op("pe", lambda e, S=S, lhsT=lhsT, rhs=rhs, xi=xi, nx=len(ex): e.matmul(
                out=S[:, 0:nh * 128], lhsT=lhsT, rhs=rhs, start=False, stop=(xi == nx - 1), skip_group_check=True),
                reads=reads, writes=[b_S], inc=(xi == len(ex) - 1))
        Pt, b_P = R.nextP()
        if bias_ap is None:
            P.op("act", lambda e, S=S, Pt=Pt: e.activation(out=Pt[:, 0:nh * 128], in_=S[:, 0:nh * 128], func=AF.Exp, scale=scale),
                 reads=[b_S], writes=[b_P])
        else:
            P.op("act", lambda e, S=S, Pt=Pt: e.activation(out=Pt[:, 0:nh * 128], in_=S[:, 0:nh * 128], func=AF.Exp, scale=scale,
                                                           bias=bias_ap), reads=[b_S, b_bias], writes=[b_P])
        return Pt, b_P

    def emit_pv(ii, kt, Pt, b_P):
        for hh in range(nh):
            rhs, reads = v_fn(kt, hh)
            P.op("pe", lambda e, Pt=Pt, hh=hh, rhs=rhs, ii=ii: e.matmul(
                out=O[:, hh, 0:nv], lhsT=Pt[:, hh * 128:(hh + 1) * 128], rhs=rhs, start=(ii == 0 and hh == 0),
                stop=(ii == n - 1 and hh == nh - 1), skip_group_check=True),
                reads=[b_P] + reads, writes=[b_O], inc=(hh == nh - 1 and post_fn is None))
        if post_fn is not None:
            post_fn(kt, ii, n, Pt, b_P)

    pend = None
    for ii, kt in enumerate(kts):
        cur = emit_qk(kt)
        if pend is not None:
            emit_pv(*pend)
        pend = (ii, kt, cur[0], cur[1])
    if pend is not None:
        emit_pv(*pend)


def moba_static(NT, STRIDE, j):
    LC = 11 * 128 + 127
    y = np.arange(LC)
    d = y - 127 - 384 + 128 * j
    oh_rel = onehot_table(d, "rel")
    ohsel = np.zeros((32, 32, 128), np.float32)
    for n in range(32):
        ohsel[n, n, :] = 1.0
    negvalid = np.zeros((NT, 32), np.float32)
    own1h = np.zeros((NT, 32), np.float32)
    for lt in range(NT):
        own = (STRIDE * lt + j) // 2
        negvalid[lt, own:] = -1e30
        own1h[lt, own] = 1.0
    return dict(oh_rel=oh_rel, ohsel=ohsel.reshape(32, 4096).astype(NPBF), negvalid=negvalid.reshape(1, -1),
                own1h=own1h.reshape(1, -1))


def gelu_tanh_ops(P, u, b_u, t, b_t, out_bf, b_out, width):
    P.op("dve", lambda e: e.tensor_tensor(out=t[:, 0:width], in0=u[:, 0:width], in1=u[:, 0:width], op=ALU.mult),
         reads=[b_u], writes=[b_t])
    P.op("dve", lambda e: e.tensor_scalar(out=t[:, 0:width], in0=t[:, 0:width], scalar1=0.044715, scalar2=1.0,
                                          op0=ALU.mult, op1=ALU.add), reads=[b_t], writes=[b_t])
    P.op("dve", lambda e: e.tensor_tensor(out=t[:, 0:width], in0=t[:, 0:width], in1=u[:, 0:width], op=ALU.mult),
         reads=[b_t, b_u], writes=[b_t])
    P.op("act", lambda e: e.activation(out=t[:, 0:width], in_=t[:, 0:width], func=AF.Sigmoid, scale=1.5957691216057308),
         reads=[b_t], writes=[b_t])
    P.op("dve", lambda e: e.tensor_tensor(out=out_bf, in0=t[:, 0:width], in1=u[:, 0:width], op=ALU.mult),
         reads=[b_t, b_u], writes=[b_out])


def attn0_static(NT, STRIDE, j, NKT=64):
    st = moba_static(NT, STRIDE, j)
    LW = 8 * 128 + 127
    y = np.arange(LW)
    st["oh_win"] = onehot_table(y - 127 - 384 + 128 * j, "abs", win=512)
    NRC = -(-2853 // (128 * STRIDE))
    LCM = 128 * STRIDE * (NRC - 1) + 127 + 16 * 127 + 1
    y = np.arange(LCM)
    st["oh_cmp"] = onehot_table(y + 128 * j - 2063, "abs")
    n = np.arange(512)
    cs = n * 16
    ce = cs + 31
    m = np.arange(128) * 64
    ov = ((cs[:, None] <= m[None, :] + 63) & (ce[:, None] >= m[None, :])).astype(np.float32)
    ov[511] = 0
    st["overlap"] = ov.reshape(4, 128, 128).transpose(1, 0, 2).reshape(128, 512).astype(NPBF)
    E = np.zeros((128, NKT * 128), np.float32)
    keys = np.arange(NKT * 128)
    E[keys // 64, keys] = 1.0
    st["E"] = E.astype(NPBF)
    OFF = 2 * STRIDE * (NT - 1)
    width = OFF + 128
    Fw = np.zeros((128, width), np.float32)
    q = np.arange(128)[:, None]
    xx = np.arange(width)[None, :]
    rel = xx - OFF - 2 * j
    hq = (q >= 64).astype(np.int64)
    Fw[np.broadcast_to(rel > hq, Fw.shape)] = -1e30
    Fw[np.broadcast_to((rel == hq) | (rel == hq - 1), Fw.shape)] = 1e30
    st["fwide"] = Fw
    st["cnt_win"] = pad_counts(NT, STRIDE, j, 512)
    return st


def n_aff(STRIDE, win):
    return -(-(win // 128) // STRIDE)


def pad_counts(NT, STRIDE, j, win):
    na = n_aff(STRIDE, win)
    cnt = np.zeros((32, na * 128), np.float32)
    for a in range(na):
        for q in range(128):
            t = 128 * (STRIDE * a + j) + q
            if t + 1 <= win - 1:
                d = np.arange(t + 1, win)
                bb = rel_bucket_np(d)
                cnt[:, a * 128 + q] = np.bincount(bb, minlength=32)
    return cnt


def build_attn0(NT=16, STRIDE=4, NKT=64, do_nsa=True, do_moba=True):
    nc = bass.Bass("TRN2", target_bir_lowering=False)
    NTOK = NT * 128
    NK = NKT * 128
    LC = 11 * 128 + 127
    LW = 8 * 128 + 127
    NRC = -(-2853 // (128 * STRIDE))
    LCM = 128 * STRIDE * (NRC - 1) + 127 + 16 * 127 + 1
    OFFW = 2 * STRIDE * (NT - 1)
    dt = lambda n, s, d, k: nc.dram_tensor(n, s, d, kind=k).ap()
    qT_d = dt("qT", [128, 16, NTOK], BF16, "ExternalInput")
    gates_d = dt("gates", [NTOK, 24], F32, "ExternalInput")
    kbT = dt("kbT", [128, 4, NK], BF16, "ExternalInput")
    ksT = dt("ksT", [128, 2, NK], BF16, "ExternalInput")
    kwT = dt("kwT", [128, 2, NK], BF16, "ExternalInput")
    kc2 = dt("kc2", [128, 4, NK // 2], BF16, "ExternalInput")
    vtok = dt("vtok", [NK, 768], BF16, "ExternalInput")
    tab33_d = dt("tab33", [33, 8], F32, "ExternalInput")
    oh_rel = dt("oh_rel", [33, LC], F32, "ExternalInput")
    oh_win = dt("oh_win", [33, LW], F32, "ExternalInput")
    oh_cmp = dt("oh_cmp", [33, LCM], F32, "ExternalInput")
    ohsel_d = dt("ohsel", [32, 32 * 128], BF16, "ExternalInput")
    negvalid_d = dt("negvalid", [1, NT * 32], F32, "ExternalInput")
    own1h_d = dt("own1h", [1, NT * 32], F32, "ExternalInput")
    overlap_d = dt("overlap", [128, 512], BF16, "ExternalInput")
    E_d = dt("E", [128, NK], BF16, "ExternalInput")
    fwide_d = dt("fwide", [128, OFFW + 128], F32, "ExternalInput")
    NAFF = n_aff(STRIDE, 512)
    cnt_win_d = dt("cnt_win", [32, NAFF * 128], F32, "ExternalInput")
    posc_d = dt("posc", [128, 2, 16], F32, "ExternalInput")
    w1_d = dt("cw1", [2, 2048, 256], F32, "ExternalInput")
    w2k_d = dt("cw2k", [256, 128], F32, "ExternalInput")
    w2v_d = dt("cw2v", [256, 64], F32, "ExternalInput")
    ident = dt("ident", [128, 128], F32, "ExternalInput")
    o_out = dt("o_attn", [NTOK, 1024], BF16, "ExternalOutput")
    crel_scr = dt("crel_scr", [8, LC], BF16, "Internal")
    cwin_scr = dt("cwin_scr", [8, LW], BF16, "Internal")
    ccmp_scr = dt("ccmp_scr", [8, LCM], BF16, "Internal")

    P = Prog(nc)
    C = Consts(P, nc, ident[:, :])
    R = AttnRes(P)
    ps_misc = P.psum("ps_misc", [128, 512], F32); b_ps_misc = Buf()
    ps_trf = P.psum("ps_trb", [128, 8, 128], BF16); b_ps_tr = Buf()
    Ops = [(P.psum(f"O{i}", [128, 4, 128], F32), Buf()) for i in range(3)]
    IMP = P.psum("IMP", [128, 4, 128], F32); b_IMP = Buf()
    tab33 = P.sbuf("tab33", [33, 8], F32); b_tab33 = Buf()
    P.dma("sp", lambda e: e.dma_start(out=tab33[:], in_=tab33_d[:, :]), writes=[b_tab33])
    b_scr_rel = build_ctab(P, nc, C, tab33, b_tab33, oh_rel, LC, "crel", crel_scr, ps_misc, b_ps_misc)
    b31 = P.sbuf("b31", [128, 8], F32); b_b31 = Buf()
    P.dma("sp", lambda e: e.dma_start(out=b31[:], in_=tab33_d[31:32, :].to_broadcast([128, 8])), writes=[b_b31])
    P.op("dve", lambda e: e.tensor_scalar(out=b31[:], in0=b31[:], scalar1=8.0, scalar2=None, op0=ALU.mult),
         reads=[b_b31], writes=[b_b31])
    QT = P.sbuf("QT", [128, 8, NTOK], BF16); b_QT = Buf()
    KV = P.sbuf("KV", [128, 33280], BF16); b_KV = Buf()
    o_sb = P.sbuf("o_sb", [128, NT, 1024], BF16); b_osb = Buf()
    W = P.sbuf("W", [128, 11, 4, 128], BF16); b_W = Buf()
    rz = P.sbuf("rz", [128, 4, 1], F32); b_rz = Buf()
    outs = []
    Esb = P.sbuf("Esb", [128, NK], BF16); b_E = Buf()
    u32 = P.sbuf("u32", [128, 512], F32); b_u32 = Buf()
    t32 = P.sbuf("t32", [128, 512], F32); b_t32 = Buf()
    Wwin = P.sbuf("Wwin", [128, 8, 4, 128], BF16); b_Wwin = Buf()
    Wc = P.sbuf("Wc", [128, NRC, 4, 128], BF16); b_Wc = Buf()

    if do_nsa:
        b_scr_win = build_ctab(P, nc, C, tab33, b_tab33, oh_win, LW, "cwin", cwin_scr, ps_misc, b_ps_misc)
        b_scr_cmp = build_ctab(P, nc, C, tab33, b_tab33, oh_cmp, LCM, "ccmp", ccmp_scr, ps_misc, b_ps_misc)
        P.dma("sp", lambda e: e.dma_start(out=Esb[:], in_=E_d[:, :]), writes=[b_E])
        ovl = P.sbuf("ovl", [128, 4, 128], BF16); b_ovl = Buf()
        P.dma("sp", lambda e: e.dma_start(out=ovl[:].rearrange("p a m -> p (a m)"), in_=overlap_d[:, :]), writes=[b_ovl])
        fw = P.sbuf("fw", [128, OFFW + 128], F32); b_fw = Buf()
        P.dma("sp", lambda e: e.dma_start(out=fw[:], in_=fwide_d[:, :]), writes=[b_fw])
        exptab = P.sbuf("exptab", [32, 8], F32); b_exptab = Buf()
        P.op("act", lambda e: e.activation(out=exptab[:], in_=tab33[0:32, :], func=AF.Exp), reads=[b_tab33], writes=[b_exptab])
        cntw = P.sbuf("cntw", [32, NAFF * 128], F32); b_cntw = Buf()
        P.dma("sp", lambda e: e.dma_start(out=cntw[:], in_=cnt_win_d[:, :]), writes=[b_cntw])
        zpad = P.sbuf("zpad", [128, NAFF, 8], F32); b_zpad = Buf()
        for a in range(NAFF):
            P.op("pe", lambda e, a=a: e.matmul(out=ps_misc[:, 0:8], lhsT=cntw[:, a * 128:(a + 1) * 128], rhs=exptab[:, :],
                                               start=True, stop=True), reads=[b_cntw, b_exptab], writes=[b_ps_misc])
            P.op("act", lambda e, a=a: e.copy(out=zpad[:, a, :], in_=ps_misc[:, 0:8]), reads=[b_ps_misc], writes=[b_zpad])
        gts = P.sbuf("gts", [128, NT, 24], F32); b_gts = Buf()
        P.dma("sp", lambda e: e.dma_start(out=gts[:], in_=gates_d.rearrange("(t p) n -> p t n", p=128)), writes=[b_gts])
        P.dma("sp", lambda e: e.dma_start(out=QT[:], in_=qT_d[:, 0:8, :]), writes=[b_QT])
        posc = P.sbuf("posc", [128, 2, 16], F32); b_posc = Buf()
        poscb = P.sbuf("poscb", [128, 2, 16], BF16); b_poscb = Buf()
        P.dma("sp", lambda e: e.dma_start(out=posc[:], in_=posc_d[:, :, :]), writes=[b_posc])
        P.op("dve", lambda e: e.tensor_copy(out=poscb[:], in_=posc[:]), reads=[b_posc], writes=[b_poscb])
        w2k = P.sbuf("w2k", [128, 2, 128], BF16); b_w2k = Buf()
        w2v = P.sbuf("w2v", [128, 2, 64], BF16); b_w2v = Buf()
        P.dma("pool", lambda e: e.dma_start(out=w2k[:], in_=w2k_d.rearrange("(k p) n -> p k n", p=128)), writes=[b_w2k])
        P.dma("pool", lambda e: e.dma_start(out=w2v[:], in_=w2v_d.rearrange("(k p) n -> p k n", p=128)), writes=[b_w2v])
        w1 = KV[:, 8192:8192 + 4096].rearrange("p (k n) -> p k n", n=256); b_w1 = Buf()
        kc2sb = KV[:, 0:NK // 2]; b_kc2 = Buf()
        bias1 = P.sbuf("bias1", [128, 2], F32); b_bias1 = Buf()
        GT = P.sbuf("GT", [128, 2, 512], BF16); b_GT = Buf()
        P.op("pool", lambda e: e.memset(GT[:], 0.0), writes=[b_GT])
        KcT = P.sbuf("KcT", [128, 2, 512], BF16); b_KcT = Buf()
        Vc = P.sbuf("Vc", [128, 2, 4, 65], BF16); b_Vc = Buf()
        P.op("pool", lambda e: e.memset(KcT[:], 0.0), writes=[b_KcT])
        P.op("pool", lambda e: e.memset(Vc[:], 1.0), writes=[b_Vc])
        ncmp = NK // 16 - 1
        for kvt in range(2):
            for k in range(16):
                P.dma("pool", lambda e, k=k, kvt=kvt: e.dma_start(out=w1[:, k, :], in_=w1_d[kvt, k * 128:(k + 1) * 128, :]),
                      writes=[b_w1])
            for hc in range(2):
                for k in range(16):
                    P.op("pe", lambda e, hc=hc, k=k, kvt=kvt: e.matmul(
                        out=ps_misc[:, 0:1], lhsT=w1[:, k, hc * 128:(hc + 1) * 128], rhs=poscb[:, kvt, k:k + 1],
                        start=(k == 0), stop=(k == 15)), reads=[b_w1, b_poscb], writes=[b_ps_misc], inc=(k == 15))
                P.op("act", lambda e, hc=hc: e.copy(out=bias1[:, hc:hc + 1], in_=ps_misc[:, 0:1]),
                     reads=[b_ps_misc], writes=[b_bias1])
            for kv in range(2):
                X = kvt * 2 + kv
                P.dma("sp", lambda e, X=X: e.dma_start(out=kc2sb, in_=kc2[:, X, :]), writes=[b_kc2])
                for hc in range(2):
                    for k in range(16):
                        P.op("pe", lambda e, hc=hc, k=k: e.matmul(
                            out=ps_misc[:, 0:ncmp], lhsT=w1[:, k, hc * 128:(hc + 1) * 128],
                            rhs=kc2sb[:, k:k + 8 * (ncmp - 1) + 1:8], start=(k == 0), stop=(k == 15)),
                            reads=[b_w1, b_kc2], writes=[b_ps_misc], inc=(k == 15))
                    P.op("act", lambda e, hc=hc: e.activation(out=u32[:, 0:ncmp], in_=ps_misc[:, 0:ncmp], func=AF.Identity,
                                                              bias=bias1[:, hc:hc + 1]),
                         reads=[b_ps_misc, b_bias1], writes=[b_u32])
                    gelu_tanh_ops(P, u32, b_u32, t32, b_t32, GT[:, hc, 0:ncmp], b_GT, ncmp)
                if kvt == 0:
                    for hc in range(2):
                        P.op("pe", lambda e, hc=hc: e.matmul(out=ps_misc[:, 0:512], lhsT=w2k[:, hc, :], rhs=GT[:, hc, :],
                                                             start=(hc == 0), stop=(hc == 1)),
                             reads=[b_w2k, b_GT], writes=[b_ps_misc], inc=(hc == 1))
                    P.op("act", lambda e, kv=kv: e.copy(out=KcT[:, kv, :], in_=ps_misc[:, 0:512]),
                         reads=[b_ps_misc], writes=[b_KcT])
                else:
                    for nc_ in range(4):
                        for hc in range(2):
                            P.op("pe", lambda e, hc=hc, nc_=nc_: e.matmul(
                                out=ps_misc[:, nc_ * 64:(nc_ + 1) * 64], lhsT=GT[:, hc, nc_ * 128:(nc_ + 1) * 128],
                                rhs=w2v[:, hc, :], start=(hc == 0 and nc_ == 0), stop=(hc == 1 and nc_ == 3),
                                skip_group_check=True),
                                reads=[b_w2v, b_GT], writes=[b_ps_misc], inc=(hc == 1 and nc_ == 3))
                    P.op("act", lambda e, kv=kv: e.copy(out=Vc[:, kv, :, 0:64],
                                                        in_=ps_misc[:, 0:256].rearrange("p (a d) -> p a d", d=64)),
                         reads=[b_ps_misc], writes=[b_Vc])
        KsT = KV[:, 0:NK]
        KwT = KV[:, NK:2 * NK]
        Vs = KV[:, 2 * NK:2 * NK + NKT * 65].rearrange("p (k d) -> p k d", d=65)
        Vw = KV[:, 2 * NK + NKT * 65:2 * NK + 2 * NKT * 65].rearrange("p (k d) -> p k d", d=65)
        imp = P.sbuf("imp", [128, 128], F32); b_imp = Buf()
        sc2 = P.sbuf("sc2", [128, 128], F32); b_sc2 = Buf()
        m8 = P.sbuf("m8", [128, 2, 8], F32); b_m8 = Buf()
        negm4 = P.sbuf("negm4", [128, 4, 128], BF16); b_negm4 = Buf()
        nT4 = [(P.sbuf(f"nT4_{i}", [128, 4, 128], BF16), Buf()) for i in range(2)]
        b31row = P.sbuf("b31row", [1, 2, 4, 128], BF16); b_b31row = Buf()
        for kv in range(2):
            P.op("dve", lambda e, kv=kv: e.tensor_copy(
                out=b31row[0:1, kv, :, :], in_=b31[0:1, 4 * kv:4 * kv + 4].unsqueeze(2).to_broadcast([1, 4, 128])),
                reads=[b_b31], writes=[b_b31row])
        ones_b = P.sbuf("ones_b", [1, 128], BF16); b_ones_b = Buf()
        P.op("dve", lambda e: e.memset(ones_b[:], 1.0), writes=[b_ones_b])
        rzg = P.sbuf("rzg", [128, 4, 1], F32); b_rzg = Buf()
        oacc = P.sbuf("oacc", [128, 4, 64], F32); b_oacc = Buf()
        otmp = P.sbuf("otmp", [128, 4, 64], F32); b_otmp = Buf()
        for kv in range(2):
            P.dma("sp", lambda e, kv=kv: e.dma_start(out=KsT, in_=ksT[:, kv, :]), writes=[b_KV, b_w1, b_kc2])
            P.dma("sp", lambda e, kv=kv: e.dma_start(out=KwT, in_=kwT[:, kv, :]), writes=[b_KV])
            P.op("pool", lambda e: e.memset(Vs[:, :, 64:65], 1.0), writes=[b_KV])
            P.op("pool", lambda e: e.memset(Vw[:, :, 64:65], 1.0), writes=[b_KV])
            for k0 in range(0, NKT, 8):
                P.dma("sp", lambda e, kv=kv, k0=k0: e.dma_start(
                    out=Vs[:, k0:k0 + 8, 0:64], in_=vtok[k0 * 128:(k0 + 8) * 128, kv * 64:(kv + 1) * 64].rearrange(
                        "(kt p) d -> p kt d", p=128)), writes=[b_KV])
                P.dma("sp", lambda e, kv=kv, k0=k0: e.dma_start(
                    out=Vw[:, k0:k0 + 8, 0:64], in_=vtok[k0 * 128:(k0 + 8) * 128, 128 + kv * 64:128 + (kv + 1) * 64].rearrange(
                        "(kt p) d -> p kt d", p=128)), writes=[b_KV])
            toeplitz_load(P, W, b_W, crel_scr, b_scr_rel, LC, 4 * kv, 11)
            toeplitz_load(P, Wwin, b_Wwin, cwin_scr, b_scr_win, LW, 4 * kv, 8)
            toeplitz_load(P, Wc, b_Wc, ccmp_scr, b_scr_cmp, LCM, 4 * kv, NRC, rstride=128 * STRIDE, pstride=16)
            for lt in range(NT):
                Oc, b_Oc = Ops[0]; Os, b_Os = Ops[1]; Ow, b_Ow = Ops[2]

                def qk_gen(Ksrc, bK, lt=lt, kv=kv):
                    def qk_fn(kt):
                        return [(hh * 128, (hh + 1) * 128,
                                 [(Ksrc(kt), QT[:, 4 * kv + hh, lt * 128:(lt + 1) * 128])], [bK, b_QT])
                                for hh in range(4)]
                    return qk_fn
                ncs = list(range(0, min(4, (STRIDE * lt + STRIDE - 1) // 16 + 1)))

                def extra_c(nc_, lt=lt, kv=kv):
                    r = (STRIDE * lt - 16 * nc_) // STRIDE
                    if r < NRC:
                        return [(C.antib[:], Wc[:, r, :, :].rearrange("p h q -> p (h q)"), [C.b_antib, b_Wc])]
                    return [(ones_b[0:1, :], b31row[0:1, kv, :, :].rearrange("p h q -> p (h q)"), [b_ones_b, b_b31row])]

                def post_c(nc_, ii, n, Pt, b_P):
                    for g in range(4):
                        P.op("pe", lambda e, g=g, nc_=nc_, ii=ii, n=n, Pt=Pt: e.matmul(
                            out=IMP[:, g, :], lhsT=Pt[:, g * 128:(g + 1) * 128], rhs=ovl[:, nc_, :],
                            start=(ii == 0 and g == 0), stop=(ii == n - 1 and g == 3), skip_group_check=True),
                            reads=[b_P, b_ovl], writes=[b_IMP], inc=(g == 3))
                attn_steps(P, R, ncs, qk_gen(lambda nc_, kv=kv: KcT[:, kv, nc_ * 128:(nc_ + 1) * 128], b_KcT), extra_c,
                           lambda nc_, hh, kv=kv: (Vc[:, kv, nc_, :], [b_Vc]), Oc, b_Oc, 0.125, post_fn=post_c)
                P.op("dve", lambda e, Oc=Oc: e.tensor_scalar(out=rz[:], in0=Oc[:, :, 64:65], scalar1=1e-30, scalar2=None,
                                                             op0=ALU.max), reads=[b_Oc], writes=[b_rz])
                P.op("dve", lambda e: e.reciprocal(out=rz[:], in_=rz[:]), reads=[b_rz], writes=[b_rz])
                P.op("dve", lambda e: e.tensor_scalar(out=imp[:], in0=IMP[:, 0, :], scalar1=rz[:, 0, :], scalar2=None,
                                                      op0=ALU.mult), reads=[b_IMP, b_rz], writes=[b_imp])
                for g in range(1, 4):
                    P.op("dve", lambda e, g=g: e.scalar_tensor_tensor(out=imp[:], in0=IMP[:, g, :], scalar=rz[:, g, :],
                                                                      in1=imp[:], op0=ALU.mult, op1=ALU.add),
                         reads=[b_IMP, b_rz, b_imp], writes=[b_imp])
                f0 = OFFW - 2 * STRIDE * lt
                P.op("dve", lambda e, f0=f0: e.tensor_tensor(out=imp[:], in0=imp[:], in1=fw[:, f0:f0 + 128], op=ALU.add),
                     reads=[b_imp, b_fw], writes=[b_imp])
                P.op("dve", lambda e: e.memset(imp[:, 0:1], 1e30), reads=[b_imp], writes=[b_imp])
                P.op("dve", lambda e: e.max(out=m8[:, 0, :], in_=imp[:]), reads=[b_imp], writes=[b_m8])
                P.op("dve", lambda e: e.match_replace(out=sc2[:], in_to_replace=m8[:, 0, :], in_values=imp[:],
                                                      imm_value=-3.0e38), reads=[b_imp, b_m8], writes=[b_sc2])
                P.op("dve", lambda e: e.max(out=m8[:, 1, :], in_=sc2[:]), reads=[b_sc2], writes=[b_m8])
                P.op("dve", lambda e: e.tensor_scalar(out=sc2[:], in0=imp[:], scalar1=m8[:, 1, 7:8], scalar2=None,
                                                      op0=ALU.is_ge), reads=[b_imp, b_m8], writes=[b_sc2])
                P.op("dve", lambda e: e.tensor_scalar(out=sc2[:], in0=sc2[:], scalar1=-1.0, scalar2=-NEGM,
                                                      op0=ALU.add, op1=ALU.mult), reads=[b_sc2], writes=[b_sc2])
                for hh in range(4):
                    P.op("dve", lambda e, hh=hh, kv=kv: e.tensor_scalar(
                        out=negm4[:, hh, :], in0=sc2[:], scalar1=b31[:, 4 * kv + hh:4 * kv + hh + 1], scalar2=None,
                        op0=ALU.add), reads=[b_sc2, b_b31], writes=[b_negm4], inc=(hh == 3))
                for hh in range(4):
                    P.op("pe", lambda e, hh=hh: e.transpose(out=ps_trf[:, hh, :], in_=negm4[:, hh, :], identity=C.idb[:]),
                         reads=[b_negm4, C.b_idb], writes=[b_ps_tr], inc=(hh == 3))
                nT, b_nT = nT4[lt % 2]
                P.op("act", lambda e, nT=nT: e.copy(out=nT[:], in_=ps_trf[:, 0:4, :]), reads=[b_ps_tr], writes=[b_nT])
                kts = list(range(0, min(STRIDE * lt + STRIDE, NKT)))

                def extra_s(kt, lt=lt, nT=nT, b_nT=b_nT):
                    ex = [(Esb[:, kt * 128:(kt + 1) * 128], nT[:].rearrange("m h q -> m (h q)"), [b_E, b_nT])]
                    dl = STRIDE * lt - kt
                    if dl <= 7:
                        ex.append((C.antib[:], W[:, dl + 3, :, :].rearrange("p h q -> p (h q)"), [C.b_antib, b_W]))
                    return ex
                attn_steps(P, R, kts, qk_gen(lambda kt: KsT[:, kt * 128:(kt + 1) * 128], b_KV), extra_s,
                           lambda kt, hh: (Vs[:, kt, :], [b_KV]), Os, b_Os, 0.125)
                ktw = list(range(max(0, STRIDE * lt - 4), min(STRIDE * lt + STRIDE, NKT)))

                def extra_w(kt, lt=lt):
                    dl = STRIDE * lt - kt
                    return [(C.antib[:], Wwin[:, dl + 3, :, :].rearrange("p h q -> p (h q)"), [C.b_antib, b_Wwin])]
                attn_steps(P, R, ktw, qk_gen(lambda kt: KwT[:, kt * 128:(kt + 1) * 128], b_KV), extra_w,
                           lambda kt, hh: (Vw[:, kt, :], [b_KV]), Ow, b_Ow, 0.125)
                for br, (O, b_O) in enumerate([(Oc, b_Oc), (Os, b_Os), (Ow, b_Ow)]):
                    if br == 2 and lt < NAFF:
                        P.op("dve", lambda e, O=O, lt=lt, kv=kv: e.tensor_tensor(
                            out=rz[:], in0=O[:, :, 64:65], in1=zpad[:, lt, 4 * kv:4 * kv + 4].unsqueeze(2), op=ALU.add),
                            reads=[b_O, b_zpad], writes=[b_rz])
                    else:
                        P.op("dve", lambda e, O=O: e.tensor_scalar(out=rz[:], in0=O[:, :, 64:65], scalar1=1e-30, scalar2=None,
                                                                   op0=ALU.max), reads=[b_O], writes=[b_rz])
                    P.op("dve", lambda e: e.reciprocal(out=rz[:], in_=rz[:]), reads=[b_rz], writes=[b_rz])
                    gsl = gts[:, lt, 12 * kv:12 * kv + 12].rearrange("p (h b) -> p h b", b=3)[:, :, br:br + 1]
                    P.op("dve", lambda e, gsl=gsl: e.tensor_tensor(out=rzg[:], in0=rz[:], in1=gsl, op=ALU.mult),
                         reads=[b_rz, b_gts], writes=[b_rzg])
                    if br == 0:
                        P.op("dve", lambda e, O=O: e.tensor_tensor(out=oacc[:], in0=O[:, :, 0:64],
                                                                   in1=rzg[:].to_broadcast([128, 4, 64]), op=ALU.mult),
                             reads=[b_O, b_rzg], writes=[b_oacc])
                    else:
                        P.op("dve", lambda e, O=O: e.tensor_tensor(out=otmp[:], in0=O[:, :, 0:64],
                                                                   in1=rzg[:].to_broadcast([128, 4, 64]), op=ALU.mult),
                             reads=[b_O, b_rzg], writes=[b_otmp])
                        if br == 1:
                            P.op("pool", lambda e: e.tensor_tensor(out=oacc[:], in0=oacc[:], in1=otmp[:], op=ALU.add),
                                 reads=[b_oacc, b_otmp], writes=[b_oacc])
                        else:
                            P.op("pool", lambda e, lt=lt, kv=kv: e.tensor_tensor(
                                out=o_sb[:, lt, kv * 256:(kv + 1) * 256].rearrange("p (h d) -> p h d", d=64),
                                in0=oacc[:], in1=otmp[:], op=ALU.add), reads=[b_oacc, b_otmp], writes=[b_osb])

    if do_moba:
        ohsel = Esb[0:32, 0:32 * 128]; b_ohsel = b_E
        P.dma("sp", lambda e: e.dma_start(out=ohsel, in_=ohsel_d[:, :]), writes=[b_ohsel])
        assert NT * 32 <= 512
        negvalid = u32[:, 0:NT * 32].rearrange("p (a n) -> p a n", n=32); b_nv = b_u32
        own1h = t32[:, 0:NT * 32].rearrange("p (a n) -> p a n", n=32); b_own = b_t32
        P.dma("sp", lambda e: e.dma_start(out=u32[:, 0:NT * 32],
                                          in_=negvalid_d[0:1, :].to_broadcast([128, NT * 32])), writes=[b_nv])
        P.dma("sp", lambda e: e.dma_start(out=t32[:, 0:NT * 32],
                                          in_=own1h_d[0:1, :].to_broadcast([128, NT * 32])), writes=[b_own])
        P.dma("sp", lambda e: e.dma_start(out=QT[:], in_=qT_d[:, 8:16, :]), writes=[b_QT])
        KT = KV[:, 0:2 * NK].rearrange("p (c k) -> p c k", c=2)
        V = KV[:, 2 * NK:2 * NK + NKT * 4 * 65].rearrange("p (k h d) -> p k h d", h=4, d=65)
        kmT = P.sbuf("kmT", [128, 2, 32], BF16); b_kmT = Buf()
        kms = P.sbuf("kms", [128, 32], F32); b_kms = Buf()
        gate = P.sbuf("gate", [128, 4, 32], F32); b_gate = Buf()
        mx8 = P.sbuf("mx8", [128, 4, 8], F32); b_mx8 = Buf()
        sel = P.sbuf("sel", [128, 4, 32], F32); b_sel = Buf()
        negm = P.sbuf("negm", [128, 4, 32], BF16); b_negm = Buf()
        negmT = [(Wwin[0:32, i, :, :], b_Wwin) for i in range(2)]
        for g in range(2):
            for cc in range(2):
                P.dma("sp", lambda e, cc=cc, g=g: e.dma_start(out=KT[:, cc, :], in_=kbT[:, 2 * g + cc, :]), writes=[b_KV])
            P.op("pool", lambda e: e.memset(V[:, :, :, 64:65], 1.0), writes=[b_KV])
            for hh in range(4):
                for k0 in range(0, NKT, 8):
                    P.dma("sp", lambda e, hh=hh, g=g, k0=k0: e.dma_start(
                        out=V[:, k0:k0 + 8, hh, 0:64],
                        in_=vtok[k0 * 128:(k0 + 8) * 128, 256 + (4 * g + hh) * 64:256 + (4 * g + hh + 1) * 64].rearrange(
                            "(kt p) d -> p kt d", p=128)), writes=[b_KV])
            toeplitz_load(P, W, b_W, crel_scr, b_scr_rel, LC, 4 * g, 11)
            for cc in range(2):
                P.op("dve", lambda e, cc=cc: e.tensor_reduce(out=kms[:], in_=KT[:, cc, :].rearrange("p (n k) -> p n k", k=256),
                                                             axis=AX.X, op=ALU.add), reads=[b_KV], writes=[b_kms])
                P.op("dve", lambda e, cc=cc: e.tensor_scalar(out=kmT[:, cc, :], in0=kms[:], scalar1=1.0 / 256, scalar2=None,
                                                             op0=ALU.mult), reads=[b_kms], writes=[b_kmT])
            for lt in range(NT):
                for hh in range(4):
                    cc = hh // 2
                    P.op("pe", lambda e, hh=hh, cc=cc, lt=lt, g=g: e.matmul(
                        out=ps_misc[:, hh * 32:(hh + 1) * 32], lhsT=QT[:, 4 * g + hh, lt * 128:(lt + 1) * 128],
                        rhs=kmT[:, cc, :], start=(hh == 0), stop=(hh == 3), skip_group_check=True),
                        reads=[b_QT, b_kmT], writes=[b_ps_misc], inc=(hh == 3))
                P.op("dve", lambda e, lt=lt: e.tensor_tensor(
                    out=gate[:], in0=ps_misc[:, 0:128].rearrange("p (h n) -> p h n", n=32),
                    in1=negvalid[:, lt:lt + 1, :].to_broadcast([128, 4, 32]), op=ALU.add),
                    reads=[b_ps_misc, b_nv], writes=[b_gate])
                for hh in range(4):
                    P.op("dve", lambda e, hh=hh: e.max(out=mx8[:, hh, :], in_=gate[:, hh, :]),
                         reads=[b_gate], writes=[b_mx8], inc=(hh == 3))
                P.op("dve", lambda e: e.tensor_tensor(out=sel[:], in0=gate[:], in1=mx8[:, :, 2:3].to_broadcast([128, 4, 32]),
                                                      op=ALU.is_ge), reads=[b_gate, b_mx8], writes=[b_sel])
                P.op("dve", lambda e, lt=lt: e.tensor_tensor(out=sel[:], in0=sel[:],
                                                             in1=own1h[:, lt:lt + 1, :].to_broadcast([128, 4, 32]),
                                                             op=ALU.max), reads=[b_sel, b_own], writes=[b_sel])
                P.op("dve", lambda e: e.tensor_scalar(out=sel[:], in0=sel[:], scalar1=-1.0, scalar2=-NEGM,
                                                      op0=ALU.add, op1=ALU.mult), reads=[b_sel], writes=[b_sel])
                P.op("dve", lambda e, g=g: e.tensor_tensor(
                    out=negm[:], in0=sel[:], in1=b31[:, 4 * g:4 * g + 4].unsqueeze(2).to_broadcast([128, 4, 32]),
                    op=ALU.add), reads=[b_sel, b_b31], writes=[b_negm])
                for hh in range(4):
                    P.op("pe", lambda e, hh=hh: e.transpose(out=ps_trf[0:32, hh, :], in_=negm[:, hh, :], identity=C.idb[:]),
                         reads=[b_negm, C.b_idb], writes=[b_ps_tr], inc=(hh == 3))
                nT, b_nT = negmT[lt % 2]
                P.op("act", lambda e, nT=nT: e.copy(out=nT, in_=ps_trf[0:32, 0:4, :]), reads=[b_ps_tr], writes=[b_nT])
                O, b_O = Ops[lt % 2]
                kts = list(range(0, min(STRIDE * lt + STRIDE, NKT)))

                def qk_fn(kt, lt=lt, g=g):
                    return [(hh * 128, (hh + 1) * 128,
                             [(KT[:, hh // 2, kt * 128:(kt + 1) * 128], QT[:, 4 * g + hh, lt * 128:(lt + 1) * 128])],
                             [b_KV, b_QT]) for hh in range(4)]

                def extra_fn(kt, lt=lt, nT=nT, b_nT=b_nT):
                    ex = [(ohsel[:, (kt // 2) * 128:(kt // 2 + 1) * 128], nT.rearrange("n h q -> n (h q)"),
                           [b_ohsel, b_nT])]
                    dl = STRIDE * lt - kt
                    if dl <= 7:
                        ex.append((C.antib[:], W[:, dl + 3, :, :].rearrange("p h q -> p (h q)"), [C.b_antib, b_W]))
                    return ex
                attn_steps(P, R, kts, qk_fn, extra_fn, lambda kt, hh: (V[:, kt, hh, :], [b_KV]), O, b_O, 0.125)
                P.op("dve", lambda e, O=O: e.tensor_scalar(out=rz[:], in0=O[:, :, 64:65], scalar1=1e-30, scalar2=None,
                                                           op0=ALU.max), reads=[b_O], writes=[b_rz])
                P.op("dve", lambda e: e.reciprocal(out=rz[:], in_=rz[:]), reads=[b_rz], writes=[b_rz])
                P.op("dve", lambda e, O=O, lt=lt, g=g: e.tensor_tensor(
                    out=o_sb[:, lt, 512 + g * 256:512 + (g + 1) * 256].rearrange("p (h d) -> p h d", d=64),
                    in0=O[:, :, 0:64], in1=rz[:].to_broadcast([128, 4, 64]), op=ALU.mult),
                    reads=[b_O, b_rz], writes=[b_osb])
    for t in range(NT):
        outs.append(P.dma("sp", lambda e, t=t: e.dma_start(out=o_out[t * 128:(t + 1) * 128, :], in_=o_sb[:, t, :]),
                          reads=[b_osb]))
    P.wait_all("sp", outs)
    P.finish()
    return nc


def build_post(NT=16, final=False, NEXP=16):
    nc = bass.Bass("TRN2", target_bir_lowering=False)
    NTOK = NT * 128
    dt = lambda n, s, d, k: nc.dram_tensor(n, s, d, kind=k).ap()
    o_attn = dt("o_attn", [NTOK, 1024], BF16, "ExternalInput")
    x_d = dt("x", [NTOK, D], F32, "ExternalInput")
    mod_d = dt("mod", [6, D], F32, "ExternalInput")
    w_out_d = dt("w_out", [D, D], F32, "ExternalInput")
    g_ffn_d = dt("g_ffn", [1, D], F32, "ExternalInput")
    g_fin_d = dt("g_fin", [1, D], F32, "ExternalInput")
    rw_d = dt("router_w", [D, 16], F32, "ExternalInput")
    rb_d = dt("router_b", [1, 16], F32, "ExternalInput")
    wg_d = dt("wg", [16, D, 512], F32, "ExternalInput")
    wu_d = dt("wu", [16, D, 512], F32, "ExternalInput")
    wd_d = dt("wd", [16, 512, D], F32, "ExternalInput")
    oh16_d = dt("oh16", [16, 16 * 128], F32, "ExternalInput")
    ident = dt("ident", [128, 128], F32, "ExternalInput")
    x_out = dt("x_out", [NTOK, D], F32, "ExternalOutput")

    P = Prog(nc)
    C = Consts(P, nc, ident[:, :])
    ps_a = [(P.psum(f"pa{i}", [128, 512], F32), Buf()) for i in range(4)]
    ps_y = [(P.psum(f"py{i}", [128, 512], F32), Buf()) for i in range(2)]
    ps_w = (P.psum("pw", [128, 512], F32), Buf())
    ps_tr = P.psum("ptr", [128, 8, 128], BF16); b_ps_tr = Buf()
    x_sb = P.sbuf("x_sb", [128, NT, D], F32); b_x = [Buf() for _ in range(NT)]
    hfT = P.sbuf("hfT", [128, 8, NTOK], BF16); b_hfT = Buf()
    WB = [P.sbuf(f"WB{i}", [128, 12288], BF16) for i in range(2)]; b_WB = [Buf(), Buf()]
    bc = P.sbuf("bc", [128, 4, D], F32); b_bc = Buf()
    nsc = norm_scratch(P, "p")
    gf = nsc["h32"]; b_gf = nsc["b"][3]
    P.dma("sp", lambda e: e.dma_start(out=bc[:, 0, :], in_=mod_d[2:3, :].to_broadcast([128, D])), writes=[b_bc])
    P.dma("sp", lambda e: e.dma_start(out=bc[:, 1, :], in_=mod_d[4:5, :].to_broadcast([128, D])), writes=[b_bc])
    P.dma("sp", lambda e: e.dma_start(out=bc[:, 2, :], in_=mod_d[3:4, :].to_broadcast([128, D])), writes=[b_bc])
    P.dma("sp", lambda e: e.dma_start(out=bc[:, 3, :], in_=mod_d[5:6, :].to_broadcast([128, D])), writes=[b_bc])
    P.dma("sp", lambda e: e.dma_start(out=gf[:], in_=g_ffn_d[0:1, :].to_broadcast([128, D])), writes=[b_gf])
    P.op("dve", lambda e: e.scalar_tensor_tensor(out=bc[:, 1, :], in0=bc[:, 1, :], scalar=1.0, in1=gf[:],
                                                 op0=ALU.add, op1=ALU.mult), reads=[b_bc, b_gf], writes=[b_bc])
    wo = WB[1][:, 0:8192].rearrange("p (k n) -> p k n", n=1024)
    wov = w_out_d.rearrange("(k p) n -> p k n", p=128)
    for k in range(8):
        P.dma("pool", lambda e, k=k: e.dma_start(out=wo[:, k, :], in_=wov[:, k, :]), writes=[b_WB[1]])
    for k in range(8):
        P.op("dve", lambda e, k=k: e.tensor_tensor(out=wo[:, k, :], in0=wo[:, k, :], in1=bc[:, 0, :], op=ALU.mult),
             reads=[b_WB[1], b_bc], writes=[b_WB[1]])
    rw = P.sbuf("rw", [128, 8, 16], F32); b_rw = Buf()
    P.dma("sp", lambda e: e.dma_start(out=rw[:], in_=rw_d.rearrange("(k p) n -> p k n", p=128)), writes=[b_rw])
    rb = P.sbuf("rb", [128, 16], F32); b_rb = Buf()
    P.dma("sp", lambda e: e.dma_start(out=rb[:], in_=rb_d[0:1, :].to_broadcast([128, 16])), writes=[b_rb])
    oh16 = P.sbuf("oh16", [16, 16 * 128], F32); b_oh16 = Buf()
    P.dma("sp", lambda e: e.dma_start(out=oh16[:], in_=oh16_d[:, :]), writes=[b_oh16])
    wT = P.sbuf("wT", [16, NTOK], F32); b_wT = Buf()
    ob = [P.sbuf("ob0", [128, 1024], BF16)] * 2; b_ob = [Buf()] * 2
    oT = P.sbuf("oT", [128, 8, 128], BF16); b_oT = Buf()
    h32T = nsc["sq"][:].rearrange("p (k n) -> p k n", n=128); b_h32T = nsc["b"][0]
    r_aff = P.sbuf("r_aff", [128, 16], F32); b_aff = Buf()
    r_b = P.sbuf("r_b", [128, 4, 4], F32); b_rbias = Buf()
    r_t = P.sbuf("r_t", [128, 8, 4], F32); b_rt = Buf()
    r_w = P.sbuf("r_w", [128, 4, 4], F32); b_rw2 = Buf()
    r_s = P.sbuf("r_s", [128, 2], F32); b_rs = Buf()
    for t in range(NT):
        o_t = ob[t % 2]; bo = b_ob[t % 2]
        P.dma("sp", lambda e, o_t=o_t, t=t: e.dma_start(out=o_t[:], in_=o_attn[t * 128:(t + 1) * 128, :]), writes=[bo])
        P.dma("sp", lambda e, t=t: e.dma_start(out=x_sb[:, t, :], in_=x_d[t * 128:(t + 1) * 128, :]), writes=[b_x[t]])
        for k in range(8):
            P.op("pe", lambda e, k=k, o_t=o_t: e.transpose(out=ps_tr[:, k, :], in_=o_t[:, k * 128:(k + 1) * 128],
                                                           identity=C.idb[:]),
                 reads=[bo, C.b_idb], writes=[b_ps_tr], inc=(k == 7))
        P.op("act", lambda e: e.copy(out=oT[:], in_=ps_tr[:]), reads=[b_ps_tr], writes=[b_oT])
        for half in range(2):
            py, b_py = ps_y[half]
            for k in range(8):
                P.op("pe", lambda e, k=k, half=half, py=py: e.matmul(out=py[:], lhsT=oT[:, k, :],
                                                                   rhs=wo[:, k, half * 512:(half + 1) * 512],
                                                                   start=(k == 0), stop=(k == 7)),
                     reads=[b_oT, b_WB[1]], writes=[b_py], inc=(k == 7))
            P.op("dve", lambda e, half=half, py=py, t=t: e.tensor_tensor(
                out=x_sb[:, t, half * 512:(half + 1) * 512], in0=py[:], in1=x_sb[:, t, half * 512:(half + 1) * 512],
                op=ALU.add), reads=[b_py, b_x[t]], writes=[b_x[t]])
        emit_norm_tile(P, C, x_sb[:, t, :], b_x[t], bc[:, 1, :], b_bc, bc[:, 2, :], b_bc,
                       hfT[:, :, t * 128:(t + 1) * 128], b_hfT, nsc, ps_tr, b_ps_tr)
        h32 = nsc["h32"]; b_h32 = nsc["b"][3]
        P.op("dve", lambda e: e.tensor_tensor(out=h32[:], in0=h32[:], in1=bc[:, 2, :], op=ALU.add),
             reads=[b_h32, b_bc], writes=[b_h32])
        for hf_ in range(2):
            pa, b_pa = ps_a[hf_]
            for k in range(4):
                kk = hf_ * 4 + k
                P.op("pe", lambda e, k=k, kk=kk, pa=pa: e.transpose(out=pa[:, k * 128:(k + 1) * 128],
                                                                  in_=h32[:, kk * 128:(kk + 1) * 128], identity=C.idf[:]),
                     reads=[b_h32, C.b_idf], writes=[b_pa], inc=(k == 3))
            P.op("act", lambda e, hf_=hf_, pa=pa: e.copy(out=h32T[:, hf_ * 4:(hf_ + 1) * 4, :],
                                                         in_=pa[:].rearrange("p (k n) -> p k n", n=128)),
                 reads=[b_pa], writes=[b_h32T])
        pw, b_pw = ps_w
        for k in range(8):
            P.op("pe", lambda e, k=k: e.matmul(out=pw[:, 0:16], lhsT=h32T[:, k, :], rhs=rw[:, k, :],
                                               start=(k == 0), stop=(k == 7)),
                 reads=[b_h32T, b_rw], writes=[b_pw], inc=(k == 7))
        P.op("act", lambda e: e.activation(out=r_aff[:], in_=pw[:, 0:16], func=AF.Sigmoid), reads=[b_pw], writes=[b_aff])
        r_bf = r_b[:].rearrange("p g e -> p (g e)")
        P.op("dve", lambda e: e.tensor_tensor(out=r_bf, in0=r_aff[:], in1=rb[:], op=ALU.add),
             reads=[b_aff, b_rb], writes=[b_rbias])
        a_, b_, c_, d_ = (r_b[:, :, i] for i in range(4))
        T = lambda i: r_t[:, i, :]
        seq = [(T(0), a_, b_, ALU.max), (T(1), a_, b_, ALU.min), (T(2), c_, d_, ALU.max), (T(3), c_, d_, ALU.min),
               (T(4), T(0), T(2), ALU.max), (T(5), T(0), T(2), ALU.min), (T(6), T(1), T(3), ALU.max),
               (T(7), T(5), T(6), ALU.max),
               (T(0), T(4), T(7), ALU.add)]
        for (o_, i0, i1, op_) in seq:
            P.op("dve", lambda e, o_=o_, i0=i0, i1=i1, op_=op_: e.tensor_tensor(out=o_, in0=i0, in1=i1, op=op_),
                 reads=[b_rbias, b_rt], writes=[b_rt])
        P.op("dve", lambda e: e.tensor_reduce(out=r_s[:, 0:1], in_=r_t[:, 0, :], axis=AX.X, op=ALU.max),
             reads=[b_rt], writes=[b_rs])
        P.op("dve", lambda e: e.tensor_scalar(out=r_t[:, 1, :], in0=r_t[:, 0, :], scalar1=r_s[:, 0:1], scalar2=None,
                                              op0=ALU.is_ge), reads=[b_rt, b_rs], writes=[b_rt])
        P.op("dve", lambda e: e.tensor_tensor(out=r_w[:], in0=r_b[:], in1=r_t[:, 7, :].unsqueeze(2).to_broadcast([128, 4, 4]),
                                              op=ALU.is_ge), reads=[b_rbias, b_rt], writes=[b_rw2])
        P.op("dve", lambda e: e.tensor_tensor(out=r_w[:], in0=r_w[:], in1=r_t[:, 1, :].unsqueeze(2).to_broadcast([128, 4, 4]),
                                              op=ALU.mult), reads=[b_rw2, b_rt], writes=[b_rw2])
        r_wf = r_w[:].rearrange("p g e -> p (g e)")
        P.op("dve", lambda e: e.tensor_tensor(out=r_wf, in0=r_wf, in1=r_aff[:], op=ALU.mult),
             reads=[b_rw2, b_aff], writes=[b_rw2])
        P.op("dve", lambda e: e.tensor_reduce(out=r_s[:, 1:2], in_=r_wf, axis=AX.X, op=ALU.add),
             reads=[b_rw2], writes=[b_rs])
        P.op("dve", lambda e: e.reciprocal(out=r_s[:, 1:2], in_=r_s[:, 1:2]), reads=[b_rs], writes=[b_rs])
        P.op("dve", lambda e: e.tensor_scalar(out=r_wf, in0=r_wf, scalar1=r_s[:, 1:2], scalar2=None, op0=ALU.mult),
             reads=[b_rw2, b_rs], writes=[b_rw2])
        pa, b_pa = ps_a[2]
        P.op("pe", lambda e, pa=pa: e.transpose(out=pa[0:16, 0:128], in_=r_wf, identity=C.idf[:]),
             reads=[b_rw2, C.b_idf], writes=[b_pa])
        P.op("act", lambda e, pa=pa, t=t: e.copy(out=wT[:, t * 128:(t + 1) * 128], in_=pa[0:16, 0:128]),
             reads=[b_pa], writes=[b_wT])
    wbc = [P.sbuf("wbc0", [128, 512], F32)] * 2; b_wbc = [Buf()] * 2
    sg = [P.sbuf(f"sg{i}", [128, 512], BF16) for i in range(2)]; b_sg = [Buf(), Buf()]
    uw = [P.sbuf(f"uw{i}", [128, 512], BF16) for i in range(2)]; b_uw = [Buf(), Buf()]
    hid = [P.sbuf("hid0", [128, 4, 512], BF16)] * 2; b_hid = [Buf()] * 2
    NTG = NT // 4
    it = 0
    pai = 0
    for ex in range(NEXP):
        Wb = WB[ex % 2]; bW = b_WB[ex % 2]
        wg = Wb[:, 0:4096].rearrange("p (k n) -> p k n", n=512)
        wu = Wb[:, 4096:8192].rearrange("p (k n) -> p k n", n=512)
        wd = Wb[:, 8192:12288].rearrange("p (k n) -> p k n", n=1024)
        wgv = wg_d[ex].rearrange("(k p) n -> p k n", p=128)
        wuv = wu_d[ex].rearrange("(k p) n -> p k n", p=128)
        wdv = wd_d[ex].rearrange("(k p) n -> p k n", p=128)
        for k in range(8):
            P.dma("pool", lambda e, k=k, wg=wg, wgv=wgv: e.dma_start(out=wg[:, k, :], in_=wgv[:, k, :]), writes=[bW])
            P.dma("pool", lambda e, k=k, wu=wu, wuv=wuv: e.dma_start(out=wu[:, k, :], in_=wuv[:, k, :]), writes=[bW])
        for k in range(4):
            P.dma("pool", lambda e, k=k, wd=wd, wdv=wdv: e.dma_start(out=wd[:, k, :], in_=wdv[:, k, :]), writes=[bW])
        for k in range(4):
            P.op("pool", lambda e, k=k, wd=wd: e.tensor_tensor(out=wd[:, k, :], in0=wd[:, k, :], in1=bc[:, 3, :], op=ALU.mult),
                 reads=[bW, b_bc], writes=[bW])
        for tg in range(NTG):
            wb_ = wbc[it % 2]; bwb = b_wbc[it % 2]
            hd = hid[it % 2]; bhd = b_hid[it % 2]
            it += 1
            pw, b_pw = ps_w
            P.op("pe", lambda e, ex=ex, tg=tg: e.matmul(out=pw[:], lhsT=oh16[:, ex * 128:(ex + 1) * 128],
                                                        rhs=wT[:, tg * 512:(tg + 1) * 512], start=True, stop=True),
                 reads=[b_oh16, b_wT], writes=[b_pw])
            P.op("act", lambda e, wb_=wb_: e.copy(out=wb_[:], in_=pw[:]), reads=[b_pw], writes=[bwb])
            for fc in range(4):
                pg, b_pg = ps_a[pai % 4]; pai += 1
                pu, b_pu = ps_a[pai % 4]; pai += 1
                for k in range(8):
                    P.op("pe", lambda e, k=k, fc=fc, pg=pg, wg=wg, tg=tg: e.matmul(
                        out=pg[:], lhsT=wg[:, k, fc * 128:(fc + 1) * 128], rhs=hfT[:, k, tg * 512:(tg + 1) * 512],
                        start=(k == 0), stop=(k == 7)), reads=[bW, b_hfT], writes=[b_pg], inc=(k == 7))
                for k in range(8):
                    P.op("pe", lambda e, k=k, fc=fc, pu=pu, wu=wu, tg=tg: e.matmul(
                        out=pu[:], lhsT=wu[:, k, fc * 128:(fc + 1) * 128], rhs=hfT[:, k, tg * 512:(tg + 1) * 512],
                        start=(k == 0), stop=(k == 7)), reads=[bW, b_hfT], writes=[b_pu], inc=(k == 7))
                s_ = sg[fc % 2]; bs_ = b_sg[fc % 2]
                u_ = uw[fc % 2]; bu_ = b_uw[fc % 2]
                P.op("act", lambda e, pg=pg, s_=s_: e.activation(out=s_[:], in_=pg[:], func=AF.Silu), reads=[b_pg], writes=[bs_])
                P.op("dve", lambda e, pu=pu, u_=u_, wb_=wb_: e.tensor_tensor(out=u_[:], in0=pu[:], in1=wb_[:], op=ALU.mult),
                     reads=[b_pu, bwb], writes=[bu_])
                P.op("pool", lambda e, fc=fc, hd=hd, s_=s_, u_=u_: e.tensor_tensor(out=hd[:, fc, :], in0=s_[:], in1=u_[:],
                                                                                  op=ALU.mult),
                     reads=[bs_, bu_], writes=[bhd])
            for tt in range(4):
                t = tg * 4 + tt
                for half in range(2):
                    py, b_py = ps_y[half]
                    for fc in range(4):
                        P.op("pe", lambda e, fc=fc, tt=tt, half=half, py=py, hd=hd, wd=wd: e.matmul(
                            out=py[:], lhsT=hd[:, fc, tt * 128:(tt + 1) * 128], rhs=wd[:, fc, half * 512:(half + 1) * 512],
                            start=(fc == 0), stop=(fc == 3)), reads=[bhd, bW], writes=[b_py], inc=(fc == 3))
                    P.op("dve", lambda e, half=half, py=py, t=t: e.tensor_tensor(
                        out=x_sb[:, t, half * 512:(half + 1) * 512], in0=py[:], in1=x_sb[:, t, half * 512:(half + 1) * 512],
                        op=ALU.add), reads=[b_py, b_x[t]], writes=[b_x[t]])
    outs = []
    if final:
        gfin = bc[:, 0, :]
        b_gf = b_bc
        P.dma("sp", lambda e: e.dma_start(out=gfin, in_=g_fin_d[0:1, :].to_broadcast([128, D])), writes=[b_gf])
        sq, ss, rstd = nsc["sq"], nsc["ss"], nsc["rstd"]
        b_sq, b_ss, b_rstd = nsc["b"][0:3]
        for t in range(NT):
            P.op("act", lambda e, t=t: e.activation(out=sq[:], in_=x_sb[:, t, :], func=AF.Square, accum_out=ss[:]),
                 reads=[b_x[t]], writes=[b_sq, b_ss])
            P.op("dve", lambda e: e.tensor_scalar(out=rstd[:], in0=ss[:], scalar1=1.0 / D, scalar2=1e-6,
                                                  op0=ALU.mult, op1=ALU.add), reads=[b_ss], writes=[b_rstd])
            P.op("act", lambda e: e.activation(out=rstd[:], in_=rstd[:], func=AF.Sqrt), reads=[b_rstd], writes=[b_rstd])
            P.op("dve", lambda e: e.reciprocal(out=rstd[:], in_=rstd[:]), reads=[b_rstd], writes=[b_rstd])
            P.op("dve", lambda e, t=t: e.scalar_tensor_tensor(out=x_sb[:, t, :], in0=x_sb[:, t, :], scalar=rstd[:, 0:1],
                                                              in1=gfin, op0=ALU.mult, op1=ALU.mult),
                 reads=[b_x[t], b_rstd, b_gf], writes=[b_x[t]])
    for t in range(NT):
        outs.append(P.dma("sp", lambda e, t=t: e.dma_start(out=x_out[t * 128:(t + 1) * 128, :], in_=x_sb[:, t, :]),
                          reads=[b_x[t]]))
    P.wait_all("sp", outs)
    P.finish()
    return nc


def oh16_static():
    oh = np.zeros((16, 16, 128), np.float32)
    for e in range(16):
        oh[e, e, :] = 1.0
    return oh.reshape(16, 2048)


OD = dict(c_q=(0, 256), c_kv=(256, 384), k_rope=(384, 448), q_d=(448, 960), k_d=(960, 1088), v_d=(1088, 1216))
NU1 = 10


def host_w_in_odd(w, wq_up, wkv_up):
    sl = lambda n: w[:, OD[n][0]:OD[n][1]]
    units = []
    for h in range(8):
        u = np.zeros((1024, 128), np.float32)
        u[:, (h % 2) * 64:(h % 2 + 1) * 64] = sl("q_d")[:, h * 64:(h + 1) * 64]
        units.append(u)
    for kv in range(2):
        c = sl("k_d")[:, kv * 64:(kv + 1) * 64]
        units.append(np.concatenate([c, c], axis=1))
    WF = np.concatenate(units, axis=1)
    WT = np.concatenate([sl("c_q"), sl("c_kv"), sl("k_rope"), sl("v_d")], axis=1)
    wq = wq_up.reshape(256, 4, 192)
    wq_nope = np.ascontiguousarray(wq[:, :, 0:128].reshape(256, 512))
    wq_rope = np.ascontiguousarray(wq[:, :, 128:192].reshape(256, 256))
    wkv = wkv_up.reshape(128, 4, 256)
    wk_nope = np.ascontiguousarray(wkv[:, :, 0:128].reshape(128, 512))
    wv = np.ascontiguousarray(wkv[:, :, 128:256].reshape(128, 512))
    return dict(wf=np.ascontiguousarray(WF), wt=np.ascontiguousarray(WT), wq_nope=wq_nope, wq_rope=wq_rope,
                wk_nope=wk_nope, wv=wv)


def rope_static(positions):
    inv = (10000.0 ** (-np.arange(0, 64, 2, dtype=np.float32) / 64)).astype(np.float32)
    ang = positions.astype(np.float32)[:, None] * inv[None, :]
    return np.cos(ang).astype(np.float32), np.sin(ang).astype(np.float32)


def build_L1odd(NT=16):
    nc = bass.Bass("TRN2", target_bir_lowering=False)
    NTOK = NT * 128
    dt = lambda n, s, d, k: nc.dram_tensor(n, s, d, kind=k).ap()
    x = dt("x", [NTOK, D], F32, "ExternalInput")
    c_cols = dt("c_cols", [128, 8], F32, "ExternalInput")
    ada_w = dt("ada_w", [D, 6 * D], F32, "ExternalInput")
    ada_b = dt("ada_b", [1, 6 * D], F32, "ExternalInput")
    g_mix = dt("g_mix", [1, D], F32, "ExternalInput")
    wf_d = dt("wf", [D, NU1 * 128], F32, "ExternalInput")
    wt_d = dt("wt", [D, 576], F32, "ExternalInput")
    wqn_d = dt("wq_nope", [256, 512], F32, "ExternalInput")
    wqr_d = dt("wq_rope", [256, 256], F32, "ExternalInput")
    wkn_d = dt("wk_nope", [128, 512], F32, "ExternalInput")
    wv_d = dt("wv", [128, 512], F32, "ExternalInput")
    qn_g = dt("q_norm", [1, 256], F32, "ExternalInput")
    kvn_g = dt("kv_norm", [1, 128], F32, "ExternalInput")
    cos_d = dt("cos", [NTOK, 32], F32, "ExternalInput")
    sin_d = dt("sin", [NTOK, 32], F32, "ExternalInput")
    ident = dt("ident", [128, 128], F32, "ExternalInput")
    o_fm = dt("o_fm", [128, NU1, NTOK], BF16, "ExternalOutput")
    o_qn = dt("o_qn", [128, 4, NTOK], BF16, "ExternalOutput")
    o_qr = dt("o_qr", [64, 4, NTOK], BF16, "ExternalOutput")
    o_kn = dt("o_kn", [128, 4, NTOK], BF16, "ExternalOutput")
    o_kr = dt("o_kr", [64, NTOK], BF16, "ExternalOutput")
    o_vtok = dt("o_vtok", [NTOK, 640], BF16, "ExternalOutput")
    o_mod = dt("o_mod", [6, D], F32, "ExternalOutput")

    P = Prog(nc)
    C = Consts(P, nc, ident[:, :])
    ps_row = P.psum("ps_row", [128, 512], F32); b_ps_row = Buf()
    ps_bc = P.psum("ps_bc", [128, 512], F32); b_ps_bc = Buf()
    ps_tr = P.psum("ps_tr", [128, 8, 128], BF16); b_ps_tr = Buf()
    ps_mm = [P.psum(f"ps_mm{i}", [128, 512], F32) for i in range(4)]
    b_ps_mm = [Buf() for _ in range(4)]
    mod, b_mod = emit_adaln(P, nc, C, c_cols[:, :], ada_w, ada_b[:, :], "1", ps_row, b_ps_row, ps_bc, b_ps_bc)
    outs = []
    outs.append(P.dma("sp", lambda e: e.dma_start(out=o_mod[:, :], in_=mod[0:1, :, :]), reads=[b_mod]))
    gm = P.sbuf("gm", [128, 1024], F32); b_gm = Buf()
    A = P.sbuf("A_m", [128, 1024], F32); b_A = Buf()
    P.dma("sp", lambda e: e.dma_start(out=gm[:], in_=g_mix[0:1, :].to_broadcast([128, 1024])), writes=[b_gm])
    P.op("dve", lambda e: e.scalar_tensor_tensor(out=A[:], in0=mod[:, 1, :], scalar=1.0, in1=gm[:],
                                                 op0=ALU.add, op1=ALU.mult), reads=[b_mod, b_gm], writes=[b_A])
    Bt = mod[:, 0, :]
    wf, b_wf = load_w_bf16(P, nc, "wf_sb", wf_d, NU1 * 128)
    wt, b_wt = load_w_bf16(P, nc, "wt_sb", wt_d, 576)
    wqn, b_wqn = load_w_bf16(P, nc, "wqn_sb", wqn_d, 512, rows=256)
    wqr, b_wqr = load_w_bf16(P, nc, "wqr_sb", wqr_d, 256, rows=256)
    wkn, b_wkn = load_w_bf16(P, nc, "wkn_sb", wkn_d, 512, rows=128)
    wv, b_wv = load_w_bf16(P, nc, "wv_sb", wv_d, 512, rows=128)
    qng = P.sbuf("qng", [128, 256], F32); b_qng = Buf()
    kvng = P.sbuf("kvng", [128, 128], F32); b_kvng = Buf()
    P.dma("sp", lambda e: e.dma_start(out=qng[:], in_=qn_g[0:1, :].to_broadcast([128, 256])), writes=[b_qng])
    P.dma("sp", lambda e: e.dma_start(out=kvng[:], in_=kvn_g[0:1, :].to_broadcast([128, 128])), writes=[b_kvng])
    cs = P.sbuf("cs", [128, NT, 2, 32], F32); b_cs = Buf()
    P.dma("sp", lambda e: e.dma_start(out=cs[:, :, 0, :], in_=cos_d.rearrange("(t p) n -> p t n", p=128)), writes=[b_cs])
    P.dma("sp", lambda e: e.dma_start(out=cs[:, :, 1, :], in_=sin_d.rearrange("(t p) n -> p t n", p=128)), writes=[b_cs])
    xt = [P.sbuf(f"xt{i}", [128, 1024], F32) for i in range(2)]
    b_xt = [Buf(), Buf()]
    hT = [P.sbuf(f"hT{i}", [128, 8, 512], BF16) for i in range(2)]
    b_hT = [Buf(), Buf()]
    nsc = norm_scratch(P, "a")
    stg = [P.sbuf(f"stg{i}", [128, 512], BF16) for i in range(4)]
    b_stg = [Buf() for _ in range(4)]
    cq = P.sbuf("cq", [128, 384], F32); b_cq = Buf()
    cqn = P.sbuf("cqn", [128, 384], BF16); b_cqn = Buf()
    cT = P.sbuf("cT", [128, 3, 128], BF16); b_cT = Buf()
    mss = P.sbuf("mss", [128, 4], F32); b_mss = Buf()
    junk = P.sbuf("junk", [128, 256], F32); b_junk = Buf()
    rp = P.sbuf("rp", [128, 5, 64], F32); b_rp = Buf()
    rt = P.sbuf("rt", [128, 4, 5, 32], F32); b_rt = Buf()
    rpb = P.sbuf("rpb", [128, 5, 64], BF16); b_rpb = Buf()
    rT = P.sbuf("rT", [64, 5, 128], BF16); b_rT = Buf()
    rr = RR()
    si = 0
    mi = 0

    def next_ps():
        nonlocal mi
        r = (ps_mm[mi % 4], b_ps_mm[mi % 4]); mi += 1
        return r

    def next_stg():
        nonlocal si
        r = (stg[si % 4], b_stg[si % 4]); si += 1
        return r
    for tg in range(NT // 4):
        h = hT[tg % 2]; bh = b_hT[tg % 2]
        for tt in range(4):
            t = tg * 4 + tt
            xb = xt[t % 2]; bx = b_xt[t % 2]
            P.dma("sp", lambda e, xb=xb, t=t: e.dma_start(out=xb[:], in_=x[t * 128:(t + 1) * 128, :]), writes=[bx])
            emit_norm_tile(P, C, xb[:], bx, A[:], b_A, Bt, b_mod, h[:, :, tt * 128:(tt + 1) * 128], bh,
                           nsc, ps_tr, b_ps_tr)
        for u in range(NU1):
            ps, bps = next_ps()
            for k in range(8):
                P.op("pe", lambda e, ps=ps, u=u, k=k, h=h: e.matmul(out=ps[:], lhsT=wf[:, k, u * 128:(u + 1) * 128],
                                                                  rhs=h[:, k, :], start=(k == 0), stop=(k == 7)),
                     reads=[b_wf, bh], writes=[bps], inc=(k == 7))
            st, bst = next_stg()
            evac(P, rr.next(), st[:], ps[:], [bps], [bst])
            outs.append(P.dma("sp", lambda e, st=st, u=u, tg=tg: e.dma_start(
                out=o_fm[:, u, tg * 512:(tg + 1) * 512], in_=st[:]), reads=[bst]))
        for tt in range(4):
            t = tg * 4 + tt
            tsl = slice(tt * 128, (tt + 1) * 128)
            ps, bps = next_ps()
            for k in range(8):
                P.op("pe", lambda e, ps=ps, k=k, h=h, tsl=tsl: e.matmul(out=ps[:, 0:384], lhsT=h[:, k, tsl], rhs=wt[:, k, 0:384],
                                                                      start=(k == 0), stop=(k == 7)),
                     reads=[b_wt, bh], writes=[bps], inc=(k == 7))
            P.op("act", lambda e, ps=ps: e.copy(out=cq[:], in_=ps[:, 0:384]), reads=[bps], writes=[b_cq])
            psB, bpsB = next_ps()
            for k in range(8):
                P.op("pe", lambda e, psB=psB, k=k, h=h, tsl=tsl: e.matmul(out=psB[:, 0:192], lhsT=h[:, k, tsl], rhs=wt[:, k, 384:576],
                                                                        start=(k == 0), stop=(k == 7)),
                     reads=[b_wt, bh], writes=[bpsB], inc=(k == 7))
            st, bst = next_stg()
            P.op("act", lambda e, st=st, psB=psB: e.copy(out=st[:, 0:128], in_=psB[:, 64:192]), reads=[bpsB], writes=[bst])
            outs.append(P.dma("sp", lambda e, st=st, t=t: e.dma_start(out=o_vtok[t * 128:(t + 1) * 128, 512:640], in_=st[:, 0:128]),
                              reads=[bst]))
            P.op("act", lambda e, psB=psB: e.copy(out=rp[:, 4, :], in_=psB[:, 0:64]), reads=[bpsB], writes=[b_rp])
            P.op("act", lambda e: e.activation(out=junk[:, 0:256], in_=cq[:, 0:256], func=AF.Square, accum_out=mss[:, 0:1]),
                 reads=[b_cq], writes=[b_junk, b_mss])
            P.op("act", lambda e: e.activation(out=junk[:, 0:128], in_=cq[:, 256:384], func=AF.Square, accum_out=mss[:, 1:2]),
                 reads=[b_cq], writes=[b_junk, b_mss])
            P.op("dve", lambda e: e.tensor_scalar(out=mss[:, 2:3], in0=mss[:, 0:1], scalar1=1.0 / 256, scalar2=1e-6,
                                                  op0=ALU.mult, op1=ALU.add), reads=[b_mss], writes=[b_mss])
            P.op("dve", lambda e: e.tensor_scalar(out=mss[:, 3:4], in0=mss[:, 1:2], scalar1=1.0 / 128, scalar2=1e-6,
                                                  op0=ALU.mult, op1=ALU.add), reads=[b_mss], writes=[b_mss])
            P.op("act", lambda e: e.activation(out=mss[:, 2:4], in_=mss[:, 2:4], func=AF.Sqrt), reads=[b_mss], writes=[b_mss])
            P.op("dve", lambda e: e.reciprocal(out=mss[:, 2:4], in_=mss[:, 2:4]), reads=[b_mss], writes=[b_mss])
            P.op("dve", lambda e: e.scalar_tensor_tensor(out=cqn[:, 0:256], in0=cq[:, 0:256], scalar=mss[:, 2:3], in1=qng[:],
                                                         op0=ALU.mult, op1=ALU.mult), reads=[b_cq, b_mss, b_qng], writes=[b_cqn])
            P.op("dve", lambda e: e.scalar_tensor_tensor(out=cqn[:, 256:384], in0=cq[:, 256:384], scalar=mss[:, 3:4], in1=kvng[:],
                                                         op0=ALU.mult, op1=ALU.mult), reads=[b_cq, b_mss, b_kvng], writes=[b_cqn])
            for k in range(3):
                P.op("pe", lambda e, k=k: e.transpose(out=ps_tr[:, k, :], in_=cqn[:, k * 128:(k + 1) * 128], identity=C.idb[:]),
                     reads=[b_cqn, C.b_idb], writes=[b_ps_tr], inc=(k == 2))
            P.op("act", lambda e: e.copy(out=cT[:], in_=ps_tr[:, 0:3, :]), reads=[b_ps_tr], writes=[b_cT])
            ps, bps = next_ps()
            for hh in range(4):
                for k in range(2):
                    P.op("pe", lambda e, ps=ps, hh=hh, k=k: e.matmul(
                        out=ps[:, hh * 128:(hh + 1) * 128], lhsT=wqn[:, k, hh * 128:(hh + 1) * 128], rhs=cT[:, k, :],
                        start=(hh == 0 and k == 0), stop=(hh == 3 and k == 1), skip_group_check=True),
                        reads=[b_wqn, b_cT], writes=[bps], inc=(hh == 3 and k == 1))
            st, bst = next_stg()
            evac(P, rr.next(), st[:], ps[:], [bps], [bst])
            outs.append(P.dma("sp", lambda e, st=st, t=t: e.dma_start(
                out=o_qn[:, :, t * 128:(t + 1) * 128], in_=st[:].rearrange("p (h q) -> p h q", q=128)), reads=[bst]))
            ps, bps = next_ps()
            for hh in range(4):
                P.op("pe", lambda e, ps=ps, hh=hh: e.matmul(
                    out=ps[:, hh * 128:(hh + 1) * 128], lhsT=wkn[:, 0, hh * 128:(hh + 1) * 128], rhs=cT[:, 2, :],
                    start=(hh == 0), stop=(hh == 3), skip_group_check=True),
                    reads=[b_wkn, b_cT], writes=[bps], inc=(hh == 3))
            st, bst = next_stg()
            evac(P, rr.next(), st[:], ps[:], [bps], [bst])
            outs.append(P.dma("sp", lambda e, st=st, t=t: e.dma_start(
                out=o_kn[:, :, t * 128:(t + 1) * 128], in_=st[:].rearrange("p (h q) -> p h q", q=128)), reads=[bst]))
            ps, bps = next_ps()
            P.op("pe", lambda e, ps=ps: e.matmul(out=ps[:], lhsT=cT[:, 2, :], rhs=wv[:, 0, :], start=True, stop=True),
                 reads=[b_wv, b_cT], writes=[bps])
            st, bst = next_stg()
            evac(P, rr.next(), st[:], ps[:], [bps], [bst])
            outs.append(P.dma("sp", lambda e, st=st, t=t: e.dma_start(out=o_vtok[t * 128:(t + 1) * 128, 0:512], in_=st[:]),
                              reads=[bst]))
            ps, bps = next_ps()
            for k in range(2):
                P.op("pe", lambda e, ps=ps, k=k: e.matmul(out=ps[:, 0:256], lhsT=cT[:, k, :], rhs=wqr[:, k, :],
                                                          start=(k == 0), stop=(k == 1)),
                     reads=[b_wqr, b_cT], writes=[bps], inc=(k == 1))
            P.op("act", lambda e, ps=ps: e.copy(out=rp[:, 0:4, :], in_=ps[:, 0:256].rearrange("p (h d) -> p h d", d=64)),
                 reads=[bps], writes=[b_rp])
            cosb = cs[:, t, 0, :].unsqueeze(1).to_broadcast([128, 5, 32])
            sinb = cs[:, t, 1, :].unsqueeze(1).to_broadcast([128, 5, 32])
            x1 = rp[:, :, 0:32]; x2 = rp[:, :, 32:64]
            for i_, (a_, b_) in enumerate([(x1, cosb), (x2, sinb), (x1, sinb), (x2, cosb)]):
                P.op("dve", lambda e, i_=i_, a_=a_, b_=b_: e.tensor_tensor(out=rt[:, i_, :, :], in0=a_, in1=b_, op=ALU.mult),
                     reads=[b_rp, b_cs], writes=[b_rt])
            P.op("dve", lambda e: e.tensor_tensor(out=rpb[:, :, 0:32], in0=rt[:, 0, :, :], in1=rt[:, 1, :, :], op=ALU.subtract),
                 reads=[b_rt], writes=[b_rpb])
            P.op("dve", lambda e: e.tensor_tensor(out=rpb[:, :, 32:64], in0=rt[:, 2, :, :], in1=rt[:, 3, :, :], op=ALU.add),
                 reads=[b_rt], writes=[b_rpb])
            for v_ in range(5):
                P.op("pe", lambda e, v_=v_: e.transpose(out=ps_tr[0:64, v_, :], in_=rpb[:, v_, :], identity=C.idb[:]),
                     reads=[b_rpb, C.b_idb], writes=[b_ps_tr], inc=(v_ == 4))
            P.op("act", lambda e: e.copy(out=rT[:], in_=ps_tr[0:64, 0:5, :]), reads=[b_ps_tr], writes=[b_rT])
            outs.append(P.dma("sp", lambda e, t=t: e.dma_start(out=o_qr[:, :, t * 128:(t + 1) * 128], in_=rT[:, 0:4, :]),
                              reads=[b_rT]))
            outs.append(P.dma("sp", lambda e, t=t: e.dma_start(out=o_kr[:, t * 128:(t + 1) * 128], in_=rT[:, 4, :]),
                              reads=[b_rT]))
    P.wait_all("sp", outs)
    P.finish()
    return nc


def attn1_static(NT, STRIDE, j):
    S_ = STRIDE
    st = {}
    pp = np.arange(128)[:, None, None]
    r = np.arange(S_)[None, :, None]
    x = np.arange(128)[None, None, :]
    d = 128 * (r - (S_ - 1)) + 128 * j + x - (127 - pp)
    m = np.where(d >= 0, 0.0, NEGM).astype(np.float32)
    st["wm"] = np.ascontiguousarray(np.stack([m, m], axis=2)).astype(NPBF)
    L = 128 * S_ + 127 + 128
    y = np.arange(L)
    st["oh_swa"] = onehot_table(y - 127 - 128 * (S_ - 1) + 128 * j, "abs", win=128)
    st["cnt_swa"] = pad_counts(NT, STRIDE, j, 128)
    return st


def build_attn1(NT=16, STRIDE=4, NKT=64):
    nc = bass.Bass("TRN2", target_bir_lowering=False)
    NTOK = NT * 128
    NK = NKT * 128
    S_ = STRIDE
    LS = 128 * S_ + 127 + 128
    NAFF = n_aff(STRIDE, 128)
    dt = lambda n, s, d, k: nc.dram_tensor(n, s, d, kind=k).ap()
    qn_d = dt("qn", [128, 4, NTOK], BF16, "ExternalInput")
    qr_d = dt("qr", [64, 4, NTOK], BF16, "ExternalInput")
    qd_d = dt("qd", [128, 8, NTOK], BF16, "ExternalInput")
    kn_d = dt("kn", [128, 4, NK], BF16, "ExternalInput")
    kr_d = dt("kr", [64, NK], BF16, "ExternalInput")
    kd_d = dt("kd", [128, 2, NK], BF16, "ExternalInput")
    vtok = dt("vtok", [NK, 640], BF16, "ExternalInput")
    tab33_d = dt("tab33", [33, 8], F32, "ExternalInput")
    oh_swa = dt("oh_swa", [33, LS], F32, "ExternalInput")
    cnt_swa_d = dt("cnt_swa", [32, NAFF * 128], F32, "ExternalInput")
    sinks_d = dt("sinks", [1, 8], F32, "ExternalInput")
    wm_d = dt("wm", [128, S_, 2, 128], BF16, "ExternalInput")
    ident = dt("ident", [128, 128], F32, "ExternalInput")
    o_out = dt("o_attn", [NTOK, 1024], BF16, "ExternalOutput")
    cswa_scr = dt("cswa_scr", [8, LS], BF16, "Internal")

    P = Prog(nc)
    C = Consts(P, nc, ident[:, :])
    R = AttnRes(P, nS=3, nP=4)
    ps_misc = P.psum("ps_misc", [128, 512], F32); b_ps_misc = Buf()
    Ops = [(P.psum(f"O{i}", [128, 512], F32), Buf()) for i in range(2)]
    tab33 = P.sbuf("tab33", [33, 8], F32); b_tab33 = Buf()
    P.dma("sp", lambda e: e.dma_start(out=tab33[:], in_=tab33_d[:, :]), writes=[b_tab33])
    b_scr = build_ctab(P, nc, C, tab33, b_tab33, oh_swa, LS, "cswa", cswa_scr, ps_misc, b_ps_misc)
    exptab = P.sbuf("exptab", [32, 8], F32); b_exptab = Buf()
    P.op("act", lambda e: e.activation(out=exptab[:], in_=tab33[0:32, :], func=AF.Exp), reads=[b_tab33], writes=[b_exptab])
    cnts = P.sbuf("cnts", [32, NAFF * 128], F32); b_cnts = Buf()
    P.dma("sp", lambda e: e.dma_start(out=cnts[:], in_=cnt_swa_d[:, :]), writes=[b_cnts])
    zpad = P.sbuf("zpad", [128, NAFF, 8], F32); b_zpad = Buf()
    for a in range(NAFF):
        P.op("pe", lambda e, a=a: e.matmul(out=ps_misc[:, 0:8], lhsT=cnts[:, a * 128:(a + 1) * 128], rhs=exptab[:, :],
                                           start=True, stop=True), reads=[b_cnts, b_exptab], writes=[b_ps_misc])
        P.op("act", lambda e, a=a: e.copy(out=zpad[:, a, :], in_=ps_misc[:, 0:8]), reads=[b_ps_misc], writes=[b_zpad])
    esink = P.sbuf("esink", [128, 8], F32); b_esink = Buf()
    P.dma("sp", lambda e: e.dma_start(out=esink[:], in_=sinks_d[0:1, :].to_broadcast([128, 8])), writes=[b_esink])
    P.op("act", lambda e: e.activation(out=esink[:], in_=esink[:], func=AF.Exp), reads=[b_esink], writes=[b_esink])
    wm = P.sbuf("wm", [128, S_, 2, 128], BF16); b_wm = Buf()
    P.dma("sp", lambda e: e.dma_start(out=wm[:], in_=wm_d[:, :, :, :]), writes=[b_wm])
    Wswa = P.sbuf("Wswa", [128, S_ + 1, 4, 128], BF16); b_Wswa = Buf()
    QT = P.sbuf("QT", [128, 8, NTOK], BF16); b_QT = Buf()
    KV = P.sbuf("KV", [128, 33280], BF16); b_KV = Buf()
    KrT = P.sbuf("KrT", [64, NK], BF16); b_KrT = Buf()
    o_sb = P.sbuf("o_sb", [128, NT, 1024], BF16); b_osb = Buf()
    rz = P.sbuf("rz", [128, 4, 1], F32); b_rz = Buf()
    P.dma("sp", lambda e: e.dma_start(out=QT[:, 0:4, :], in_=qn_d[:, :, :]), writes=[b_QT])
    P.dma("sp", lambda e: e.dma_start(out=QT[0:64, 4:8, :], in_=qr_d[:, :, :]), writes=[b_QT])
    P.dma("sp", lambda e: e.dma_start(out=KrT[:], in_=kr_d[:, :]), writes=[b_KrT])
    KnT = KV[:, 0:2 * NK].rearrange("p (c k) -> p c k", c=2)
    Vm = KV[:, 2 * NK:2 * NK + NKT * 2 * 129].rearrange("p (k h d) -> p k h d", h=2, d=129)
    sc_mla = float(192 ** -0.5)
    for pp in range(2):
        for hh in range(2):
            P.dma("sp", lambda e, hh=hh, pp=pp: e.dma_start(out=KnT[:, hh, :], in_=kn_d[:, 2 * pp + hh, :]), writes=[b_KV])
        P.op("pool", lambda e: e.memset(Vm[:, :, :, 128:129], 1.0), writes=[b_KV])
        for hh in range(2):
            for k0 in range(0, NKT, 8):
                P.dma("sp", lambda e, hh=hh, pp=pp, k0=k0: e.dma_start(
                    out=Vm[:, k0:k0 + 8, hh, 0:128],
                    in_=vtok[k0 * 128:(k0 + 8) * 128, (2 * pp + hh) * 128:(2 * pp + hh + 1) * 128].rearrange(
                        "(kt p) d -> p kt d", p=128)), writes=[b_KV])
        for lt in range(NT):
            O, b_O = Ops[lt % 2]
            Ov = O[:].rearrange("p (h d) -> p h d", d=256)
            kts = list(range(0, min(S_ * lt + S_, NKT)))

            def qk_fn(kt, lt=lt, pp=pp):
                return [(hh * 128, (hh + 1) * 128,
                         [(KnT[:, hh, kt * 128:(kt + 1) * 128], QT[:, 2 * pp + hh, lt * 128:(lt + 1) * 128]),
                          (KrT[:, kt * 128:(kt + 1) * 128], QT[0:64, 4 + 2 * pp + hh, lt * 128:(lt + 1) * 128])],
                         [b_KV, b_QT, b_KrT]) for hh in range(2)]

            def extra_fn(kt, lt=lt):
                dl = S_ * lt - kt
                if dl <= 0:
                    return [(C.antib[:], wm[:, dl + S_ - 1, :, :].rearrange("p h q -> p (h q)"), [C.b_antib, b_wm])]
                return []
            attn_steps(P, R, kts, qk_fn, extra_fn, lambda kt, hh: (Vm[:, kt, hh, :], [b_KV]), Ov, b_O, sc_mla,
                       nv=129, nh=2)
            P.op("dve", lambda e, Ov=Ov: e.tensor_scalar(out=rz[:, 0:2, :], in0=Ov[:, :, 128:129], scalar1=1e-30, scalar2=None,
                                                         op0=ALU.max), reads=[b_O], writes=[b_rz])
            P.op("dve", lambda e: e.reciprocal(out=rz[:, 0:2, :], in_=rz[:, 0:2, :]), reads=[b_rz], writes=[b_rz])
            P.op("dve", lambda e, Ov=Ov, lt=lt, pp=pp: e.tensor_tensor(
                out=o_sb[:, lt, pp * 256:(pp + 1) * 256].rearrange("p (h d) -> p h d", d=128), in0=Ov[:, :, 0:128],
                in1=rz[:, 0:2, :].to_broadcast([128, 2, 128]), op=ALU.mult), reads=[b_O, b_rz], writes=[b_osb])
    P.dma("sp", lambda e: e.dma_start(out=QT[:], in_=qd_d[:, :, :]), writes=[b_QT])
    KdT = KV[:, 0:NK]
    Vd = KV[:, NK:NK + NKT * 65].rearrange("p (k d) -> p k d", d=65)
    for kv in range(2):
        P.dma("sp", lambda e, kv=kv: e.dma_start(out=KdT, in_=kd_d[:, kv, :]), writes=[b_KV])
        P.op("pool", lambda e: e.memset(Vd[:, :, 64:65], 1.0), writes=[b_KV])
        for k0 in range(0, NKT, 8):
            P.dma("sp", lambda e, kv=kv, k0=k0: e.dma_start(
                out=Vd[:, k0:k0 + 8, 0:64], in_=vtok[k0 * 128:(k0 + 8) * 128, 512 + kv * 64:512 + (kv + 1) * 64].rearrange(
                    "(kt p) d -> p kt d", p=128)), writes=[b_KV])
        toeplitz_load(P, Wswa, b_Wswa, cswa_scr, b_scr, LS, 4 * kv, S_ + 1)
        for lt in range(NT):
            O, b_O = Ops[lt % 2]
            Ov = O[:].rearrange("p (h d) -> p h d", d=128)
            kts = list(range(max(0, S_ * lt - 1), min(S_ * lt + S_, NKT)))

            def qk_fn(kt, lt=lt, kv=kv):
                return [(hh * 128, (hh + 1) * 128,
                         [(KdT[:, kt * 128:(kt + 1) * 128], QT[:, 4 * kv + hh, lt * 128:(lt + 1) * 128])],
                         [b_KV, b_QT]) for hh in range(4)]

            def extra_fn(kt, lt=lt):
                dl = S_ * lt - kt
                return [(C.antib[:], Wswa[:, dl + S_ - 1, :, :].rearrange("p h q -> p (h q)"), [C.b_antib, b_Wswa])]
            attn_steps(P, R, kts, qk_fn, extra_fn, lambda kt, hh: (Vd[:, kt, :], [b_KV]), Ov, b_O, 0.125)
            P.op("dve", lambda e, Ov=Ov, kv=kv: e.tensor_tensor(out=rz[:], in0=Ov[:, :, 64:65],
                                                                in1=esink[:, 4 * kv:4 * kv + 4].unsqueeze(2), op=ALU.add),
                 reads=[b_O, b_esink], writes=[b_rz])
            if lt < NAFF:
                P.op("dve", lambda e, lt=lt, kv=kv: e.tensor_tensor(out=rz[:], in0=rz[:],
                                                                    in1=zpad[:, lt, 4 * kv:4 * kv + 4].unsqueeze(2), op=ALU.add),
                     reads=[b_rz, b_zpad], writes=[b_rz])
            P.op("dve", lambda e: e.reciprocal(out=rz[:], in_=rz[:]), reads=[b_rz], writes=[b_rz])
            P.op("dve", lambda e, Ov=Ov, lt=lt, kv=kv: e.tensor_tensor(
                out=o_sb[:, lt, 512 + kv * 256:512 + (kv + 1) * 256].rearrange("p (h d) -> p h d", d=64),
                in0=Ov[:, :, 0:64], in1=rz[:].to_broadcast([128, 4, 64]), op=ALU.mult),
                reads=[b_O, b_rz], writes=[b_osb])
    outs = []
    for t in range(NT):
        outs.append(P.dma("sp", lambda e, t=t: e.dma_start(out=o_out[t * 128:(t + 1) * 128, :], in_=o_sb[:, t, :]),
                          reads=[b_osb]))
    P.wait_all("sp", outs)
    P.finish()
    return nc


from concourse.bass_utils import run_bass_kernel_spmd

_NT = 16
_STRIDE = 4
_PROGS = {}


def _prog(name, fn):
    if name not in _PROGS:
        _PROGS[name] = fn()
    return _PROGS[name]


def _own(a, j):
    sh = a.shape
    return np.ascontiguousarray(a.reshape((16, 4, 128) + sh[1:])[:, j].reshape((2048,) + sh[1:]))


def _gather_last(parts, blk=128):
    sh = parts[0].shape
    out = np.zeros(sh[:-1] + (64 * blk,), parts[0].dtype)
    o5 = out.reshape(sh[:-1] + (16, 4, blk))
    for r in range(4):
        o5[..., r, :] = parts[r].reshape(sh[:-1] + (16, blk))
    return out


def _gather_rows(parts):
    sh = parts[0].shape
    out = np.zeros((8192,) + sh[1:], parts[0].dtype)
    o5 = out.reshape((16, 4, 128) + sh[1:])
    for r in range(4):
        o5[:, r] = parts[r].reshape((16, 128) + sh[1:])
    return out


def _run(nc, ins):
    res = run_bass_kernel_spmd(nc, ins, core_ids=list(range(8)))
    return res.results


def kernel(x, c, rel_table, router_w, router_b, final_norm, norm_mix, norm_ffn, ada_w, ada_b,
           moe_w_gate, moe_w_up, moe_w_down, ev_w_in, ev_w_out, nsa_pos_k, nsa_pos_v,
           nsa_ck_w1, nsa_ck_w2, nsa_cv_w1, nsa_cv_w2, od_w_in, od_w_out, mla_q_norm,
           mla_kv_norm, mla_w_q_up, mla_w_kv_up, swa_sinks):
    f32 = lambda a: np.ascontiguousarray(np.asarray(a, dtype=np.float32))
    x = f32(x); c = f32(c)
    ident = np.eye(128, dtype=np.float32)
    tab33 = np.concatenate([f32(rel_table), np.ones((1, 8), np.float32)], 0)
    NT, ST = _NT, _STRIDE
    cores = [(cc // 4, cc % 4) for cc in range(8)]
    c_cols = [np.ascontiguousarray(c[b].reshape(8, 128).T) for b in range(2)]

    WF, WP, WT = host_w_in_even(f32(ev_w_in[0]))
    ins = [dict(x=_own(x[b], j), c_cols=c_cols[b], ada_w=f32(ada_w[0]), ada_b=f32(ada_b[0])[None],
                g_mix=f32(norm_mix[0])[None], wf=WF, wp=WP, wt=WT, ident=ident) for (b, j) in cores]
    r1 = _run(_prog("L1", lambda: build_L1(NT)), ins)
    posc = np.ascontiguousarray(np.stack([f32(nsa_pos_k[0]).reshape(16, 128).T, f32(nsa_pos_v[0]).reshape(16, 128).T], 1))
    cw1 = np.ascontiguousarray(np.stack([f32(nsa_ck_w1[0]), f32(nsa_cv_w1[0])], 0))
    cw2k = np.ascontiguousarray(np.concatenate([f32(nsa_ck_w2[0]), f32(nsa_ck_w2[0])], 1))
    G = {}
    for b in range(2):
        fm = [np.asarray(r1[4 * b + r]['o_fm']) for r in range(4)]
        G[b] = dict(kbT=_gather_last([f[:, 16:20] for f in fm]), ksT=_gather_last([f[:, 20:22] for f in fm]),
                    kwT=_gather_last([f[:, 22:24] for f in fm]),
                    kc2=_gather_last([np.asarray(r1[4 * b + r]['o_kc2']) for r in range(4)], blk=64),
                    vtok=_gather_rows([np.asarray(r1[4 * b + r]['o_vtok']) for r in range(4)]))
    ins = []
    for cc, (b, j) in enumerate(cores):
        st = attn0_static(NT, ST, j)
        ins.append(dict(qT=np.ascontiguousarray(np.asarray(r1[cc]['o_fm'])[:, 0:16]), gates=np.asarray(r1[cc]['o_gates']),
                        tab33=tab33, posc=posc, cw1=cw1, cw2k=cw2k, cw2v=f32(nsa_cv_w2[0]), ident=ident, **G[b], **st))
    ra = _run(_prog("A0", lambda: build_attn0(NT, ST, 64)), ins)
    oh16 = oh16_static()
    ins = [dict(o_attn=np.asarray(ra[cc]['o_attn']), x=_own(x[b], j), mod=np.asarray(r1[cc]['o_mod']),
                w_out=f32(ev_w_out[0]), g_ffn=f32(norm_ffn[0])[None], g_fin=f32(final_norm)[None],
                router_w=f32(router_w), router_b=f32(router_b)[None], wg=f32(moe_w_gate[0]), wu=f32(moe_w_up[0]),
                wd=f32(moe_w_down[0]), oh16=oh16, ident=ident) for cc, (b, j) in enumerate(cores)]
    rp0 = _run(_prog("P0", lambda: build_post(NT, final=False)), ins)
    hw = host_w_in_odd(f32(od_w_in[0]), f32(mla_w_q_up[0]), f32(mla_w_kv_up[0]))
    ins = []
    for cc, (b, j) in enumerate(cores):
        pos = (np.arange(64).reshape(16, 4)[:, j][:, None] * 128 + np.arange(128)[None, :]).reshape(-1)
        cos, sin = rope_static(pos)
        ins.append(dict(x=np.asarray(rp0[cc]['x_out']), c_cols=c_cols[b], ada_w=f32(ada_w[1]), ada_b=f32(ada_b[1])[None],
                        g_mix=f32(norm_mix[1])[None], q_norm=f32(mla_q_norm), kv_norm=f32(mla_kv_norm), cos=cos, sin=sin,
                        ident=ident, **hw))
    r2 = _run(_prog("L1o", lambda: build_L1odd(NT)), ins)
    G = {}
    for b in range(2):
        G[b] = dict(kn=_gather_last([np.asarray(r2[4 * b + r]['o_kn']) for r in range(4)]),
                    kr=_gather_last([np.asarray(r2[4 * b + r]['o_kr']) for r in range(4)]),
                    kd=_gather_last([np.asarray(r2[4 * b + r]['o_fm'])[:, 8:10] for r in range(4)]),
                    vtok=_gather_rows([np.asarray(r2[4 * b + r]['o_vtok']) for r in range(4)]))
    ins = []
    for cc, (b, j) in enumerate(cores):
        st = attn1_static(NT, ST, j)
        ins.append(dict(qn=np.asarray(r2[cc]['o_qn']), qr=np.asarray(r2[cc]['o_qr']),
                        qd=np.ascontiguousarray(np.asarray(r2[cc]['o_fm'])[:, 0:8]), tab33=tab33, sinks=f32(swa_sinks),
                        ident=ident, **G[b], **st))
    rb = _run(_prog("A1", lambda: build_attn1(NT, ST, 64)), ins)
    ins = [dict(o_attn=np.asarray(rb[cc]['o_attn']), x=np.asarray(rp0[cc]['x_out']), mod=np.asarray(r2[cc]['o_mod']),
                w_out=f32(od_w_out[0]), g_ffn=f32(norm_ffn[1])[None], g_fin=f32(final_norm)[None],
                router_w=f32(router_w), router_b=f32(router_b)[None], wg=f32(moe_w_gate[1]), wu=f32(moe_w_up[1]),
                wd=f32(moe_w_down[1]), oh16=oh16, ident=ident) for cc, (b, j) in enumerate(cores)]
    rp1 = _run(_prog("P1", lambda: build_post(NT, final=True)), ins)
    out = np.zeros((2, 8192, 1024), np.float32)
    o6 = out.reshape(2, 16, 4, 128, 1024)
    for cc, (b, j) in enumerate(cores):
        o6[b, :, j] = np.asarray(rp1[cc]['x_out']).reshape(16, 128, 1024)
    return out
```

```python
import numpy as np
import concourse.bass as bass
import concourse.mybir as mybir

F32 = mybir.dt.float32
BF16 = mybir.dt.bfloat16
I32 = mybir.dt.int32
U32 = mybir.dt.uint32
AF = mybir.ActivationFunctionType
ALU = mybir.AluOpType
AX = mybir.AxisListType


class Buf:
    __slots__ = ("name", "w", "r")

    def __init__(self, name=""):
        self.name = name
        self.w = None
        self.r = {}


class Prog:
    COMPUTE = ("pe", "act", "dve", "pool")
    DMAQ = ("sp", "pool")

    def __init__(self, nc, n_dma_sems=24, same_engine_sync=True):
        import os
        same_engine_sync = bool(int(os.environ.get('SES', '1' if same_engine_sync else '0')))
        self.nc = nc
        self.q = {e: [] for e in ("pe", "act", "dve", "pool", "sp")}
        self.eng_obj = {"pe": nc.tensor, "act": nc.scalar, "dve": nc.vector,
                        "pool": nc.gpsimd, "sp": nc.sync}
        self.sems = {}
        self.cnt = {}
        self.seen = {e: {} for e in self.q}
        self.same_engine_sync = same_engine_sync
        self._ctx = []
        for e in self.COMPUTE:
            self.sems[e] = self._sem("s_" + e)
            self.cnt[e] = 0
        self.dma_pool = {}
        for e in ("sp", "pool"):
            self.dma_pool[e] = [[self._sem(f"d_{e}{i}"), 0, None] for i in range(n_dma_sems)]
        self.dma_rr = {"sp": 0, "pool": 0}
        self.pending_noinc = {e: False for e in self.COMPUTE}

    def _sem(self, name):
        g = self.nc.semaphore(name)
        s = g.__enter__()
        self._ctx.append(g)
        return s

    def sbuf(self, name, shape, dt):
        g = self.nc.sbuf_tensor("sb_" + name, list(shape), dt)
        t = g.__enter__()
        self._ctx.append(g)
        return t

    def psum(self, name, shape, dt):
        g = self.nc.psum_tensor("ps_" + name, list(shape), dt)
        t = g.__enter__()
        self._ctx.append(g)
        return t

    def _collect(self, eng, reads, writes):
        deps = {}

        def add(tok):
            if tok is None:
                return
            s, v, owner = tok
            k = id(s)
            if k not in deps or deps[k][1] < v:
                deps[k] = (s, v, owner)

        for b in reads:
            add(b.w)
        for b in writes:
            add(b.w)
            for t in b.r.values():
                add(t)
        waits = []
        for k, (s, v, owner) in deps.items():
            if owner == eng and owner in self.COMPUTE:
                if eng == "pe" or not self.same_engine_sync:
                    continue
                if v > self.cnt[eng]:
                    continue
            if self.seen[eng].get(k, -1) >= v:
                continue
            self.seen[eng][k] = v
            waits.append((s, v))
        return waits

    def _mark(self, tok, reads, writes):
        k = id(tok[0])
        for b in reads:
            old = b.r.get(k)
            if old is None or old[1] < tok[1]:
                b.r[k] = tok
        for b in writes:
            b.w = tok
            b.r = {}

    def op(self, eng, fn, reads=(), writes=(), inc=True):
        assert eng in self.COMPUTE
        waits = self._collect(eng, reads, writes)
        if inc:
            self.cnt[eng] += 1
            tok = (self.sems[eng], self.cnt[eng], eng)
            self.pending_noinc[eng] = False
        else:
            tok = (self.sems[eng], self.cnt[eng] + 1, eng)
            self.pending_noinc[eng] = True
        self.q[eng].append((waits, fn, (self.sems[eng], 1) if inc else None))
        self._mark(tok, reads, writes)
        return tok

    def dma(self, eng, fn, reads=(), writes=()):
        pool = self.dma_pool[eng]
        i = self.dma_rr[eng]
        self.dma_rr[eng] = (i + 1) % len(pool)
        ent = pool[i]
        waits = self._collect(eng, reads, writes)
        if ent[2] is not None:
            s, v, _ = ent[2]
            k = id(s)
            if self.seen[eng].get(k, -1) < v:
                self.seen[eng][k] = v
                waits.append((s, v))
        ent[1] += 16
        tok = (ent[0], ent[1], "dma_" + eng)
        ent[2] = tok
        self.q[eng].append((waits, fn, (ent[0], 16)))
        self._mark(tok, reads, writes)
        return tok

    def wait_all(self, eng, toks):
        waits = []
        for tok in toks:
            s, v, _ = tok
            waits.append((s, v))
        self.q[eng].append((waits, None, None))

    def finish(self):
        nc = self.nc
        for e in self.COMPUTE:
            assert not self.pending_noinc[e], f"engine {e} ends with non-inc instruction"
        with nc.Block() as block:
            def run(engname):
                def body(e):
                    for waits, fn, inc in self.q[engname]:
                        for s, v in waits:
                            e.wait_ge(s, v)
                        if fn is not None:
                            ins = fn(e)
                            if inc is not None:
                                ins.then_inc(inc[0], inc[1])
                return body
            if self.q["sp"]:
                block.sync(run("sp"))
            if self.q["pe"]:
                block.tensor(run("pe"))
            if self.q["act"]:
                block.scalar(run("act"))
            if self.q["dve"]:
                block.vector(run("dve"))
            if self.q["pool"]:
                block.gpsimd(run("pool"))
        for g in reversed(self._ctx):
            g.__exit__(None, None, None)
        self._ctx = []


import numpy as np
import ml_dtypes

NPBF = ml_dtypes.bfloat16
D = 1024
S = 8192
NEGM = -30000.0


class RR:
    def __init__(self, engs=("act", "dve")):
        self.engs = engs
        self.i = 0

    def next(self):
        e = self.engs[self.i % len(self.engs)]
        self.i += 1
        return e


def evac(P, eng, out, in_, reads, writes):
    if eng == "act":
        return P.op("act", lambda e: e.copy(out=out, in_=in_), reads=reads, writes=writes)
    return P.op(eng, lambda e: e.tensor_copy(out=out, in_=in_), reads=reads, writes=writes)


class Consts:
    def __init__(self, P, nc, ident_ap):
        self.idf = P.sbuf("c_idf", [128, 128], F32)
        self.idb = P.sbuf("c_idb", [128, 128], BF16)
        self.b_idf = Buf("idf")
        self.b_idb = Buf("idb")
        P.dma("sp", lambda e: e.dma_start(out=self.idf[:], in_=ident_ap), writes=[self.b_idf])
        P.op("dve", lambda e: e.tensor_copy(out=self.idb[:], in_=self.idf[:]),
             reads=[self.b_idf], writes=[self.b_idb])
        self.antib = P.sbuf("c_antib", [128, 128], BF16)
        self.b_antib = Buf("antib")
        P.op("pool", lambda e: e.memset(self.antib[:], 0.0), writes=[self.b_antib])
        P.op("pool", lambda e: e.affine_select(out=self.antib[:], in_=self.antib[:], pattern=[[1, 128]],
                                               compare_op=ALU.not_equal, fill=1.0, base=-127, channel_multiplier=1),
             reads=[self.b_antib], writes=[self.b_antib])
        self.ones_f = P.sbuf("c_ones_f", [128, 128], F32)
        self.b_ones_f = Buf("ones_f")
        P.op("dve", lambda e: e.memset(self.ones_f[:], 1.0), writes=[self.b_ones_f])


def emit_adaln(P, nc, C, c_cols_ap, ada_w_ap, ada_b_ap, tag, psum_row, b_psum_row, psum_bc, b_psum_bc):
    GW = 256
    NG = 6144 // GW
    cc = P.sbuf(f"ada_c{tag}", [128, 8], F32); b_cc = Buf()
    sc = P.sbuf(f"ada_sc{tag}", [128, 8], F32); b_sc = Buf()
    row = [P.sbuf(f"ada_row{tag}{i}", [1, GW], F32) for i in range(2)]; b_row = [Buf(), Buf()]
    mod = P.sbuf(f"ada_mod{tag}", [128, 6, 1024], F32); b_mod = Buf()
    modf = mod[:].rearrange("p a n -> p (a n)")
    wst = [P.sbuf(f"ada_w{tag}_{i}", [128, 8, GW], F32) for i in range(2)]
    b_wst = [Buf(), Buf()]
    P.dma("sp", lambda e: e.dma_start(out=cc[:], in_=c_cols_ap), writes=[b_cc])
    P.dma("sp", lambda e: e.dma_start(out=modf, in_=ada_b_ap.to_broadcast([128, 6144])), writes=[b_mod])
    P.op("act", lambda e: e.activation(out=sc[:], in_=cc[:], func=AF.Silu), reads=[b_cc], writes=[b_sc])
    wv = ada_w_ap.rearrange("(k p) n -> p k n", p=128)
    for g in range(NG):
        w = wst[g % 2]; bw = b_wst[g % 2]
        r = row[g % 2]; br = b_row[g % 2]
        P.dma("sp", lambda e, w=w, g=g: e.dma_start(out=w[:], in_=wv[:, :, g * GW:(g + 1) * GW]), writes=[bw])
        for k in range(8):
            P.op("pe", lambda e, w=w, k=k: e.matmul(out=psum_row[0:1, 0:GW], lhsT=sc[:, k:k + 1], rhs=w[:, k, :],
                                                    start=(k == 0), stop=(k == 7)),
                 reads=[b_sc, bw], writes=[b_psum_row], inc=(k == 7))
        P.op("act", lambda e, r=r: e.copy(out=r[0:1, :], in_=psum_row[0:1, 0:GW]),
             reads=[b_psum_row], writes=[br])
        P.op("pe", lambda e, r=r: e.matmul(out=psum_bc[:, 0:GW], lhsT=C.ones_f[0:1, :], rhs=r[0:1, :],
                                           start=True, stop=True),
             reads=[C.b_ones_f, br], writes=[b_psum_bc])
        P.op("dve", lambda e, g=g: e.tensor_tensor(out=modf[:, g * GW:(g + 1) * GW], in0=psum_bc[:, 0:GW],
                                                   in1=modf[:, g * GW:(g + 1) * GW], op=ALU.add),
             reads=[b_psum_bc, b_mod], writes=[b_mod])
    return mod, b_mod


def emit_norm_tile(P, C, x_t, b_x, A, b_A, Bt, b_B, hT_out, b_hT, scratch, psum_tr, b_psum_tr, tag=""):
    sq, ss, rstd, h32, hb = scratch["sq"], scratch["ss"], scratch["rstd"], scratch["h32"], scratch["hb"]
    b_sq, b_ss, b_rstd, b_h32, b_hb = scratch["b"]
    P.op("act", lambda e: e.activation(out=sq[:], in_=x_t, func=AF.Square, accum_out=ss[:]),
         reads=[b_x], writes=[b_sq, b_ss])
    P.op("dve", lambda e: e.tensor_scalar(out=rstd[:], in0=ss[:], scalar1=1.0 / D, scalar2=1e-6,
                                          op0=ALU.mult, op1=ALU.add), reads=[b_ss], writes=[b_rstd])
    P.op("act", lambda e: e.activation(out=rstd[:], in_=rstd[:], func=AF.Sqrt), reads=[b_rstd], writes=[b_rstd])
    P.op("dve", lambda e: e.reciprocal(out=rstd[:], in_=rstd[:]), reads=[b_rstd], writes=[b_rstd])
    P.op("dve", lambda e: e.scalar_tensor_tensor(out=h32[:], in0=x_t, scalar=rstd[:, 0:1], in1=A,
                                                 op0=ALU.mult, op1=ALU.mult),
         reads=[b_x, b_rstd, b_A], writes=[b_h32])
    P.op("pool", lambda e: e.tensor_tensor(out=hb[:], in0=h32[:], in1=Bt, op=ALU.add),
         reads=[b_h32, b_B], writes=[b_hb])
    for k in range(8):
        P.op("pe", lambda e, k=k: e.transpose(out=psum_tr[:, k, :], in_=hb[:, k * 128:(k + 1) * 128],
                                              identity=C.idb[:]),
             reads=[b_hb, C.b_idb], writes=[b_psum_tr], inc=(k == 7))
    P.op("act", lambda e: e.copy(out=hT_out, in_=psum_tr[:]), reads=[b_psum_tr], writes=[b_hT])


EV = dict(q_a=(0, 512), kc=(512, 640), vc=(640, 768), ks=(768, 896), vs=(896, 1024), kw=(1024, 1152),
          vw=(1152, 1280), gates=(1280, 1304), q_b=(1304, 1816), k_b=(1816, 2328), v_b=(2328, 2840))


def host_w_in_even(w):
    sl = lambda n: w[:, EV[n][0]:EV[n][1]]
    units = []
    for nm in ("q_a", "q_b"):
        for h in range(8):
            u = np.zeros((1024, 128), np.float32)
            u[:, (h % 2) * 64:(h % 2 + 1) * 64] = sl(nm)[:, h * 64:(h + 1) * 64]
            units.append(u)
    for cc in range(4):
        units.append(sl("k_b")[:, cc * 128:(cc + 1) * 128])
    for nm in ("ks", "kw"):
        for kv in range(2):
            c = sl(nm)[:, kv * 64:(kv + 1) * 64]
            units.append(np.concatenate([c, c], axis=1))
    WF = np.concatenate(units, axis=1)
    WP = np.zeros((1024, 4, 2, 128), np.float32)
    for X, (nm, kv) in enumerate([("kc", 0), ("kc", 1), ("vc", 0), ("vc", 1)]):
        cols = sl(nm)[:, kv * 64:(kv + 1) * 64]
        WP[:, X, 0, 0:64] = cols
        WP[:, X, 1, 64:128] = cols
    WT = np.concatenate([sl("vs"), sl("vw"), sl("gates"), sl("v_b")], axis=1)
    return np.ascontiguousarray(WF), np.ascontiguousarray(WP.reshape(1024, 1024)), np.ascontiguousarray(WT)


NU0 = 24
def load_w_bf16(P, nc, name, ap2d, ncols, rows=1024):
    kc = rows // 128
    t = P.sbuf(name, [128, kc, ncols], BF16)
    b = Buf(name)
    v = ap2d.rearrange("(k p) n -> p k n", p=128)
    for k in range(kc):
        P.dma("pool", lambda e, k=k: e.dma_start(out=t[:, k, :], in_=v[:, k, :]), writes=[b])
    return t, b


def norm_scratch(P, tag, hb=None):
    sc = dict(sq=P.sbuf(f"n_sq{tag}", [128, 1024], F32), ss=P.sbuf(f"n_ss{tag}", [128, 1], F32),
              rstd=P.sbuf(f"n_rstd{tag}", [128, 1], F32), h32=P.sbuf(f"n_h32{tag}", [128, 1024], F32),
              hb=hb if hb is not None else P.sbuf(f"n_hb{tag}", [128, 1024], BF16))
    sc["b"] = [Buf() for _ in range(5)]
    return sc


def build_L1(NT=16):
    nc = bass.Bass("TRN2", target_bir_lowering=False)
    NTOK = NT * 128
    dt = lambda n, s, d, k: nc.dram_tensor(n, s, d, kind=k).ap()
    x = dt("x", [NTOK, D], F32, "ExternalInput")
    c_cols = dt("c_cols", [128, 8], F32, "ExternalInput")
    ada_w = dt("ada_w", [D, 6 * D], F32, "ExternalInput")
    ada_b = dt("ada_b", [1, 6 * D], F32, "ExternalInput")
    g_mix = dt("g_mix", [1, D], F32, "ExternalInput")
    wf_d = dt("wf", [D, NU0 * 128], F32, "ExternalInput")
    wp_d = dt("wp", [D, 1024], F32, "ExternalInput")
    wt_d = dt("wt", [D, 792], F32, "ExternalInput")
    ident = dt("ident", [128, 128], F32, "ExternalInput")
    o_fm = dt("o_fm", [128, NU0, NTOK], BF16, "ExternalOutput")
    o_kc2 = dt("o_kc2", [128, 4, NTOK // 2], BF16, "ExternalOutput")
    o_vtok = dt("o_vtok", [NTOK, 768], BF16, "ExternalOutput")
    o_gates = dt("o_gates", [NTOK, 24], F32, "ExternalOutput")
    o_mod = dt("o_mod", [6, D], F32, "ExternalOutput")

    P = Prog(nc)
    C = Consts(P, nc, ident[:, :])
    ps_row = P.psum("ps_row", [1, 512], F32); b_ps_row = Buf()
    ps_bc = P.psum("ps_bc", [128, 512], F32); b_ps_bc = Buf()
    ps_tr = P.psum("ps_tr", [128, 8, 128], BF16); b_ps_tr = Buf()
    ps_mm = [P.psum(f"ps_mm{i}", [128, 512], F32) for i in range(4)]
    b_ps_mm = [Buf() for _ in range(4)]

    mod, b_mod = emit_adaln(P, nc, C, c_cols[:, :], ada_w, ada_b[:, :], "0", ps_row, b_ps_row, ps_bc, b_ps_bc)
    outs = []
    outs.append(P.dma("sp", lambda e: e.dma_start(out=o_mod[:, :], in_=mod[0:1, :, :]), reads=[b_mod]))
    gm = P.sbuf("gm", [128, 1024], F32); b_gm = Buf()
    A = P.sbuf("A_m", [128, 1024], F32); b_A = Buf()
    P.dma("sp", lambda e: e.dma_start(out=gm[:], in_=g_mix[0:1, :].to_broadcast([128, 1024])), writes=[b_gm])
    P.op("dve", lambda e: e.scalar_tensor_tensor(out=A[:], in0=mod[:, 1, :], scalar=1.0, in1=gm[:],
                                                 op0=ALU.add, op1=ALU.mult), reads=[b_mod, b_gm], writes=[b_A])
    Bt = mod[:, 0, :]
    wf, b_wf = load_w_bf16(P, nc, "wf_sb", wf_d, NU0 * 128)
    wp, b_wp = load_w_bf16(P, nc, "wp_sb", wp_d, 1024)
    wt, b_wt = load_w_bf16(P, nc, "wt_sb", wt_d, 792)
    xt = [P.sbuf(f"xt{i}", [128, 1024], F32) for i in range(2)]
    b_xt = [Buf(), Buf()]
    hT = [P.sbuf(f"hT{i}", [128, 8, 512], BF16) for i in range(2)]
    b_hT = [Buf(), Buf()]
    nsc = norm_scratch(P, "a")
    stg = [P.sbuf(f"stg{i}", [128, 512], BF16) for i in range(4)]
    b_stg = [Buf() for _ in range(4)]
    gst = [P.sbuf(f"gst{i}", [128, 24], F32) for i in range(2)]
    b_gst = [Buf(), Buf()]
    rr = RR()
    si = 0
    mi = 0
    for tg in range(NT // 4):
        h = hT[tg % 2]; bh = b_hT[tg % 2]
        for tt in range(4):
            t = tg * 4 + tt
            xb = xt[t % 2]; bx = b_xt[t % 2]
            P.dma("sp", lambda e, xb=xb, t=t: e.dma_start(out=xb[:], in_=x[t * 128:(t + 1) * 128, :]), writes=[bx])
            emit_norm_tile(P, C, xb[:], bx, A[:], b_A, Bt, b_mod, h[:, :, tt * 128:(tt + 1) * 128], bh,
                           nsc, ps_tr, b_ps_tr)
        for u in range(NU0):
            ps = ps_mm[mi % 4]; bps = b_ps_mm[mi % 4]; mi += 1
            for k in range(8):
                P.op("pe", lambda e, ps=ps, u=u, k=k, h=h: e.matmul(out=ps[:], lhsT=wf[:, k, u * 128:(u + 1) * 128],
                                                                  rhs=h[:, k, :], start=(k == 0), stop=(k == 7)),
                     reads=[b_wf, bh], writes=[bps], inc=(k == 7))
            st = stg[si % 4]; bst = b_stg[si % 4]; si += 1
            evac(P, rr.next(), st[:], ps[:], [bps], [bst])
            outs.append(P.dma("sp", lambda e, st=st, u=u, tg=tg: e.dma_start(
                out=o_fm[:, u, tg * 512:(tg + 1) * 512], in_=st[:]), reads=[bst]))
        for X in range(4):
            ps = ps_mm[mi % 4]; bps = b_ps_mm[mi % 4]; mi += 1
            n = 0
            for lo in range(2):
                for k in range(8):
                    P.op("pe", lambda e, ps=ps, X=X, lo=lo, k=k, h=h, n=n: e.matmul(
                        out=ps[:, 0:256], lhsT=wp[:, k, (X * 2 + lo) * 128:(X * 2 + lo + 1) * 128],
                        rhs=h[:, k, lo:512:2], start=(n == 0), stop=(n == 15)),
                        reads=[b_wp, bh], writes=[bps], inc=(n == 15))
                    n += 1
            st = stg[si % 4]; bst = b_stg[si % 4]; si += 1
            evac(P, rr.next(), st[:, 0:256], ps[:, 0:256], [bps], [bst])
            outs.append(P.dma("sp", lambda e, st=st, X=X, tg=tg: e.dma_start(
                out=o_kc2[:, X, tg * 256:(tg + 1) * 256], in_=st[:, 0:256]), reads=[bst]))
        for tt in range(4):
            t = tg * 4 + tt
            for grp, (c0, c1) in enumerate([(0, 280), (280, 792)]):
                ps = ps_mm[mi % 4]; bps = b_ps_mm[mi % 4]; mi += 1
                w_ = c1 - c0
                for k in range(8):
                    P.op("pe", lambda e, ps=ps, k=k, h=h, tt=tt, c0=c0, c1=c1, w_=w_: e.matmul(
                        out=ps[:, 0:w_], lhsT=h[:, k, tt * 128:(tt + 1) * 128], rhs=wt[:, k, c0:c1],
                        start=(k == 0), stop=(k == 7)), reads=[b_wt, bh], writes=[bps], inc=(k == 7))
                st = stg[si % 4]; bst = b_stg[si % 4]; si += 1
                if grp == 0:
                    evac(P, rr.next(), st[:, 0:256], ps[:, 0:256], [bps], [bst])
                    outs.append(P.dma("sp", lambda e, st=st, t=t: e.dma_start(
                        out=o_vtok[t * 128:(t + 1) * 128, 0:256], in_=st[:, 0:256]), reads=[bst]))
                    g = gst[t % 2]; bg = b_gst[t % 2]
                    P.op("act", lambda e, g=g, ps=ps: e.activation(out=g[:], in_=ps[:, 256:280], func=AF.Sigmoid),
                         reads=[bps], writes=[bg])
                    outs.append(P.dma("sp", lambda e, g=g, t=t: e.dma_start(
                        out=o_gates[t * 128:(t + 1) * 128, :], in_=g[:]), reads=[bg]))
                else:
                    evac(P, rr.next(), st[:], ps[:], [bps], [bst])
                    outs.append(P.dma("sp", lambda e, st=st, t=t: e.dma_start(
                        out=o_vtok[t * 128:(t + 1) * 128, 256:768], in_=st[:]), reads=[bst]))
    P.wait_all("sp", outs)
    P.finish()
    return nc


def rel_bucket_np(d):
    d = np.maximum(d, 0)
    lp = 16 + (np.log(np.maximum(d, 1).astype(np.float32) / 16) / np.float32(np.log(1024 / 16)) * 16).astype(np.int32)
    return np.where(d < 16, d, np.minimum(lp, 31))


def onehot_table(dvals, mode, win=None):
    L = len(dvals)
    oh = np.zeros((33, L), np.float32)
    ok = dvals >= 0
    if win is not None:
        ok &= dvals < win
    b = rel_bucket_np(dvals)
    idx = np.nonzero(ok)[0]
    oh[b[idx], idx] += 8.0
    if mode == "rel":
        oh[31, idx] -= 8.0
    oh[32, ~ok] = NEGM
    return oh


def build_ctab(P, nc, C, tab33, b_tab33, oh_dram, L, name, scratch_dram, ps, b_ps):
    b_scr = Buf(name + "_scr")
    oh = P.sbuf(name + "_oh", [33, 512], F32); b_oh = Buf()
    cb = P.sbuf(name + "_cb", [8, 512], BF16); b_cb = Buf()
    for c0 in range(0, L, 512):
        w = min(512, L - c0)
        P.dma("sp", lambda e, c0=c0, w=w: e.dma_start(out=oh[:, 0:w], in_=oh_dram[:, c0:c0 + w]), writes=[b_oh])
        P.op("pe", lambda e, w=w: e.matmul(out=ps[0:8, 0:w], lhsT=tab33[:, :], rhs=oh[:, 0:w], start=True, stop=True),
             reads=[b_tab33, b_oh], writes=[b_ps])
        P.op("act", lambda e, w=w: e.copy(out=cb[:, 0:w], in_=ps[0:8, 0:w]), reads=[b_ps], writes=[b_cb])
        P.dma("sp", lambda e, c0=c0, w=w: e.dma_start(out=scratch_dram[:, c0:c0 + w], in_=cb[:, 0:w]),
              reads=[b_cb], writes=[b_scr])
    return b_scr


def toeplitz_load(P, Wt, b_W, scratch_dram, b_scr, L, h0, nR, rstride=128, pstride=1, base=0, nh=4, width=128):
    from concourse.bass_types import AP
    for hh in range(nh):
        src = AP(scratch_dram.tensor, scratch_dram.offset + (h0 + hh) * L + base,
                 [[pstride, 128], [rstride, nR], [1, width]])
        P.dma("sp", lambda e, hh=hh, src=src: e.dma_start(out=Wt[:, :, hh, :], in_=src),
              reads=[b_scr], writes=[b_W])


class AttnRes:
    def __init__(self, P, nS=2, nP=3):
        self.S = [(P.psum(f"at_S{i}", [128, 512], F32), Buf()) for i in range(nS)]
        self.Pt = [(P.sbuf(f"at_P{i}", [128, 512], BF16), Buf()) for i in range(nP)]
        self.si = 0
        self.pi = 0

    def nextS(self):
        r = self.S[self.si % len(self.S)]; self.si += 1
        return r

    def nextP(self):
        r = self.Pt[self.pi % len(self.Pt)]; self.pi += 1
        return r


def attn_steps(P, R, kts, qk_fn, extra_fn, v_fn, O, b_O, scale, nv=65, post_fn=None, bias_ap=None, b_bias=None, nh=4):
    n = len(kts)

    def emit_qk(kt):
        S, b_S = R.nextS()
        ex = extra_fn(kt) if extra_fn else []
        qk = qk_fn(kt)
        for qi, (c0, c1, mms, reads) in enumerate(qk):
            for mi, (lhsT, rhs) in enumerate(mms):
                last = (not ex) and qi == len(qk) - 1 and mi == len(mms) - 1
                P.op("pe", lambda e, S=S, c0=c0, c1=c1, lhsT=lhsT, rhs=rhs, mi=mi, qi=qi, last=last: e.matmul(
                    out=S[:, c0:c1], lhsT=lhsT, rhs=rhs, start=(mi == 0 and qi == 0), stop=last,
                    skip_group_check=True),
                    reads=reads, writes=[b_S], inc=last)
        for xi, (lhsT, rhs, reads) in enumerate(ex):
            P.op("pe", lambda e, S=S, lhsT=lhsT, rhs=rhs, xi=xi, nx=len(ex): e.matmul(
                out=S[:, 0:nh * 128], lhsT=lhsT, rhs=rhs, start=False, stop=(xi == nx - 1), skip_group_check=True),
                reads=reads, writes=[b_S], inc=(xi == len(ex) - 1))
        Pt, b_P = R.nextP()
        if bias_ap is None:
            P.op("act", lambda e, S=S, Pt=Pt: e.activation(out=Pt[:, 0:nh * 128], in_=S[:, 0:nh * 128], func=AF.Exp, scale=scale),
                 reads=[b_S], writes=[b_P])
        else:
            P.op("act", lambda e, S=S, Pt=Pt: e.activation(out=Pt[:, 0:nh * 128], in_=S[:, 0:nh * 128], func=AF.Exp, scale=scale,
                                                           bias=bias_ap), reads=[b_S, b_bias], writes=[b_P])
        return Pt, b_P

    def emit_pv(ii, kt, Pt, b_P):
        for hh in range(nh):
            rhs, reads = v_fn(kt, hh)
            P.op("pe", lambda e, Pt=Pt, hh=hh, rhs=rhs, ii=ii: e.matmul(
                out=O[:, hh, 0:nv], lhsT=Pt[:, hh * 128:(hh + 1) * 128], rhs=rhs, start=(ii == 0 and hh == 0),
                stop=(ii == n - 1 and hh == nh - 1), skip_group_check=True),
                reads=[b_P] + reads, writes=[b_O], inc=(hh == nh - 1 and post_fn is None))
        if post_fn is not None:
            post_fn(kt, ii, n, Pt, b_P)

    pend = None
    for ii, kt in enumerate(kts):
        cur = emit_qk(kt)
        if pend is not None:
            emit_pv(*pend)
        pend = (ii, kt, cur[0], cur[1])
    if pend is not None:
        emit_pv(*pend)


def moba_static(NT, STRIDE, j):
    LC = 11 * 128 + 127
    y = np.arange(LC)
    d = y - 127 - 384 + 128 * j
    oh_rel = onehot_table(d, "rel")
    ohsel = np.zeros((32, 32, 128), np.float32)
    for n in range(32):
        ohsel[n, n, :] = 1.0
    negvalid = np.zeros((NT, 32), np.float32)
    own1h = np.zeros((NT, 32), np.float32)
    for lt in range(NT):
        own = (STRIDE * lt + j) // 2
        negvalid[lt, own:] = -1e30
        own1h[lt, own] = 1.0
    return dict(oh_rel=oh_rel, ohsel=ohsel.reshape(32, 4096).astype(NPBF), negvalid=negvalid.reshape(1, -1),
                own1h=own1h.reshape(1, -1))


def gelu_tanh_ops(P, u, b_u, t, b_t, out_bf, b_out, width):
    P.op("dve", lambda e: e.tensor_tensor(out=t[:, 0:width], in0=u[:, 0:width], in1=u[:, 0:width], op=ALU.mult),
         reads=[b_u], writes=[b_t])
    P.op("dve", lambda e: e.tensor_scalar(out=t[:, 0:width], in0=t[:, 0:width], scalar1=0.044715, scalar2=1.0,
                                          op0=ALU.mult, op1=ALU.add), reads=[b_t], writes=[b_t])
    P.op("dve", lambda e: e.tensor_tensor(out=t[:, 0:width], in0=t[:, 0:width], in1=u[:, 0:width], op=ALU.mult),
         reads=[b_t, b_u], writes=[b_t])
    P.op("act", lambda e: e.activation(out=t[:, 0:width], in_=t[:, 0:width], func=AF.Sigmoid, scale=1.5957691216057308),
         reads=[b_t], writes=[b_t])
    P.op("dve", lambda e: e.tensor_tensor(out=out_bf, in0=t[:, 0:width], in1=u[:, 0:width], op=ALU.mult),
         reads=[b_t, b_u], writes=[b_out])


def attn0_static(NT, STRIDE, j, NKT=64):
    st = moba_static(NT, STRIDE, j)
    LW = 8 * 128 + 127
    y = np.arange(LW)
    st["oh_win"] = onehot_table(y - 127 - 384 + 128 * j, "abs", win=512)
    NRC = -(-2853 // (128 * STRIDE))
    LCM = 128 * STRIDE * (NRC - 1) + 127 + 16 * 127 + 1
    y = np.arange(LCM)
    st["oh_cmp"] = onehot_table(y + 128 * j - 2063, "abs")
    n = np.arange(512)
    cs = n * 16
    ce = cs + 31
    m = np.arange(128) * 64
    ov = ((cs[:, None] <= m[None, :] + 63) & (ce[:, None] >= m[None, :])).astype(np.float32)
    ov[511] = 0
    st["overlap"] = ov.reshape(4, 128, 128).transpose(1, 0, 2).reshape(128, 512).astype(NPBF)
    E = np.zeros((128, NKT * 128), np.float32)
    keys = np.arange(NKT * 128)
    E[keys // 64, keys] = 1.0
    st["E"] = E.astype(NPBF)
    OFF = 2 * STRIDE * (NT - 1)
    width = OFF + 128
    Fw = np.zeros((128, width), np.float32)
    q = np.arange(128)[:, None]
    xx = np.arange(width)[None, :]
    rel = xx - OFF - 2 * j
    hq = (q >= 64).astype(np.int64)
    Fw[np.broadcast_to(rel > hq, Fw.shape)] = -1e30
    Fw[np.broadcast_to((rel == hq) | (rel == hq - 1), Fw.shape)] = 1e30
    st["fwide"] = Fw
    st["cnt_win"] = pad_counts(NT, STRIDE, j, 512)
    return st


def n_aff(STRIDE, win):
    return -(-(win // 128) // STRIDE)


def pad_counts(NT, STRIDE, j, win):
    na = n_aff(STRIDE, win)
    cnt = np.zeros((32, na * 128), np.float32)
    for a in range(na):
        for q in range(128):
            t = 128 * (STRIDE * a + j) + q
            if t + 1 <= win - 1:
                d = np.arange(t + 1, win)
                bb = rel_bucket_np(d)
                cnt[:, a * 128 + q] = np.bincount(bb, minlength=32)
    return cnt


def build_attn0(NT=16, STRIDE=4, NKT=64, do_nsa=True, do_moba=True):
    nc = bass.Bass("TRN2", target_bir_lowering=False)
    NTOK = NT * 128
    NK = NKT * 128
    LC = 11 * 128 + 127
    LW = 8 * 128 + 127
    NRC = -(-2853 // (128 * STRIDE))
    LCM = 128 * STRIDE * (NRC - 1) + 127 + 16 * 127 + 1
    OFFW = 2 * STRIDE * (NT - 1)
    dt = lambda n, s, d, k: nc.dram_tensor(n, s, d, kind=k).ap()
    qT_d = dt("qT", [128, 16, NTOK], BF16, "ExternalInput")
    gates_d = dt("gates", [NTOK, 24], F32, "ExternalInput")
    kbT = dt("kbT", [128, 4, NK], BF16, "ExternalInput")
    ksT = dt("ksT", [128, 2, NK], BF16, "ExternalInput")
    kwT = dt("kwT", [128, 2, NK], BF16, "ExternalInput")
    kc2 = dt("kc2", [128, 4, NK // 2], BF16, "ExternalInput")
    vtok = dt("vtok", [NK, 768], BF16, "ExternalInput")
    tab33_d = dt("tab33", [33, 8], F32, "ExternalInput")
    oh_rel = dt("oh_rel", [33, LC], F32, "ExternalInput")
    oh_win = dt("oh_win", [33, LW], F32, "ExternalInput")
    oh_cmp = dt("oh_cmp", [33, LCM], F32, "ExternalInput")
    ohsel_d = dt("ohsel", [32, 32 * 128], BF16, "ExternalInput")
    negvalid_d = dt("negvalid", [1, NT * 32], F32, "ExternalInput")
    own1h_d = dt("own1h", [1, NT * 32], F32, "ExternalInput")
    overlap_d = dt("overlap", [128, 512], BF16, "ExternalInput")
    E_d = dt("E", [128, NK], BF16, "ExternalInput")
    fwide_d = dt("fwide", [128, OFFW + 128], F32, "ExternalInput")
    NAFF = n_aff(STRIDE, 512)
    cnt_win_d = dt("cnt_win", [32, NAFF * 128], F32, "ExternalInput")
    posc_d = dt("posc", [128, 2, 16], F32, "ExternalInput")
    w1_d = dt("cw1", [2, 2048, 256], F32, "ExternalInput")
    w2k_d = dt("cw2k", [256, 128], F32, "ExternalInput")
    w2v_d = dt("cw2v", [256, 64], F32, "ExternalInput")
    ident = dt("ident", [128, 128], F32, "ExternalInput")
    o_out = dt("o_attn", [NTOK, 1024], BF16, "ExternalOutput")
    crel_scr = dt("crel_scr", [8, LC], BF16, "Internal")
    cwin_scr = dt("cwin_scr", [8, LW], BF16, "Internal")
    ccmp_scr = dt("ccmp_scr", [8, LCM], BF16, "Internal")

    P = Prog(nc)
    C = Consts(P, nc, ident[:, :])
    R = AttnRes(P)
    ps_misc = P.psum("ps_misc", [128, 512], F32); b_ps_misc = Buf()
    ps_trf = P.psum("ps_trb", [128, 8, 128], BF16); b_ps_tr = Buf()
    Ops = [(P.psum(f"O{i}", [128, 4, 128], F32), Buf()) for i in range(3)]
    IMP = P.psum("IMP", [128, 4, 128], F32); b_IMP = Buf()
    tab33 = P.sbuf("tab33", [33, 8], F32); b_tab33 = Buf()
    P.dma("sp", lambda e: e.dma_start(out=tab33[:], in_=tab33_d[:, :]), writes=[b_tab33])
    b_scr_rel = build_ctab(P, nc, C, tab33, b_tab33, oh_rel, LC, "crel", crel_scr, ps_misc, b_ps_misc)
    b31 = P.sbuf("b31", [128, 8], F32); b_b31 = Buf()
    P.dma("sp", lambda e: e.dma_start(out=b31[:], in_=tab33_d[31:32, :].to_broadcast([128, 8])), writes=[b_b31])
    P.op("dve", lambda e: e.tensor_scalar(out=b31[:], in0=b31[:], scalar1=8.0, scalar2=None, op0=ALU.mult),
         reads=[b_b31], writes=[b_b31])
    QT = P.sbuf("QT", [128, 8, NTOK], BF16); b_QT = Buf()
    KV = P.sbuf("KV", [128, 33280], BF16); b_KV = Buf()
    o_sb = P.sbuf("o_sb", [128, NT, 1024], BF16); b_osb = Buf()
    W = P.sbuf("W", [128, 11, 4, 128], BF16); b_W = Buf()
    rz = P.sbuf("rz", [128, 4, 1], F32); b_rz = Buf()
    outs = []
    Esb = P.sbuf("Esb", [128, NK], BF16); b_E = Buf()
    u32 = P.sbuf("u32", [128, 512], F32); b_u32 = Buf()
    t32 = P.sbuf("t32", [128, 512], F32); b_t32 = Buf()
    Wwin = P.sbuf("Wwin", [128, 8, 4, 128], BF16); b_Wwin = Buf()
    Wc = P.sbuf("Wc", [128, NRC, 4, 128], BF16); b_Wc = Buf()

    if do_nsa:
        b_scr_win = build_ctab(P, nc, C, tab33, b_tab33, oh_win, LW, "cwin", cwin_scr, ps_misc, b_ps_misc)
        b_scr_cmp = build_ctab(P, nc, C, tab33, b_tab33, oh_cmp, LCM, "ccmp", ccmp_scr, ps_misc, b_ps_misc)
        P.dma("sp", lambda e: e.dma_start(out=Esb[:], in_=E_d[:, :]), writes=[b_E])
        ovl = P.sbuf("ovl", [128, 4, 128], BF16); b_ovl = Buf()
        P.dma("sp", lambda e: e.dma_start(out=ovl[:].rearrange("p a m -> p (a m)"), in_=overlap_d[:, :]), writes=[b_ovl])
        fw = P.sbuf("fw", [128, OFFW + 128], F32); b_fw = Buf()
        P.dma("sp", lambda e: e.dma_start(out=fw[:], in_=fwide_d[:, :]), writes=[b_fw])
        exptab = P.sbuf("exptab", [32, 8], F32); b_exptab = Buf()
        P.op("act", lambda e: e.activation(out=exptab[:], in_=tab33[0:32, :], func=AF.Exp), reads=[b_tab33], writes=[b_exptab])
        cntw = P.sbuf("cntw", [32, NAFF * 128], F32); b_cntw = Buf()
        P.dma("sp", lambda e: e.dma_start(out=cntw[:], in_=cnt_win_d[:, :]), writes=[b_cntw])
        zpad = P.sbuf("zpad", [128, NAFF, 8], F32); b_zpad = Buf()
        for a in range(NAFF):
            P.op("pe", lambda e, a=a: e.matmul(out=ps_misc[:, 0:8], lhsT=cntw[:, a * 128:(a + 1) * 128], rhs=exptab[:, :],
                                               start=True, stop=True), reads=[b_cntw, b_exptab], writes=[b_ps_misc])
            P.op("act", lambda e, a=a: e.copy(out=zpad[:, a, :], in_=ps_misc[:, 0:8]), reads=[b_ps_misc], writes=[b_zpad])
        gts = P.sbuf("gts", [128, NT, 24], F32); b_gts = Buf()
        P.dma("sp", lambda e: e.dma_start(out=gts[:], in_=gates_d.rearrange("(t p) n -> p t n", p=128)), writes=[b_gts])
        P.dma("sp", lambda e: e.dma_start(out=QT[:], in_=qT_d[:, 0:8, :]), writes=[b_QT])
        posc = P.sbuf("posc", [128, 2, 16], F32); b_posc = Buf()
        poscb = P.sbuf("poscb", [128, 2, 16], BF16); b_poscb = Buf()
        P.dma("sp", lambda e: e.dma_start(out=posc[:], in_=posc_d[:, :, :]), writes=[b_posc])
        P.op("dve", lambda e: e.tensor_copy(out=poscb[:], in_=posc[:]), reads=[b_posc], writes=[b_poscb])
        w2k = P.sbuf("w2k", [128, 2, 128], BF16); b_w2k = Buf()
        w2v = P.sbuf("w2v", [128, 2, 64], BF16); b_w2v = Buf()
        P.dma("pool", lambda e: e.dma_start(out=w2k[:], in_=w2k_d.rearrange("(k p) n -> p k n", p=128)), writes=[b_w2k])
        P.dma("pool", lambda e: e.dma_start(out=w2v[:], in_=w2v_d.rearrange("(k p) n -> p k n", p=128)), writes=[b_w2v])
        w1 = KV[:, 8192:8192 + 4096].rearrange("p (k n) -> p k n", n=256); b_w1 = Buf()
        kc2sb = KV[:, 0:NK // 2]; b_kc2 = Buf()
        bias1 = P.sbuf("bias1", [128, 2], F32); b_bias1 = Buf()
        GT = P.sbuf("GT", [128, 2, 512], BF16); b_GT = Buf()
        P.op("pool", lambda e: e.memset(GT[:], 0.0), writes=[b_GT])
        KcT = P.sbuf("KcT", [128, 2, 512], BF16); b_KcT = Buf()
        Vc = P.sbuf("Vc", [128, 2, 4, 65], BF16); b_Vc = Buf()
        P.op("pool", lambda e: e.memset(KcT[:], 0.0), writes=[b_KcT])
        P.op("pool", lambda e: e.memset(Vc[:], 1.0), writes=[b_Vc])
        ncmp = NK // 16 - 1
        for kvt in range(2):
            for k in range(16):
                P.dma("pool", lambda e, k=k, kvt=kvt: e.dma_start(out=w1[:, k, :], in_=w1_d[kvt, k * 128:(k + 1) * 128, :]),
                      writes=[b_w1])
            for hc in range(2):
                for k in range(16):
                    P.op("pe", lambda e, hc=hc, k=k, kvt=kvt: e.matmul(
                        out=ps_misc[:, 0:1], lhsT=w1[:, k, hc * 128:(hc + 1) * 128], rhs=poscb[:, kvt, k:k + 1],
                        start=(k == 0), stop=(k == 15)), reads=[b_w1, b_poscb], writes=[b_ps_misc], inc=(k == 15))
                P.op("act", lambda e, hc=hc: e.copy(out=bias1[:, hc:hc + 1], in_=ps_misc[:, 0:1]),
                     reads=[b_ps_misc], writes=[b_bias1])
            for kv in range(2):
                X = kvt * 2 + kv
                P.dma("sp", lambda e, X=X: e.dma_start(out=kc2sb, in_=kc2[:, X, :]), writes=[b_kc2])
                for hc in range(2):
                    for k in range(16):
                        P.op("pe", lambda e, hc=hc, k=k: e.matmul(
                            out=ps_misc[:, 0:ncmp], lhsT=w1[:, k, hc * 128:(hc + 1) * 128],
                            rhs=kc2sb[:, k:k + 8 * (ncmp - 1) + 1:8], start=(k == 0), stop=(k == 15)),
                            reads=[b_w1, b_kc2], writes=[b_ps_misc], inc=(k == 15))
                    P.op("act", lambda e, hc=hc: e.activation(out=u32[:, 0:ncmp], in_=ps_misc[:, 0:ncmp], func=AF.Identity,
                                                              bias=bias1[:, hc:hc + 1]),
                         reads=[b_ps_misc, b_bias1], writes=[b_u32])
                    gelu_tanh_ops(P, u32, b_u32, t32, b_t32, GT[:, hc, 0:ncmp], b_GT, ncmp)
                if kvt == 0:
                    for hc in range(2):
                        P.op("pe", lambda e, hc=hc: e.matmul(out=ps_misc[:, 0:512], lhsT=w2k[:, hc, :], rhs=GT[:, hc, :],
                                                             start=(hc == 0), stop=(hc == 1)),
                             reads=[b_w2k, b_GT], writes=[b_ps_misc], inc=(hc == 1))
                    P.op("act", lambda e, kv=kv: e.copy(out=KcT[:, kv, :], in_=ps_misc[:, 0:512]),
                         reads=[b_ps_misc], writes=[b_KcT])
                else:
                    for nc_ in range(4):
                        for hc in range(2):
                            P.op("pe", lambda e, hc=hc, nc_=nc_: e.matmul(
                                out=ps_misc[:, nc_ * 64:(nc_ + 1) * 64], lhsT=GT[:, hc, nc_ * 128:(nc_ + 1) * 128],
                                rhs=w2v[:, hc, :], start=(hc == 0 and nc_ == 0), stop=(hc == 1 and nc_ == 3),
                                skip_group_check=True),
                                reads=[b_w2v, b_GT], writes=[b_ps_misc], inc=(hc == 1 and nc_ == 3))
                    P.op("act", lambda e, kv=kv: e.copy(out=Vc[:, kv, :, 0:64],
                                                        in_=ps_misc[:, 0:256].rearrange("p (a d) -> p a d", d=64)),
                         reads=[b_ps_misc], writes=[b_Vc])
        KsT = KV[:, 0:NK]
        KwT = KV[:, NK:2 * NK]
        Vs = KV[:, 2 * NK:2 * NK + NKT * 65].rearrange("p (k d) -> p k d", d=65)
        Vw = KV[:, 2 * NK + NKT * 65:2 * NK + 2 * NKT * 65].rearrange("p (k d) -> p k d", d=65)
        imp = P.sbuf("imp", [128, 128], F32); b_imp = Buf()
        sc2 = P.sbuf("sc2", [128, 128], F32); b_sc2 = Buf()
        m8 = P.sbuf("m8", [128, 2, 8], F32); b_m8 = Buf()
        negm4 = P.sbuf("negm4", [128, 4, 128], BF16); b_negm4 = Buf()
        nT4 = [(P.sbuf(f"nT4_{i}", [128, 4, 128], BF16), Buf()) for i in range(2)]
        b31row = P.sbuf("b31row", [1, 2, 4, 128], BF16); b_b31row = Buf()
        for kv in range(2):
            P.op("dve", lambda e, kv=kv: e.tensor_copy(
                out=b31row[0:1, kv, :, :], in_=b31[0:1, 4 * kv:4 * kv + 4].unsqueeze(2).to_broadcast([1, 4, 128])),
                reads=[b_b31], writes=[b_b31row])
        ones_b = P.sbuf("ones_b", [1, 128], BF16); b_ones_b = Buf()
        P.op("dve", lambda e: e.memset(ones_b[:], 1.0), writes=[b_ones_b])
        rzg = P.sbuf("rzg", [128, 4, 1], F32); b_rzg = Buf()
        oacc = P.sbuf("oacc", [128, 4, 64], F32); b_oacc = Buf()
        otmp = P.sbuf("otmp", [128, 4, 64], F32); b_otmp = Buf()
        for kv in range(2):
            P.dma("sp", lambda e, kv=kv: e.dma_start(out=KsT, in_=ksT[:, kv, :]), writes=[b_KV, b_w1, b_kc2])
            P.dma("sp", lambda e, kv=kv: e.dma_start(out=KwT, in_=kwT[:, kv, :]), writes=[b_KV])
            P.op("pool", lambda e: e.memset(Vs[:, :, 64:65], 1.0), writes=[b_KV])
            P.op("pool", lambda e: e.memset(Vw[:, :, 64:65], 1.0), writes=[b_KV])
            for k0 in range(0, NKT, 8):
                P.dma("sp", lambda e, kv=kv, k0=k0: e.dma_start(
                    out=Vs[:, k0:k0 + 8, 0:64], in_=vtok[k0 * 128:(k0 + 8) * 128, kv * 64:(kv + 1) * 64].rearrange(
                        "(kt p) d -> p kt d", p=128)), writes=[b_KV])
                P.dma("sp", lambda e, kv=kv, k0=k0: e.dma_start(
                    out=Vw[:, k0:k0 + 8, 0:64], in_=vtok[k0 * 128:(k0 + 8) * 128, 128 + kv * 64:128 + (kv + 1) * 64].rearrange(
                        "(kt p) d -> p kt d", p=128)), writes=[b_KV])
            toeplitz_load(P, W, b_W, crel_scr, b_scr_rel, LC, 4 * kv, 11)
            toeplitz_load(P, Wwin, b_Wwin, cwin_scr, b_scr_win, LW, 4 * kv, 8)
            toeplitz_load(P, Wc, b_Wc, ccmp_scr, b_scr_cmp, LCM, 4 * kv, NRC, rstride=128 * STRIDE, pstride=16)
            for lt in range(NT):
                Oc, b_Oc = Ops[0]; Os, b_Os = Ops[1]; Ow, b_Ow = Ops[2]

                def qk_gen(Ksrc, bK, lt=lt, kv=kv):
                    def qk_fn(kt):
                        return [(hh * 128, (hh + 1) * 128,
                                 [(Ksrc(kt), QT[:, 4 * kv + hh, lt * 128:(lt + 1) * 128])], [bK, b_QT])
                                for hh in range(4)]
                    return qk_fn
                ncs = list(range(0, min(4, (STRIDE * lt + STRIDE - 1) // 16 + 1)))

                def extra_c(nc_, lt=lt, kv=kv):
                    r = (STRIDE * lt - 16 * nc_) // STRIDE
                    if r < NRC:
                        return [(C.antib[:], Wc[:, r, :, :].rearrange("p h q -> p (h q)"), [C.b_antib, b_Wc])]
                    return [(ones_b[0:1, :], b31row[0:1, kv, :, :].rearrange("p h q -> p (h q)"), [b_ones_b, b_b31row])]

                def post_c(nc_, ii, n, Pt, b_P):
                    for g in range(4):
                        P.op("pe", lambda e, g=g, nc_=nc_, ii=ii, n=n, Pt=Pt: e.matmul(
                            out=IMP[:, g, :], lhsT=Pt[:, g * 128:(g + 1) * 128], rhs=ovl[:, nc_, :],
                            start=(ii == 0 and g == 0), stop=(ii == n - 1 and g == 3), skip_group_check=True),
                            reads=[b_P, b_ovl], writes=[b_IMP], inc=(g == 3))
                attn_steps(P, R, ncs, qk_gen(lambda nc_, kv=kv: KcT[:, kv, nc_ * 128:(nc_ + 1) * 128], b_KcT), extra_c,
                           lambda nc_, hh, kv=kv: (Vc[:, kv, nc_, :], [b_Vc]), Oc, b_Oc, 0.125, post_fn=post_c)
                P.op("dve", lambda e, Oc=Oc: e.tensor_scalar(out=rz[:], in0=Oc[:, :, 64:65], scalar1=1e-30, scalar2=None,
                                                             op0=ALU.max), reads=[b_Oc], writes=[b_rz])
                P.op("dve", lambda e: e.reciprocal(out=rz[:], in_=rz[:]), reads=[b_rz], writes=[b_rz])
                P.op("dve", lambda e: e.tensor_scalar(out=imp[:], in0=IMP[:, 0, :], scalar1=rz[:, 0, :], scalar2=None,
                                                      op0=ALU.mult), reads=[b_IMP, b_rz], writes=[b_imp])
                for g in range(1, 4):
                    P.op("dve", lambda e, g=g: e.scalar_tensor_tensor(out=imp[:], in0=IMP[:, g, :], scalar=rz[:, g, :],
                                                                      in1=imp[:], op0=ALU.mult, op1=ALU.add),
                         reads=[b_IMP, b_rz, b_imp], writes=[b_imp])
                f0 = OFFW - 2 * STRIDE * lt
                P.op("dve", lambda e, f0=f0: e.tensor_tensor(out=imp[:], in0=imp[:], in1=fw[:, f0:f0 + 128], op=ALU.add),
                     reads=[b_imp, b_fw], writes=[b_imp])
                P.op("dve", lambda e: e.memset(imp[:, 0:1], 1e30), reads=[b_imp], writes=[b_imp])
                P.op("dve", lambda e: e.max(out=m8[:, 0, :], in_=imp[:]), reads=[b_imp], writes=[b_m8])
                P.op("dve", lambda e: e.match_replace(out=sc2[:], in_to_replace=m8[:, 0, :], in_values=imp[:],
                                                      imm_value=-3.0e38), reads=[b_imp, b_m8], writes=[b_sc2])
                P.op("dve", lambda e: e.max(out=m8[:, 1, :], in_=sc2[:]), reads=[b_sc2], writes=[b_m8])
                P.op("dve", lambda e: e.tensor_scalar(out=sc2[:], in0=imp[:], scalar1=m8[:, 1, 7:8], scalar2=None,
                                                      op0=ALU.is_ge), reads=[b_imp, b_m8], writes=[b_sc2])
                P.op("dve", lambda e: e.tensor_scalar(out=sc2[:], in0=sc2[:], scalar1=-1.0, scalar2=-NEGM,
                                                      op0=ALU.add, op1=ALU.mult), reads=[b_sc2], writes=[b_sc2])
                for hh in range(4):
                    P.op("dve", lambda e, hh=hh, kv=kv: e.tensor_scalar(
                        out=negm4[:, hh, :], in0=sc2[:], scalar1=b31[:, 4 * kv + hh:4 * kv + hh + 1], scalar2=None,
                        op0=ALU.add), reads=[b_sc2, b_b31], writes=[b_negm4], inc=(hh == 3))
                for hh in range(4):
                    P.op("pe", lambda e, hh=hh: e.transpose(out=ps_trf[:, hh, :], in_=negm4[:, hh, :], identity=C.idb[:]),
                         reads=[b_negm4, C.b_idb], writes=[b_ps_tr], inc=(hh == 3))
                nT, b_nT = nT4[lt % 2]
                P.op("act", lambda e, nT=nT: e.copy(out=nT[:], in_=ps_trf[:, 0:4, :]), reads=[b_ps_tr], writes=[b_nT])
                kts = list(range(0, min(STRIDE * lt + STRIDE, NKT)))

                def extra_s(kt, lt=lt, nT=nT, b_nT=b_nT):
                    ex = [(Esb[:, kt * 128:(kt + 1) * 128], nT[:].rearrange("m h q -> m (h q)"), [b_E, b_nT])]
                    dl = STRIDE * lt - kt
                    if dl <= 7:
                        ex.append((C.antib[:], W[:, dl + 3, :, :].rearrange("p h q -> p (h q)"), [C.b_antib, b_W]))
                    return ex
                attn_steps(P, R, kts, qk_gen(lambda kt: KsT[:, kt * 128:(kt + 1) * 128], b_KV), extra_s,
                           lambda kt, hh: (Vs[:, kt, :], [b_KV]), Os, b_Os, 0.125)
                ktw = list(range(max(0, STRIDE * lt - 4), min(STRIDE * lt + STRIDE, NKT)))

                def extra_w(kt, lt=lt):
                    dl = STRIDE * lt - kt
                    return [(C.antib[:], Wwin[:, dl + 3, :, :].rearrange("p h q -> p (h q)"), [C.b_antib, b_Wwin])]
                attn_steps(P, R, ktw, qk_gen(lambda kt: KwT[:, kt * 128:(kt + 1) * 128], b_KV), extra_w,
                           lambda kt, hh: (Vw[:, kt, :], [b_KV]), Ow, b_Ow, 0.125)
                for br, (O, b_O) in enumerate([(Oc, b_Oc), (Os, b_Os), (Ow, b_Ow)]):
                    if br == 2 and lt < NAFF:
                        P.op("dve", lambda e, O=O, lt=lt, kv=kv: e.tensor_tensor(
                            out=rz[:], in0=O[:, :, 64:65], in1=zpad[:, lt, 4 * kv:4 * kv + 4].unsqueeze(2), op=ALU.add),
                            reads=[b_O, b_zpad], writes=[b_rz])
                    else:
                        P.op("dve", lambda e, O=O: e.tensor_scalar(out=rz[:], in0=O[:, :, 64:65], scalar1=1e-30, scalar2=None,
                                                                   op0=ALU.max), reads=[b_O], writes=[b_rz])
                    P.op("dve", lambda e: e.reciprocal(out=rz[:], in_=rz[:]), reads=[b_rz], writes=[b_rz])
                    gsl = gts[:, lt, 12 * kv:12 * kv + 12].rearrange("p (h b) -> p h b", b=3)[:, :, br:br + 1]
                    P.op("dve", lambda e, gsl=gsl: e.tensor_tensor(out=rzg[:], in0=rz[:], in1=gsl, op=ALU.mult),
                         reads=[b_rz, b_gts], writes=[b_rzg])
                    if br == 0:
                        P.op("dve", lambda e, O=O: e.tensor_tensor(out=oacc[:], in0=O[:, :, 0:64],
                                                                   in1=rzg[:].to_broadcast([128, 4, 64]), op=ALU.mult),
                             reads=[b_O, b_rzg], writes=[b_oacc])
                    else:
                        P.op("dve", lambda e, O=O: e.tensor_tensor(out=otmp[:], in0=O[:, :, 0:64],
                                                                   in1=rzg[:].to_broadcast([128, 4, 64]), op=ALU.mult),
                             reads=[b_O, b_rzg], writes=[b_otmp])
                        if br == 1:
                            P.op("pool", lambda e: e.tensor_tensor(out=oacc[:], in0=oacc[:], in1=otmp[:], op=ALU.add),
                                 reads=[b_oacc, b_otmp], writes=[b_oacc])
                        else:
                            P.op("pool", lambda e, lt=lt, kv=kv: e.tensor_tensor(
                                out=o_sb[:, lt, kv * 256:(kv + 1) * 256].rearrange("p (h d) -> p h d", d=64),
                                in0=oacc[:], in1=otmp[:], op=ALU.add), reads=[b_oacc, b_otmp], writes=[b_osb])

    if do_moba:
        ohsel = Esb[0:32, 0:32 * 128]; b_ohsel = b_E
        P.dma("sp", lambda e: e.dma_start(out=ohsel, in_=ohsel_d[:, :]), writes=[b_ohsel])
        assert NT * 32 <= 512
        negvalid = u32[:, 0:NT * 32].rearrange("p (a n) -> p a n", n=32); b_nv = b_u32
        own1h = t32[:, 0:NT * 32].rearrange("p (a n) -> p a n", n=32); b_own = b_t32
        P.dma("sp", lambda e: e.dma_start(out=u32[:, 0:NT * 32],
                                          in_=negvalid_d[0:1, :].to_broadcast([128, NT * 32])), writes=[b_nv])
        P.dma("sp", lambda e: e.dma_start(out=t32[:, 0:NT * 32],
                                          in_=own1h_d[0:1, :].to_broadcast([128, NT * 32])), writes=[b_own])
        P.dma("sp", lambda e: e.dma_start(out=QT[:], in_=qT_d[:, 8:16, :]), writes=[b_QT])
        KT = KV[:, 0:2 * NK].rearrange("p (c k) -> p c k", c=2)
        V = KV[:, 2 * NK:2 * NK + NKT * 4 * 65].rearrange("p (k h d) -> p k h d", h=4, d=65)
        kmT = P.sbuf("kmT", [128, 2, 32], BF16); b_kmT = Buf()
        kms = P.sbuf("kms", [128, 32], F32); b_kms = Buf()
        gate = P.sbuf("gate", [128, 4, 32], F32); b_gate = Buf()
        mx8 = P.sbuf("mx8", [128, 4, 8], F32); b_mx8 = Buf()
        sel = P.sbuf("sel", [128, 4, 32], F32); b_sel = Buf()
        negm = P.sbuf("negm", [128, 4, 32], BF16); b_negm = Buf()
        negmT = [(Wwin[0:32, i, :, :], b_Wwin) for i in range(2)]
        for g in range(2):
            for cc in range(2):
                P.dma("sp", lambda e, cc=cc, g=g: e.dma_start(out=KT[:, cc, :], in_=kbT[:, 2 * g + cc, :]), writes=[b_KV])
            P.op("pool", lambda e: e.memset(V[:, :, :, 64:65], 1.0), writes=[b_KV])
            for hh in range(4):
                for k0 in range(0, NKT, 8):
                    P.dma("sp", lambda e, hh=hh, g=g, k0=k0: e.dma_start(
                        out=V[:, k0:k0 + 8, hh, 0:64],
                        in_=vtok[k0 * 128:(k0 + 8) * 128, 256 + (4 * g + hh) * 64:256 + (4 * g + hh + 1) * 64].rearrange(
                            "(kt p) d -> p kt d", p=128)), writes=[b_KV])
            toeplitz_load(P, W, b_W, crel_scr, b_scr_rel, LC, 4 * g, 11)
            for cc in range(2):
                P.op("dve", lambda e, cc=cc: e.tensor_reduce(out=kms[:], in_=KT[:, cc, :].rearrange("p (n k) -> p n k", k=256),
                                                             axis=AX.X, op=ALU.add), reads=[b_KV], writes=[b_kms])
                P.op("dve", lambda e, cc=cc: e.tensor_scalar(out=kmT[:, cc, :], in0=kms[:], scalar1=1.0 / 256, scalar2=None,
                                                             op0=ALU.mult), reads=[b_kms], writes=[b_kmT])
            for lt in range(NT):
                for hh in range(4):
                    cc = hh // 2
                    P.op("pe", lambda e, hh=hh, cc=cc, lt=lt, g=g: e.matmul(
                        out=ps_misc[:, hh * 32:(hh + 1) * 32], lhsT=QT[:, 4 * g + hh, lt * 128:(lt + 1) * 128],
                        rhs=kmT[:, cc, :], start=(hh == 0), stop=(hh == 3), skip_group_check=True),
                        reads=[b_QT, b_kmT], writes=[b_ps_misc], inc=(hh == 3))
                P.op("dve", lambda e, lt=lt: e.tensor_tensor(
                    out=gate[:], in0=ps_misc[:, 0:128].rearrange("p (h n) -> p h n", n=32),
                    in1=negvalid[:, lt:lt + 1, :].to_broadcast([128, 4, 32]), op=ALU.add),
                    reads=[b_ps_misc, b_nv], writes=[b_gate])
                for hh in range(4):
                    P.op("dve", lambda e, hh=hh: e.max(out=mx8[:, hh, :], in_=gate[:, hh, :]),
                         reads=[b_gate], writes=[b_mx8], inc=(hh == 3))
                P.op("dve", lambda e: e.tensor_tensor(out=sel[:], in0=gate[:], in1=mx8[:, :, 2:3].to_broadcast([128, 4, 32]),
                                                      op=ALU.is_ge), reads=[b_gate, b_mx8], writes=[b_sel])
                P.op("dve", lambda e, lt=lt: e.tensor_tensor(out=sel[:], in0=sel[:],
                                                             in1=own1h[:, lt:lt + 1, :].to_broadcast([128, 4, 32]),
                                                             op=ALU.max), reads=[b_sel, b_own], writes=[b_sel])
                P.op("dve", lambda e: e.tensor_scalar(out=sel[:], in0=sel[:], scalar1=-1.0, scalar2=-NEGM,
                                                      op0=ALU.add, op1=ALU.mult), reads=[b_sel], writes=[b_sel])
                P.op("dve", lambda e, g=g: e.tensor_tensor(
                    out=negm[:], in0=sel[:], in1=b31[:, 4 * g:4 * g + 4].unsqueeze(2).to_broadcast([128, 4, 32]),
                    op=ALU.add), reads=[b_sel, b_b31], writes=[b_negm])
                for hh in range(4):
                    P.op("pe", lambda e, hh=hh: e.transpose(out=ps_trf[0:32, hh, :], in_=negm[:, hh, :], identity=C.idb[:]),
                         reads=[b_negm, C.b_idb], writes=[b_ps_tr], inc=(hh == 3))
                nT, b_nT = negmT[lt % 2]
                P.op("act", lambda e, nT=nT: e.copy(out=nT, in_=ps_trf[0:32, 0:4, :]), reads=[b_ps_tr], writes=[b_nT])
                O, b_O = Ops[lt % 2]
                kts = list(range(0, min(STRIDE * lt + STRIDE, NKT)))

                def qk_fn(kt, lt=lt, g=g):
                    return [(hh * 128, (hh + 1) * 128,
                             [(KT[:, hh // 2, kt * 128:(kt + 1) * 128], QT[:, 4 * g + hh, lt * 128:(lt + 1) * 128])],
                             [b_KV, b_QT]) for hh in range(4)]

                def extra_fn(kt, lt=lt, nT=nT, b_nT=b_nT):
                    ex = [(ohsel[:, (kt // 2) * 128:(kt // 2 + 1) * 128], nT.rearrange("n h q -> n (h q)"),
                           [b_ohsel, b_nT])]
                    dl = STRIDE * lt - kt
                    if dl <= 7:
                        ex.append((C.antib[:], W[:, dl + 3, :, :].rearrange("p h q -> p (h q)"), [C.b_antib, b_W]))
                    return ex
                attn_steps(P, R, kts, qk_fn, extra_fn, lambda kt, hh: (V[:, kt, hh, :], [b_KV]), O, b_O, 0.125)
                P.op("dve", lambda e, O=O: e.tensor_scalar(out=rz[:], in0=O[:, :, 64:65], scalar1=1e-30, scalar2=None,
                                                           op0=ALU.max), reads=[b_O], writes=[b_rz])
                P.op("dve", lambda e: e.reciprocal(out=rz[:], in_=rz[:]), reads=[b_rz], writes=[b_rz])
                P.op("dve", lambda e, O=O, lt=lt, g=g: e.tensor_tensor(
                    out=o_sb[:, lt, 512 + g * 256:512 + (g + 1) * 256].rearrange("p (h d) -> p h d", d=64),
                    in0=O[:, :, 0:64], in1=rz[:].to_broadcast([128, 4, 64]), op=ALU.mult),
                    reads=[b_O, b_rz], writes=[b_osb])
    for t in range(NT):
        outs.append(P.dma("sp", lambda e, t=t: e.dma_start(out=o_out[t * 128:(t + 1) * 128, :], in_=o_sb[:, t, :]),
                          reads=[b_osb]))
    P.wait_all("sp", outs)
    P.finish()
    return nc


def build_post(NT=16, final=False, NEXP=16):
    nc = bass.Bass("TRN2", target_bir_lowering=False)
    NTOK = NT * 128
    dt = lambda n, s, d, k: nc.dram_tensor(n, s, d, kind=k).ap()
    o_attn = dt("o_attn", [NTOK, 1024], BF16, "ExternalInput")
    x_d = dt("x", [NTOK, D], F32, "ExternalInput")
    mod_d = dt("mod", [6, D], F32, "ExternalInput")
    w_out_d = dt("w_out", [D, D], F32, "ExternalInput")
    g_ffn_d = dt("g_ffn", [1, D], F32, "ExternalInput")
    g_fin_d = dt("g_fin", [1, D], F32, "ExternalInput")
    rw_d = dt("router_w", [D, 16], F32, "ExternalInput")
    rb_d = dt("router_b", [1, 16], F32, "ExternalInput")
    wg_d = dt("wg", [16, D, 512], F32, "ExternalInput")
    wu_d = dt("wu", [16, D, 512], F32, "ExternalInput")
    wd_d = dt("wd", [16, 512, D], F32, "ExternalInput")
    oh16_d = dt("oh16", [16, 16 * 128], F32, "ExternalInput")
    ident = dt("ident", [128, 128], F32, "ExternalInput")
    x_out = dt("x_out", [NTOK, D], F32, "ExternalOutput")

    P = Prog(nc)
    C = Consts(P, nc, ident[:, :])
    ps_a = [(P.psum(f"pa{i}", [128, 512], F32), Buf()) for i in range(4)]
    ps_y = [(P.psum(f"py{i}", [128, 512], F32), Buf()) for i in range(2)]
    ps_w = (P.psum("pw", [128, 512], F32), Buf())
    ps_tr = P.psum("ptr", [128, 8, 128], BF16); b_ps_tr = Buf()
    x_sb = P.sbuf("x_sb", [128, NT, D], F32); b_x = [Buf() for _ in range(NT)]
    hfT = P.sbuf("hfT", [128, 8, NTOK], BF16); b_hfT = Buf()
    WB = [P.sbuf(f"WB{i}", [128, 12288], BF16) for i in range(2)]; b_WB = [Buf(), Buf()]
    bc = P.sbuf("bc", [128, 4, D], F32); b_bc = Buf()
    scr_b = P.sbuf("scr_b", [128, 2048], BF16)
    nsc = norm_scratch(P, "p", hb=scr_b[:, 1024:2048])
    gf = nsc["h32"]; b_gf = nsc["b"][3]
    P.dma("sp", lambda e: e.dma_start(out=bc[:, 0, :], in_=mod_d[2:3, :].to_broadcast([128, D])), writes=[b_bc])
    P.dma("sp", lambda e: e.dma_start(out=bc[:, 1, :], in_=mod_d[4:5, :].to_broadcast([128, D])), writes=[b_bc])
    P.dma("sp", lambda e: e.dma_start(out=bc[:, 2, :], in_=mod_d[3:4, :].to_broadcast([128, D])), writes=[b_bc])
    P.dma("sp", lambda e: e.dma_start(out=bc[:, 3, :], in_=mod_d[5:6, :].to_broadcast([128, D])), writes=[b_bc])
    P.dma("sp", lambda e: e.dma_start(out=gf[:], in_=g_ffn_d[0:1, :].to_broadcast([128, D])), writes=[b_gf])
    P.op("dve", lambda e: e.scalar_tensor_tensor(out=bc[:, 1, :], in0=bc[:, 1, :], scalar=1.0, in1=gf[:],
                                                 op0=ALU.add, op1=ALU.mult), reads=[b_bc, b_gf], writes=[b_bc])
    wo = WB[1][:, 0:8192].rearrange("p (k n) -> p k n", n=1024)
    wov = w_out_d.rearrange("(k p) n -> p k n", p=128)
    for k in range(8):
        P.dma("pool", lambda e, k=k: e.dma_start(out=wo[:, k, :], in_=wov[:, k, :]), writes=[b_WB[1]])
    for k in range(8):
        P.op("dve", lambda e, k=k: e.tensor_tensor(out=wo[:, k, :], in0=wo[:, k, :], in1=bc[:, 0, :], op=ALU.mult),
             reads=[b_WB[1], b_bc], writes=[b_WB[1]])
    rw = P.sbuf("rw", [128, 8, 16], F32); b_rw = Buf()
    P.dma("sp", lambda e: e.dma_start(out=rw[:], in_=rw_d.rearrange("(k p) n -> p k n", p=128)), writes=[b_rw])
    rb = P.sbuf("rb", [128, 16], F32); b_rb = Buf()
    P.dma("sp", lambda e: e.dma_start(out=rb[:], in_=rb_d[0:1, :].to_broadcast([128, 16])), writes=[b_rb])
    oh16 = P.sbuf("oh16", [16, 16 * 128], F32); b_oh16 = Buf()
    P.dma("sp", lambda e: e.dma_start(out=oh16[:], in_=oh16_d[:, :]), writes=[b_oh16])
    wT = P.sbuf("wT", [16, NTOK], F32); b_wT = Buf()
    ob = [scr_b[:, 0:1024]] * 2; b_ob = [Buf()] * 2
    oT = P.sbuf("oT", [128, 8, 128], BF16); b_oT = Buf()
    h32T = nsc["sq"][:].rearrange("p (k n) -> p k n", n=128); b_h32T = nsc["b"][0]
    r_aff = P.sbuf("r_aff", [128, 16], F32); b_aff = Buf()
    r_b = P.sbuf("r_b", [128, 4, 4], F32); b_rbias = Buf()
    r_t = P.sbuf("r_t", [128, 8, 4], F32); b_rt = Buf()
    r_w = P.sbuf("r_w", [128, 4, 4], F32); b_rw2 = Buf()
    r_s = P.sbuf("r_s", [128, 2], F32); b_rs = Buf()
    for t in range(NT):
        o_t = ob[t % 2]; bo = b_ob[t % 2]
        P.dma("sp", lambda e, o_t=o_t, t=t: e.dma_start(out=o_t[:], in_=o_attn[t * 128:(t + 1) * 128, :]), writes=[bo])
        P.dma("sp", lambda e, t=t: e.dma_start(out=x_sb[:, t, :], in_=x_d[t * 128:(t + 1) * 128, :]), writes=[b_x[t]])
        for k in range(8):
            P.op("pe", lambda e, k=k, o_t=o_t: e.transpose(out=ps_tr[:, k, :], in_=o_t[:, k * 128:(k + 1) * 128],
                                                           identity=C.idb[:]),
                 reads=[bo, C.b_idb], writes=[b_ps_tr], inc=(k == 7))
        P.op("act", lambda e: e.copy(out=oT[:], in_=ps_tr[:]), reads=[b_ps_tr], writes=[b_oT])
        for half in range(2):
            py, b_py = ps_y[half]
            for k in range(8):
                P.op("pe", lambda e, k=k, half=half, py=py: e.matmul(out=py[:], lhsT=oT[:, k, :],
                                                                   rhs=wo[:, k, half * 512:(half + 1) * 512],
                                                                   start=(k == 0), stop=(k == 7)),
                     reads=[b_oT, b_WB[1]], writes=[b_py], inc=(k == 7))
            P.op("dve", lambda e, half=half, py=py, t=t: e.tensor_tensor(
                out=x_sb[:, t, half * 512:(half + 1) * 512], in0=py[:], in1=x_sb[:, t, half * 512:(half + 1) * 512],
                op=ALU.add), reads=[b_py, b_x[t]], writes=[b_x[t]])
        emit_norm_tile(P, C, x_sb[:, t, :], b_x[t], bc[:, 1, :], b_bc, bc[:, 2, :], b_bc,
                       hfT[:, :, t * 128:(t + 1) * 128], b_hfT, nsc, ps_tr, b_ps_tr)
        h32 = nsc["h32"]; b_h32 = nsc["b"][3]
        P.op("dve", lambda e: e.tensor_tensor(out=h32[:], in0=h32[:], in1=bc[:, 2, :], op=ALU.add),
             reads=[b_h32, b_bc], writes=[b_h32])
        for hf_ in range(2):
            pa, b_pa = ps_a[hf_]
            for k in range(4):
                kk = hf_ * 4 + k
                P.op("pe", lambda e, k=k, kk=kk, pa=pa: e.transpose(out=pa[:, k * 128:(k + 1) * 128],
                                                                  in_=h32[:, kk * 128:(kk + 1) * 128], identity=C.idf[:]),
                     reads=[b_h32, C.b_idf], writes=[b_pa], inc=(k == 3))
            P.op("act", lambda e, hf_=hf_, pa=pa: e.copy(out=h32T[:, hf_ * 4:(hf_ + 1) * 4, :],
                                                         in_=pa[:].rearrange("p (k n) -> p k n", n=128)),
                 reads=[b_pa], writes=[b_h32T])
        pw, b_pw = ps_w
        for k in range(8):
            P.op("pe", lambda e, k=k: e.matmul(out=pw[:, 0:16], lhsT=h32T[:, k, :], rhs=rw[:, k, :],
                                               start=(k == 0), stop=(k == 7)),
                 reads=[b_h32T, b_rw], writes=[b_pw], inc=(k == 7))
        P.op("act", lambda e: e.activation(out=r_aff[:], in_=pw[:, 0:16], func=AF.Sigmoid), reads=[b_pw], writes=[b_aff])
        r_bf = r_b[:].rearrange("p g e -> p (g e)")
        P.op("dve", lambda e: e.tensor_tensor(out=r_bf, in0=r_aff[:], in1=rb[:], op=ALU.add),
             reads=[b_aff, b_rb], writes=[b_rbias])
        a_, b_, c_, d_ = (r_b[:, :, i] for i in range(4))
        T = lambda i: r_t[:, i, :]
        seq = [(T(0), a_, b_, ALU.max), (T(1), a_, b_, ALU.min), (T(2), c_, d_, ALU.max), (T(3), c_, d_, ALU.min),
               (T(4), T(0), T(2), ALU.max), (T(5), T(0), T(2), ALU.min), (T(6), T(1), T(3), ALU.max),
               (T(7), T(5), T(6), ALU.max),
               (T(0), T(4), T(7), ALU.add)]
        for (o_, i0, i1, op_) in seq:
            P.op("dve", lambda e, o_=o_, i0=i0, i1=i1, op_=op_: e.tensor_tensor(out=o_, in0=i0, in1=i1, op=op_),
                 reads=[b_rbias, b_rt], writes=[b_rt])
        P.op("dve", lambda e: e.tensor_reduce(out=r_s[:, 0:1], in_=r_t[:, 0, :], axis=AX.X, op=ALU.max),
             reads=[b_rt], writes=[b_rs])
        P.op("dve", lambda e: e.tensor_scalar(out=r_t[:, 1, :], in0=r_t[:, 0, :], scalar1=r_s[:, 0:1], scalar2=None,
                                              op0=ALU.is_ge), reads=[b_rt, b_rs], writes=[b_rt])
        P.op("dve", lambda e: e.tensor_tensor(out=r_w[:], in0=r_b[:], in1=r_t[:, 7, :].unsqueeze(2).to_broadcast([128, 4, 4]),
                                              op=ALU.is_ge), reads=[b_rbias, b_rt], writes=[b_rw2])
        P.op("dve", lambda e: e.tensor_tensor(out=r_w[:], in0=r_w[:], in1=r_t[:, 1, :].unsqueeze(2).to_broadcast([128, 4, 4]),
                                              op=ALU.mult), reads=[b_rw2, b_rt], writes=[b_rw2])
        r_wf = r_w[:].rearrange("p g e -> p (g e)")
        P.op("dve", lambda e: e.tensor_tensor(out=r_wf, in0=r_wf, in1=r_aff[:], op=ALU.mult),
             reads=[b_rw2, b_aff], writes=[b_rw2])
        P.op("dve", lambda e: e.tensor_reduce(out=r_s[:, 1:2], in_=r_wf, axis=AX.X, op=ALU.add),
             reads=[b_rw2], writes=[b_rs])
        P.op("dve", lambda e: e.reciprocal(out=r_s[:, 1:2], in_=r_s[:, 1:2]), reads=[b_rs], writes=[b_rs])
        P.op("dve", lambda e: e.tensor_scalar(out=r_wf, in0=r_wf, scalar1=r_s[:, 1:2], scalar2=None, op0=ALU.mult),
             reads=[b_rw2, b_rs], writes=[b_rw2])
        pa, b_pa = ps_a[2]
        P.op("pe", lambda e, pa=pa: e.transpose(out=pa[0:16, 0:128], in_=r_wf, identity=C.idf[:]),
             reads=[b_rw2, C.b_idf], writes=[b_pa])
        P.op("act", lambda e, pa=pa, t=t: e.copy(out=wT[:, t * 128:(t + 1) * 128], in_=pa[0:16, 0:128]),
             reads=[b_pa], writes=[b_wT])
    wbc = [P.sbuf("wbc0", [128, 512], F32)] * 2; b_wbc = [Buf()] * 2
    sg = [P.sbuf(f"sg{i}", [128, 512], BF16) for i in range(2)]; b_sg = [Buf(), Buf()]
    uw = [P.sbuf(f"uw{i}", [128, 512], BF16) for i in range(2)]; b_uw = [Buf(), Buf()]
    hid = [P.sbuf("hid0", [128, 4, 512], BF16), scr_b[:, :].rearrange("p (f n) -> p f n", n=512)]
    b_hid = [Buf(), Buf()]
    hid1_first = [True]
    NTG = NT // 4
    pai = [0]

    def stage_a(ex, tg, it, wg, wu, bW):
        wb_ = wbc[it % 2]; bwb = b_wbc[it % 2]
        hd = hid[it % 2]; bhd = b_hid[it % 2]
        pw, b_pw = ps_w
        P.op("pe", lambda e: e.matmul(out=pw[:], lhsT=oh16[:, ex * 128:(ex + 1) * 128],
                                      rhs=wT[:, tg * 512:(tg + 1) * 512], start=True, stop=True),
             reads=[b_oh16, b_wT], writes=[b_pw])
        P.op("act", lambda e: e.copy(out=wb_[:], in_=pw[:]), reads=[b_pw], writes=[bwb])
        for fc in range(4):
            pg, b_pg = ps_a[pai[0] % 4]; pai[0] += 1
            pu, b_pu = ps_a[pai[0] % 4]; pai[0] += 1
            for k in range(8):
                P.op("pe", lambda e, k=k, fc=fc, pg=pg: e.matmul(
                    out=pg[:], lhsT=wg[:, k, fc * 128:(fc + 1) * 128], rhs=hfT[:, k, tg * 512:(tg + 1) * 512],
                    start=(k == 0), stop=(k == 7)), reads=[bW, b_hfT], writes=[b_pg], inc=(k == 7))
            for k in range(8):
                P.op("pe", lambda e, k=k, fc=fc, pu=pu: e.matmul(
                    out=pu[:], lhsT=wu[:, k, fc * 128:(fc + 1) * 128], rhs=hfT[:, k, tg * 512:(tg + 1) * 512],
                    start=(k == 0), stop=(k == 7)), reads=[bW, b_hfT], writes=[b_pu], inc=(k == 7))
            s_ = sg[fc % 2]; bs_ = b_sg[fc % 2]
            u_ = uw[fc % 2]; bu_ = b_uw[fc % 2]
            P.op("act", lambda e, pg=pg, s_=s_: e.activation(out=s_[:], in_=pg[:], func=AF.Silu), reads=[b_pg], writes=[bs_])
            P.op("dve", lambda e, pu=pu, u_=u_: e.tensor_tensor(out=u_[:], in0=pu[:], in1=wb_[:], op=ALU.mult),
                 reads=[b_pu, bwb], writes=[bu_])
            wr = [bhd]
            if it % 2 == 1 and hid1_first[0]:
                wr = [bhd, b_ob[0], nsc["b"][4]]
                hid1_first[0] = False
            P.op("dve", lambda e, fc=fc, s_=s_, u_=u_: e.tensor_tensor(out=hd[:, fc, :], in0=s_[:], in1=u_[:], op=ALU.mult),
                 reads=[bs_, bu_], writes=wr)

    def stage_b(ex, tg, it, wd, bW):
        hd = hid[it % 2]; bhd = b_hid[it % 2]
        for tt in range(4):
            t = tg * 4 + tt
            for half in range(2):
                py, b_py = ps_y[half]
                for fc in range(4):
                    P.op("pe", lambda e, fc=fc, tt=tt, half=half, py=py: e.matmul(
                        out=py[:], lhsT=hd[:, fc, tt * 128:(tt + 1) * 128], rhs=wd[:, fc, half * 512:(half + 1) * 512],
                        start=(fc == 0), stop=(fc == 3)), reads=[bhd, bW], writes=[b_py], inc=(fc == 3))
                P.op("dve", lambda e, half=half, py=py, t=t: e.tensor_tensor(
                    out=x_sb[:, t, half * 512:(half + 1) * 512], in0=py[:], in1=x_sb[:, t, half * 512:(half + 1) * 512],
                    op=ALU.add), reads=[b_py, b_x[t]], writes=[b_x[t]])

    pend = None
    it = 0
    for ex in range(NEXP):
        Wb = WB[ex % 2]; bW = b_WB[ex % 2]
        wg = Wb[:, 0:4096].rearrange("p (k n) -> p k n", n=512)
        wu = Wb[:, 4096:8192].rearrange("p (k n) -> p k n", n=512)
        wd = Wb[:, 8192:12288].rearrange("p (k n) -> p k n", n=1024)
        wgv = wg_d[ex].rearrange("(k p) n -> p k n", p=128)
        wuv = wu_d[ex].rearrange("(k p) n -> p k n", p=128)
        wdv = wd_d[ex].rearrange("(k p) n -> p k n", p=128)
        for k in range(8):
            P.dma("pool", lambda e, k=k, wg=wg, wgv=wgv: e.dma_start(out=wg[:, k, :], in_=wgv[:, k, :]), writes=[bW])
            P.dma("pool", lambda e, k=k, wu=wu, wuv=wuv: e.dma_start(out=wu[:, k, :], in_=wuv[:, k, :]), writes=[bW])
        for k in range(4):
            P.dma("pool", lambda e, k=k, wd=wd, wdv=wdv: e.dma_start(out=wd[:, k, :], in_=wdv[:, k, :]), writes=[bW])
        for k in range(4):
            P.op("pool", lambda e, k=k, wd=wd: e.tensor_tensor(out=wd[:, k, :], in0=wd[:, k, :], in1=bc[:, 3, :], op=ALU.mult),
                 reads=[bW, b_bc], writes=[bW])
        for tg in range(NTG):
            stage_a(ex, tg, it, wg, wu, bW)
            if pend is not None:
                stage_b(*pend)
            pend = (ex, tg, it, wd, bW)
            it += 1
    if pend is not None:
        stage_b(*pend)
    outs = []
    if final:
        gfin = bc[:, 0, :]
        b_gf = b_bc
        P.dma("sp", lambda e: e.dma_start(out=gfin, in_=g_fin_d[0:1, :].to_broadcast([128, D])), writes=[b_gf])
        sq, ss, rstd = nsc["sq"], nsc["ss"], nsc["rstd"]
        b_sq, b_ss, b_rstd = nsc["b"][0:3]
        for t in range(NT):
            P.op("act", lambda e, t=t: e.activation(out=sq[:], in_=x_sb[:, t, :], func=AF.Square, accum_out=ss[:]),
                 reads=[b_x[t]], writes=[b_sq, b_ss])
            P.op("dve", lambda e: e.tensor_scalar(out=rstd[:], in0=ss[:], scalar1=1.0 / D, scalar2=1e-6,
                                                  op0=ALU.mult, op1=ALU.add), reads=[b_ss], writes=[b_rstd])
            P.op("act", lambda e: e.activation(out=rstd[:], in_=rstd[:], func=AF.Sqrt), reads=[b_rstd], writes=[b_rstd])
            P.op("dve", lambda e: e.reciprocal(out=rstd[:], in_=rstd[:]), reads=[b_rstd], writes=[b_rstd])
            P.op("dve", lambda e, t=t: e.scalar_tensor_tensor(out=x_sb[:, t, :], in0=x_sb[:, t, :], scalar=rstd[:, 0:1],
                                                              in1=gfin, op0=ALU.mult, op1=ALU.mult),
                 reads=[b_x[t], b_rstd, b_gf], writes=[b_x[t]])
    for t in range(NT):
        outs.append(P.dma("sp", lambda e, t=t: e.dma_start(out=x_out[t * 128:(t + 1) * 128, :], in_=x_sb[:, t, :]),
                          reads=[b_x[t]]))
    P.wait_all("sp", outs)
    P.finish()
    return nc


def oh16_static():
    oh = np.zeros((16, 16, 128), np.float32)
    for e in range(16):
        oh[e, e, :] = 1.0
    return oh.reshape(16, 2048)


OD = dict(c_q=(0, 256), c_kv=(256, 384), k_rope=(384, 448), q_d=(448, 960), k_d=(960, 1088), v_d=(1088, 1216))
NU1 = 10


def host_w_in_odd(w, wq_up, wkv_up):
    sl = lambda n: w[:, OD[n][0]:OD[n][1]]
    units = []
    for h in range(8):
        u = np.zeros((1024, 128), np.float32)
        u[:, (h % 2) * 64:(h % 2 + 1) * 64] = sl("q_d")[:, h * 64:(h + 1) * 64]
        units.append(u)
    for kv in range(2):
        c = sl("k_d")[:, kv * 64:(kv + 1) * 64]
        units.append(np.concatenate([c, c], axis=1))
    WF = np.concatenate(units, axis=1)
    WT = np.concatenate([sl("c_q"), sl("c_kv"), sl("k_rope"), sl("v_d")], axis=1)
    wq = wq_up.reshape(256, 4, 192)
    wq_nope = np.ascontiguousarray(wq[:, :, 0:128].reshape(256, 512))
    wq_rope = np.ascontiguousarray(wq[:, :, 128:192].reshape(256, 256))
    wkv = wkv_up.reshape(128, 4, 256)
    wk_nope = np.ascontiguousarray(wkv[:, :, 0:128].reshape(128, 512))
    wv = np.ascontiguousarray(wkv[:, :, 128:256].reshape(128, 512))
    return dict(wf=np.ascontiguousarray(WF), wt=np.ascontiguousarray(WT), wq_nope=wq_nope, wq_rope=wq_rope,
                wk_nope=wk_nope, wv=wv)


def rope_static(positions):
    inv = (10000.0 ** (-np.arange(0, 64, 2, dtype=np.float32) / 64)).astype(np.float32)
    ang = positions.astype(np.float32)[:, None] * inv[None, :]
    return np.cos(ang).astype(np.float32), np.sin(ang).astype(np.float32)


def build_L1odd(NT=16):
    nc = bass.Bass("TRN2", target_bir_lowering=False)
    NTOK = NT * 128
    dt = lambda n, s, d, k: nc.dram_tensor(n, s, d, kind=k).ap()
    x = dt("x", [NTOK, D], F32, "ExternalInput")
    c_cols = dt("c_cols", [128, 8], F32, "ExternalInput")
    ada_w = dt("ada_w", [D, 6 * D], F32, "ExternalInput")
    ada_b = dt("ada_b", [1, 6 * D], F32, "ExternalInput")
    g_mix = dt("g_mix", [1, D], F32, "ExternalInput")
    wf_d = dt("wf", [D, NU1 * 128], F32, "ExternalInput")
    wt_d = dt("wt", [D, 576], F32, "ExternalInput")
    wqn_d = dt("wq_nope", [256, 512], F32, "ExternalInput")
    wqr_d = dt("wq_rope", [256, 256], F32, "ExternalInput")
    wkn_d = dt("wk_nope", [128, 512], F32, "ExternalInput")
    wv_d = dt("wv", [128, 512], F32, "ExternalInput")
    qn_g = dt("q_norm", [1, 256], F32, "ExternalInput")
    kvn_g = dt("kv_norm", [1, 128], F32, "ExternalInput")
    cos_d = dt("cos", [NTOK, 32], F32, "ExternalInput")
    sin_d = dt("sin", [NTOK, 32], F32, "ExternalInput")
    ident = dt("ident", [128, 128], F32, "ExternalInput")
    o_fm = dt("o_fm", [128, NU1, NTOK], BF16, "ExternalOutput")
    o_qn = dt("o_qn", [128, 4, NTOK], BF16, "ExternalOutput")
    o_qr = dt("o_qr", [64, 4, NTOK], BF16, "ExternalOutput")
    o_kn = dt("o_kn", [128, 4, NTOK], BF16, "ExternalOutput")
    o_kr = dt("o_kr", [64, NTOK], BF16, "ExternalOutput")
    o_vtok = dt("o_vtok", [NTOK, 640], BF16, "ExternalOutput")
    o_mod = dt("o_mod", [6, D], F32, "ExternalOutput")

    P = Prog(nc)
    C = Consts(P, nc, ident[:, :])
    ps_row = P.psum("ps_row", [128, 512], F32); b_ps_row = Buf()
    ps_bc = P.psum("ps_bc", [128, 512], F32); b_ps_bc = Buf()
    ps_tr = P.psum("ps_tr", [128, 8, 128], BF16); b_ps_tr = Buf()
    ps_mm = [P.psum(f"ps_mm{i}", [128, 512], F32) for i in range(4)]
    b_ps_mm = [Buf() for _ in range(4)]
    mod, b_mod = emit_adaln(P, nc, C, c_cols[:, :], ada_w, ada_b[:, :], "1", ps_row, b_ps_row, ps_bc, b_ps_bc)
    outs = []
    outs.append(P.dma("sp", lambda e: e.dma_start(out=o_mod[:, :], in_=mod[0:1, :, :]), reads=[b_mod]))
    gm = P.sbuf("gm", [128, 1024], F32); b_gm = Buf()
    A = P.sbuf("A_m", [128, 1024], F32); b_A = Buf()
    P.dma("sp", lambda e: e.dma_start(out=gm[:], in_=g_mix[0:1, :].to_broadcast([128, 1024])), writes=[b_gm])
    P.op("dve", lambda e: e.scalar_tensor_tensor(out=A[:], in0=mod[:, 1, :], scalar=1.0, in1=gm[:],
                                                 op0=ALU.add, op1=ALU.mult), reads=[b_mod, b_gm], writes=[b_A])
    Bt = mod[:, 0, :]
    wf, b_wf = load_w_bf16(P, nc, "wf_sb", wf_d, NU1 * 128)
    wt, b_wt = load_w_bf16(P, nc, "wt_sb", wt_d, 576)
    wqn, b_wqn = load_w_bf16(P, nc, "wqn_sb", wqn_d, 512, rows=256)
    wqr, b_wqr = load_w_bf16(P, nc, "wqr_sb", wqr_d, 256, rows=256)
    wkn, b_wkn = load_w_bf16(P, nc, "wkn_sb", wkn_d, 512, rows=128)
    wv, b_wv = load_w_bf16(P, nc, "wv_sb", wv_d, 512, rows=128)
    qng = P.sbuf("qng", [128, 256], F32); b_qng = Buf()
    kvng = P.sbuf("kvng", [128, 128], F32); b_kvng = Buf()
    P.dma("sp", lambda e: e.dma_start(out=qng[:], in_=qn_g[0:1, :].to_broadcast([128, 256])), writes=[b_qng])
    P.dma("sp", lambda e: e.dma_start(out=kvng[:], in_=kvn_g[0:1, :].to_broadcast([128, 128])), writes=[b_kvng])
    cs = P.sbuf("cs", [128, NT, 2, 32], F32); b_cs = Buf()
    P.dma("sp", lambda e: e.dma_start(out=cs[:, :, 0, :], in_=cos_d.rearrange("(t p) n -> p t n", p=128)), writes=[b_cs])
    P.dma("sp", lambda e: e.dma_start(out=cs[:, :, 1, :], in_=sin_d.rearrange("(t p) n -> p t n", p=128)), writes=[b_cs])
    xt = [P.sbuf(f"xt{i}", [128, 1024], F32) for i in range(2)]
    b_xt = [Buf(), Buf()]
    hT = [P.sbuf(f"hT{i}", [128, 8, 512], BF16) for i in range(2)]
    b_hT = [Buf(), Buf()]
    nsc = norm_scratch(P, "a")
    stg = [P.sbuf(f"stg{i}", [128, 512], BF16) for i in range(4)]
    b_stg = [Buf() for _ in range(4)]
    cq = P.sbuf("cq", [128, 384], F32); b_cq = Buf()
    cqn = P.sbuf("cqn", [128, 384], BF16); b_cqn = Buf()
    cT = P.sbuf("cT", [128, 3, 128], BF16); b_cT = Buf()
    mss = P.sbuf("mss", [128, 4], F32); b_mss = Buf()
    junk = P.sbuf("junk", [128, 256], F32); b_junk = Buf()
    rp = P.sbuf("rp", [128, 5, 64], F32); b_rp = Buf()
    rt = P.sbuf("rt", [128, 4, 5, 32], F32); b_rt = Buf()
    rpb = P.sbuf("rpb", [128, 5, 64], BF16); b_rpb = Buf()
    rT = P.sbuf("rT", [64, 5, 128], BF16); b_rT = Buf()
    rr = RR()
    si = 0
    mi = 0

    def next_ps():
        nonlocal mi
        r = (ps_mm[mi % 4], b_ps_mm[mi % 4]); mi += 1
        return r

    def next_stg():
        nonlocal si
        r = (stg[si % 4], b_stg[si % 4]); si += 1
        return r
    for tg in range(NT // 4):
        h = hT[tg % 2]; bh = b_hT[tg % 2]
        for tt in range(4):
            t = tg * 4 + tt
            xb = xt[t % 2]; bx = b_xt[t % 2]
            P.dma("sp", lambda e, xb=xb, t=t: e.dma_start(out=xb[:], in_=x[t * 128:(t + 1) * 128, :]), writes=[bx])
            emit_norm_tile(P, C, xb[:], bx, A[:], b_A, Bt, b_mod, h[:, :, tt * 128:(tt + 1) * 128], bh,
                           nsc, ps_tr, b_ps_tr)
        for u in range(NU1):
            ps, bps = next_ps()
            for k in range(8):
                P.op("pe", lambda e, ps=ps, u=u, k=k, h=h: e.matmul(out=ps[:], lhsT=wf[:, k, u * 128:(u + 1) * 128],
                                                                  rhs=h[:, k, :], start=(k == 0), stop=(k == 7)),
                     reads=[b_wf, bh], writes=[bps], inc=(k == 7))
            st, bst = next_stg()
            evac(P, rr.next(), st[:], ps[:], [bps], [bst])
            outs.append(P.dma("sp", lambda e, st=st, u=u, tg=tg: e.dma_start(
                out=o_fm[:, u, tg * 512:(tg + 1) * 512], in_=st[:]), reads=[bst]))
        for tt in range(4):
            t = tg * 4 + tt
            tsl = slice(tt * 128, (tt + 1) * 128)
            ps, bps = next_ps()
            for k in range(8):
                P.op("pe", lambda e, ps=ps, k=k, h=h, tsl=tsl: e.matmul(out=ps[:, 0:384], lhsT=h[:, k, tsl], rhs=wt[:, k, 0:384],
                                                                      start=(k == 0), stop=(k == 7)),
                     reads=[b_wt, bh], writes=[bps], inc=(k == 7))
            P.op("act", lambda e, ps=ps: e.copy(out=cq[:], in_=ps[:, 0:384]), reads=[bps], writes=[b_cq])
            psB, bpsB = next_ps()
            for k in range(8):
                P.op("pe", lambda e, psB=psB, k=k, h=h, tsl=tsl: e.matmul(out=psB[:, 0:192], lhsT=h[:, k, tsl], rhs=wt[:, k, 384:576],
                                                                        start=(k == 0), stop=(k == 7)),
                     reads=[b_wt, bh], writes=[bpsB], inc=(k == 7))
            st, bst = next_stg()
            P.op("act", lambda e, st=st, psB=psB: e.copy(out=st[:, 0:128], in_=psB[:, 64:192]), reads=[bpsB], writes=[bst])
            outs.append(P.dma("sp", lambda e, st=st, t=t: e.dma_start(out=o_vtok[t * 128:(t + 1) * 128, 512:640], in_=st[:, 0:128]),
                              reads=[bst]))
            P.op("act", lambda e, psB=psB: e.copy(out=rp[:, 4, :], in_=psB[:, 0:64]), reads=[bpsB], writes=[b_rp])
            P.op("act", lambda e: e.activation(out=junk[:, 0:256], in_=cq[:, 0:256], func=AF.Square, accum_out=mss[:, 0:1]),
                 reads=[b_cq], writes=[b_junk, b_mss])
            P.op("act", lambda e: e.activation(out=junk[:, 0:128], in_=cq[:, 256:384], func=AF.Square, accum_out=mss[:, 1:2]),
                 reads=[b_cq], writes=[b_junk, b_mss])
            P.op("dve", lambda e: e.tensor_scalar(out=mss[:, 2:3], in0=mss[:, 0:1], scalar1=1.0 / 256, scalar2=1e-6,
                                                  op0=ALU.mult, op1=ALU.add), reads=[b_mss], writes=[b_mss])
            P.op("dve", lambda e: e.tensor_scalar(out=mss[:, 3:4], in0=mss[:, 1:2], scalar1=1.0 / 128, scalar2=1e-6,
                                                  op0=ALU.mult, op1=ALU.add), reads=[b_mss], writes=[b_mss])
            P.op("act", lambda e: e.activation(out=mss[:, 2:4], in_=mss[:, 2:4], func=AF.Sqrt), reads=[b_mss], writes=[b_mss])
            P.op("dve", lambda e: e.reciprocal(out=mss[:, 2:4], in_=mss[:, 2:4]), reads=[b_mss], writes=[b_mss])
            P.op("dve", lambda e: e.scalar_tensor_tensor(out=cqn[:, 0:256], in0=cq[:, 0:256], scalar=mss[:, 2:3], in1=qng[:],
                                                         op0=ALU.mult, op1=ALU.mult), reads=[b_cq, b_mss, b_qng], writes=[b_cqn])
            P.op("dve", lambda e: e.scalar_tensor_tensor(out=cqn[:, 256:384], in0=cq[:, 256:384], scalar=mss[:, 3:4], in1=kvng[:],
                                                         op0=ALU.mult, op1=ALU.mult), reads=[b_cq, b_mss, b_kvng], writes=[b_cqn])
            for k in range(3):
                P.op("pe", lambda e, k=k: e.transpose(out=ps_tr[:, k, :], in_=cqn[:, k * 128:(k + 1) * 128], identity=C.idb[:]),
                     reads=[b_cqn, C.b_idb], writes=[b_ps_tr], inc=(k == 2))
            P.op("act", lambda e: e.copy(out=cT[:], in_=ps_tr[:, 0:3, :]), reads=[b_ps_tr], writes=[b_cT])
            ps, bps = next_ps()
            for hh in range(4):
                for k in range(2):
                    P.op("pe", lambda e, ps=ps, hh=hh, k=k: e.matmul(
                        out=ps[:, hh * 128:(hh + 1) * 128], lhsT=wqn[:, k, hh * 128:(hh + 1) * 128], rhs=cT[:, k, :],
                        start=(hh == 0 and k == 0), stop=(hh == 3 and k == 1), skip_group_check=True),
                        reads=[b_wqn, b_cT], writes=[bps], inc=(hh == 3 and k == 1))
            st, bst = next_stg()
            evac(P, rr.next(), st[:], ps[:], [bps], [bst])
            outs.append(P.dma("sp", lambda e, st=st, t=t: e.dma_start(
                out=o_qn[:, :, t * 128:(t + 1) * 128], in_=st[:].rearrange("p (h q) -> p h q", q=128)), reads=[bst]))
            ps, bps = next_ps()
            for hh in range(4):
                P.op("pe", lambda e, ps=ps, hh=hh: e.matmul(
                    out=ps[:, hh * 128:(hh + 1) * 128], lhsT=wkn[:, 0, hh * 128:(hh + 1) * 128], rhs=cT[:, 2, :],
                    start=(hh == 0), stop=(hh == 3), skip_group_check=True),
                    reads=[b_wkn, b_cT], writes=[bps], inc=(hh == 3))
            st, bst = next_stg()
            evac(P, rr.next(), st[:], ps[:], [bps], [bst])
            outs.append(P.dma("sp", lambda e, st=st, t=t: e.dma_start(
                out=o_kn[:, :, t * 128:(t + 1) * 128], in_=st[:].rearrange("p (h q) -> p h q", q=128)), reads=[bst]))
            ps, bps = next_ps()
            P.op("pe", lambda e, ps=ps: e.matmul(out=ps[:], lhsT=cT[:, 2, :], rhs=wv[:, 0, :], start=True, stop=True),
                 reads=[b_wv, b_cT], writes=[bps])
            st, bst = next_stg()
            evac(P, rr.next(), st[:], ps[:], [bps], [bst])
            outs.append(P.dma("sp", lambda e, st=st, t=t: e.dma_start(out=o_vtok[t * 128:(t + 1) * 128, 0:512], in_=st[:]),
                              reads=[bst]))
            ps, bps = next_ps()
            for k in range(2):
                P.op("pe", lambda e, ps=ps, k=k: e.matmul(out=ps[:, 0:256], lhsT=cT[:, k, :], rhs=wqr[:, k, :],
                                                          start=(k == 0), stop=(k == 1)),
                     reads=[b_wqr, b_cT], writes=[bps], inc=(k == 1))
            P.op("act", lambda e, ps=ps: e.copy(out=rp[:, 0:4, :], in_=ps[:, 0:256].rearrange("p (h d) -> p h d", d=64)),
                 reads=[bps], writes=[b_rp])
            cosb = cs[:, t, 0, :].unsqueeze(1).to_broadcast([128, 5, 32])
            sinb = cs[:, t, 1, :].unsqueeze(1).to_broadcast([128, 5, 32])
            x1 = rp[:, :, 0:32]; x2 = rp[:, :, 32:64]
            for i_, (a_, b_) in enumerate([(x1, cosb), (x2, sinb), (x1, sinb), (x2, cosb)]):
                P.op("dve", lambda e, i_=i_, a_=a_, b_=b_: e.tensor_tensor(out=rt[:, i_, :, :], in0=a_, in1=b_, op=ALU.mult),
                     reads=[b_rp, b_cs], writes=[b_rt])
            P.op("dve", lambda e: e.tensor_tensor(out=rpb[:, :, 0:32], in0=rt[:, 0, :, :], in1=rt[:, 1, :, :], op=ALU.subtract),
                 reads=[b_rt], writes=[b_rpb])
            P.op("dve", lambda e: e.tensor_tensor(out=rpb[:, :, 32:64], in0=rt[:, 2, :, :], in1=rt[:, 3, :, :], op=ALU.add),
                 reads=[b_rt], writes=[b_rpb])
            for v_ in range(5):
                P.op("pe", lambda e, v_=v_: e.transpose(out=ps_tr[0:64, v_, :], in_=rpb[:, v_, :], identity=C.idb[:]),
                     reads=[b_rpb, C.b_idb], writes=[b_ps_tr], inc=(v_ == 4))
            P.op("act", lambda e: e.copy(out=rT[:], in_=ps_tr[0:64, 0:5, :]), reads=[b_ps_tr], writes=[b_rT])
            outs.append(P.dma("sp", lambda e, t=t: e.dma_start(out=o_qr[:, :, t * 128:(t + 1) * 128], in_=rT[:, 0:4, :]),
                              reads=[b_rT]))
            outs.append(P.dma("sp", lambda e, t=t: e.dma_start(out=o_kr[:, t * 128:(t + 1) * 128], in_=rT[:, 4, :]),
                              reads=[b_rT]))
    P.wait_all("sp", outs)
    P.finish()
    return nc


def attn1_static(NT, STRIDE, j):
    S_ = STRIDE
    st = {}
    pp = np.arange(128)[:, None, None]
    r = np.arange(S_)[None, :, None]
    x = np.arange(128)[None, None, :]
    d = 128 * (r - (S_ - 1)) + 128 * j + x - (127 - pp)
    m = np.where(d >= 0, 0.0, NEGM).astype(np.float32)
    st["wm"] = np.ascontiguousarray(np.stack([m, m], axis=2)).astype(NPBF)
    L = 128 * S_ + 127 + 128
    y = np.arange(L)
    st["oh_swa"] = onehot_table(y - 127 - 128 * (S_ - 1) + 128 * j, "abs", win=128)
    st["cnt_swa"] = pad_counts(NT, STRIDE, j, 128)
    return st


def build_attn1(NT=16, STRIDE=4, NKT=64):
    nc = bass.Bass("TRN2", target_bir_lowering=False)
    NTOK = NT * 128
    NK = NKT * 128
    S_ = STRIDE
    LS = 128 * S_ + 127 + 128
    NAFF = n_aff(STRIDE, 128)
    dt = lambda n, s, d, k: nc.dram_tensor(n, s, d, kind=k).ap()
    qn_d = dt("qn", [128, 4, NTOK], BF16, "ExternalInput")
    qr_d = dt("qr", [64, 4, NTOK], BF16, "ExternalInput")
    qd_d = dt("qd", [128, 8, NTOK], BF16, "ExternalInput")
    kn_d = dt("kn", [128, 4, NK], BF16, "ExternalInput")
    kr_d = dt("kr", [64, NK], BF16, "ExternalInput")
    kd_d = dt("kd", [128, 2, NK], BF16, "ExternalInput")
    vtok = dt("vtok", [NK, 640], BF16, "ExternalInput")
    tab33_d = dt("tab33", [33, 8], F32, "ExternalInput")
    oh_swa = dt("oh_swa", [33, LS], F32, "ExternalInput")
    cnt_swa_d = dt("cnt_swa", [32, NAFF * 128], F32, "ExternalInput")
    sinks_d = dt("sinks", [1, 8], F32, "ExternalInput")
    wm_d = dt("wm", [128, S_, 2, 128], BF16, "ExternalInput")
    ident = dt("ident", [128, 128], F32, "ExternalInput")
    o_out = dt("o_attn", [NTOK, 1024], BF16, "ExternalOutput")
    cswa_scr = dt("cswa_scr", [8, LS], BF16, "Internal")

    P = Prog(nc)
    C = Consts(P, nc, ident[:, :])
    R = AttnRes(P, nS=3, nP=4)
    ps_misc = P.psum("ps_misc", [128, 512], F32); b_ps_misc = Buf()
    Ops = [(P.psum(f"O{i}", [128, 512], F32), Buf()) for i in range(2)]
    tab33 = P.sbuf("tab33", [33, 8], F32); b_tab33 = Buf()
    P.dma("sp", lambda e: e.dma_start(out=tab33[:], in_=tab33_d[:, :]), writes=[b_tab33])
    b_scr = build_ctab(P, nc, C, tab33, b_tab33, oh_swa, LS, "cswa", cswa_scr, ps_misc, b_ps_misc)
    exptab = P.sbuf("exptab", [32, 8], F32); b_exptab = Buf()
    P.op("act", lambda e: e.activation(out=exptab[:], in_=tab33[0:32, :], func=AF.Exp), reads=[b_tab33], writes=[b_exptab])
    cnts = P.sbuf("cnts", [32, NAFF * 128], F32); b_cnts = Buf()
    P.dma("sp", lambda e: e.dma_start(out=cnts[:], in_=cnt_swa_d[:, :]), writes=[b_cnts])
    zpad = P.sbuf("zpad", [128, NAFF, 8], F32); b_zpad = Buf()
    for a in range(NAFF):
        P.op("pe", lambda e, a=a: e.matmul(out=ps_misc[:, 0:8], lhsT=cnts[:, a * 128:(a + 1) * 128], rhs=exptab[:, :],
                                           start=True, stop=True), reads=[b_cnts, b_exptab], writes=[b_ps_misc])
        P.op("act", lambda e, a=a: e.copy(out=zpad[:, a, :], in_=ps_misc[:, 0:8]), reads=[b_ps_misc], writes=[b_zpad])
    esink = P.sbuf("esink", [128, 8], F32); b_esink = Buf()
    P.dma("sp", lambda e: e.dma_start(out=esink[:], in_=sinks_d[0:1, :].to_broadcast([128, 8])), writes=[b_esink])
    P.op("act", lambda e: e.activation(out=esink[:], in_=esink[:], func=AF.Exp), reads=[b_esink], writes=[b_esink])
    wm = P.sbuf("wm", [128, S_, 2, 128], BF16); b_wm = Buf()
    P.dma("sp", lambda e: e.dma_start(out=wm[:], in_=wm_d[:, :, :, :]), writes=[b_wm])
    Wswa = P.sbuf("Wswa", [128, S_ + 1, 4, 128], BF16); b_Wswa = Buf()
    QT = P.sbuf("QT", [128, 8, NTOK], BF16); b_QT = Buf()
    KV = P.sbuf("KV", [128, 33280], BF16); b_KV = Buf()
    KrT = P.sbuf("KrT", [64, NK], BF16); b_KrT = Buf()
    o_sb = P.sbuf("o_sb", [128, NT, 1024], BF16); b_osb = Buf()
    rz = P.sbuf("rz", [128, 4, 1], F32); b_rz = Buf()
    P.dma("sp", lambda e: e.dma_start(out=QT[:, 0:4, :], in_=qn_d[:, :, :]), writes=[b_QT])
    P.dma("sp", lambda e: e.dma_start(out=QT[0:64, 4:8, :], in_=qr_d[:, :, :]), writes=[b_QT])
    P.dma("sp", lambda e: e.dma_start(out=KrT[:], in_=kr_d[:, :]), writes=[b_KrT])
    KnT = KV[:, 0:2 * NK].rearrange("p (c k) -> p c k", c=2)
    Vm = KV[:, 2 * NK:2 * NK + NKT * 2 * 129].rearrange("p (k h d) -> p k h d", h=2, d=129)
    sc_mla = float(192 ** -0.5)
    for pp in range(2):
        for hh in range(2):
            P.dma("sp", lambda e, hh=hh, pp=pp: e.dma_start(out=KnT[:, hh, :], in_=kn_d[:, 2 * pp + hh, :]), writes=[b_KV])
        P.op("pool", lambda e: e.memset(Vm[:, :, :, 128:129], 1.0), writes=[b_KV])
        for hh in range(2):
            for k0 in range(0, NKT, 8):
                P.dma("sp", lambda e, hh=hh, pp=pp, k0=k0: e.dma_start(
                    out=Vm[:, k0:k0 + 8, hh, 0:128],
                    in_=vtok[k0 * 128:(k0 + 8) * 128, (2 * pp + hh) * 128:(2 * pp + hh + 1) * 128].rearrange(
                        "(kt p) d -> p kt d", p=128)), writes=[b_KV])
        for lt in range(NT):
            O, b_O = Ops[lt % 2]
            Ov = O[:].rearrange("p (h d) -> p h d", d=256)
            kts = list(range(0, min(S_ * lt + S_, NKT)))

            def qk_fn(kt, lt=lt, pp=pp):
                return [(hh * 128, (hh + 1) * 128,
                         [(KnT[:, hh, kt * 128:(kt + 1) * 128], QT[:, 2 * pp + hh, lt * 128:(lt + 1) * 128]),
                          (KrT[:, kt * 128:(kt + 1) * 128], QT[0:64, 4 + 2 * pp + hh, lt * 128:(lt + 1) * 128])],
                         [b_KV, b_QT, b_KrT]) for hh in range(2)]

            def extra_fn(kt, lt=lt):
                dl = S_ * lt - kt
                if dl <= 0:
                    return [(C.antib[:], wm[:, dl + S_ - 1, :, :].rearrange("p h q -> p (h q)"), [C.b_antib, b_wm])]
                return []
            attn_steps(P, R, kts, qk_fn, extra_fn, lambda kt, hh: (Vm[:, kt, hh, :], [b_KV]), Ov, b_O, sc_mla,
                       nv=129, nh=2)
            P.op("dve", lambda e, Ov=Ov: e.tensor_scalar(out=rz[:, 0:2, :], in0=Ov[:, :, 128:129], scalar1=1e-30, scalar2=None,
                                                         op0=ALU.max), reads=[b_O], writes=[b_rz])
            P.op("dve", lambda e: e.reciprocal(out=rz[:, 0:2, :], in_=rz[:, 0:2, :]), reads=[b_rz], writes=[b_rz])
            P.op("dve", lambda e, Ov=Ov, lt=lt, pp=pp: e.tensor_tensor(
                out=o_sb[:, lt, pp * 256:(pp + 1) * 256].rearrange("p (h d) -> p h d", d=128), in0=Ov[:, :, 0:128],
                in1=rz[:, 0:2, :].to_broadcast([128, 2, 128]), op=ALU.mult), reads=[b_O, b_rz], writes=[b_osb])
    P.dma("sp", lambda e: e.dma_start(out=QT[:], in_=qd_d[:, :, :]), writes=[b_QT])
    KdT = KV[:, 0:NK]
    Vd = KV[:, NK:NK + NKT * 65].rearrange("p (k d) -> p k d", d=65)
    for kv in range(2):
        P.dma("sp", lambda e, kv=kv: e.dma_start(out=KdT, in_=kd_d[:, kv, :]), writes=[b_KV])
        P.op("pool", lambda e: e.memset(Vd[:, :, 64:65], 1.0), writes=[b_KV])
        for k0 in range(0, NKT, 8):
            P.dma("sp", lambda e, kv=kv, k0=k0: e.dma_start(
                out=Vd[:, k0:k0 + 8, 0:64], in_=vtok[k0 * 128:(k0 + 8) * 128, 512 + kv * 64:512 + (kv + 1) * 64].rearrange(
                    "(kt p) d -> p kt d", p=128)), writes=[b_KV])
        toeplitz_load(P, Wswa, b_Wswa, cswa_scr, b_scr, LS, 4 * kv, S_ + 1)
        for lt in range(NT):
            O, b_O = Ops[lt % 2]
            Ov = O[:].rearrange("p (h d) -> p h d", d=128)
            kts = list(range(max(0, S_ * lt - 1), min(S_ * lt + S_, NKT)))

            def qk_fn(kt, lt=lt, kv=kv):
                return [(hh * 128, (hh + 1) * 128,
                         [(KdT[:, kt * 128:(kt + 1) * 128], QT[:, 4 * kv + hh, lt * 128:(lt + 1) * 128])],
                         [b_KV, b_QT]) for hh in range(4)]

            def extra_fn(kt, lt=lt):
                dl = S_ * lt - kt
                return [(C.antib[:], Wswa[:, dl + S_ - 1, :, :].rearrange("p h q -> p (h q)"), [C.b_antib, b_Wswa])]
            attn_steps(P, R, kts, qk_fn, extra_fn, lambda kt, hh: (Vd[:, kt, :], [b_KV]), Ov, b_O, 0.125)
            P.op("dve", lambda e, Ov=Ov, kv=kv: e.tensor_tensor(out=rz[:], in0=Ov[:, :, 64:65],
                                                                in1=esink[:, 4 * kv:4 * kv + 4].unsqueeze(2), op=ALU.add),
                 reads=[b_O, b_esink], writes=[b_rz])
            if lt < NAFF:
                P.op("dve", lambda e, lt=lt, kv=kv: e.tensor_tensor(out=rz[:], in0=rz[:],
                                                                    in1=zpad[:, lt, 4 * kv:4 * kv + 4].unsqueeze(2), op=ALU.add),
                     reads=[b_rz, b_zpad], writes=[b_rz])
            P.op("dve", lambda e: e.reciprocal(out=rz[:], in_=rz[:]), reads=[b_rz], writes=[b_rz])
            P.op("dve", lambda e, Ov=Ov, lt=lt, kv=kv: e.tensor_tensor(
                out=o_sb[:, lt, 512 + kv * 256:512 + (kv + 1) * 256].rearrange("p (h d) -> p h d", d=64),
                in0=Ov[:, :, 0:64], in1=rz[:].to_broadcast([128, 4, 64]), op=ALU.mult),
                reads=[b_O, b_rz], writes=[b_osb])
    outs = []
    for t in range(NT):
        outs.append(P.dma("sp", lambda e, t=t: e.dma_start(out=o_out[t * 128:(t + 1) * 128, :], in_=o_sb[:, t, :]),
                          reads=[b_osb]))
    P.wait_all("sp", outs)
    P.finish()
    return nc


from concourse.bass_utils import run_bass_kernel_spmd

_NT = 16
_STRIDE = 4
_PROGS = {}


def _prog(name, fn):
    if name not in _PROGS:
        _PROGS[name] = fn()
    return _PROGS[name]


def _own(a, j):
    sh = a.shape
    return np.ascontiguousarray(a.reshape((16, 4, 128) + sh[1:])[:, j].reshape((2048,) + sh[1:]))


def _gather_last(parts, blk=128):
    sh = parts[0].shape
    out = np.zeros(sh[:-1] + (64 * blk,), parts[0].dtype)
    o5 = out.reshape(sh[:-1] + (16, 4, blk))
    for r in range(4):
        o5[..., r, :] = parts[r].reshape(sh[:-1] + (16, blk))
    return out


def _gather_rows(parts):
    sh = parts[0].shape
    out = np.zeros((8192,) + sh[1:], parts[0].dtype)
    o5 = out.reshape((16, 4, 128) + sh[1:])
    for r in range(4):
        o5[:, r] = parts[r].reshape((16, 128) + sh[1:])
    return out


def _run(nc, ins):
    res = run_bass_kernel_spmd(nc, ins, core_ids=list(range(8)))
    return res.results


def kernel(x, c, rel_table, router_w, router_b, final_norm, norm_mix, norm_ffn, ada_w, ada_b,
           moe_w_gate, moe_w_up, moe_w_down, ev_w_in, ev_w_out, nsa_pos_k, nsa_pos_v,
           nsa_ck_w1, nsa_ck_w2, nsa_cv_w1, nsa_cv_w2, od_w_in, od_w_out, mla_q_norm,
           mla_kv_norm, mla_w_q_up, mla_w_kv_up, swa_sinks):
    f32 = lambda a: np.ascontiguousarray(np.asarray(a, dtype=np.float32))
    x = f32(x); c = f32(c)
    ident = np.eye(128, dtype=np.float32)
    tab33 = np.concatenate([f32(rel_table), np.ones((1, 8), np.float32)], 0)
    NT, ST = _NT, _STRIDE
    cores = [(cc // 4, cc % 4) for cc in range(8)]
    c_cols = [np.ascontiguousarray(c[b].reshape(8, 128).T) for b in range(2)]

    WF, WP, WT = host_w_in_even(f32(ev_w_in[0]))
    ins = [dict(x=_own(x[b], j), c_cols=c_cols[b], ada_w=f32(ada_w[0]), ada_b=f32(ada_b[0])[None],
                g_mix=f32(norm_mix[0])[None], wf=WF, wp=WP, wt=WT, ident=ident) for (b, j) in cores]
    r1 = _run(_prog("L1", lambda: build_L1(NT)), ins)
    posc = np.ascontiguousarray(np.stack([f32(nsa_pos_k[0]).reshape(16, 128).T, f32(nsa_pos_v[0]).reshape(16, 128).T], 1))
    cw1 = np.ascontiguousarray(np.stack([f32(nsa_ck_w1[0]), f32(nsa_cv_w1[0])], 0))
    cw2k = np.ascontiguousarray(np.concatenate([f32(nsa_ck_w2[0]), f32(nsa_ck_w2[0])], 1))
    G = {}
    for b in range(2):
        fm = [np.asarray(r1[4 * b + r]['o_fm']) for r in range(4)]
        G[b] = dict(kbT=_gather_last([f[:, 16:20] for f in fm]), ksT=_gather_last([f[:, 20:22] for f in fm]),
                    kwT=_gather_last([f[:, 22:24] for f in fm]),
                    kc2=_gather_last([np.asarray(r1[4 * b + r]['o_kc2']) for r in range(4)], blk=64),
                    vtok=_gather_rows([np.asarray(r1[4 * b + r]['o_vtok']) for r in range(4)]))
    ins = []
    for cc, (b, j) in enumerate(cores):
        st = attn0_static(NT, ST, j)
        ins.append(dict(qT=np.ascontiguousarray(np.asarray(r1[cc]['o_fm'])[:, 0:16]), gates=np.asarray(r1[cc]['o_gates']),
                        tab33=tab33, posc=posc, cw1=cw1, cw2k=cw2k, cw2v=f32(nsa_cv_w2[0]), ident=ident, **G[b], **st))
    ra = _run(_prog("A0", lambda: build_attn0(NT, ST, 64)), ins)
    oh16 = oh16_static()
    ins = [dict(o_attn=np.asarray(ra[cc]['o_attn']), x=_own(x[b], j), mod=np.asarray(r1[cc]['o_mod']),
                w_out=f32(ev_w_out[0]), g_ffn=f32(norm_ffn[0])[None], g_fin=f32(final_norm)[None],
                router_w=f32(router_w), router_b=f32(router_b)[None], wg=f32(moe_w_gate[0]), wu=f32(moe_w_up[0]),
                wd=f32(moe_w_down[0]), oh16=oh16, ident=ident) for cc, (b, j) in enumerate(cores)]
    rp0 = _run(_prog("P0", lambda: build_post(NT, final=False)), ins)
    hw = host_w_in_odd(f32(od_w_in[0]), f32(mla_w_q_up[0]), f32(mla_w_kv_up[0]))
    ins = []
    for cc, (b, j) in enumerate(cores):
        pos = (np.arange(64).reshape(16, 4)[:, j][:, None] * 128 + np.arange(128)[None, :]).reshape(-1)
        cos, sin = rope_static(pos)
        ins.append(dict(x=np.asarray(rp0[cc]['x_out']), c_cols=c_cols[b], ada_w=f32(ada_w[1]), ada_b=f32(ada_b[1])[None],
                        g_mix=f32(norm_mix[1])[None], q_norm=f32(mla_q_norm), kv_norm=f32(mla_kv_norm), cos=cos, sin=sin,
                        ident=ident, **hw))
    r2 = _run(_prog("L1o", lambda: build_L1odd(NT)), ins)
    G = {}
    for b in range(2):
        G[b] = dict(kn=_gather_last([np.asarray(r2[4 * b + r]['o_kn']) for r in range(4)]),
                    kr=_gather_last([np.asarray(r2[4 * b + r]['o_kr']) for r in range(4)]),
                    kd=_gather_last([np.asarray(r2[4 * b + r]['o_fm'])[:, 8:10] for r in range(4)]),
                    vtok=_gather_rows([np.asarray(r2[4 * b + r]['o_vtok']) for r in range(4)]))
    ins = []
    for cc, (b, j) in enumerate(cores):
        st = attn1_static(NT, ST, j)
        ins.append(dict(qn=np.asarray(r2[cc]['o_qn']), qr=np.asarray(r2[cc]['o_qr']),
                        qd=np.ascontiguousarray(np.asarray(r2[cc]['o_fm'])[:, 0:8]), tab33=tab33, sinks=f32(swa_sinks),
                        ident=ident, **G[b], **st))
    rb = _run(_prog("A1", lambda: build_attn1(NT, ST, 64)), ins)
    ins = [dict(o_attn=np.asarray(rb[cc]['o_attn']), x=np.asarray(rp0[cc]['x_out']), mod=np.asarray(r2[cc]['o_mod']),
                w_out=f32(od_w_out[0]), g_ffn=f32(norm_ffn[1])[None], g_fin=f32(final_norm)[None],
                router_w=f32(router_w), router_b=f32(router_b)[None], wg=f32(moe_w_gate[1]), wu=f32(moe_w_up[1]),
                wd=f32(moe_w_down[1]), oh16=oh16, ident=ident) for cc, (b, j) in enumerate(cores)]
    rp1 = _run(_prog("P1", lambda: build_post(NT, final=True)), ins)
    out = np.zeros((2, 8192, 1024), np.float32)
    o6 = out.reshape(2, 16, 4, 128, 1024)
    for cc, (b, j) in enumerate(cores):
        o6[b, :, j] = np.asarray(rp1[cc]['x_out']).reshape(16, 128, 1024)
    return out
```

```python
import numpy as np
import concourse.bass as bass
import concourse.mybir as mybir

F32 = mybir.dt.float32
BF16 = mybir.dt.bfloat16
I32 = mybir.dt.int32
U32 = mybir.dt.uint32
AF = mybir.ActivationFunctionType
ALU = mybir.AluOpType
AX = mybir.AxisListType


class Buf:
    __slots__ = ("name", "w", "r")

    def __init__(self, name=""):
        self.name = name
        self.w = None
        self.r = {}


class Prog:
    COMPUTE = ("pe", "act", "dve", "pool")
    DMAQ = ("sp", "pool")

    def __init__(self, nc, n_dma_sems=24, same_engine_sync=True):
        import os
        same_engine_sync = bool(int(os.environ.get('SES', '1' if same_engine_sync else '0')))
        self.nc = nc
        self.q = {e: [] for e in ("pe", "act", "dve", "pool", "sp")}
        self.eng_obj = {"pe": nc.tensor, "act": nc.scalar, "dve": nc.vector,
                        "pool": nc.gpsimd, "sp": nc.sync}
        self.sems = {}
        self.cnt = {}
        self.seen = {e: {} for e in self.q}
        self.same_engine_sync = same_engine_sync
        self._ctx = []
        for e in self.COMPUTE:
            self.sems[e] = self._sem("s_" + e)
            self.cnt[e] = 0
        self.dma_pool = {}
        for e in ("sp", "pool"):
            self.dma_pool[e] = [[self._sem(f"d_{e}{i}"), 0, None] for i in range(n_dma_sems)]
        self.dma_rr = {"sp": 0, "pool": 0}
        self.pending_noinc = {e: False for e in self.COMPUTE}

    def _sem(self, name):
        g = self.nc.semaphore(name)
        s = g.__enter__()
        self._ctx.append(g)
        return s

    def sbuf(self, name, shape, dt):
        g = self.nc.sbuf_tensor("sb_" + name, list(shape), dt)
        t = g.__enter__()
        self._ctx.append(g)
        return t

    def psum(self, name, shape, dt):
        g = self.nc.psum_tensor("ps_" + name, list(shape), dt)
        t = g.__enter__()
        self._ctx.append(g)
        return t

    def _collect(self, eng, reads, writes):
        deps = {}

        def add(tok):
            if tok is None:
                return
            s, v, owner = tok
            k = id(s)
            if k not in deps or deps[k][1] < v:
                deps[k] = (s, v, owner)

        for b in reads:
            add(b.w)
        for b in writes:
            add(b.w)
            for t in b.r.values():
                add(t)
        waits = []
        for k, (s, v, owner) in deps.items():
            if owner == eng and owner in self.COMPUTE:
                if eng == "pe" or not self.same_engine_sync:
                    continue
                if v > self.cnt[eng]:
                    continue
            if self.seen[eng].get(k, -1) >= v:
                continue
            self.seen[eng][k] = v
            waits.append((s, v))
        return waits

    def _mark(self, tok, reads, writes):
        k = id(tok[0])
        for b in reads:
            old = b.r.get(k)
            if old is None or old[1] < tok[1]:
                b.r[k] = tok
        for b in writes:
            b.w = tok
            b.r = {}

    def op(self, eng, fn, reads=(), writes=(), inc=True):
        assert eng in self.COMPUTE
        waits = self._collect(eng, reads, writes)
        if inc:
            self.cnt[eng] += 1
            tok = (self.sems[eng], self.cnt[eng], eng)
            self.pending_noinc[eng] = False
        else:
            tok = (self.sems[eng], self.cnt[eng] + 1, eng)
            self.pending_noinc[eng] = True
        self.q[eng].append((waits, fn, (self.sems[eng], 1) if inc else None))
        self._mark(tok, reads, writes)
        return tok

    def dma(self, eng, fn, reads=(), writes=()):
        pool = self.dma_pool[eng]
        i = self.dma_rr[eng]
        self.dma_rr[eng] = (i + 1) % len(pool)
        ent = pool[i]
        waits = self._collect(eng, reads, writes)
        if ent[2] is not None:
            s, v, _ = ent[2]
            k = id(s)
            if self.seen[eng].get(k, -1) < v:
                self.seen[eng][k] = v
                waits.append((s, v))
        ent[1] += 16
        tok = (ent[0], ent[1], "dma_" + eng)
        ent[2] = tok
        self.q[eng].append((waits, fn, (ent[0], 16)))
        self._mark(tok, reads, writes)
        return tok

    def wait_all(self, eng, toks):
        waits = []
        for tok in toks:
            s, v, _ = tok
            waits.append((s, v))
        self.q[eng].append((waits, None, None))

    def finish(self):
        nc = self.nc
        for e in self.COMPUTE:
            assert not self.pending_noinc[e], f"engine {e} ends with non-inc instruction"
        with nc.Block() as block:
            def run(engname):
                def body(e):
                    for waits, fn, inc in self.q[engname]:
                        for s, v in waits:
                            e.wait_ge(s, v)
                        if fn is not None:
                            ins = fn(e)
                            if inc is not None:
                                ins.then_inc(inc[0], inc[1])
                return body
            if self.q["sp"]:
                block.sync(run("sp"))
            if self.q["pe"]:
                block.tensor(run("pe"))
            if self.q["act"]:
                block.scalar(run("act"))
            if self.q["dve"]:
                block.vector(run("dve"))
            if self.q["pool"]:
                block.gpsimd(run("pool"))
        for g in reversed(self._ctx):
            g.__exit__(None, None, None)
        self._ctx = []


import numpy as np
import ml_dtypes

NPBF = ml_dtypes.bfloat16
D = 1024
S = 8192
NEGM = -30000.0


class RR:
    def __init__(self, engs=("act", "dve")):
        self.engs = engs
        self.i = 0

    def next(self):
        e = self.engs[self.i % len(self.engs)]
        self.i += 1
        return e


def evac(P, eng, out, in_, reads, writes):
    if eng == "act":
        return P.op("act", lambda e: e.copy(out=out, in_=in_), reads=reads, writes=writes)
    return P.op(eng, lambda e: e.tensor_copy(out=out, in_=in_), reads=reads, writes=writes)


class Consts:
    def __init__(self, P, nc, ident_ap):
        self.idf = P.sbuf("c_idf", [128, 128], F32)
        self.idb = P.sbuf("c_idb", [128, 128], BF16)
        self.b_idf = Buf("idf")
        self.b_idb = Buf("idb")
        P.dma("sp", lambda e: e.dma_start(out=self.idf[:], in_=ident_ap), writes=[self.b_idf])
        P.op("dve", lambda e: e.tensor_copy(out=self.idb[:], in_=self.idf[:]),
             reads=[self.b_idf], writes=[self.b_idb])
        self.antib = P.sbuf("c_antib", [128, 128], BF16)
        self.b_antib = Buf("antib")
        P.op("pool", lambda e: e.memset(self.antib[:], 0.0), writes=[self.b_antib])
        P.op("pool", lambda e: e.affine_select(out=self.antib[:], in_=self.antib[:], pattern=[[1, 128]],
                                               compare_op=ALU.not_equal, fill=1.0, base=-127, channel_multiplier=1),
             reads=[self.b_antib], writes=[self.b_antib])
        self.ones_f = P.sbuf("c_ones_f", [128, 128], F32)
        self.b_ones_f = Buf("ones_f")
        P.op("dve", lambda e: e.memset(self.ones_f[:], 1.0), writes=[self.b_ones_f])


def emit_adaln(P, nc, C, c_cols_ap, ada_w_ap, ada_b_ap, tag, psum_row, b_psum_row, psum_bc, b_psum_bc):
    GW = 256
    NG = 6144 // GW
    cc = P.sbuf(f"ada_c{tag}", [128, 8], F32); b_cc = Buf()
    sc = P.sbuf(f"ada_sc{tag}", [128, 8], F32); b_sc = Buf()
    row = [P.sbuf(f"ada_row{tag}{i}", [1, GW], F32) for i in range(2)]; b_row = [Buf(), Buf()]
    mod = P.sbuf(f"ada_mod{tag}", [128, 6, 1024], F32); b_mod = Buf()
    modf = mod[:].rearrange("p a n -> p (a n)")
    wst = [P.sbuf(f"ada_w{tag}_{i}", [128, 8, GW], F32) for i in range(2)]
    b_wst = [Buf(), Buf()]
    P.dma("sp", lambda e: e.dma_start(out=cc[:], in_=c_cols_ap), writes=[b_cc])
    P.dma("sp", lambda e: e.dma_start(out=modf, in_=ada_b_ap.to_broadcast([128, 6144])), writes=[b_mod])
    P.op("act", lambda e: e.activation(out=sc[:], in_=cc[:], func=AF.Silu), reads=[b_cc], writes=[b_sc])
    wv = ada_w_ap.rearrange("(k p) n -> p k n", p=128)
    for g in range(NG):
        w = wst[g % 2]; bw = b_wst[g % 2]
        r = row[g % 2]; br = b_row[g % 2]
        P.dma("sp", lambda e, w=w, g=g: e.dma_start(out=w[:], in_=wv[:, :, g * GW:(g + 1) * GW]), writes=[bw])
        for k in range(8):
            P.op("pe", lambda e, w=w, k=k: e.matmul(out=psum_row[0:1, 0:GW], lhsT=sc[:, k:k + 1], rhs=w[:, k, :],
                                                    start=(k == 0), stop=(k == 7)),
                 reads=[b_sc, bw], writes=[b_psum_row], inc=(k == 7))
        P.op("act", lambda e, r=r: e.copy(out=r[0:1, :], in_=psum_row[0:1, 0:GW]),
             reads=[b_psum_row], writes=[br])
        P.op("pe", lambda e, r=r: e.matmul(out=psum_bc[:, 0:GW], lhsT=C.ones_f[0:1, :], rhs=r[0:1, :],
                                           start=True, stop=True),
             reads=[C.b_ones_f, br], writes=[b_psum_bc])
        P.op("dve", lambda e, g=g: e.tensor_tensor(out=modf[:, g * GW:(g + 1) * GW], in0=psum_bc[:, 0:GW],
                                                   in1=modf[:, g * GW:(g + 1) * GW], op=ALU.add),
             reads=[b_psum_bc, b_mod], writes=[b_mod])
    return mod, b_mod


def emit_norm_tile(P, C, x_t, b_x, A, b_A, Bt, b_B, hT_out, b_hT, scratch, psum_tr, b_psum_tr, tag=""):
    sq, ss, rstd, h32, hb = scratch["sq"], scratch["ss"], scratch["rstd"], scratch["h32"], scratch["hb"]
    b_sq, b_ss, b_rstd, b_h32, b_hb = scratch["b"]
    P.op("act", lambda e: e.activation(out=sq[:], in_=x_t, func=AF.Square, accum_out=ss[:]),
         reads=[b_x], writes=[b_sq, b_ss])
    P.op("dve", lambda e: e.tensor_scalar(out=rstd[:], in0=ss[:], scalar1=1.0 / D, scalar2=1e-6,
                                          op0=ALU.mult, op1=ALU.add), reads=[b_ss], writes=[b_rstd])
    P.op("act", lambda e: e.activation(out=rstd[:], in_=rstd[:], func=AF.Sqrt), reads=[b_rstd], writes=[b_rstd])
    P.op("dve", lambda e: e.reciprocal(out=rstd[:], in_=rstd[:]), reads=[b_rstd], writes=[b_rstd])
    P.op("dve", lambda e: e.scalar_tensor_tensor(out=h32[:], in0=x_t, scalar=rstd[:, 0:1], in1=A,
                                                 op0=ALU.mult, op1=ALU.mult),
         reads=[b_x, b_rstd, b_A], writes=[b_h32])
    P.op("pool", lambda e: e.tensor_tensor(out=hb[:], in0=h32[:], in1=Bt, op=ALU.add),
         reads=[b_h32, b_B], writes=[b_hb])
    for k in range(8):
        P.op("pe", lambda e, k=k: e.transpose(out=psum_tr[:, k, :], in_=hb[:, k * 128:(k + 1) * 128],
                                              identity=C.idb[:]),
             reads=[b_hb, C.b_idb], writes=[b_psum_tr], inc=(k == 7))
    P.op("act", lambda e: e.copy(out=hT_out, in_=psum_tr[:]), reads=[b_psum_tr], writes=[b_hT])


EV = dict(q_a=(0, 512), kc=(512, 640), vc=(640, 768), ks=(768, 896), vs=(896, 1024), kw=(1024, 1152),
          vw=(1152, 1280), gates=(1280, 1304), q_b=(1304, 1816), k_b=(1816, 2328), v_b=(2328, 2840))


def host_w_in_even(w):
    sl = lambda n: w[:, EV[n][0]:EV[n][1]]
    units = []
    for nm in ("q_a", "q_b"):
        for h in range(8):
            u = np.zeros((1024, 128), np.float32)
            u[:, (h % 2) * 64:(h % 2 + 1) * 64] = sl(nm)[:, h * 64:(h + 1) * 64]
            units.append(u)
    for cc in range(4):
        units.append(sl("k_b")[:, cc * 128:(cc + 1) * 128])
    for nm in ("ks", "kw"):
        for kv in range(2):
            c = sl(nm)[:, kv * 64:(kv + 1) * 64]
            units.append(np.concatenate([c, c], axis=1))
    WF = np.concatenate(units, axis=1)
    WP = np.zeros((1024, 4, 2, 128), np.float32)
    for X, (nm, kv) in enumerate([("kc", 0), ("kc", 1), ("vc", 0), ("vc", 1)]):
        cols = sl(nm)[:, kv * 64:(kv + 1) * 64]
        WP[:, X, 0, 0:64] = cols
        WP[:, X, 1, 64:128] = cols
    WT = np.concatenate([sl("vs"), sl("vw"), sl("gates"), sl("v_b")], axis=1)
    return np.ascontiguousarray(WF), np.ascontiguousarray(WP.reshape(1024, 1024)), np.ascontiguousarray(WT)


NU0 = 24
def load_w_bf16(P, nc, name, ap2d, ncols, rows=1024):
    kc = rows // 128
    t = P.sbuf(name, [128, kc, ncols], BF16)
    b = Buf(name)
    v = ap2d.rearrange("(k p) n -> p k n", p=128)
    for k in range(kc):
        P.dma("pool", lambda e, k=k: e.dma_start(out=t[:, k, :], in_=v[:, k, :]), writes=[b])
    return t, b


def norm_scratch(P, tag, hb=None):
    sc = dict(sq=P.sbuf(f"n_sq{tag}", [128, 1024], F32), ss=P.sbuf(f"n_ss{tag}", [128, 1], F32),
              rstd=P.sbuf(f"n_rstd{tag}", [128, 1], F32), h32=P.sbuf(f"n_h32{tag}", [128, 1024], F32),
              hb=hb if hb is not None else P.sbuf(f"n_hb{tag}", [128, 1024], BF16))
    sc["b"] = [Buf() for _ in range(5)]
    return sc


def build_L1(NT=16):
    nc = bass.Bass("TRN2", target_bir_lowering=False)
    NTOK = NT * 128
    dt = lambda n, s, d, k: nc.dram_tensor(n, s, d, kind=k).ap()
    x = dt("x", [NTOK, D], F32, "ExternalInput")
    c_cols = dt("c_cols", [128, 8], F32, "ExternalInput")
    ada_w = dt("ada_w", [D, 6 * D], F32, "ExternalInput")
    ada_b = dt("ada_b", [1, 6 * D], F32, "ExternalInput")
    g_mix = dt("g_mix", [1, D], F32, "ExternalInput")
    wf_d = dt("wf", [D, NU0 * 128], F32, "ExternalInput")
    wp_d = dt("wp", [D, 1024], F32, "ExternalInput")
    wt_d = dt("wt", [D, 792], F32, "ExternalInput")
    ident = dt("ident", [128, 128], F32, "ExternalInput")
    o_fm = dt("o_fm", [128, NU0, NTOK], BF16, "ExternalOutput")
    o_kc2 = dt("o_kc2", [128, 4, NTOK // 2], BF16, "ExternalOutput")
    o_vtok = dt("o_vtok", [NTOK, 768], BF16, "ExternalOutput")
    o_gates = dt("o_gates", [NTOK, 24], F32, "ExternalOutput")
    o_mod = dt("o_mod", [6, D], F32, "ExternalOutput")

    P = Prog(nc)
    C = Consts(P, nc, ident[:, :])
    ps_row = P.psum("ps_row", [1, 512], F32); b_ps_row = Buf()
    ps_bc = P.psum("ps_bc", [128, 512], F32); b_ps_bc = Buf()
    ps_tr = P.psum("ps_tr", [128, 8, 128], BF16); b_ps_tr = Buf()
    ps_mm = [P.psum(f"ps_mm{i}", [128, 512], F32) for i in range(4)]
    b_ps_mm = [Buf() for _ in range(4)]

    mod, b_mod = emit_adaln(P, nc, C, c_cols[:, :], ada_w, ada_b[:, :], "0", ps_row, b_ps_row, ps_bc, b_ps_bc)
    outs = []
    outs.append(P.dma("sp", lambda e: e.dma_start(out=o_mod[:, :], in_=mod[0:1, :, :]), reads=[b_mod]))
    gm = P.sbuf("gm", [128, 1024], F32); b_gm = Buf()
    A = P.sbuf("A_m", [128, 1024], F32); b_A = Buf()
    P.dma("sp", lambda e: e.dma_start(out=gm[:], in_=g_mix[0:1, :].to_broadcast([128, 1024])), writes=[b_gm])
    P.op("dve", lambda e: e.scalar_tensor_tensor(out=A[:], in0=mod[:, 1, :], scalar=1.0, in1=gm[:],
                                                 op0=ALU.add, op1=ALU.mult), reads=[b_mod, b_gm], writes=[b_A])
    Bt = mod[:, 0, :]
    wf, b_wf = load_w_bf16(P, nc, "wf_sb", wf_d, NU0 * 128)
    wp, b_wp = load_w_bf16(P, nc, "wp_sb", wp_d, 1024)
    wt, b_wt = load_w_bf16(P, nc, "wt_sb", wt_d, 792)
    xt = [P.sbuf(f"xt{i}", [128, 1024], F32) for i in range(2)]
    b_xt = [Buf(), Buf()]
    hT = [P.sbuf(f"hT{i}", [128, 8, 512], BF16) for i in range(2)]
    b_hT = [Buf(), Buf()]
    nsc = norm_scratch(P, "a")
    stg = [P.sbuf(f"stg{i}", [128, 512], BF16) for i in range(4)]
    b_stg = [Buf() for _ in range(4)]
    gst = [P.sbuf(f"gst{i}", [128, 24], F32) for i in range(2)]
    b_gst = [Buf(), Buf()]
    rr = RR()
    si = 0
    mi = 0
    for tg in range(NT // 4):
        h = hT[tg % 2]; bh = b_hT[tg % 2]
        for tt in range(4):
            t = tg * 4 + tt
            xb = xt[t % 2]; bx = b_xt[t % 2]
            P.dma("sp", lambda e, xb=xb, t=t: e.dma_start(out=xb[:], in_=x[t * 128:(t + 1) * 128, :]), writes=[bx])
            emit_norm_tile(P, C, xb[:], bx, A[:], b_A, Bt, b_mod, h[:, :, tt * 128:(tt + 1) * 128], bh,
                           nsc, ps_tr, b_ps_tr)
        for u in range(NU0):
            ps = ps_mm[mi % 4]; bps = b_ps_mm[mi % 4]; mi += 1
            for k in range(8):
                P.op("pe", lambda e, ps=ps, u=u, k=k, h=h: e.matmul(out=ps[:], lhsT=wf[:, k, u * 128:(u + 1) * 128],
                                                                  rhs=h[:, k, :], start=(k == 0), stop=(k == 7)),
                     reads=[b_wf, bh], writes=[bps], inc=(k == 7))
            st = stg[si % 4]; bst = b_stg[si % 4]; si += 1
            evac(P, rr.next(), st[:], ps[:], [bps], [bst])
            outs.append(P.dma("sp", lambda e, st=st, u=u, tg=tg: e.dma_start(
                out=o_fm[:, u, tg * 512:(tg + 1) * 512], in_=st[:]), reads=[bst]))
        for X in range(4):
            ps = ps_mm[mi % 4]; bps = b_ps_mm[mi % 4]; mi += 1
            n = 0
            for lo in range(2):
                for k in range(8):
                    P.op("pe", lambda e, ps=ps, X=X, lo=lo, k=k, h=h, n=n: e.matmul(
                        out=ps[:, 0:256], lhsT=wp[:, k, (X * 2 + lo) * 128:(X * 2 + lo + 1) * 128],
                        rhs=h[:, k, lo:512:2], start=(n == 0), stop=(n == 15)),
                        reads=[b_wp, bh], writes=[bps], inc=(n == 15))
                    n += 1
            st = stg[si % 4]; bst = b_stg[si % 4]; si += 1
            evac(P, rr.next(), st[:, 0:256], ps[:, 0:256], [bps], [bst])
            outs.append(P.dma("sp", lambda e, st=st, X=X, tg=tg: e.dma_start(
                out=o_kc2[:, X, tg * 256:(tg + 1) * 256], in_=st[:, 0:256]), reads=[bst]))
        for tt in range(4):
            t = tg * 4 + tt
            for grp, (c0, c1) in enumerate([(0, 280), (280, 792)]):
                ps = ps_mm[mi % 4]; bps = b_ps_mm[mi % 4]; mi += 1
                w_ = c1 - c0
                for k in range(8):
                    P.op("pe", lambda e, ps=ps, k=k, h=h, tt=tt, c0=c0, c1=c1, w_=w_: e.matmul(
                        out=ps[:, 0:w_], lhsT=h[:, k, tt * 128:(tt + 1) * 128], rhs=wt[:, k, c0:c1],
                        start=(k == 0), stop=(k == 7)), reads=[b_wt, bh], writes=[bps], inc=(k == 7))
                st = stg[si % 4]; bst = b_stg[si % 4]; si += 1
                if grp == 0:
                    evac(P, rr.next(), st[:, 0:256], ps[:, 0:256], [bps], [bst])
                    outs.append(P.dma("sp", lambda e, st=st, t=t: e.dma_start(
                        out=o_vtok[t * 128:(t + 1) * 128, 0:256], in_=st[:, 0:256]), reads=[bst]))
                    g = gst[t % 2]; bg = b_gst[t % 2]
                    P.op("act", lambda e, g=g, ps=ps: e.activation(out=g[:], in_=ps[:, 256:280], func=AF.Sigmoid),
                         reads=[bps], writes=[bg])
                    outs.append(P.dma("sp", lambda e, g=g, t=t: e.dma_start(
                        out=o_gates[t * 128:(t + 1) * 128, :], in_=g[:]), reads=[bg]))
                else:
                    evac(P, rr.next(), st[:], ps[:], [bps], [bst])
                    outs.append(P.dma("sp", lambda e, st=st, t=t: e.dma_start(
                        out=o_vtok[t * 128:(t + 1) * 128, 256:768], in_=st[:]), reads=[bst]))
    P.wait_all("sp", outs)
    P.finish()
    return nc


def rel_bucket_np(d):
    d = np.maximum(d, 0)
    lp = 16 + (np.log(np.maximum(d, 1).astype(np.float32) / 16) / np.float32(np.log(1024 / 16)) * 16).astype(np.int32)
    return np.where(d < 16, d, np.minimum(lp, 31))


def onehot_table(dvals, mode, win=None):
    L = len(dvals)
    oh = np.zeros((33, L), np.float32)
    ok = dvals >= 0
    if win is not None:
        ok &= dvals < win
    b = rel_bucket_np(dvals)
    idx = np.nonzero(ok)[0]
    oh[b[idx], idx] += 8.0
    if mode == "rel":
        oh[31, idx] -= 8.0
    oh[32, ~ok] = NEGM
    return oh


def build_ctab(P, nc, C, tab33, b_tab33, oh_dram, L, name, scratch_dram, ps, b_ps):
    b_scr = Buf(name + "_scr")
    oh = P.sbuf(name + "_oh", [33, 512], F32); b_oh = Buf()
    cb = P.sbuf(name + "_cb", [8, 512], BF16); b_cb = Buf()
    for c0 in range(0, L, 512):
        w = min(512, L - c0)
        P.dma("sp", lambda e, c0=c0, w=w: e.dma_start(out=oh[:, 0:w], in_=oh_dram[:, c0:c0 + w]), writes=[b_oh])
        P.op("pe", lambda e, w=w: e.matmul(out=ps[0:8, 0:w], lhsT=tab33[:, :], rhs=oh[:, 0:w], start=True, stop=True),
             reads=[b_tab33, b_oh], writes=[b_ps])
        P.op("act", lambda e, w=w: e.copy(out=cb[:, 0:w], in_=ps[0:8, 0:w]), reads=[b_ps], writes=[b_cb])
        P.dma("sp", lambda e, c0=c0, w=w: e.dma_start(out=scratch_dram[:, c0:c0 + w], in_=cb[:, 0:w]),
              reads=[b_cb], writes=[b_scr])
    return b_scr


def toeplitz_load(P, Wt, b_W, scratch_dram, b_scr, L, h0, nR, rstride=128, pstride=1, base=0, nh=4, width=128):
    from concourse.bass_types import AP
    for hh in range(nh):
        src = AP(scratch_dram.tensor, scratch_dram.offset + (h0 + hh) * L + base,
                 [[pstride, 128], [rstride, nR], [1, width]])
        P.dma("sp", lambda e, hh=hh, src=src: e.dma_start(out=Wt[:, :, hh, :], in_=src),
              reads=[b_scr], writes=[b_W])


class AttnRes:
    def __init__(self, P, nS=2, nP=3):
        self.S = [(P.psum(f"at_S{i}", [128, 512], F32), Buf()) for i in range(nS)]
        self.Pt = [(P.sbuf(f"at_P{i}", [128, 512], BF16), Buf()) for i in range(nP)]
        self.si = 0
        self.pi = 0

    def nextS(self):
        r = self.S[self.si % len(self.S)]; self.si += 1
        return r

    def nextP(self):
        r = self.Pt[self.pi % len(self.Pt)]; self.pi += 1
        return r


def attn_steps(P, R, kts, qk_fn, extra_fn, v_fn, O, b_O, scale, nv=65, post_fn=None, bias_ap=None, b_bias=None, nh=4):
    n = len(kts)

    def emit_qk(kt):
        S, b_S = R.nextS()
        ex = extra_fn(kt) if extra_fn else []
        qk = qk_fn(kt)
        for qi, (c0, c1, mms, reads) in enumerate(qk):
            for mi, (lhsT, rhs) in enumerate(mms):
                last = (not ex) and qi == len(qk) - 1 and mi == len(mms) - 1
                P.op("pe", lambda e, S=S, c0=c0, c1=c1, lhsT=lhsT, rhs=rhs, mi=mi, qi=qi, last=last: e.matmul(
                    out=S[:, c0:c1], lhsT=lhsT, rhs=rhs, start=(mi == 0 and qi == 0), stop=last,
                    skip_group_check=True),
                    reads=reads, writes=[b_S], inc=last)
        for xi, (lhsT, rhs, reads) in enumerate(ex):
            P.op("pe", lambda e, S=S, lhsT=lhsT, rhs=rhs, xi=xi, nx=len(ex): e.matmul(
                out=S[:, 0:nh * 128], lhsT=lhsT, rhs=rhs, start=False, stop=(xi == nx - 1), skip_group_check=True),
                reads=reads, writes=[b_S], inc=(xi == len(ex) - 1))
        Pt, b_P = R.nextP()
        if bias_ap is None:
            P.op("act", lambda e, S=S, Pt=Pt: e.activation(out=Pt[:, 0:nh * 128], in_=S[:, 0:nh * 128], func=AF.Exp, scale=scale),
                 reads=[b_S], writes=[b_P])
        else:
            P.op("act", lambda e, S=S, Pt=Pt: e.activation(out=Pt[:, 0:nh * 128], in_=S[:, 0:nh * 128], func=AF.Exp, scale=scale,
                                                           bias=bias_ap), reads=[b_S, b_bias], writes=[b_P])
        return Pt, b_P

    def emit_pv(ii, kt, Pt, b_P):
        for hh in range(nh):
            rhs, reads = v_fn(kt, hh)
            P.op("pe", lambda e, Pt=Pt, hh=hh, rhs=rhs, ii=ii: e.matmul(
                out=O[:, hh, 0:nv], lhsT=Pt[:, hh * 128:(hh + 1) * 128], rhs=rhs, start=(ii == 0 and hh == 0),
                stop=(ii == n - 1 and hh == nh - 1), skip_group_check=True),
                reads=[b_P] + reads, writes=[b_O], inc=(hh == nh - 1 and post_fn is None))
        if post_fn is not None:
            post_fn(kt, ii, n, Pt, b_P)

    pend = None
    for ii, kt in enumerate(kts):
        cur = emit_qk(kt)
        if pend is not None:
            emit_pv(*pend)
        pend = (ii, kt, cur[0], cur[1])
    if pend is not None:
        emit_pv(*pend)


def moba_static(NT, STRIDE, j):
    LC = 11 * 128 + 127
    y = np.arange(LC)
    d = y - 127 - 384 + 128 * j
    oh_rel = onehot_table(d, "rel")
    ohsel = np.zeros((32, 32, 128), np.float32)
    for n in range(32):
        ohsel[n, n, :] = 1.0
    negvalid = np.zeros((NT, 32), np.float32)
    own1h = np.zeros((NT, 32), np.float32)
    for lt in range(NT):
        own = (STRIDE * lt + j) // 2
        negvalid[lt, own:] = -1e30
        own1h[lt, own] = 1.0
    return dict(oh_rel=oh_rel, ohsel=ohsel.reshape(32, 4096).astype(NPBF), negvalid=negvalid.reshape(1, -1),
                own1h=own1h.reshape(1, -1))


def gelu_tanh_ops(P, u, b_u, t, b_t, out_bf, b_out, width):
    P.op("dve", lambda e: e.tensor_tensor(out=t[:, 0:width], in0=u[:, 0:width], in1=u[:, 0:width], op=ALU.mult),
         reads=[b_u], writes=[b_t])
    P.op("dve", lambda e: e.tensor_scalar(out=t[:, 0:width], in0=t[:, 0:width], scalar1=0.044715, scalar2=1.0,
                                          op0=ALU.mult, op1=ALU.add), reads=[b_t], writes=[b_t])
    P.op("dve", lambda e: e.tensor_tensor(out=t[:, 0:width], in0=t[:, 0:width], in1=u[:, 0:width], op=ALU.mult),
         reads=[b_t, b_u], writes=[b_t])
    P.op("act", lambda e: e.activation(out=t[:, 0:width], in_=t[:, 0:width], func=AF.Sigmoid, scale=1.5957691216057308),
         reads=[b_t], writes=[b_t])
    P.op("dve", lambda e: e.tensor_tensor(out=out_bf, in0=t[:, 0:width], in1=u[:, 0:width], op=ALU.mult),
         reads=[b_t, b_u], writes=[b_out])


def attn0_static(NT, STRIDE, j, NKT=64):
    st = moba_static(NT, STRIDE, j)
    LW = 8 * 128 + 127
    y = np.arange(LW)
    st["oh_win"] = onehot_table(y - 127 - 384 + 128 * j, "abs", win=512)
    NRC = -(-2853 // (128 * STRIDE))
    LCM = 128 * STRIDE * (NRC - 1) + 127 + 16 * 127 + 1
    y = np.arange(LCM)
    st["oh_cmp"] = onehot_table(y + 128 * j - 2063, "abs")
    n = np.arange(512)
    cs = n * 16
    ce = cs + 31
    m = np.arange(128) * 64
    ov = ((cs[:, None] <= m[None, :] + 63) & (ce[:, None] >= m[None, :])).astype(np.float32)
    ov[511] = 0
    st["overlap"] = ov.reshape(4, 128, 128).transpose(1, 0, 2).reshape(128, 512).astype(NPBF)
    E = np.zeros((128, NKT * 128), np.float32)
    keys = np.arange(NKT * 128)
    E[keys // 64, keys] = 1.0
    st["E"] = E.astype(NPBF)
    OFF = 2 * STRIDE * (NT - 1)
    width = OFF + 128
    Fw = np.zeros((128, width), np.float32)
    q = np.arange(128)[:, None]
    xx = np.arange(width)[None, :]
    rel = xx - OFF - 2 * j
    hq = (q >= 64).astype(np.int64)
    Fw[np.broadcast_to(rel > hq, Fw.shape)] = -1e30
    Fw[np.broadcast_to((rel == hq) | (rel == hq - 1), Fw.shape)] = 1e30
    st["fwide"] = Fw
    st["cnt_win"] = pad_counts(NT, STRIDE, j, 512)
    return st


def n_aff(STRIDE, win):
    return -(-(win // 128) // STRIDE)


def pad_counts(NT, STRIDE, j, win):
    na = n_aff(STRIDE, win)
    cnt = np.zeros((32, na * 128), np.float32)
    for a in range(na):
        for q in range(128):
            t = 128 * (STRIDE * a + j) + q
            if t + 1 <= win - 1:
                d = np.arange(t + 1, win)
                bb = rel_bucket_np(d)
                cnt[:, a * 128 + q] = np.bincount(bb, minlength=32)
    return cnt


def build_attn0(NT=16, STRIDE=4, NKT=64, do_nsa=True, do_moba=True):
    nc = bass.Bass("TRN2", target_bir_lowering=False)
    NTOK = NT * 128
    NK = NKT * 128
    LC = 11 * 128 + 127
    LW = 8 * 128 + 127
    NRC = -(-2853 // (128 * STRIDE))
    LCM = 128 * STRIDE * (NRC - 1) + 127 + 16 * 127 + 1
    OFFW = 2 * STRIDE * (NT - 1)
    dt = lambda n, s, d, k: nc.dram_tensor(n, s, d, kind=k).ap()
    qT_d = dt("qT", [128, 16, NTOK], BF16, "ExternalInput")
    gates_d = dt("gates", [NTOK, 24], F32, "ExternalInput")
    kbT = dt("kbT", [128, 4, NK], BF16, "ExternalInput")
    ksT = dt("ksT", [128, 2, NK], BF16, "ExternalInput")
    kwT = dt("kwT", [128, 2, NK], BF16, "ExternalInput")
    kc2 = dt("kc2", [128, 4, NK // 2], BF16, "ExternalInput")
    vtok = dt("vtok", [NK, 768], BF16, "ExternalInput")
    tab33_d = dt("tab33", [33, 8], F32, "ExternalInput")
    oh_rel = dt("oh_rel", [33, LC], F32, "ExternalInput")
    oh_win = dt("oh_win", [33, LW], F32, "ExternalInput")
    oh_cmp = dt("oh_cmp", [33, LCM], F32, "ExternalInput")
    ohsel_d = dt("ohsel", [32, 32 * 128], BF16, "ExternalInput")
    negvalid_d = dt("negvalid", [1, NT * 32], F32, "ExternalInput")
    own1h_d = dt("own1h", [1, NT * 32], F32, "ExternalInput")
    overlap_d = dt("overlap", [128, 512], BF16, "ExternalInput")
    E_d = dt("E", [128, NK], BF16, "ExternalInput")
    fwide_d = dt("fwide", [128, OFFW + 128], F32, "ExternalInput")
    NAFF = n_aff(STRIDE, 512)
    cnt_win_d = dt("cnt_win", [32, NAFF * 128], F32, "ExternalInput")
    posc_d = dt("posc", [128, 2, 16], F32, "ExternalInput")
    w1_d = dt("cw1", [2, 2048, 256], F32, "ExternalInput")
    w2k_d = dt("cw2k", [256, 128], F32, "ExternalInput")
    w2v_d = dt("cw2v", [256, 64], F32, "ExternalInput")
    ident = dt("ident", [128, 128], F32, "ExternalInput")
    o_out = dt("o_attn", [NTOK, 1024], BF16, "ExternalOutput")
    crel_scr = dt("crel_scr", [8, LC], BF16, "Internal")
    cwin_scr = dt("cwin_scr", [8, LW], BF16, "Internal")
    ccmp_scr = dt("ccmp_scr", [8, LCM], BF16, "Internal")

    P = Prog(nc)
    C = Consts(P, nc, ident[:, :])
    R = AttnRes(P)
    ps_misc = P.psum("ps_misc", [128, 512], F32); b_ps_misc = Buf()
    ps_trf = P.psum("ps_trb", [128, 8, 128], BF16); b_ps_tr = Buf()
    Ops = [(P.psum(f"O{i}", [128, 4, 128], F32), Buf()) for i in range(3)]
    IMP = P.psum("IMP", [128, 4, 128], F32); b_IMP = Buf()
    tab33 = P.sbuf("tab33", [33, 8], F32); b_tab33 = Buf()
    P.dma("sp", lambda e: e.dma_start(out=tab33[:], in_=tab33_d[:, :]), writes=[b_tab33])
    b_scr_rel = build_ctab(P, nc, C, tab33, b_tab33, oh_rel, LC, "crel", crel_scr, ps_misc, b_ps_misc)
    b31 = P.sbuf("b31", [128, 8], F32); b_b31 = Buf()
    P.dma("sp", lambda e: e.dma_start(out=b31[:], in_=tab33_d[31:32, :].to_broadcast([128, 8])), writes=[b_b31])
    P.op("dve", lambda e: e.tensor_scalar(out=b31[:], in0=b31[:], scalar1=8.0, scalar2=None, op0=ALU.mult),
         reads=[b_b31], writes=[b_b31])
    QT = P.sbuf("QT", [128, 8, NTOK], BF16); b_QT = Buf()
    KV = P.sbuf("KV", [128, 33280], BF16); b_KV = Buf()
    o_sb = P.sbuf("o_sb", [128, NT, 1024], BF16); b_osb = Buf()
    W = P.sbuf("W", [128, 11, 4, 128], BF16); b_W = Buf()
    rz = P.sbuf("rz", [128, 4, 1], F32); b_rz = Buf()
    outs = []
    Esb = P.sbuf("Esb", [128, NK], BF16); b_E = Buf()
    u32 = P.sbuf("u32", [128, 512], F32); b_u32 = Buf()
    t32 = P.sbuf("t32", [128, 512], F32); b_t32 = Buf()
    Wwin = P.sbuf("Wwin", [128, 8, 4, 128], BF16); b_Wwin = Buf()
    Wc = P.sbuf("Wc", [128, NRC, 4, 128], BF16); b_Wc = Buf()

    if do_nsa:
        b_scr_win = build_ctab(P, nc, C, tab33, b_tab33, oh_win, LW, "cwin", cwin_scr, ps_misc, b_ps_misc)
        b_scr_cmp = build_ctab(P, nc, C, tab33, b_tab33, oh_cmp, LCM, "ccmp", ccmp_scr, ps_misc, b_ps_misc)
        P.dma("sp", lambda e: e.dma_start(out=Esb[:], in_=E_d[:, :]), writes=[b_E])
        ovl = P.sbuf("ovl", [128, 4, 128], BF16); b_ovl = Buf()
        P.dma("sp", lambda e: e.dma_start(out=ovl[:].rearrange("p a m -> p (a m)"), in_=overlap_d[:, :]), writes=[b_ovl])
        fw = P.sbuf("fw", [128, OFFW + 128], F32); b_fw = Buf()
        P.dma("sp", lambda e: e.dma_start(out=fw[:], in_=fwide_d[:, :]), writes=[b_fw])
        exptab = P.sbuf("exptab", [32, 8], F32); b_exptab = Buf()
        P.op("act", lambda e: e.activation(out=exptab[:], in_=tab33[0:32, :], func=AF.Exp), reads=[b_tab33], writes=[b_exptab])
        cntw = P.sbuf("cntw", [32, NAFF * 128], F32); b_cntw = Buf()
        P.dma("sp", lambda e: e.dma_start(out=cntw[:], in_=cnt_win_d[:, :]), writes=[b_cntw])
        zpad = P.sbuf("zpad", [128, NAFF, 8], F32); b_zpad = Buf()
        for a in range(NAFF):
            P.op("pe", lambda e, a=a: e.matmul(out=ps_misc[:, 0:8], lhsT=cntw[:, a * 128:(a + 1) * 128], rhs=exptab[:, :],
                                               start=True, stop=True), reads=[b_cntw, b_exptab], writes=[b_ps_misc])
            P.op("act", lambda e, a=a: e.copy(out=zpad[:, a, :], in_=ps_misc[:, 0:8]), reads=[b_ps_misc], writes=[b_zpad])
        gts = P.sbuf("gts", [128, NT, 24], F32); b_gts = Buf()
        P.dma("sp", lambda e: e.dma_start(out=gts[:], in_=gates_d.rearrange("(t p) n -> p t n", p=128)), writes=[b_gts])
        P.dma("sp", lambda e: e.dma_start(out=QT[:], in_=qT_d[:, 0:8, :]), writes=[b_QT])
        posc = P.sbuf("posc", [128, 2, 16], F32); b_posc = Buf()
        poscb = P.sbuf("poscb", [128, 2, 16], BF16); b_poscb = Buf()
        P.dma("sp", lambda e: e.dma_start(out=posc[:], in_=posc_d[:, :, :]), writes=[b_posc])
        P.op("dve", lambda e: e.tensor_copy(out=poscb[:], in_=posc[:]), reads=[b_posc], writes=[b_poscb])
        w2k = P.sbuf("w2k", [128, 2, 128], BF16); b_w2k = Buf()
        w2v = P.sbuf("w2v", [128, 2, 64], BF16); b_w2v = Buf()
        P.dma("pool", lambda e: e.dma_start(out=w2k[:], in_=w2k_d.rearrange("(k p) n -> p k n", p=128)), writes=[b_w2k])
        P.dma("pool", lambda e: e.dma_start(out=w2v[:], in_=w2v_d.rearrange("(k p) n -> p k n", p=128)), writes=[b_w2v])
        w1 = KV[:, 8192:8192 + 4096].rearrange("p (k n) -> p k n", n=256); b_w1 = Buf()
        kc2sb = KV[:, 0:NK // 2]; b_kc2 = Buf()
        bias1 = P.sbuf("bias1", [128, 2], F32); b_bias1 = Buf()
        GT = P.sbuf("GT", [128, 2, 512], BF16); b_GT = Buf()
        P.op("pool", lambda e: e.memset(GT[:], 0.0), writes=[b_GT])
        KcT = P.sbuf("KcT", [128, 2, 512], BF16); b_KcT = Buf()
        Vc = P.sbuf("Vc", [128, 2, 4, 65], BF16); b_Vc = Buf()
        P.op("pool", lambda e: e.memset(KcT[:], 0.0), writes=[b_KcT])
        P.op("pool", lambda e: e.memset(Vc[:], 1.0), writes=[b_Vc])
        ncmp = NK // 16 - 1
        for kvt in range(2):
            for k in range(16):
                P.dma("pool", lambda e, k=k, kvt=kvt: e.dma_start(out=w1[:, k, :], in_=w1_d[kvt, k * 128:(k + 1) * 128, :]),
                      writes=[b_w1])
            for hc in range(2):
                for k in range(16):
                    P.op("pe", lambda e, hc=hc, k=k, kvt=kvt: e.matmul(
                        out=ps_misc[:, 0:1], lhsT=w1[:, k, hc * 128:(hc + 1) * 128], rhs=poscb[:, kvt, k:k + 1],
                        start=(k == 0), stop=(k == 15)), reads=[b_w1, b_poscb], writes=[b_ps_misc], inc=(k == 15))
                P.op("act", lambda e, hc=hc: e.copy(out=bias1[:, hc:hc + 1], in_=ps_misc[:, 0:1]),
                     reads=[b_ps_misc], writes=[b_bias1])
            for kv in range(2):
                X = kvt * 2 + kv
                P.dma("sp", lambda e, X=X: e.dma_start(out=kc2sb, in_=kc2[:, X, :]), writes=[b_kc2])
                for hc in range(2):
                    for k in range(16):
                        P.op("pe", lambda e, hc=hc, k=k: e.matmul(
                            out=ps_misc[:, 0:ncmp], lhsT=w1[:, k, hc * 128:(hc + 1) * 128],
                            rhs=kc2sb[:, k:k + 8 * (ncmp - 1) + 1:8], start=(k == 0), stop=(k == 15)),
                            reads=[b_w1, b_kc2], writes=[b_ps_misc], inc=(k == 15))
                    P.op("act", lambda e, hc=hc: e.activation(out=u32[:, 0:ncmp], in_=ps_misc[:, 0:ncmp], func=AF.Identity,
                                                              bias=bias1[:, hc:hc + 1]),
                         reads=[b_ps_misc, b_bias1], writes=[b_u32])
                    gelu_tanh_ops(P, u32, b_u32, t32, b_t32, GT[:, hc, 0:ncmp], b_GT, ncmp)
                if kvt == 0:
                    for hc in range(2):
                        P.op("pe", lambda e, hc=hc: e.matmul(out=ps_misc[:, 0:512], lhsT=w2k[:, hc, :], rhs=GT[:, hc, :],
                                                             start=(hc == 0), stop=(hc == 1)),
                             reads=[b_w2k, b_GT], writes=[b_ps_misc], inc=(hc == 1))
                    P.op("act", lambda e, kv=kv: e.copy(out=KcT[:, kv, :], in_=ps_misc[:, 0:512]),
                         reads=[b_ps_misc], writes=[b_KcT])
                else:
                    for nc_ in range(4):
                        for hc in range(2):
                            P.op("pe", lambda e, hc=hc, nc_=nc_: e.matmul(
                                out=ps_misc[:, nc_ * 64:(nc_ + 1) * 64], lhsT=GT[:, hc, nc_ * 128:(nc_ + 1) * 128],
                                rhs=w2v[:, hc, :], start=(hc == 0 and nc_ == 0), stop=(hc == 1 and nc_ == 3),
                                skip_group_check=True),
                                reads=[b_w2v, b_GT], writes=[b_ps_misc], inc=(hc == 1 and nc_ == 3))
                    P.op("act", lambda e, kv=kv: e.copy(out=Vc[:, kv, :, 0:64],
                                                        in_=ps_misc[:, 0:256].rearrange("p (a d) -> p a d", d=64)),
                         reads=[b_ps_misc], writes=[b_Vc])
        KsT = KV[:, 0:NK]
        KwT = KV[:, NK:2 * NK]
        Vs = KV[:, 2 * NK:2 * NK + NKT * 65].rearrange("p (k d) -> p k d", d=65)
        Vw = KV[:, 2 * NK + NKT * 65:2 * NK + 2 * NKT * 65].rearrange("p (k d) -> p k d", d=65)
        imp = P.sbuf("imp", [128, 128], F32); b_imp = Buf()
        sc2 = P.sbuf("sc2", [128, 128], F32); b_sc2 = Buf()
        m8 = P.sbuf("m8", [128, 2, 8], F32); b_m8 = Buf()
        negm4 = P.sbuf("negm4", [128, 4, 128], BF16); b_negm4 = Buf()
        nT4 = [(P.sbuf(f"nT4_{i}", [128, 4, 128], BF16), Buf()) for i in range(2)]
        b31row = P.sbuf("b31row", [1, 2, 4, 128], BF16); b_b31row = Buf()
        for kv in range(2):
            P.op("dve", lambda e, kv=kv: e.tensor_copy(
                out=b31row[0:1, kv, :, :], in_=b31[0:1, 4 * kv:4 * kv + 4].unsqueeze(2).to_broadcast([1, 4, 128])),
                reads=[b_b31], writes=[b_b31row])
        ones_b = P.sbuf("ones_b", [1, 128], BF16); b_ones_b = Buf()
        P.op("dve", lambda e: e.memset(ones_b[:], 1.0), writes=[b_ones_b])
        rzg = P.sbuf("rzg", [128, 4, 1], F32); b_rzg = Buf()
        oacc = P.sbuf("oacc", [128, 4, 64], F32); b_oacc = Buf()
        otmp = P.sbuf("otmp", [128, 4, 64], F32); b_otmp = Buf()
        for kv in range(2):
            P.dma("sp", lambda e, kv=kv: e.dma_start(out=KsT, in_=ksT[:, kv, :]), writes=[b_KV, b_w1, b_kc2])
            P.dma("sp", lambda e, kv=kv: e.dma_start(out=KwT, in_=kwT[:, kv, :]), writes=[b_KV])
            P.op("pool", lambda e: e.memset(Vs[:, :, 64:65], 1.0), writes=[b_KV])
            P.op("pool", lambda e: e.memset(Vw[:, :, 64:65], 1.0), writes=[b_KV])
            for k0 in range(0, NKT, 8):
                P.dma("sp", lambda e, kv=kv, k0=k0: e.dma_start(
                    out=Vs[:, k0:k0 + 8, 0:64], in_=vtok[k0 * 128:(k0 + 8) * 128, kv * 64:(kv + 1) * 64].rearrange(
                        "(kt p) d -> p kt d", p=128)), writes=[b_KV])
                P.dma("sp", lambda e, kv=kv, k0=k0: e.dma_start(
                    out=Vw[:, k0:k0 + 8, 0:64], in_=vtok[k0 * 128:(k0 + 8) * 128, 128 + kv * 64:128 + (kv + 1) * 64].rearrange(
                        "(kt p) d -> p kt d", p=128)), writes=[b_KV])
            toeplitz_load(P, W, b_W, crel_scr, b_scr_rel, LC, 4 * kv, 11)
            toeplitz_load(P, Wwin, b_Wwin, cwin_scr, b_scr_win, LW, 4 * kv, 8)
            toeplitz_load(P, Wc, b_Wc, ccmp_scr, b_scr_cmp, LCM, 4 * kv, NRC, rstride=128 * STRIDE, pstride=16)
            for lt in range(NT):
                Oc, b_Oc = Ops[0]; Os, b_Os = Ops[1]; Ow, b_Ow = Ops[2]

                def qk_gen(Ksrc, bK, lt=lt, kv=kv):
                    def qk_fn(kt):
                        return [(hh * 128, (hh + 1) * 128,
                                 [(Ksrc(kt), QT[:, 4 * kv + hh, lt * 128:(lt + 1) * 128])], [bK, b_QT])
                                for hh in range(4)]
                    return qk_fn
                ncs = list(range(0, min(4, (STRIDE * lt + STRIDE - 1) // 16 + 1)))

                def extra_c(nc_, lt=lt, kv=kv):
                    r = (STRIDE * lt - 16 * nc_) // STRIDE
                    if r < NRC:
                        return [(C.antib[:], Wc[:, r, :, :].rearrange("p h q -> p (h q)"), [C.b_antib, b_Wc])]
                    return [(ones_b[0:1, :], b31row[0:1, kv, :, :].rearrange("p h q -> p (h q)"), [b_ones_b, b_b31row])]

                def post_c(nc_, ii, n, Pt, b_P):
                    for g in range(4):
                        P.op("pe", lambda e, g=g, nc_=nc_, ii=ii, n=n, Pt=Pt: e.matmul(
                            out=IMP[:, g, :], lhsT=Pt[:, g * 128:(g + 1) * 128], rhs=ovl[:, nc_, :],
                            start=(ii == 0 and g == 0), stop=(ii == n - 1 and g == 3), skip_group_check=True),
                            reads=[b_P, b_ovl], writes=[b_IMP], inc=(g == 3))
                attn_steps(P, R, ncs, qk_gen(lambda nc_, kv=kv: KcT[:, kv, nc_ * 128:(nc_ + 1) * 128], b_KcT), extra_c,
                           lambda nc_, hh, kv=kv: (Vc[:, kv, nc_, :], [b_Vc]), Oc, b_Oc, 0.125, post_fn=post_c)
                P.op("dve", lambda e, Oc=Oc: e.tensor_scalar(out=rz[:], in0=Oc[:, :, 64:65], scalar1=1e-30, scalar2=None,
                                                             op0=ALU.max), reads=[b_Oc], writes=[b_rz])
                P.op("dve", lambda e: e.reciprocal(out=rz[:], in_=rz[:]), reads=[b_rz], writes=[b_rz])
                P.op("dve", lambda e: e.tensor_scalar(out=imp[:], in0=IMP[:, 0, :], scalar1=rz[:, 0, :], scalar2=None,
                                                      op0=ALU.mult), reads=[b_IMP, b_rz], writes=[b_imp])
                for g in range(1, 4):
                    P.op("dve", lambda e, g=g: e.scalar_tensor_tensor(out=imp[:], in0=IMP[:, g, :], scalar=rz[:, g, :],
                                                                      in1=imp[:], op0=ALU.mult, op1=ALU.add),
                         reads=[b_IMP, b_rz, b_imp], writes=[b_imp])
                f0 = OFFW - 2 * STRIDE * lt
                P.op("dve", lambda e, f0=f0: e.tensor_tensor(out=imp[:], in0=imp[:], in1=fw[:, f0:f0 + 128], op=ALU.add),
                     reads=[b_imp, b_fw], writes=[b_imp])
                P.op("dve", lambda e: e.memset(imp[:, 0:1], 1e30), reads=[b_imp], writes=[b_imp])
                P.op("dve", lambda e: e.max(out=m8[:, 0, :], in_=imp[:]), reads=[b_imp], writes=[b_m8])
                P.op("dve", lambda e: e.match_replace(out=sc2[:], in_to_replace=m8[:, 0, :], in_values=imp[:],
                                                      imm_value=-3.0e38), reads=[b_imp, b_m8], writes=[b_sc2])
                P.op("dve", lambda e: e.max(out=m8[:, 1, :], in_=sc2[:]), reads=[b_sc2], writes=[b_m8])
                P.op("dve", lambda e: e.tensor_scalar(out=sc2[:], in0=imp[:], scalar1=m8[:, 1, 7:8], scalar2=None,
                                                      op0=ALU.is_ge), reads=[b_imp, b_m8], writes=[b_sc2])
                P.op("dve", lambda e: e.tensor_scalar(out=sc2[:], in0=sc2[:], scalar1=-1.0, scalar2=-NEGM,
                                                      op0=ALU.add, op1=ALU.mult), reads=[b_sc2], writes=[b_sc2])
                for hh in range(4):
                    P.op("dve", lambda e, hh=hh, kv=kv: e.tensor_scalar(
                        out=negm4[:, hh, :], in0=sc2[:], scalar1=b31[:, 4 * kv + hh:4 * kv + hh + 1], scalar2=None,
                        op0=ALU.add), reads=[b_sc2, b_b31], writes=[b_negm4], inc=(hh == 3))
                for hh in range(4):
                    P.op("pe", lambda e, hh=hh: e.transpose(out=ps_trf[:, hh, :], in_=negm4[:, hh, :], identity=C.idb[:]),
                         reads=[b_negm4, C.b_idb], writes=[b_ps_tr], inc=(hh == 3))
                nT, b_nT = nT4[lt % 2]
                P.op("act", lambda e, nT=nT: e.copy(out=nT[:], in_=ps_trf[:, 0:4, :]), reads=[b_ps_tr], writes=[b_nT])
                kts = list(range(0, min(STRIDE * lt + STRIDE, NKT)))

                def extra_s(kt, lt=lt, nT=nT, b_nT=b_nT):
                    ex = [(Esb[:, kt * 128:(kt + 1) * 128], nT[:].rearrange("m h q -> m (h q)"), [b_E, b_nT])]
                    dl = STRIDE * lt - kt
                    if dl <= 7:
                        ex.append((C.antib[:], W[:, dl + 3, :, :].rearrange("p h q -> p (h q)"), [C.b_antib, b_W]))
                    return ex
                attn_steps(P, R, kts, qk_gen(lambda kt: KsT[:, kt * 128:(kt + 1) * 128], b_KV), extra_s,
                           lambda kt, hh: (Vs[:, kt, :], [b_KV]), Os, b_Os, 0.125)
                ktw = list(range(max(0, STRIDE * lt - 4), min(STRIDE * lt + STRIDE, NKT)))

                def extra_w(kt, lt=lt):
                    dl = STRIDE * lt - kt
                    return [(C.antib[:], Wwin[:, dl + 3, :, :].rearrange("p h q -> p (h q)"), [C.b_antib, b_Wwin])]
                attn_steps(P, R, ktw, qk_gen(lambda kt: KwT[:, kt * 128:(kt + 1) * 128], b_KV), extra_w,
                           lambda kt, hh: (Vw[:, kt, :], [b_KV]), Ow, b_Ow, 0.125)
                for br, (O, b_O) in enumerate([(Oc, b_Oc), (Os, b_Os), (Ow, b_Ow)]):
                    if br == 2 and lt < NAFF:
                        P.op("dve", lambda e, O=O, lt=lt, kv=kv: e.tensor_tensor(
                            out=rz[:], in0=O[:, :, 64:65], in1=zpad[:, lt, 4 * kv:4 * kv + 4].unsqueeze(2), op=ALU.add),
                            reads=[b_O, b_zpad], writes=[b_rz])
                    else:
                        P.op("dve", lambda e, O=O: e.tensor_scalar(out=rz[:], in0=O[:, :, 64:65], scalar1=1e-30, scalar2=None,
                                                                   op0=ALU.max), reads=[b_O], writes=[b_rz])
                    P.op("dve", lambda e: e.reciprocal(out=rz[:], in_=rz[:]), reads=[b_rz], writes=[b_rz])
                    gsl = gts[:, lt, 12 * kv:12 * kv + 12].rearrange("p (h b) -> p h b", b=3)[:, :, br:br + 1]
                    P.op("dve", lambda e, gsl=gsl: e.tensor_tensor(out=rzg[:], in0=rz[:], in1=gsl, op=ALU.mult),
                         reads=[b_rz, b_gts], writes=[b_rzg])
                    if br == 0:
                        P.op("dve", lambda e, O=O: e.tensor_tensor(out=oacc[:], in0=O[:, :, 0:64],
                                                                   in1=rzg[:].to_broadcast([128, 4, 64]), op=ALU.mult),
                             reads=[b_O, b_rzg], writes=[b_oacc])
                    else:
                        P.op("dve", lambda e, O=O: e.tensor_tensor(out=otmp[:], in0=O[:, :, 0:64],
                                                                   in1=rzg[:].to_broadcast([128, 4, 64]), op=ALU.mult),
                             reads=[b_O, b_rzg], writes=[b_otmp])
                        if br == 1:
                            P.op("pool", lambda e: e.tensor_tensor(out=oacc[:], in0=oacc[:], in1=otmp[:], op=ALU.add),
                                 reads=[b_oacc, b_otmp], writes=[b_oacc])
                        else:
                            P.op("pool", lambda e, lt=lt, kv=kv: e.tensor_tensor(
                                out=o_sb[:, lt, kv * 256:(kv + 1) * 256].rearrange("p (h d) -> p h d", d=64),
                                in0=oacc[:], in1=otmp[:], op=ALU.add), reads=[b_oacc, b_otmp], writes=[b_osb])

    if do_moba:
        ohsel = Esb[0:32, 0:32 * 128]; b_ohsel = b_E
        P.dma("sp", lambda e: e.dma_start(out=ohsel, in_=ohsel_d[:, :]), writes=[b_ohsel])
        assert NT * 32 <= 512
        negvalid = u32[:, 0:NT * 32].rearrange("p (a n) -> p a n", n=32); b_nv = b_u32
        own1h = t32[:, 0:NT * 32].rearrange("p (a n) -> p a n", n=32); b_own = b_t32
        P.dma("sp", lambda e: e.dma_start(out=u32[:, 0:NT * 32],
                                          in_=negvalid_d[0:1, :].to_broadcast([128, NT * 32])), writes=[b_nv])
        P.dma("sp", lambda e: e.dma_start(out=t32[:, 0:NT * 32],
                                          in_=own1h_d[0:1, :].to_broadcast([128, NT * 32])), writes=[b_own])
        P.dma("sp", lambda e: e.dma_start(out=QT[:], in_=qT_d[:, 8:16, :]), writes=[b_QT])
        KT = KV[:, 0:2 * NK].rearrange("p (c k) -> p c k", c=2)
        V = KV[:, 2 * NK:2 * NK + NKT * 4 * 65].rearrange("p (k h d) -> p k h d", h=4, d=65)
        kmT = P.sbuf("kmT", [128, 2, 32], BF16); b_kmT = Buf()
        kms = P.sbuf("kms", [128, 32], F32); b_kms = Buf()
        gate = P.sbuf("gate", [128, 4, 32], F32); b_gate = Buf()
        mx8 = P.sbuf("mx8", [128, 4, 8], F32); b_mx8 = Buf()
        sel = P.sbuf("sel", [128, 4, 32], F32); b_sel = Buf()
        negm = P.sbuf("negm", [128, 4, 32], BF16); b_negm = Buf()
        negmT = [(Wwin[0:32, i, :, :], b_Wwin) for i in range(2)]
        for g in range(2):
            for cc in range(2):
                P.dma("sp", lambda e, cc=cc, g=g: e.dma_start(out=KT[:, cc, :], in_=kbT[:, 2 * g + cc, :]), writes=[b_KV])
            P.op("pool", lambda e: e.memset(V[:, :, :, 64:65], 1.0), writes=[b_KV])
            for hh in range(4):
                for k0 in range(0, NKT, 8):
                    P.dma("sp", lambda e, hh=hh, g=g, k0=k0: e.dma_start(
                        out=V[:, k0:k0 + 8, hh, 0:64],
                        in_=vtok[k0 * 128:(k0 + 8) * 128, 256 + (4 * g + hh) * 64:256 + (4 * g + hh + 1) * 64].rearrange(
                            "(kt p) d -> p kt d", p=128)), writes=[b_KV])
            toeplitz_load(P, W, b_W, crel_scr, b_scr_rel, LC, 4 * g, 11)
            for cc in range(2):
                P.op("dve", lambda e, cc=cc: e.tensor_reduce(out=kms[:], in_=KT[:, cc, :].rearrange("p (n k) -> p n k", k=256),
                                                             axis=AX.X, op=ALU.add), reads=[b_KV], writes=[b_kms])
                P.op("dve", lambda e, cc=cc: e.tensor_scalar(out=kmT[:, cc, :], in0=kms[:], scalar1=1.0 / 256, scalar2=None,
                                                             op0=ALU.mult), reads=[b_kms], writes=[b_kmT])
            for lt in range(NT):
                for hh in range(4):
                    cc = hh // 2
                    P.op("pe", lambda e, hh=hh, cc=cc, lt=lt, g=g: e.matmul(
                        out=ps_misc[:, hh * 32:(hh + 1) * 32], lhsT=QT[:, 4 * g + hh, lt * 128:(lt + 1) * 128],
                        rhs=kmT[:, cc, :], start=(hh == 0), stop=(hh == 3), skip_group_check=True),
                        reads=[b_QT, b_kmT], writes=[b_ps_misc], inc=(hh == 3))
                P.op("dve", lambda e, lt=lt: e.tensor_tensor(
                    out=gate[:], in0=ps_misc[:, 0:128].rearrange("p (h n) -> p h n", n=32),
                    in1=negvalid[:, lt:lt + 1, :].to_broadcast([128, 4, 32]), op=ALU.add),
                    reads=[b_ps_misc, b_nv], writes=[b_gate])
                for hh in range(4):
                    P.op("dve", lambda e, hh=hh: e.max(out=mx8[:, hh, :], in_=gate[:, hh, :]),
                         reads=[b_gate], writes=[b_mx8], inc=(hh == 3))
                P.op("dve", lambda e: e.tensor_tensor(out=sel[:], in0=gate[:], in1=mx8[:, :, 2:3].to_broadcast([128, 4, 32]),
                                                      op=ALU.is_ge), reads=[b_gate, b_mx8], writes=[b_sel])
                P.op("dve", lambda e, lt=lt: e.tensor_tensor(out=sel[:], in0=sel[:],
                                                             in1=own1h[:, lt:lt + 1, :].to_broadcast([128, 4, 32]),
                                                             op=ALU.max), reads=[b_sel, b_own], writes=[b_sel])
                P.op("dve", lambda e: e.tensor_scalar(out=sel[:], in0=sel[:], scalar1=-1.0, scalar2=-NEGM,
                                                      op0=ALU.add, op1=ALU.mult), reads=[b_sel], writes=[b_sel])
                P.op("dve", lambda e, g=g: e.tensor_tensor(
                    out=negm[:], in0=sel[:], in1=b31[:, 4 * g:4 * g + 4].unsqueeze(2).to_broadcast([128, 4, 32]),
                    op=ALU.add), reads=[b_sel, b_b31], writes=[b_negm])
                for hh in range(4):
                    P.op("pe", lambda e, hh=hh: e.transpose(out=ps_trf[0:32, hh, :], in_=negm[:, hh, :], identity=C.idb[:]),
                         reads=[b_negm, C.b_idb], writes=[b_ps_tr], inc=(hh == 3))
                nT, b_nT = negmT[lt % 2]
                P.op("act", lambda e, nT=nT: e.copy(out=nT, in_=ps_trf[0:32, 0:4, :]), reads=[b_ps_tr], writes=[b_nT])
                O, b_O = Ops[lt % 2]
                kts = list(range(0, min(STRIDE * lt + STRIDE, NKT)))

                def qk_fn(kt, lt=lt, g=g):
                    return [(hh * 128, (hh + 1) * 128,
                             [(KT[:, hh // 2, kt * 128:(kt + 1) * 128], QT[:, 4 * g + hh, lt * 128:(lt + 1) * 128])],
                             [b_KV, b_QT]) for hh in range(4)]

                def extra_fn(kt, lt=lt, nT=nT, b_nT=b_nT):
                    ex = [(ohsel[:, (kt // 2) * 128:(kt // 2 + 1) * 128], nT.rearrange("n h q -> n (h q)"),
                           [b_ohsel, b_nT])]
                    dl = STRIDE * lt - kt
                    if dl <= 7:
                        ex.append((C.antib[:], W[:, dl + 3, :, :].rearrange("p h q -> p (h q)"), [C.b_antib, b_W]))
                    return ex
                attn_steps(P, R, kts, qk_fn, extra_fn, lambda kt, hh: (V[:, kt, hh, :], [b_KV]), O, b_O, 0.125)
                P.op("dve", lambda e, O=O: e.tensor_scalar(out=rz[:], in0=O[:, :, 64:65], scalar1=1e-30, scalar2=None,
                                                           op0=ALU.max), reads=[b_O], writes=[b_rz])
                P.op("dve", lambda e: e.reciprocal(out=rz[:], in_=rz[:]), reads=[b_rz], writes=[b_rz])
                P.op("dve", lambda e, O=O, lt=lt, g=g: e.tensor_tensor(
                    out=o_sb[:, lt, 512 + g * 256:512 + (g + 1) * 256].rearrange("p (h d) -> p h d", d=64),
                    in0=O[:, :, 0:64], in1=rz[:].to_broadcast([128, 4, 64]), op=ALU.mult),
                    reads=[b_O, b_rz], writes=[b_osb])
    for t in range(NT):
        outs.append(P.dma("sp", lambda e, t=t: e.dma_start(out=o_out[t * 128:(t + 1) * 128, :], in_=o_sb[:, t, :]),
                          reads=[b_osb]))
    P.wait_all("sp", outs)
    P.finish()
    return nc


def build_post(NT=16, final=False, NEXP=16):
    nc = bass.Bass("TRN2", target_bir_lowering=False)
    NTOK = NT * 128
    dt = lambda n, s, d, k: nc.dram_tensor(n, s, d, kind=k).ap()
    o_attn = dt("o_attn", [NTOK, 1024], BF16, "ExternalInput")
    x_d = dt("x", [NTOK, D], F32, "ExternalInput")
    mod_d = dt("mod", [6, D], F32, "ExternalInput")
    w_out_d = dt("w_out", [D, D], F32, "ExternalInput")
    g_ffn_d = dt("g_ffn", [1, D], F32, "ExternalInput")
    g_fin_d = dt("g_fin", [1, D], F32, "ExternalInput")
    rw_d = dt("router_w", [D, 16], F32, "ExternalInput")
    rb_d = dt("router_b", [1, 16], F32, "ExternalInput")
    wg_d = dt("wg", [16, D, 512], F32, "ExternalInput")
    wu_d = dt("wu", [16, D, 512], F32, "ExternalInput")
    wd_d = dt("wd", [16, 512, D], F32, "ExternalInput")
    oh16_d = dt("oh16", [16, 16 * 128], F32, "ExternalInput")
    ident = dt("ident", [128, 128], F32, "ExternalInput")
    x_out = dt("x_out", [NTOK, D], F32, "ExternalOutput")

    P = Prog(nc)
    C = Consts(P, nc, ident[:, :])
    ps_a = [(P.psum(f"pa{i}", [128, 512], F32), Buf()) for i in range(4)]
    ps_y = [(P.psum(f"py{i}", [128, 512], F32), Buf()) for i in range(2)]
    ps_w = (P.psum("pw", [128, 512], F32), Buf())
    ps_tr = P.psum("ptr", [128, 8, 128], BF16); b_ps_tr = Buf()
    x_sb = P.sbuf("x_sb", [128, NT, D], F32); b_x = [Buf() for _ in range(NT)]
    hfT = P.sbuf("hfT", [128, 8, NTOK], BF16); b_hfT = Buf()
    WB = [P.sbuf(f"WB{i}", [128, 12288], BF16) for i in range(2)]; b_WB = [Buf(), Buf()]
    bc = P.sbuf("bc", [128, 4, D], F32); b_bc = Buf()
    scr_b = P.sbuf("scr_b", [128, 2048], BF16)
    nsc = norm_scratch(P, "p", hb=scr_b[:, 1024:2048])
    gf = nsc["h32"]; b_gf = nsc["b"][3]
    P.dma("sp", lambda e: e.dma_start(out=bc[:, 0, :], in_=mod_d[2:3, :].to_broadcast([128, D])), writes=[b_bc])
    P.dma("sp", lambda e: e.dma_start(out=bc[:, 1, :], in_=mod_d[4:5, :].to_broadcast([128, D])), writes=[b_bc])
    P.dma("sp", lambda e: e.dma_start(out=bc[:, 2, :], in_=mod_d[3:4, :].to_broadcast([128, D])), writes=[b_bc])
    P.dma("sp", lambda e: e.dma_start(out=bc[:, 3, :], in_=mod_d[5:6, :].to_broadcast([128, D])), writes=[b_bc])
    P.dma("sp", lambda e: e.dma_start(out=gf[:], in_=g_ffn_d[0:1, :].to_broadcast([128, D])), writes=[b_gf])
    P.op("dve", lambda e: e.scalar_tensor_tensor(out=bc[:, 1, :], in0=bc[:, 1, :], scalar=1.0, in1=gf[:],
                                                 op0=ALU.add, op1=ALU.mult), reads=[b_bc, b_gf], writes=[b_bc])
    wo = WB[1][:, 0:8192].rearrange("p (k n) -> p k n", n=1024)
    wov = w_out_d.rearrange("(k p) n -> p k n", p=128)
    for k in range(8):
        P.dma("pool", lambda e, k=k: e.dma_start(out=wo[:, k, :], in_=wov[:, k, :]), writes=[b_WB[1]])
    for k in range(8):
        P.op("dve", lambda e, k=k: e.tensor_tensor(out=wo[:, k, :], in0=wo[:, k, :], in1=bc[:, 0, :], op=ALU.mult),
             reads=[b_WB[1], b_bc], writes=[b_WB[1]])
    rw = P.sbuf("rw", [128, 8, 16], F32); b_rw = Buf()
    P.dma("sp", lambda e: e.dma_start(out=rw[:], in_=rw_d.rearrange("(k p) n -> p k n", p=128)), writes=[b_rw])
    rb = P.sbuf("rb", [128, 16], F32); b_rb = Buf()
    P.dma("sp", lambda e: e.dma_start(out=rb[:], in_=rb_d[0:1, :].to_broadcast([128, 16])), writes=[b_rb])
    oh16 = P.sbuf("oh16", [16, 16 * 128], F32); b_oh16 = Buf()
    P.dma("sp", lambda e: e.dma_start(out=oh16[:], in_=oh16_d[:, :]), writes=[b_oh16])
    wT = P.sbuf("wT", [16, NTOK], F32); b_wT = Buf()
    ob = [scr_b[:, 0:1024]] * 2; b_ob = [Buf()] * 2
    oT = P.sbuf("oT", [128, 8, 128], BF16); b_oT = Buf()
    h32T = nsc["sq"][:].rearrange("p (k n) -> p k n", n=128); b_h32T = nsc["b"][0]
    r_aff = P.sbuf("r_aff", [128, 16], F32); b_aff = Buf()
    r_b = P.sbuf("r_b", [128, 4, 4], F32); b_rbias = Buf()
    r_t = P.sbuf("r_t", [128, 8, 4], F32); b_rt = Buf()
    r_w = P.sbuf("r_w", [128, 4, 4], F32); b_rw2 = Buf()
    r_s = P.sbuf("r_s", [128, 2], F32); b_rs = Buf()
    def stage_pa(t):
        o_t = ob[t % 2]; bo = b_ob[t % 2]
        P.dma("sp", lambda e, o_t=o_t, t=t: e.dma_start(out=o_t[:], in_=o_attn[t * 128:(t + 1) * 128, :]), writes=[bo])
        P.dma("sp", lambda e, t=t: e.dma_start(out=x_sb[:, t, :], in_=x_d[t * 128:(t + 1) * 128, :]), writes=[b_x[t]])
        for k in range(8):
            P.op("pe", lambda e, k=k, o_t=o_t: e.transpose(out=ps_tr[:, k, :], in_=o_t[:, k * 128:(k + 1) * 128],
                                                           identity=C.idb[:]),
                 reads=[bo, C.b_idb], writes=[b_ps_tr], inc=(k == 7))
        P.op("act", lambda e: e.copy(out=oT[:], in_=ps_tr[:]), reads=[b_ps_tr], writes=[b_oT])
        for half in range(2):
            py, b_py = ps_y[half]
            for k in range(8):
                P.op("pe", lambda e, k=k, half=half, py=py: e.matmul(out=py[:], lhsT=oT[:, k, :],
                                                                   rhs=wo[:, k, half * 512:(half + 1) * 512],
                                                                   start=(k == 0), stop=(k == 7)),
                     reads=[b_oT, b_WB[1]], writes=[b_py], inc=(k == 7))
            P.op("dve", lambda e, half=half, py=py, t=t: e.tensor_tensor(
                out=x_sb[:, t, half * 512:(half + 1) * 512], in0=py[:], in1=x_sb[:, t, half * 512:(half + 1) * 512],
                op=ALU.add), reads=[b_py, b_x[t]], writes=[b_x[t]])
    def stage_pb(t):
        emit_norm_tile(P, C, x_sb[:, t, :], b_x[t], bc[:, 1, :], b_bc, bc[:, 2, :], b_bc,
                       hfT[:, :, t * 128:(t + 1) * 128], b_hfT, nsc, ps_tr, b_ps_tr)
        h32 = nsc["h32"]; b_h32 = nsc["b"][3]
        P.op("dve", lambda e: e.tensor_tensor(out=h32[:], in0=h32[:], in1=bc[:, 2, :], op=ALU.add),
             reads=[b_h32, b_bc], writes=[b_h32])
        for hf_ in range(2):
            pa, b_pa = ps_a[hf_]
            for k in range(4):
                kk = hf_ * 4 + k
                P.op("pe", lambda e, k=k, kk=kk, pa=pa: e.transpose(out=pa[:, k * 128:(k + 1) * 128],
                                                                  in_=h32[:, kk * 128:(kk + 1) * 128], identity=C.idf[:]),
                     reads=[b_h32, C.b_idf], writes=[b_pa], inc=(k == 3))
            P.op("act", lambda e, hf_=hf_, pa=pa: e.copy(out=h32T[:, hf_ * 4:(hf_ + 1) * 4, :],
                                                         in_=pa[:].rearrange("p (k n) -> p k n", n=128)),
                 reads=[b_pa], writes=[b_h32T])
        pw, b_pw = ps_w
        for k in range(8):
            P.op("pe", lambda e, k=k: e.matmul(out=pw[:, 0:16], lhsT=h32T[:, k, :], rhs=rw[:, k, :],
                                               start=(k == 0), stop=(k == 7)),
                 reads=[b_h32T, b_rw], writes=[b_pw], inc=(k == 7))
        P.op("act", lambda e: e.activation(out=r_aff[:], in_=pw[:, 0:16], func=AF.Sigmoid), reads=[b_pw], writes=[b_aff])
        r_bf = r_b[:].rearrange("p g e -> p (g e)")
        P.op("dve", lambda e: e.tensor_tensor(out=r_bf, in0=r_aff[:], in1=rb[:], op=ALU.add),
             reads=[b_aff, b_rb], writes=[b_rbias])
        a_, b_, c_, d_ = (r_b[:, :, i] for i in range(4))
        T = lambda i: r_t[:, i, :]
        seq = [(T(0), a_, b_, ALU.max), (T(1), a_, b_, ALU.min), (T(2), c_, d_, ALU.max), (T(3), c_, d_, ALU.min),
               (T(4), T(0), T(2), ALU.max), (T(5), T(0), T(2), ALU.min), (T(6), T(1), T(3), ALU.max),
               (T(7), T(5), T(6), ALU.max),
               (T(0), T(4), T(7), ALU.add)]
        for (o_, i0, i1, op_) in seq:
            P.op("dve", lambda e, o_=o_, i0=i0, i1=i1, op_=op_: e.tensor_tensor(out=o_, in0=i0, in1=i1, op=op_),
                 reads=[b_rbias, b_rt], writes=[b_rt])
        P.op("dve", lambda e: e.tensor_reduce(out=r_s[:, 0:1], in_=r_t[:, 0, :], axis=AX.X, op=ALU.max),
             reads=[b_rt], writes=[b_rs])
        P.op("dve", lambda e: e.tensor_scalar(out=r_t[:, 1, :], in0=r_t[:, 0, :], scalar1=r_s[:, 0:1], scalar2=None,
                                              op0=ALU.is_ge), reads=[b_rt, b_rs], writes=[b_rt])
        P.op("dve", lambda e: e.tensor_tensor(out=r_w[:], in0=r_b[:], in1=r_t[:, 7, :].unsqueeze(2).to_broadcast([128, 4, 4]),
                                              op=ALU.is_ge), reads=[b_rbias, b_rt], writes=[b_rw2])
        P.op("dve", lambda e: e.tensor_tensor(out=r_w[:], in0=r_w[:], in1=r_t[:, 1, :].unsqueeze(2).to_broadcast([128, 4, 4]),
                                              op=ALU.mult), reads=[b_rw2, b_rt], writes=[b_rw2])
        r_wf = r_w[:].rearrange("p g e -> p (g e)")
        P.op("dve", lambda e: e.tensor_tensor(out=r_wf, in0=r_wf, in1=r_aff[:], op=ALU.mult),
             reads=[b_rw2, b_aff], writes=[b_rw2])
        P.op("dve", lambda e: e.tensor_reduce(out=r_s[:, 1:2], in_=r_wf, axis=AX.X, op=ALU.add),
             reads=[b_rw2], writes=[b_rs])
        P.op("dve", lambda e: e.reciprocal(out=r_s[:, 1:2], in_=r_s[:, 1:2]), reads=[b_rs], writes=[b_rs])
        P.op("dve", lambda e: e.tensor_scalar(out=r_wf, in0=r_wf, scalar1=r_s[:, 1:2], scalar2=None, op0=ALU.mult),
             reads=[b_rw2, b_rs], writes=[b_rw2])
        pa, b_pa = ps_a[2]
        P.op("pe", lambda e, pa=pa: e.transpose(out=pa[0:16, 0:128], in_=r_wf, identity=C.idf[:]),
             reads=[b_rw2, C.b_idf], writes=[b_pa])
        P.op("act", lambda e, pa=pa, t=t: e.copy(out=wT[:, t * 128:(t + 1) * 128], in_=pa[0:16, 0:128]),
             reads=[b_pa], writes=[b_wT])

    for t in range(NT + 1):
        if t < NT:
            stage_pa(t)
        if t >= 1:
            stage_pb(t - 1)

    wbc = [P.sbuf("wbc0", [128, 512], F32)] * 2; b_wbc = [Buf()] * 2
    sg = [P.sbuf(f"sg{i}", [128, 512], BF16) for i in range(2)]; b_sg = [Buf(), Buf()]
    uw = [P.sbuf(f"uw{i}", [128, 512], BF16) for i in range(2)]; b_uw = [Buf(), Buf()]
    hid = [P.sbuf("hid0", [128, 4, 512], BF16), scr_b[:, :].rearrange("p (f n) -> p f n", n=512)]
    b_hid = [Buf(), Buf()]
    hid1_first = [True]
    NTG = NT // 4
    pai = [0]

    def stage_a(ex, tg, it, wg, wu, bW):
        wb_ = wbc[it % 2]; bwb = b_wbc[it % 2]
        hd = hid[it % 2]; bhd = b_hid[it % 2]
        pw, b_pw = ps_w
        P.op("pe", lambda e: e.matmul(out=pw[:], lhsT=oh16[:, ex * 128:(ex + 1) * 128],
                                      rhs=wT[:, tg * 512:(tg + 1) * 512], start=True, stop=True),
             reads=[b_oh16, b_wT], writes=[b_pw])
        P.op("act", lambda e: e.copy(out=wb_[:], in_=pw[:]), reads=[b_pw], writes=[bwb])
        for fc in range(4):
            pg, b_pg = ps_a[pai[0] % 4]; pai[0] += 1
            pu, b_pu = ps_a[pai[0] % 4]; pai[0] += 1
            for k in range(8):
                P.op("pe", lambda e, k=k, fc=fc, pg=pg: e.matmul(
                    out=pg[:], lhsT=wg[:, k, fc * 128:(fc + 1) * 128], rhs=hfT[:, k, tg * 512:(tg + 1) * 512],
                    start=(k == 0), stop=(k == 7)), reads=[bW, b_hfT], writes=[b_pg], inc=(k == 7))
            for k in range(8):
                P.op("pe", lambda e, k=k, fc=fc, pu=pu: e.matmul(
                    out=pu[:], lhsT=wu[:, k, fc * 128:(fc + 1) * 128], rhs=hfT[:, k, tg * 512:(tg + 1) * 512],
                    start=(k == 0), stop=(k == 7)), reads=[bW, b_hfT], writes=[b_pu], inc=(k == 7))
            s_ = sg[fc % 2]; bs_ = b_sg[fc % 2]
            u_ = uw[fc % 2]; bu_ = b_uw[fc % 2]
            P.op("act", lambda e, pg=pg, s_=s_: e.activation(out=s_[:], in_=pg[:], func=AF.Silu), reads=[b_pg], writes=[bs_])
            P.op("dve", lambda e, pu=pu, u_=u_: e.tensor_tensor(out=u_[:], in0=pu[:], in1=wb_[:], op=ALU.mult),
                 reads=[b_pu, bwb], writes=[bu_])
            wr = [bhd]
            if it % 2 == 1 and hid1_first[0]:
                wr = [bhd, b_ob[0], nsc["b"][4]]
                hid1_first[0] = False
            P.op("dve", lambda e, fc=fc, s_=s_, u_=u_: e.tensor_tensor(out=hd[:, fc, :], in0=s_[:], in1=u_[:], op=ALU.mult),
                 reads=[bs_, bu_], writes=wr)

    def stage_b(ex, tg, it, wd, bW):
        hd = hid[it % 2]; bhd = b_hid[it % 2]
        for tt in range(4):
            t = tg * 4 + tt
            for half in range(2):
                py, b_py = ps_y[half]
                for fc in range(4):
                    P.op("pe", lambda e, fc=fc, tt=tt, half=half, py=py: e.matmul(
                        out=py[:], lhsT=hd[:, fc, tt * 128:(tt + 1) * 128], rhs=wd[:, fc, half * 512:(half + 1) * 512],
                        start=(fc == 0), stop=(fc == 3)), reads=[bhd, bW], writes=[b_py], inc=(fc == 3))
                P.op("dve", lambda e, half=half, py=py, t=t: e.tensor_tensor(
                    out=x_sb[:, t, half * 512:(half + 1) * 512], in0=py[:], in1=x_sb[:, t, half * 512:(half + 1) * 512],
                    op=ALU.add), reads=[b_py, b_x[t]], writes=[b_x[t]])

    pend = None
    it = 0
    for ex in range(NEXP):
        Wb = WB[ex % 2]; bW = b_WB[ex % 2]
        wg = Wb[:, 0:4096].rearrange("p (k n) -> p k n", n=512)
        wu = Wb[:, 4096:8192].rearrange("p (k n) -> p k n", n=512)
        wd = Wb[:, 8192:12288].rearrange("p (k n) -> p k n", n=1024)
        wgv = wg_d[ex].rearrange("(k p) n -> p k n", p=128)
        wuv = wu_d[ex].rearrange("(k p) n -> p k n", p=128)
        wdv = wd_d[ex].rearrange("(k p) n -> p k n", p=128)
        for k in range(8):
            P.dma("pool", lambda e, k=k, wg=wg, wgv=wgv: e.dma_start(out=wg[:, k, :], in_=wgv[:, k, :]), writes=[bW])
            P.dma("pool", lambda e, k=k, wu=wu, wuv=wuv: e.dma_start(out=wu[:, k, :], in_=wuv[:, k, :]), writes=[bW])
        for k in range(4):
            P.dma("pool", lambda e, k=k, wd=wd, wdv=wdv: e.dma_start(out=wd[:, k, :], in_=wdv[:, k, :]), writes=[bW])
        for k in range(4):
            P.op("pool", lambda e, k=k, wd=wd: e.tensor_tensor(out=wd[:, k, :], in0=wd[:, k, :], in1=bc[:, 3, :], op=ALU.mult),
                 reads=[bW, b_bc], writes=[bW])
        for tg in range(NTG):
            stage_a(ex, tg, it, wg, wu, bW)
            if pend is not None:
                stage_b(*pend)
            pend = (ex, tg, it, wd, bW)
            it += 1
    if pend is not None:
        stage_b(*pend)
    outs = []
    if final:
        gfin = bc[:, 0, :]
        b_gf = b_bc
        P.dma("sp", lambda e: e.dma_start(out=gfin, in_=g_fin_d[0:1, :].to_broadcast([128, D])), writes=[b_gf])
        sq, ss, rstd = nsc["sq"], nsc["ss"], nsc["rstd"]
        b_sq, b_ss, b_rstd = nsc["b"][0:3]
        for t in range(NT):
            P.op("act", lambda e, t=t: e.activation(out=sq[:], in_=x_sb[:, t, :], func=AF.Square, accum_out=ss[:]),
                 reads=[b_x[t]], writes=[b_sq, b_ss])
            P.op("dve", lambda e: e.tensor_scalar(out=rstd[:], in0=ss[:], scalar1=1.0 / D, scalar2=1e-6,
                                                  op0=ALU.mult, op1=ALU.add), reads=[b_ss], writes=[b_rstd])
            P.op("act", lambda e: e.activation(out=rstd[:], in_=rstd[:], func=AF.Sqrt), reads=[b_rstd], writes=[b_rstd])
            P.op("dve", lambda e: e.reciprocal(out=rstd[:], in_=rstd[:]), reads=[b_rstd], writes=[b_rstd])
            P.op("dve", lambda e, t=t: e.scalar_tensor_tensor(out=x_sb[:, t, :], in0=x_sb[:, t, :], scalar=rstd[:, 0:1],
                                                              in1=gfin, op0=ALU.mult, op1=ALU.mult),
                 reads=[b_x[t], b_rstd, b_gf], writes=[b_x[t]])
    for t in range(NT):
        outs.append(P.dma("sp", lambda e, t=t: e.dma_start(out=x_out[t * 128:(t + 1) * 128, :], in_=x_sb[:, t, :]),
                          reads=[b_x[t]]))
    P.wait_all("sp", outs)
    P.finish()
    return nc


def oh16_static():
    oh = np.zeros((16, 16, 128), np.float32)
    for e in range(16):
        oh[e, e, :] = 1.0
    return oh.reshape(16, 2048)


OD = dict(c_q=(0, 256), c_kv=(256, 384), k_rope=(384, 448), q_d=(448, 960), k_d=(960, 1088), v_d=(1088, 1216))
NU1 = 10


def host_w_in_odd(w, wq_up, wkv_up):
    sl = lambda n: w[:, OD[n][0]:OD[n][1]]
    units = []
    for h in range(8):
        u = np.zeros((1024, 128), np.float32)
        u[:, (h % 2) * 64:(h % 2 + 1) * 64] = sl("q_d")[:, h * 64:(h + 1) * 64]
        units.append(u)
    for kv in range(2):
        c = sl("k_d")[:, kv * 64:(kv + 1) * 64]
        units.append(np.concatenate([c, c], axis=1))
    WF = np.concatenate(units, axis=1)
    WT = np.concatenate([sl("c_q"), sl("c_kv"), sl("k_rope"), sl("v_d")], axis=1)
    wq = wq_up.reshape(256, 4, 192)
    wq_nope = np.ascontiguousarray(wq[:, :, 0:128].reshape(256, 512))
    wq_rope = np.ascontiguousarray(wq[:, :, 128:192].reshape(256, 256))
    wkv = wkv_up.reshape(128, 4, 256)
    wk_nope = np.ascontiguousarray(wkv[:, :, 0:128].reshape(128, 512))
    wv = np.ascontiguousarray(wkv[:, :, 128:256].reshape(128, 512))
    return dict(wf=np.ascontiguousarray(WF), wt=np.ascontiguousarray(WT), wq_nope=wq_nope, wq_rope=wq_rope,
                wk_nope=wk_nope, wv=wv)


def rope_static(positions):
    inv = (10000.0 ** (-np.arange(0, 64, 2, dtype=np.float32) / 64)).astype(np.float32)
    ang = positions.astype(np.float32)[:, None] * inv[None, :]
    return np.cos(ang).astype(np.float32), np.sin(ang).astype(np.float32)


def build_L1odd(NT=16):
    nc = bass.Bass("TRN2", target_bir_lowering=False)
    NTOK = NT * 128
    dt = lambda n, s, d, k: nc.dram_tensor(n, s, d, kind=k).ap()
    x = dt("x", [NTOK, D], F32, "ExternalInput")
    c_cols = dt("c_cols", [128, 8], F32, "ExternalInput")
    ada_w = dt("ada_w", [D, 6 * D], F32, "ExternalInput")
    ada_b = dt("ada_b", [1, 6 * D], F32, "ExternalInput")
    g_mix = dt("g_mix", [1, D], F32, "ExternalInput")
    wf_d = dt("wf", [D, NU1 * 128], F32, "ExternalInput")
    wt_d = dt("wt", [D, 576], F32, "ExternalInput")
    wqn_d = dt("wq_nope", [256, 512], F32, "ExternalInput")
    wqr_d = dt("wq_rope", [256, 256], F32, "ExternalInput")
    wkn_d = dt("wk_nope", [128, 512], F32, "ExternalInput")
    wv_d = dt("wv", [128, 512], F32, "ExternalInput")
    qn_g = dt("q_norm", [1, 256], F32, "ExternalInput")
    kvn_g = dt("kv_norm", [1, 128], F32, "ExternalInput")
    cos_d = dt("cos", [NTOK, 32], F32, "ExternalInput")
    sin_d = dt("sin", [NTOK, 32], F32, "ExternalInput")
    ident = dt("ident", [128, 128], F32, "ExternalInput")
    o_fm = dt("o_fm", [128, NU1, NTOK], BF16, "ExternalOutput")
    o_qn = dt("o_qn", [128, 4, NTOK], BF16, "ExternalOutput")
    o_qr = dt("o_qr", [64, 4, NTOK], BF16, "ExternalOutput")
    o_kn = dt("o_kn", [128, 4, NTOK], BF16, "ExternalOutput")
    o_kr = dt("o_kr", [64, NTOK], BF16, "ExternalOutput")
    o_vtok = dt("o_vtok", [NTOK, 640], BF16, "ExternalOutput")
    o_mod = dt("o_mod", [6, D], F32, "ExternalOutput")

    P = Prog(nc)
    C = Consts(P, nc, ident[:, :])
    ps_row = P.psum("ps_row", [128, 512], F32); b_ps_row = Buf()
    ps_bc = P.psum("ps_bc", [128, 512], F32); b_ps_bc = Buf()
    ps_tr = P.psum("ps_tr", [128, 8, 128], BF16); b_ps_tr = Buf()
    ps_mm = [P.psum(f"ps_mm{i}", [128, 512], F32) for i in range(4)]
    b_ps_mm = [Buf() for _ in range(4)]
    mod, b_mod = emit_adaln(P, nc, C, c_cols[:, :], ada_w, ada_b[:, :], "1", ps_row, b_ps_row, ps_bc, b_ps_bc)
    outs = []
    outs.append(P.dma("sp", lambda e: e.dma_start(out=o_mod[:, :], in_=mod[0:1, :, :]), reads=[b_mod]))
    gm = P.sbuf("gm", [128, 1024], F32); b_gm = Buf()
    A = P.sbuf("A_m", [128, 1024], F32); b_A = Buf()
    P.dma("sp", lambda e: e.dma_start(out=gm[:], in_=g_mix[0:1, :].to_broadcast([128, 1024])), writes=[b_gm])
    P.op("dve", lambda e: e.scalar_tensor_tensor(out=A[:], in0=mod[:, 1, :], scalar=1.0, in1=gm[:],
                                                 op0=ALU.add, op1=ALU.mult), reads=[b_mod, b_gm], writes=[b_A])
    Bt = mod[:, 0, :]
    wf, b_wf = load_w_bf16(P, nc, "wf_sb", wf_d, NU1 * 128)
    wt, b_wt = load_w_bf16(P, nc, "wt_sb", wt_d, 576)
    wqn, b_wqn = load_w_bf16(P, nc, "wqn_sb", wqn_d, 512, rows=256)
    wqr, b_wqr = load_w_bf16(P, nc, "wqr_sb", wqr_d, 256, rows=256)
    wkn, b_wkn = load_w_bf16(P, nc, "wkn_sb", wkn_d, 512, rows=128)
    wv, b_wv = load_w_bf16(P, nc, "wv_sb", wv_d, 512, rows=128)
    qng = P.sbuf("qng", [128, 256], F32); b_qng = Buf()
    kvng = P.sbuf("kvng", [128, 128], F32); b_kvng = Buf()
    P.dma("sp", lambda e: e.dma_start(out=qng[:], in_=qn_g[0:1, :].to_broadcast([128, 256])), writes=[b_qng])
    P.dma("sp", lambda e: e.dma_start(out=kvng[:], in_=kvn_g[0:1, :].to_broadcast([128, 128])), writes=[b_kvng])
    cs = P.sbuf("cs", [128, NT, 2, 32], F32); b_cs = Buf()
    P.dma("sp", lambda e: e.dma_start(out=cs[:, :, 0, :], in_=cos_d.rearrange("(t p) n -> p t n", p=128)), writes=[b_cs])
    P.dma("sp", lambda e: e.dma_start(out=cs[:, :, 1, :], in_=sin_d.rearrange("(t p) n -> p t n", p=128)), writes=[b_cs])
    xt = [P.sbuf(f"xt{i}", [128, 1024], F32) for i in range(2)]
    b_xt = [Buf(), Buf()]
    hT = [P.sbuf(f"hT{i}", [128, 8, 512], BF16) for i in range(2)]
    b_hT = [Buf(), Buf()]
    nsc = norm_scratch(P, "a")
    stg = [P.sbuf(f"stg{i}", [128, 512], BF16) for i in range(4)]
    b_stg = [Buf() for _ in range(4)]
    cq = P.sbuf("cq", [128, 384], F32); b_cq = Buf()
    cqn = P.sbuf("cqn", [128, 384], BF16); b_cqn = Buf()
    cT = P.sbuf("cT", [128, 3, 128], BF16); b_cT = Buf()
    mss = P.sbuf("mss", [128, 4], F32); b_mss = Buf()
    junk = P.sbuf("junk", [128, 256], F32); b_junk = Buf()
    rp = P.sbuf("rp", [128, 5, 64], F32); b_rp = Buf()
    rt = P.sbuf("rt", [128, 4, 5, 32], F32); b_rt = Buf()
    rpb = P.sbuf("rpb", [128, 5, 64], BF16); b_rpb = Buf()
    rT = P.sbuf("rT", [64, 5, 128], BF16); b_rT = Buf()
    rr = RR()
    si = 0
    mi = 0

    def next_ps():
        nonlocal mi
        r = (ps_mm[mi % 4], b_ps_mm[mi % 4]); mi += 1
        return r

    def next_stg():
        nonlocal si
        r = (stg[si % 4], b_stg[si % 4]); si += 1
        return r
    for tg in range(NT // 4):
        h = hT[tg % 2]; bh = b_hT[tg % 2]
        for tt in range(4):
            t = tg * 4 + tt
            xb = xt[t % 2]; bx = b_xt[t % 2]
            P.dma("sp", lambda e, xb=xb, t=t: e.dma_start(out=xb[:], in_=x[t * 128:(t + 1) * 128, :]), writes=[bx])
            emit_norm_tile(P, C, xb[:], bx, A[:], b_A, Bt, b_mod, h[:, :, tt * 128:(tt + 1) * 128], bh,
                           nsc, ps_tr, b_ps_tr)
        for u in range(NU1):
            ps, bps = next_ps()
            for k in range(8):
                P.op("pe", lambda e, ps=ps, u=u, k=k, h=h: e.matmul(out=ps[:], lhsT=wf[:, k, u * 128:(u + 1) * 128],
                                                                  rhs=h[:, k, :], start=(k == 0), stop=(k == 7)),
                     reads=[b_wf, bh], writes=[bps], inc=(k == 7))
            st, bst = next_stg()
            evac(P, rr.next(), st[:], ps[:], [bps], [bst])
            outs.append(P.dma("sp", lambda e, st=st, u=u, tg=tg: e.dma_start(
                out=o_fm[:, u, tg * 512:(tg + 1) * 512], in_=st[:]), reads=[bst]))
        for tt in range(4):
            t = tg * 4 + tt
            tsl = slice(tt * 128, (tt + 1) * 128)
            ps, bps = next_ps()
            for k in range(8):
                P.op("pe", lambda e, ps=ps, k=k, h=h, tsl=tsl: e.matmul(out=ps[:, 0:384], lhsT=h[:, k, tsl], rhs=wt[:, k, 0:384],
                                                                      start=(k == 0), stop=(k == 7)),
                     reads=[b_wt, bh], writes=[bps], inc=(k == 7))
            P.op("act", lambda e, ps=ps: e.copy(out=cq[:], in_=ps[:, 0:384]), reads=[bps], writes=[b_cq])
            psB, bpsB = next_ps()
            for k in range(8):
                P.op("pe", lambda e, psB=psB, k=k, h=h, tsl=tsl: e.matmul(out=psB[:, 0:192], lhsT=h[:, k, tsl], rhs=wt[:, k, 384:576],
                                                                        start=(k == 0), stop=(k == 7)),
                     reads=[b_wt, bh], writes=[bpsB], inc=(k == 7))
            st, bst = next_stg()
            P.op("act", lambda e, st=st, psB=psB: e.copy(out=st[:, 0:128], in_=psB[:, 64:192]), reads=[bpsB], writes=[bst])
            outs.append(P.dma("sp", lambda e, st=st, t=t: e.dma_start(out=o_vtok[t * 128:(t + 1) * 128, 512:640], in_=st[:, 0:128]),
                              reads=[bst]))
            P.op("act", lambda e, psB=psB: e.copy(out=rp[:, 4, :], in_=psB[:, 0:64]), reads=[bpsB], writes=[b_rp])
            P.op("act", lambda e: e.activation(out=junk[:, 0:256], in_=cq[:, 0:256], func=AF.Square, accum_out=mss[:, 0:1]),
                 reads=[b_cq], writes=[b_junk, b_mss])
            P.op("act", lambda e: e.activation(out=junk[:, 0:128], in_=cq[:, 256:384], func=AF.Square, accum_out=mss[:, 1:2]),
                 reads=[b_cq], writes=[b_junk, b_mss])
            P.op("dve", lambda e: e.tensor_scalar(out=mss[:, 2:3], in0=mss[:, 0:1], scalar1=1.0 / 256, scalar2=1e-6,
                                                  op0=ALU.mult, op1=ALU.add), reads=[b_mss], writes=[b_mss])
            P.op("dve", lambda e: e.tensor_scalar(out=mss[:, 3:4], in0=mss[:, 1:2], scalar1=1.0 / 128, scalar2=1e-6,
                                                  op0=ALU.mult, op1=ALU.add), reads=[b_mss], writes=[b_mss])
            P.op("act", lambda e: e.activation(out=mss[:, 2:4], in_=mss[:, 2:4], func=AF.Sqrt), reads=[b_mss], writes=[b_mss])
            P.op("dve", lambda e: e.reciprocal(out=mss[:, 2:4], in_=mss[:, 2:4]), reads=[b_mss], writes=[b_mss])
            P.op("dve", lambda e: e.scalar_tensor_tensor(out=cqn[:, 0:256], in0=cq[:, 0:256], scalar=mss[:, 2:3], in1=qng[:],
                                                         op0=ALU.mult, op1=ALU.mult), reads=[b_cq, b_mss, b_qng], writes=[b_cqn])
            P.op("dve", lambda e: e.scalar_tensor_tensor(out=cqn[:, 256:384], in0=cq[:, 256:384], scalar=mss[:, 3:4], in1=kvng[:],
                                                         op0=ALU.mult, op1=ALU.mult), reads=[b_cq, b_mss, b_kvng], writes=[b_cqn])
            for k in range(3):
                P.op("pe", lambda e, k=k: e.transpose(out=ps_tr[:, k, :], in_=cqn[:, k * 128:(k + 1) * 128], identity=C.idb[:]),
                     reads=[b_cqn, C.b_idb], writes=[b_ps_tr], inc=(k == 2))
            P.op("act", lambda e: e.copy(out=cT[:], in_=ps_tr[:, 0:3, :]), reads=[b_ps_tr], writes=[b_cT])
            ps, bps = next_ps()
            for hh in range(4):
                for k in range(2):
                    P.op("pe", lambda e, ps=ps, hh=hh, k=k: e.matmul(
                        out=ps[:, hh * 128:(hh + 1) * 128], lhsT=wqn[:, k, hh * 128:(hh + 1) * 128], rhs=cT[:, k, :],
                        start=(hh == 0 and k == 0), stop=(hh == 3 and k == 1), skip_group_check=True),
                        reads=[b_wqn, b_cT], writes=[bps], inc=(hh == 3 and k == 1))
            st, bst = next_stg()
            evac(P, rr.next(), st[:], ps[:], [bps], [bst])
            outs.append(P.dma("sp", lambda e, st=st, t=t: e.dma_start(
                out=o_qn[:, :, t * 128:(t + 1) * 128], in_=st[:].rearrange("p (h q) -> p h q", q=128)), reads=[bst]))
            ps, bps = next_ps()
            for hh in range(4):
                P.op("pe", lambda e, ps=ps, hh=hh: e.matmul(
                    out=ps[:, hh * 128:(hh + 1) * 128], lhsT=wkn[:, 0, hh * 128:(hh + 1) * 128], rhs=cT[:, 2, :],
                    start=(hh == 0), stop=(hh == 3), skip_group_check=True),
                    reads=[b_wkn, b_cT], writes=[bps], inc=(hh == 3))
            st, bst = next_stg()
            evac(P, rr.next(), st[:], ps[:], [bps], [bst])
            outs.append(P.dma("sp", lambda e, st=st, t=t: e.dma_start(
                out=o_kn[:, :, t * 128:(t + 1) * 128], in_=st[:].rearrange("p (h q) -> p h q", q=128)), reads=[bst]))
            ps, bps = next_ps()
            P.op("pe", lambda e, ps=ps: e.matmul(out=ps[:], lhsT=cT[:, 2, :], rhs=wv[:, 0, :], start=True, stop=True),
                 reads=[b_wv, b_cT], writes=[bps])
            st, bst = next_stg()
            evac(P, rr.next(), st[:], ps[:], [bps], [bst])
            outs.append(P.dma("sp", lambda e, st=st, t=t: e.dma_start(out=o_vtok[t * 128:(t + 1) * 128, 0:512], in_=st[:]),
                              reads=[bst]))
            ps, bps = next_ps()
            for k in range(2):
                P.op("pe", lambda e, ps=ps, k=k: e.matmul(out=ps[:, 0:256], lhsT=cT[:, k, :], rhs=wqr[:, k, :],
                                                          start=(k == 0), stop=(k == 1)),
                     reads=[b_wqr, b_cT], writes=[bps], inc=(k == 1))
            P.op("act", lambda e, ps=ps: e.copy(out=rp[:, 0:4, :], in_=ps[:, 0:256].rearrange("p (h d) -> p h d", d=64)),
                 reads=[bps], writes=[b_rp])
            cosb = cs[:, t, 0, :].unsqueeze(1).to_broadcast([128, 5, 32])
            sinb = cs[:, t, 1, :].unsqueeze(1).to_broadcast([128, 5, 32])
            x1 = rp[:, :, 0:32]; x2 = rp[:, :, 32:64]
            for i_, (a_, b_) in enumerate([(x1, cosb), (x2, sinb), (x1, sinb), (x2, cosb)]):
                P.op("dve", lambda e, i_=i_, a_=a_, b_=b_: e.tensor_tensor(out=rt[:, i_, :, :], in0=a_, in1=b_, op=ALU.mult),
                     reads=[b_rp, b_cs], writes=[b_rt])
            P.op("dve", lambda e: e.tensor_tensor(out=rpb[:, :, 0:32], in0=rt[:, 0, :, :], in1=rt[:, 1, :, :], op=ALU.subtract),
                 reads=[b_rt], writes=[b_rpb])
            P.op("dve", lambda e: e.tensor_tensor(out=rpb[:, :, 32:64], in0=rt[:, 2, :, :], in1=rt[:, 3, :, :], op=ALU.add),
                 reads=[b_rt], writes=[b_rpb])
            for v_ in range(5):
                P.op("pe", lambda e, v_=v_: e.transpose(out=ps_tr[0:64, v_, :], in_=rpb[:, v_, :], identity=C.idb[:]),
                     reads=[b_rpb, C.b_idb], writes=[b_ps_tr], inc=(v_ == 4))
            P.op("act", lambda e: e.copy(out=rT[:], in_=ps_tr[0:64, 0:5, :]), reads=[b_ps_tr], writes=[b_rT])
            outs.append(P.dma("sp", lambda e, t=t: e.dma_start(out=o_qr[:, :, t * 128:(t + 1) * 128], in_=rT[:, 0:4, :]),
                              reads=[b_rT]))
            outs.append(P.dma("sp", lambda e, t=t: e.dma_start(out=o_kr[:, t * 128:(t + 1) * 128], in_=rT[:, 4, :]),
                              reads=[b_rT]))
    P.wait_all("sp", outs)
    P.finish()
    return nc


def attn1_static(NT, STRIDE, j):
    S_ = STRIDE
    st = {}
    pp = np.arange(128)[:, None, None]
    r = np.arange(S_)[None, :, None]
    x = np.arange(128)[None, None, :]
    d = 128 * (r - (S_ - 1)) + 128 * j + x - (127 - pp)
    m = np.where(d >= 0, 0.0, NEGM).astype(np.float32)
    st["wm"] = np.ascontiguousarray(np.stack([m, m], axis=2)).astype(NPBF)
    L = 128 * S_ + 127 + 128
    y = np.arange(L)
    st["oh_swa"] = onehot_table(y - 127 - 128 * (S_ - 1) + 128 * j, "abs", win=128)
    st["cnt_swa"] = pad_counts(NT, STRIDE, j, 128)
    return st


def build_attn1(NT=16, STRIDE=4, NKT=64):
    nc = bass.Bass("TRN2", target_bir_lowering=False)
    NTOK = NT * 128
    NK = NKT * 128
    S_ = STRIDE
    LS = 128 * S_ + 127 + 128
    NAFF = n_aff(STRIDE, 128)
    dt = lambda n, s, d, k: nc.dram_tensor(n, s, d, kind=k).ap()
    qn_d = dt("qn", [128, 4, NTOK], BF16, "ExternalInput")
    qr_d = dt("qr", [64, 4, NTOK], BF16, "ExternalInput")
    qd_d = dt("qd", [128, 8, NTOK], BF16, "ExternalInput")
    kn_d = dt("kn", [128, 4, NK], BF16, "ExternalInput")
    kr_d = dt("kr", [64, NK], BF16, "ExternalInput")
    kd_d = dt("kd", [128, 2, NK], BF16, "ExternalInput")
    vtok = dt("vtok", [NK, 640], BF16, "ExternalInput")
    tab33_d = dt("tab33", [33, 8], F32, "ExternalInput")
    oh_swa = dt("oh_swa", [33, LS], F32, "ExternalInput")
    cnt_swa_d = dt("cnt_swa", [32, NAFF * 128], F32, "ExternalInput")
    sinks_d = dt("sinks", [1, 8], F32, "ExternalInput")
    wm_d = dt("wm", [128, S_, 2, 128], BF16, "ExternalInput")
    ident = dt("ident", [128, 128], F32, "ExternalInput")
    o_out = dt("o_attn", [NTOK, 1024], BF16, "ExternalOutput")
    cswa_scr = dt("cswa_scr", [8, LS], BF16, "Internal")

    P = Prog(nc)
    C = Consts(P, nc, ident[:, :])
    R = AttnRes(P, nS=3, nP=4)
    ps_misc = P.psum("ps_misc", [128, 512], F32); b_ps_misc = Buf()
    Ops = [(P.psum(f"O{i}", [128, 512], F32), Buf()) for i in range(2)]
    tab33 = P.sbuf("tab33", [33, 8], F32); b_tab33 = Buf()
    P.dma("sp", lambda e: e.dma_start(out=tab33[:], in_=tab33_d[:, :]), writes=[b_tab33])
    b_scr = build_ctab(P, nc, C, tab33, b_tab33, oh_swa, LS, "cswa", cswa_scr, ps_misc, b_ps_misc)
    exptab = P.sbuf("exptab", [32, 8], F32); b_exptab = Buf()
    P.op("act", lambda e: e.activation(out=exptab[:], in_=tab33[0:32, :], func=AF.Exp), reads=[b_tab33], writes=[b_exptab])
    cnts = P.sbuf("cnts", [32, NAFF * 128], F32); b_cnts = Buf()
    P.dma("sp", lambda e: e.dma_start(out=cnts[:], in_=cnt_swa_d[:, :]), writes=[b_cnts])
    zpad = P.sbuf("zpad", [128, NAFF, 8], F32); b_zpad = Buf()
    for a in range(NAFF):
        P.op("pe", lambda e, a=a: e.matmul(out=ps_misc[:, 0:8], lhsT=cnts[:, a * 128:(a + 1) * 128], rhs=exptab[:, :],
                                           start=True, stop=True), reads=[b_cnts, b_exptab], writes=[b_ps_misc])
        P.op("act", lambda e, a=a: e.copy(out=zpad[:, a, :], in_=ps_misc[:, 0:8]), reads=[b_ps_misc], writes=[b_zpad])
    esink = P.sbuf("esink", [128, 8], F32); b_esink = Buf()
    P.dma("sp", lambda e: e.dma_start(out=esink[:], in_=sinks_d[0:1, :].to_broadcast([128, 8])), writes=[b_esink])
    P.op("act", lambda e: e.activation(out=esink[:], in_=esink[:], func=AF.Exp), reads=[b_esink], writes=[b_esink])
    wm = P.sbuf("wm", [128, S_, 2, 128], BF16); b_wm = Buf()
    P.dma("sp", lambda e: e.dma_start(out=wm[:], in_=wm_d[:, :, :, :]), writes=[b_wm])
    Wswa = P.sbuf("Wswa", [128, S_ + 1, 4, 128], BF16); b_Wswa = Buf()
    QT = P.sbuf("QT", [128, 8, NTOK], BF16); b_QT = Buf()
    KV = P.sbuf("KV", [128, 33280], BF16); b_KV = Buf()
    KrT = P.sbuf("KrT", [64, NK], BF16); b_KrT = Buf()
    o_sb = P.sbuf("o_sb", [128, NT, 1024], BF16); b_osb = Buf()
    rz = P.sbuf("rz", [128, 4, 1], F32); b_rz = Buf()
    P.dma("sp", lambda e: e.dma_start(out=QT[:, 0:4, :], in_=qn_d[:, :, :]), writes=[b_QT])
    P.dma("sp", lambda e: e.dma_start(out=QT[0:64, 4:8, :], in_=qr_d[:, :, :]), writes=[b_QT])
    P.dma("sp", lambda e: e.dma_start(out=KrT[:], in_=kr_d[:, :]), writes=[b_KrT])
    KnT = KV[:, 0:2 * NK].rearrange("p (c k) -> p c k", c=2)
    Vm = KV[:, 2 * NK:2 * NK + NKT * 2 * 129].rearrange("p (k h d) -> p k h d", h=2, d=129)
    sc_mla = float(192 ** -0.5)
    for pp in range(2):
        for hh in range(2):
            P.dma("sp", lambda e, hh=hh, pp=pp: e.dma_start(out=KnT[:, hh, :], in_=kn_d[:, 2 * pp + hh, :]), writes=[b_KV])
        P.op("pool", lambda e: e.memset(Vm[:, :, :, 128:129], 1.0), writes=[b_KV])
        for hh in range(2):
            for k0 in range(0, NKT, 8):
                P.dma("sp", lambda e, hh=hh, pp=pp, k0=k0: e.dma_start(
                    out=Vm[:, k0:k0 + 8, hh, 0:128],
                    in_=vtok[k0 * 128:(k0 + 8) * 128, (2 * pp + hh) * 128:(2 * pp + hh + 1) * 128].rearrange(
                        "(kt p) d -> p kt d", p=128)), writes=[b_KV])
        for lt in range(NT):
            O, b_O = Ops[lt % 2]
            Ov = O[:].rearrange("p (h d) -> p h d", d=256)
            kts = list(range(0, min(S_ * lt + S_, NKT)))

            def qk_fn(kt, lt=lt, pp=pp):
                return [(hh * 128, (hh + 1) * 128,
                         [(KnT[:, hh, kt * 128:(kt + 1) * 128], QT[:, 2 * pp + hh, lt * 128:(lt + 1) * 128]),
                          (KrT[:, kt * 128:(kt + 1) * 128], QT[0:64, 4 + 2 * pp + hh, lt * 128:(lt + 1) * 128])],
                         [b_KV, b_QT, b_KrT]) for hh in range(2)]

            def extra_fn(kt, lt=lt):
                dl = S_ * lt - kt
                if dl <= 0:
                    return [(C.antib[:], wm[:, dl + S_ - 1, :, :].rearrange("p h q -> p (h q)"), [C.b_antib, b_wm])]
                return []
            attn_steps(P, R, kts, qk_fn, extra_fn, lambda kt, hh: (Vm[:, kt, hh, :], [b_KV]), Ov, b_O, sc_mla,
                       nv=129, nh=2)
            P.op("dve", lambda e, Ov=Ov: e.tensor_scalar(out=rz[:, 0:2, :], in0=Ov[:, :, 128:129], scalar1=1e-30, scalar2=None,
                                                         op0=ALU.max), reads=[b_O], writes=[b_rz])
            P.op("dve", lambda e: e.reciprocal(out=rz[:, 0:2, :], in_=rz[:, 0:2, :]), reads=[b_rz], writes=[b_rz])
            P.op("dve", lambda e, Ov=Ov, lt=lt, pp=pp: e.tensor_tensor(
                out=o_sb[:, lt, pp * 256:(pp + 1) * 256].rearrange("p (h d) -> p h d", d=128), in0=Ov[:, :, 0:128],
                in1=rz[:, 0:2, :].to_broadcast([128, 2, 128]), op=ALU.mult), reads=[b_O, b_rz], writes=[b_osb])
    P.dma("sp", lambda e: e.dma_start(out=QT[:], in_=qd_d[:, :, :]), writes=[b_QT])
    KdT = KV[:, 0:NK]
    Vd = KV[:, NK:NK + NKT * 65].rearrange("p (k d) -> p k d", d=65)
    for kv in range(2):
        P.dma("sp", lambda e, kv=kv: e.dma_start(out=KdT, in_=kd_d[:, kv, :]), writes=[b_KV])
        P.op("pool", lambda e: e.memset(Vd[:, :, 64:65], 1.0), writes=[b_KV])
        for k0 in range(0, NKT, 8):
            P.dma("sp", lambda e, kv=kv, k0=k0: e.dma_start(
                out=Vd[:, k0:k0 + 8, 0:64], in_=vtok[k0 * 128:(k0 + 8) * 128, 512 + kv * 64:512 + (kv + 1) * 64].rearrange(
                    "(kt p) d -> p kt d", p=128)), writes=[b_KV])
        toeplitz_load(P, Wswa, b_Wswa, cswa_scr, b_scr, LS, 4 * kv, S_ + 1)
        for lt in range(NT):
            O, b_O = Ops[lt % 2]
            Ov = O[:].rearrange("p (h d) -> p h d", d=128)
            kts = list(range(max(0, S_ * lt - 1), min(S_ * lt + S_, NKT)))

            def qk_fn(kt, lt=lt, kv=kv):
                return [(hh * 128, (hh + 1) * 128,
                         [(KdT[:, kt * 128:(kt + 1) * 128], QT[:, 4 * kv + hh, lt * 128:(lt + 1) * 128])],
                         [b_KV, b_QT]) for hh in range(4)]

            def extra_fn(kt, lt=lt):
                dl = S_ * lt - kt
                return [(C.antib[:], Wswa[:, dl + S_ - 1, :, :].rearrange("p h q -> p (h q)"), [C.b_antib, b_Wswa])]
            attn_steps(P, R, kts, qk_fn, extra_fn, lambda kt, hh: (Vd[:, kt, :], [b_KV]), Ov, b_O, 0.125)
            P.op("dve", lambda e, Ov=Ov, kv=kv: e.tensor_tensor(out=rz[:], in0=Ov[:, :, 64:65],
                                                                in1=esink[:, 4 * kv:4 * kv + 4].unsqueeze(2), op=ALU.add),
                 reads=[b_O, b_esink], writes=[b_rz])
            if lt < NAFF:
                P.op("dve", lambda e, lt=lt, kv=kv: e.tensor_tensor(out=rz[:], in0=rz[:],
                                                                    in1=zpad[:, lt, 4 * kv:4 * kv + 4].unsqueeze(2), op=ALU.add),
                     reads=[b_rz, b_zpad], writes=[b_rz])
            P.op("dve", lambda e: e.reciprocal(out=rz[:], in_=rz[:]), reads=[b_rz], writes=[b_rz])
            P.op("dve", lambda e, Ov=Ov, lt=lt, kv=kv: e.tensor_tensor(
                out=o_sb[:, lt, 512 + kv * 256:512 + (kv + 1) * 256].rearrange("p (h d) -> p h d", d=64),
                in0=Ov[:, :, 0:64], in1=rz[:].to_broadcast([128, 4, 64]), op=ALU.mult),
                reads=[b_O, b_rz], writes=[b_osb])
    outs = []
    for t in range(NT):
        outs.append(P.dma("sp", lambda e, t=t: e.dma_start(out=o_out[t * 128:(t + 1) * 128, :], in_=o_sb[:, t, :]),
                          reads=[b_osb]))
    P.wait_all("sp", outs)
    P.finish()
    return nc


from concourse.bass_utils import run_bass_kernel_spmd

_NT = 16
_STRIDE = 4
_PROGS = {}


def _prog(name, fn):
    if name not in _PROGS:
        _PROGS[name] = fn()
    return _PROGS[name]


def _own(a, j):
    sh = a.shape
    return np.ascontiguousarray(a.reshape((16, 4, 128) + sh[1:])[:, j].reshape((2048,) + sh[1:]))


def _gather_last(parts, blk=128):
    sh = parts[0].shape
    out = np.zeros(sh[:-1] + (64 * blk,), parts[0].dtype)
    o5 = out.reshape(sh[:-1] + (16, 4, blk))
    for r in range(4):
        o5[..., r, :] = parts[r].reshape(sh[:-1] + (16, blk))
    return out


def _gather_rows(parts):
    sh = parts[0].shape
    out = np.zeros((8192,) + sh[1:], parts[0].dtype)
    o5 = out.reshape((16, 4, 128) + sh[1:])
    for r in range(4):
        o5[:, r] = parts[r].reshape((16, 128) + sh[1:])
    return out


def _run(nc, ins):
    res = run_bass_kernel_spmd(nc, ins, core_ids=list(range(8)))
    return res.results


def kernel(x, c, rel_table, router_w, router_b, final_norm, norm_mix, norm_ffn, ada_w, ada_b,
           moe_w_gate, moe_w_up, moe_w_down, ev_w_in, ev_w_out, nsa_pos_k, nsa_pos_v,
           nsa_ck_w1, nsa_ck_w2, nsa_cv_w1, nsa_cv_w2, od_w_in, od_w_out, mla_q_norm,
           mla_kv_norm, mla_w_q_up, mla_w_kv_up, swa_sinks):
    f32 = lambda a: np.ascontiguousarray(np.asarray(a, dtype=np.float32))
    x = f32(x); c = f32(c)
    ident = np.eye(128, dtype=np.float32)
    tab33 = np.concatenate([f32(rel_table), np.ones((1, 8), np.float32)], 0)
    NT, ST = _NT, _STRIDE
    cores = [(cc // 4, cc % 4) for cc in range(8)]
    c_cols = [np.ascontiguousarray(c[b].reshape(8, 128).T) for b in range(2)]

    WF, WP, WT = host_w_in_even(f32(ev_w_in[0]))
    ins = [dict(x=_own(x[b], j), c_cols=c_cols[b], ada_w=f32(ada_w[0]), ada_b=f32(ada_b[0])[None],
                g_mix=f32(norm_mix[0])[None], wf=WF, wp=WP, wt=WT, ident=ident) for (b, j) in cores]
    r1 = _run(_prog("L1", lambda: build_L1(NT)), ins)
    posc = np.ascontiguousarray(np.stack([f32(nsa_pos_k[0]).reshape(16, 128).T, f32(nsa_pos_v[0]).reshape(16, 128).T], 1))
    cw1 = np.ascontiguousarray(np.stack([f32(nsa_ck_w1[0]), f32(nsa_cv_w1[0])], 0))
    cw2k = np.ascontiguousarray(np.concatenate([f32(nsa_ck_w2[0]), f32(nsa_ck_w2[0])], 1))
    G = {}
    for b in range(2):
        fm = [np.asarray(r1[4 * b + r]['o_fm']) for r in range(4)]
        G[b] = dict(kbT=_gather_last([f[:, 16:20] for f in fm]), ksT=_gather_last([f[:, 20:22] for f in fm]),
                    kwT=_gather_last([f[:, 22:24] for f in fm]),
                    kc2=_gather_last([np.asarray(r1[4 * b + r]['o_kc2']) for r in range(4)], blk=64),
                    vtok=_gather_rows([np.asarray(r1[4 * b + r]['o_vtok']) for r in range(4)]))
    ins = []
    for cc, (b, j) in enumerate(cores):
        st = attn0_static(NT, ST, j)
        ins.append(dict(qT=np.ascontiguousarray(np.asarray(r1[cc]['o_fm'])[:, 0:16]), gates=np.asarray(r1[cc]['o_gates']),
                        tab33=tab33, posc=posc, cw1=cw1, cw2k=cw2k, cw2v=f32(nsa_cv_w2[0]), ident=ident, **G[b], **st))
    ra = _run(_prog("A0", lambda: build_attn0(NT, ST, 64)), ins)
    oh16 = oh16_static()
    ins = [dict(o_attn=np.asarray(ra[cc]['o_attn']), x=_own(x[b], j), mod=np.asarray(r1[cc]['o_mod']),
                w_out=f32(ev_w_out[0]), g_ffn=f32(norm_ffn[0])[None], g_fin=f32(final_norm)[None],
                router_w=f32(router_w), router_b=f32(router_b)[None], wg=f32(moe_w_gate[0]), wu=f32(moe_w_up[0]),
                wd=f32(moe_w_down[0]), oh16=oh16, ident=ident) for cc, (b, j) in enumerate(cores)]
    rp0 = _run(_prog("P0", lambda: build_post(NT, final=False)), ins)
    hw = host_w_in_odd(f32(od_w_in[0]), f32(mla_w_q_up[0]), f32(mla_w_kv_up[0]))
    ins = []
    for cc, (b, j) in enumerate(cores):
        pos = (np.arange(64).reshape(16, 4)[:, j][:, None] * 128 + np.arange(128)[None, :]).reshape(-1)
        cos, sin = rope_static(pos)
        ins.append(dict(x=np.asarray(rp0[cc]['x_out']), c_cols=c_cols[b], ada_w=f32(ada_w[1]), ada_b=f32(ada_b[1])[None],
                        g_mix=f32(norm_mix[1])[None], q_norm=f32(mla_q_norm), kv_norm=f32(mla_kv_norm), cos=cos, sin=sin,
                        ident=ident, **hw))
    r2 = _run(_prog("L1o", lambda: build_L1odd(NT)), ins)
    G = {}
    for b in range(2):
        G[b] = dict(kn=_gather_last([np.asarray(r2[4 * b + r]['o_kn']) for r in range(4)]),
                    kr=_gather_last([np.asarray(r2[4 * b + r]['o_kr']) for r in range(4)]),
                    kd=_gather_last([np.asarray(r2[4 * b + r]['o_fm'])[:, 8:10] for r in range(4)]),
                    vtok=_gather_rows([np.asarray(r2[4 * b + r]['o_vtok']) for r in range(4)]))
    ins = []
    for cc, (b, j) in enumerate(cores):
        st = attn1_static(NT, ST, j)
        ins.append(dict(qn=np.asarray(r2[cc]['o_qn']), qr=np.asarray(r2[cc]['o_qr']),
                        qd=np.ascontiguousarray(np.asarray(r2[cc]['o_fm'])[:, 0:8]), tab33=tab33, sinks=f32(swa_sinks),
                        ident=ident, **G[b], **st))
    rb = _run(_prog("A1", lambda: build_attn1(NT, ST, 64)), ins)
    ins = [dict(o_attn=np.asarray(rb[cc]['o_attn']), x=np.asarray(rp0[cc]['x_out']), mod=np.asarray(r2[cc]['o_mod']),
                w_out=f32(od_w_out[0]), g_ffn=f32(norm_ffn[1])[None], g_fin=f32(final_norm)[None],
                router_w=f32(router_w), router_b=f32(router_b)[None], wg=f32(moe_w_gate[1]), wu=f32(moe_w_up[1]),
                wd=f32(moe_w_down[1]), oh16=oh16, ident=ident) for cc, (b, j) in enumerate(cores)]
    rp1 = _run(_prog("P1", lambda: build_post(NT, final=True)), ins)
    out = np.zeros((2, 8192, 1024), np.float32)
    o6 = out.reshape(2, 16, 4, 128, 1024)
    for cc, (b, j) in enumerate(cores):
        o6[b, :, j] = np.asarray(rp1[cc]['x_out']).reshape(16, 128, 1024)
    return out
```

```python
import numpy as np
import concourse.bass as bass
import concourse.mybir as mybir

F32 = mybir.dt.float32
BF16 = mybir.dt.bfloat16
I32 = mybir.dt.int32
U32 = mybir.dt.uint32
AF = mybir.ActivationFunctionType
ALU = mybir.AluOpType
AX = mybir.AxisListType


class Buf:
    __slots__ = ("name", "w", "r")

    def __init__(self, name=""):
        self.name = name
        self.w = None
        self.r = {}


class Prog:
    COMPUTE = ("pe", "act", "dve", "pool")
    DMAQ = ("sp", "pool")

    def __init__(self, nc, n_dma_sems=24, same_engine_sync=True):
        import os
        same_engine_sync = bool(int(os.environ.get('SES', '1' if same_engine_sync else '0')))
        self.nc = nc
        self.q = {e: [] for e in ("pe", "act", "dve", "pool", "sp")}
        self.eng_obj = {"pe": nc.tensor, "act": nc.scalar, "dve": nc.vector,
                        "pool": nc.gpsimd, "sp": nc.sync}
        self.sems = {}
        self.cnt = {}
        self.seen = {e: {} for e in self.q}
        self.same_engine_sync = same_engine_sync
        self._ctx = []
        for e in self.COMPUTE:
            self.sems[e] = self._sem("s_" + e)
            self.cnt[e] = 0
        self.dma_pool = {}
        for e in ("sp", "pool"):
            self.dma_pool[e] = [[self._sem(f"d_{e}{i}"), 0, None] for i in range(n_dma_sems)]
        self.dma_rr = {"sp": 0, "pool": 0}
        self.pending_noinc = {e: False for e in self.COMPUTE}

    def _sem(self, name):
        g = self.nc.semaphore(name)
        s = g.__enter__()
        self._ctx.append(g)
        return s

    def sbuf(self, name, shape, dt):
        g = self.nc.sbuf_tensor("sb_" + name, list(shape), dt)
        t = g.__enter__()
        self._ctx.append(g)
        return t

    def psum(self, name, shape, dt):
        g = self.nc.psum_tensor("ps_" + name, list(shape), dt)
        t = g.__enter__()
        self._ctx.append(g)
        return t

    def _collect(self, eng, reads, writes):
        deps = {}

        def add(tok):
            if tok is None:
                return
            s, v, owner = tok
            k = id(s)
            if k not in deps or deps[k][1] < v:
                deps[k] = (s, v, owner)

        for b in reads:
            add(b.w)
        for b in writes:
            add(b.w)
            for t in b.r.values():
                add(t)
        waits = []
        for k, (s, v, owner) in deps.items():
            if owner == eng and owner in self.COMPUTE:
                if eng == "pe" or not self.same_engine_sync:
                    continue
                if v > self.cnt[eng]:
                    continue
            if self.seen[eng].get(k, -1) >= v:
                continue
            self.seen[eng][k] = v
            waits.append((s, v))
        return waits

    def _mark(self, tok, reads, writes):
        k = id(tok[0])
        for b in reads:
            old = b.r.get(k)
            if old is None or old[1] < tok[1]:
                b.r[k] = tok
        for b in writes:
            b.w = tok
            b.r = {}

    def op(self, eng, fn, reads=(), writes=(), inc=True):
        assert eng in self.COMPUTE
        waits = self._collect(eng, reads, writes)
        if inc:
            self.cnt[eng] += 1
            tok = (self.sems[eng], self.cnt[eng], eng)
            self.pending_noinc[eng] = False
        else:
            tok = (self.sems[eng], self.cnt[eng] + 1, eng)
            self.pending_noinc[eng] = True
        self.q[eng].append((waits, fn, (self.sems[eng], 1) if inc else None))
        self._mark(tok, reads, writes)
        return tok

    def dma(self, eng, fn, reads=(), writes=()):
        pool = self.dma_pool[eng]
        i = self.dma_rr[eng]
        self.dma_rr[eng] = (i + 1) % len(pool)
        ent = pool[i]
        waits = self._collect(eng, reads, writes)
        if ent[2] is not None:
            s, v, _ = ent[2]
            k = id(s)
            if self.seen[eng].get(k, -1) < v:
                self.seen[eng][k] = v
                waits.append((s, v))
        ent[1] += 16
        tok = (ent[0], ent[1], "dma_" + eng)
        ent[2] = tok
        self.q[eng].append((waits, fn, (ent[0], 16)))
        self._mark(tok, reads, writes)
        return tok

    def wait_all(self, eng, toks):
        waits = []
        for tok in toks:
            s, v, _ = tok
            waits.append((s, v))
        self.q[eng].append((waits, None, None))

    def finish(self):
        nc = self.nc
        for e in self.COMPUTE:
            assert not self.pending_noinc[e], f"engine {e} ends with non-inc instruction"
        with nc.Block() as block:
            def run(engname):
                def body(e):
                    for waits, fn, inc in self.q[engname]:
                        for s, v in waits:
                            e.wait_ge(s, v)
                        if fn is not None:
                            ins = fn(e)
                            if inc is not None:
                                ins.then_inc(inc[0], inc[1])
                return body
            if self.q["sp"]:
                block.sync(run("sp"))
            if self.q["pe"]:
                block.tensor(run("pe"))
            if self.q["act"]:
                block.scalar(run("act"))
            if self.q["dve"]:
                block.vector(run("dve"))
            if self.q["pool"]:
                block.gpsimd(run("pool"))
        for g in reversed(self._ctx):
            g.__exit__(None, None, None)
        self._ctx = []


import numpy as np
import ml_dtypes

NPBF = ml_dtypes.bfloat16
D = 1024
S = 8192
NEGM = -30000.0


class RR:
    def __init__(self, engs=("act", "dve")):
        self.engs = engs
        self.i = 0

    def next(self):
        e = self.engs[self.i % len(self.engs)]
        self.i += 1
        return e


def evac(P, eng, out, in_, reads, writes):
    if eng == "act":
        return P.op("act", lambda e: e.copy(out=out, in_=in_), reads=reads, writes=writes)
    return P.op(eng, lambda e: e.tensor_copy(out=out, in_=in_), reads=reads, writes=writes)


class Consts:
    def __init__(self, P, nc, ident_ap):
        self.idf = P.sbuf("c_idf", [128, 128], F32)
        self.idb = P.sbuf("c_idb", [128, 128], BF16)
        self.b_idf = Buf("idf")
        self.b_idb = Buf("idb")
        P.dma("sp", lambda e: e.dma_start(out=self.idf[:], in_=ident_ap), writes=[self.b_idf])
        P.op("dve", lambda e: e.tensor_copy(out=self.idb[:], in_=self.idf[:]),
             reads=[self.b_idf], writes=[self.b_idb])
        self.antib = P.sbuf("c_antib", [128, 128], BF16)
        self.b_antib = Buf("antib")
        P.op("pool", lambda e: e.memset(self.antib[:], 0.0), writes=[self.b_antib])
        P.op("pool", lambda e: e.affine_select(out=self.antib[:], in_=self.antib[:], pattern=[[1, 128]],
                                               compare_op=ALU.not_equal, fill=1.0, base=-127, channel_multiplier=1),
             reads=[self.b_antib], writes=[self.b_antib])
        self.ones_f = P.sbuf("c_ones_f", [128, 128], F32)
        self.b_ones_f = Buf("ones_f")
        P.op("dve", lambda e: e.memset(self.ones_f[:], 1.0), writes=[self.b_ones_f])


def emit_adaln(P, nc, C, c_cols_ap, ada_w_ap, ada_b_ap, tag, psum_row, b_psum_row, psum_bc, b_psum_bc):
    GW = 256
    NG = 6144 // GW
    cc = P.sbuf(f"ada_c{tag}", [128, 8], F32); b_cc = Buf()
    sc = P.sbuf(f"ada_sc{tag}", [128, 8], F32); b_sc = Buf()
    row = [P.sbuf(f"ada_row{tag}{i}", [1, GW], F32) for i in range(2)]; b_row = [Buf(), Buf()]
    mod = P.sbuf(f"ada_mod{tag}", [128, 6, 1024], F32); b_mod = Buf()
    modf = mod[:].rearrange("p a n -> p (a n)")
    wst = [P.sbuf(f"ada_w{tag}_{i}", [128, 8, GW], F32) for i in range(2)]
    b_wst = [Buf(), Buf()]
    P.dma("sp", lambda e: e.dma_start(out=cc[:], in_=c_cols_ap), writes=[b_cc])
    P.dma("sp", lambda e: e.dma_start(out=modf, in_=ada_b_ap.to_broadcast([128, 6144])), writes=[b_mod])
    P.op("act", lambda e: e.activation(out=sc[:], in_=cc[:], func=AF.Silu), reads=[b_cc], writes=[b_sc])
    wv = ada_w_ap.rearrange("(k p) n -> p k n", p=128)
    for g in range(NG):
        w = wst[g % 2]; bw = b_wst[g % 2]
        r = row[g % 2]; br = b_row[g % 2]
        P.dma("sp", lambda e, w=w, g=g: e.dma_start(out=w[:], in_=wv[:, :, g * GW:(g + 1) * GW]), writes=[bw])
        for k in range(8):
            P.op("pe", lambda e, w=w, k=k: e.matmul(out=psum_row[0:1, 0:GW], lhsT=sc[:, k:k + 1], rhs=w[:, k, :],
                                                    start=(k == 0), stop=(k == 7)),
                 reads=[b_sc, bw], writes=[b_psum_row], inc=(k == 7))
        P.op("act", lambda e, r=r: e.copy(out=r[0:1, :], in_=psum_row[0:1, 0:GW]),
             reads=[b_psum_row], writes=[br])
        P.op("pe", lambda e, r=r: e.matmul(out=psum_bc[:, 0:GW], lhsT=C.ones_f[0:1, :], rhs=r[0:1, :],
                                           start=True, stop=True),
             reads=[C.b_ones_f, br], writes=[b_psum_bc])
        P.op("dve", lambda e, g=g: e.tensor_tensor(out=modf[:, g * GW:(g + 1) * GW], in0=psum_bc[:, 0:GW],
                                                   in1=modf[:, g * GW:(g + 1) * GW], op=ALU.add),
             reads=[b_psum_bc, b_mod], writes=[b_mod])
    return mod, b_mod


def emit_norm_tile(P, C, x_t, b_x, A, b_A, Bt, b_B, hT_out, b_hT, scratch, psum_tr, b_psum_tr, tag=""):
    sq, ss, rstd, h32, hb = scratch["sq"], scratch["ss"], scratch["rstd"], scratch["h32"], scratch["hb"]
    b_sq, b_ss, b_rstd, b_h32, b_hb = scratch["b"]
    P.op("act", lambda e: e.activation(out=sq[:], in_=x_t, func=AF.Square, accum_out=ss[:]),
         reads=[b_x], writes=[b_sq, b_ss])
    P.op("dve", lambda e: e.tensor_scalar(out=rstd[:], in0=ss[:], scalar1=1.0 / D, scalar2=1e-6,
                                          op0=ALU.mult, op1=ALU.add), reads=[b_ss], writes=[b_rstd])
    P.op("act", lambda e: e.activation(out=rstd[:], in_=rstd[:], func=AF.Sqrt), reads=[b_rstd], writes=[b_rstd])
    P.op("dve", lambda e: e.reciprocal(out=rstd[:], in_=rstd[:]), reads=[b_rstd], writes=[b_rstd])
    P.op("dve", lambda e: e.scalar_tensor_tensor(out=h32[:], in0=x_t, scalar=rstd[:, 0:1], in1=A,
                                                 op0=ALU.mult, op1=ALU.mult),
         reads=[b_x, b_rstd, b_A], writes=[b_h32])
    P.op("pool", lambda e: e.tensor_tensor(out=hb[:], in0=h32[:], in1=Bt, op=ALU.add),
         reads=[b_h32, b_B], writes=[b_hb])
    for k in range(8):
        P.op("pe", lambda e, k=k: e.transpose(out=psum_tr[:, k, :], in_=hb[:, k * 128:(k + 1) * 128],
                                              identity=C.idb[:]),
             reads=[b_hb, C.b_idb], writes=[b_psum_tr], inc=(k == 7))
    P.op("act", lambda e: e.copy(out=hT_out, in_=psum_tr[:]), reads=[b_psum_tr], writes=[b_hT])


EV = dict(q_a=(0, 512), kc=(512, 640), vc=(640, 768), ks=(768, 896), vs=(896, 1024), kw=(1024, 1152),
          vw=(1152, 1280), gates=(1280, 1304), q_b=(1304, 1816), k_b=(1816, 2328), v_b=(2328, 2840))


def host_w_in_even(w):
    sl = lambda n: w[:, EV[n][0]:EV[n][1]]
    units = []
    for nm in ("q_a", "q_b"):
        for h in range(8):
            u = np.zeros((1024, 128), np.float32)
            u[:, (h % 2) * 64:(h % 2 + 1) * 64] = sl(nm)[:, h * 64:(h + 1) * 64]
            units.append(u)
    for cc in range(4):
        units.append(sl("k_b")[:, cc * 128:(cc + 1) * 128])
    for nm in ("ks", "kw"):
        for kv in range(2):
            c = sl(nm)[:, kv * 64:(kv + 1) * 64]
            units.append(np.concatenate([c, c], axis=1))
    WF = np.concatenate(units, axis=1)
    WP = np.zeros((1024, 4, 2, 128), np.float32)
    for X, (nm, kv) in enumerate([("kc", 0), ("kc", 1), ("vc", 0), ("vc", 1)]):
        cols = sl(nm)[:, kv * 64:(kv + 1) * 64]
        WP[:, X, 0, 0:64] = cols
        WP[:, X, 1, 64:128] = cols
    WT = np.concatenate([sl("vs"), sl("vw"), sl("gates"), sl("v_b")], axis=1)
    return np.ascontiguousarray(WF), np.ascontiguousarray(WP.reshape(1024, 1024)), np.ascontiguousarray(WT)


NU0 = 24
def load_w_bf16(P, nc, name, ap2d, ncols, rows=1024):
    kc = rows // 128
    t = P.sbuf(name, [128, kc, ncols], BF16)
    b = Buf(name)
    v = ap2d.rearrange("(k p) n -> p k n", p=128)
    for k in range(kc):
        P.dma("pool", lambda e, k=k: e.dma_start(out=t[:, k, :], in_=v[:, k, :]), writes=[b])
    return t, b


def norm_scratch(P, tag, hb=None):
    sc = dict(sq=P.sbuf(f"n_sq{tag}", [128, 1024], F32), ss=P.sbuf(f"n_ss{tag}", [128, 1], F32),
              rstd=P.sbuf(f"n_rstd{tag}", [128, 1], F32), h32=P.sbuf(f"n_h32{tag}", [128, 1024], F32),
              hb=hb if hb is not None else P.sbuf(f"n_hb{tag}", [128, 1024], BF16))
    sc["b"] = [Buf() for _ in range(5)]
    return sc


def build_L1(NT=16):
    nc = bass.Bass("TRN2", target_bir_lowering=False)
    NTOK = NT * 128
    dt = lambda n, s, d, k: nc.dram_tensor(n, s, d, kind=k).ap()
    x = dt("x", [NTOK, D], F32, "ExternalInput")
    c_cols = dt("c_cols", [128, 8], F32, "ExternalInput")
    ada_w = dt("ada_w", [D, 6 * D], F32, "ExternalInput")
    ada_b = dt("ada_b", [1, 6 * D], F32, "ExternalInput")
    g_mix = dt("g_mix", [1, D], F32, "ExternalInput")
    wf_d = dt("wf", [D, NU0 * 128], F32, "ExternalInput")
    wp_d = dt("wp", [D, 1024], F32, "ExternalInput")
    wt_d = dt("wt", [D, 792], F32, "ExternalInput")
    ident = dt("ident", [128, 128], F32, "ExternalInput")
    o_fm = dt("o_fm", [128, NU0, NTOK], BF16, "ExternalOutput")
    o_kc2 = dt("o_kc2", [128, 4, NTOK // 2], BF16, "ExternalOutput")
    o_vtok = dt("o_vtok", [NTOK, 768], BF16, "ExternalOutput")
    o_gates = dt("o_gates", [NTOK, 24], F32, "ExternalOutput")
    o_mod = dt("o_mod", [6, D], F32, "ExternalOutput")

    P = Prog(nc)
    C = Consts(P, nc, ident[:, :])
    ps_row = P.psum("ps_row", [1, 512], F32); b_ps_row = Buf()
    ps_bc = P.psum("ps_bc", [128, 512], F32); b_ps_bc = Buf()
    ps_tr = P.psum("ps_tr", [128, 8, 128], BF16); b_ps_tr = Buf()
    ps_mm = [P.psum(f"ps_mm{i}", [128, 512], F32) for i in range(4)]
    b_ps_mm = [Buf() for _ in range(4)]

    mod, b_mod = emit_adaln(P, nc, C, c_cols[:, :], ada_w, ada_b[:, :], "0", ps_row, b_ps_row, ps_bc, b_ps_bc)
    outs = []
    outs.append(P.dma("sp", lambda e: e.dma_start(out=o_mod[:, :], in_=mod[0:1, :, :]), reads=[b_mod]))
    gm = P.sbuf("gm", [128, 1024], F32); b_gm = Buf()
    A = P.sbuf("A_m", [128, 1024], F32); b_A = Buf()
    P.dma("sp", lambda e: e.dma_start(out=gm[:], in_=g_mix[0:1, :].to_broadcast([128, 1024])), writes=[b_gm])
    P.op("dve", lambda e: e.scalar_tensor_tensor(out=A[:], in0=mod[:, 1, :], scalar=1.0, in1=gm[:],
                                                 op0=ALU.add, op1=ALU.mult), reads=[b_mod, b_gm], writes=[b_A])
    Bt = mod[:, 0, :]
    wf, b_wf = load_w_bf16(P, nc, "wf_sb", wf_d, NU0 * 128)
    wp, b_wp = load_w_bf16(P, nc, "wp_sb", wp_d, 1024)
    wt, b_wt = load_w_bf16(P, nc, "wt_sb", wt_d, 792)
    xt = [P.sbuf(f"xt{i}", [128, 1024], F32) for i in range(2)]
    b_xt = [Buf(), Buf()]
    hT = [P.sbuf(f"hT{i}", [128, 8, 512], BF16) for i in range(2)]
    b_hT = [Buf(), Buf()]
    nsc = norm_scratch(P, "a")
    stg = [P.sbuf(f"stg{i}", [128, 512], BF16) for i in range(4)]
    b_stg = [Buf() for _ in range(4)]
    gst = [P.sbuf(f"gst{i}", [128, 24], F32) for i in range(2)]
    b_gst = [Buf(), Buf()]
    rr = RR()
    si = 0
    mi = 0
    for tg in range(NT // 4):
        h = hT[tg % 2]; bh = b_hT[tg % 2]
        for tt in range(4):
            t = tg * 4 + tt
            xb = xt[t % 2]; bx = b_xt[t % 2]
            P.dma("sp", lambda e, xb=xb, t=t: e.dma_start(out=xb[:], in_=x[t * 128:(t + 1) * 128, :]), writes=[bx])
            emit_norm_tile(P, C, xb[:], bx, A[:], b_A, Bt, b_mod, h[:, :, tt * 128:(tt + 1) * 128], bh,
                           nsc, ps_tr, b_ps_tr)
        for u in range(NU0):
            ps = ps_mm[mi % 4]; bps = b_ps_mm[mi % 4]; mi += 1
            for k in range(8):
                P.op("pe", lambda e, ps=ps, u=u, k=k, h=h: e.matmul(out=ps[:], lhsT=wf[:, k, u * 128:(u + 1) * 128],
                                                                  rhs=h[:, k, :], start=(k == 0), stop=(k == 7)),
                     reads=[b_wf, bh], writes=[bps], inc=(k == 7))
            st = stg[si % 4]; bst = b_stg[si % 4]; si += 1
            evac(P, rr.next(), st[:], ps[:], [bps], [bst])
            outs.append(P.dma("sp", lambda e, st=st, u=u, tg=tg: e.dma_start(
                out=o_fm[:, u, tg * 512:(tg + 1) * 512], in_=st[:]), reads=[bst]))
        for X in range(4):
            ps = ps_mm[mi % 4]; bps = b_ps_mm[mi % 4]; mi += 1
            n = 0
            for lo in range(2):
                for k in range(8):
                    P.op("pe", lambda e, ps=ps, X=X, lo=lo, k=k, h=h, n=n: e.matmul(
                        out=ps[:, 0:256], lhsT=wp[:, k, (X * 2 + lo) * 128:(X * 2 + lo + 1) * 128],
                        rhs=h[:, k, lo:512:2], start=(n == 0), stop=(n == 15)),
                        reads=[b_wp, bh], writes=[bps], inc=(n == 15))
                    n += 1
            st = stg[si % 4]; bst = b_stg[si % 4]; si += 1
            evac(P, rr.next(), st[:, 0:256], ps[:, 0:256], [bps], [bst])
            outs.append(P.dma("sp", lambda e, st=st, X=X, tg=tg: e.dma_start(
                out=o_kc2[:, X, tg * 256:(tg + 1) * 256], in_=st[:, 0:256]), reads=[bst]))
        for tt in range(4):
            t = tg * 4 + tt
            for grp, (c0, c1) in enumerate([(0, 280), (280, 792)]):
                ps = ps_mm[mi % 4]; bps = b_ps_mm[mi % 4]; mi += 1
                w_ = c1 - c0
                for k in range(8):
                    P.op("pe", lambda e, ps=ps, k=k, h=h, tt=tt, c0=c0, c1=c1, w_=w_: e.matmul(
                        out=ps[:, 0:w_], lhsT=h[:, k, tt * 128:(tt + 1) * 128], rhs=wt[:, k, c0:c1],
                        start=(k == 0), stop=(k == 7)), reads=[b_wt, bh], writes=[bps], inc=(k == 7))
                st = stg[si % 4]; bst = b_stg[si % 4]; si += 1
                if grp == 0:
                    evac(P, rr.next(), st[:, 0:256], ps[:, 0:256], [bps], [bst])
                    outs.append(P.dma("sp", lambda e, st=st, t=t: e.dma_start(
                        out=o_vtok[t * 128:(t + 1) * 128, 0:256], in_=st[:, 0:256]), reads=[bst]))
                    g = gst[t % 2]; bg = b_gst[t % 2]
                    P.op("act", lambda e, g=g, ps=ps: e.activation(out=g[:], in_=ps[:, 256:280], func=AF.Sigmoid),
                         reads=[bps], writes=[bg])
                    outs.append(P.dma("sp", lambda e, g=g, t=t: e.dma_start(
                        out=o_gates[t * 128:(t + 1) * 128, :], in_=g[:]), reads=[bg]))
                else:
                    evac(P, rr.next(), st[:], ps[:], [bps], [bst])
                    outs.append(P.dma("sp", lambda e, st=st, t=t: e.dma_start(
                        out=o_vtok[t * 128:(t + 1) * 128, 256:768], in_=st[:]), reads=[bst]))
    P.wait_all("sp", outs)
    P.finish()
    return nc


def rel_bucket_np(d):
    d = np.maximum(d, 0)
    lp = 16 + (np.log(np.maximum(d, 1).astype(np.float32) / 16) / np.float32(np.log(1024 / 16)) * 16).astype(np.int32)
    return np.where(d < 16, d, np.minimum(lp, 31))


def onehot_table(dvals, mode, win=None):
    L = len(dvals)
    oh = np.zeros((33, L), np.float32)
    ok = dvals >= 0
    if win is not None:
        ok &= dvals < win
    b = rel_bucket_np(dvals)
    idx = np.nonzero(ok)[0]
    oh[b[idx], idx] += 8.0
    if mode == "rel":
        oh[31, idx] -= 8.0
    oh[32, ~ok] = NEGM
    return oh


def build_ctab(P, nc, C, tab33, b_tab33, oh_dram, L, name, scratch_dram, ps, b_ps):
    b_scr = Buf(name + "_scr")
    oh = P.sbuf(name + "_oh", [33, 512], F32); b_oh = Buf()
    cb = P.sbuf(name + "_cb", [8, 512], BF16); b_cb = Buf()
    for c0 in range(0, L, 512):
        w = min(512, L - c0)
        P.dma("sp", lambda e, c0=c0, w=w: e.dma_start(out=oh[:, 0:w], in_=oh_dram[:, c0:c0 + w]), writes=[b_oh])
        P.op("pe", lambda e, w=w: e.matmul(out=ps[0:8, 0:w], lhsT=tab33[:, :], rhs=oh[:, 0:w], start=True, stop=True),
             reads=[b_tab33, b_oh], writes=[b_ps])
        P.op("act", lambda e, w=w: e.copy(out=cb[:, 0:w], in_=ps[0:8, 0:w]), reads=[b_ps], writes=[b_cb])
        P.dma("sp", lambda e, c0=c0, w=w: e.dma_start(out=scratch_dram[:, c0:c0 + w], in_=cb[:, 0:w]),
              reads=[b_cb], writes=[b_scr])
    return b_scr


def toeplitz_load(P, Wt, b_W, scratch_dram, b_scr, L, h0, nR, rstride=128, pstride=1, base=0, nh=4, width=128):
    from concourse.bass_types import AP
    for hh in range(nh):
        src = AP(scratch_dram.tensor, scratch_dram.offset + (h0 + hh) * L + base,
                 [[pstride, 128], [rstride, nR], [1, width]])
        P.dma("sp", lambda e, hh=hh, src=src: e.dma_start(out=Wt[:, :, hh, :], in_=src),
              reads=[b_scr], writes=[b_W])


class AttnRes:
    def __init__(self, P, nS=2, nP=3):
        self.S = [(P.psum(f"at_S{i}", [128, 512], F32), Buf()) for i in range(nS)]
        self.Pt = [(P.sbuf(f"at_P{i}", [128, 512], BF16), Buf()) for i in range(nP)]
        self.si = 0
        self.pi = 0

    def nextS(self):
        r = self.S[self.si % len(self.S)]; self.si += 1
        return r

    def nextP(self):
        r = self.Pt[self.pi % len(self.Pt)]; self.pi += 1
        return r


def attn_steps(P, R, kts, qk_fn, extra_fn, v_fn, O, b_O, scale, nv=65, post_fn=None, bias_ap=None, b_bias=None, nh=4):
    n = len(kts)

    def emit_qk(kt):
        S, b_S = R.nextS()
        ex = extra_fn(kt) if extra_fn else []
        qk = qk_fn(kt)
        for qi, (c0, c1, mms, reads) in enumerate(qk):
            for mi, (lhsT, rhs) in enumerate(mms):
                last = (not ex) and qi == len(qk) - 1 and mi == len(mms) - 1
                P.op("pe", lambda e, S=S, c0=c0, c1=c1, lhsT=lhsT, rhs=rhs, mi=mi, qi=qi, last=last: e.matmul(
                    out=S[:, c0:c1], lhsT=lhsT, rhs=rhs, start=(mi == 0 and qi == 0), stop=last,
                    skip_group_check=True),
                    reads=reads, writes=[b_S], inc=last)
        for xi, (lhsT, rhs, reads) in enumerate(ex):
            P.op("pe", lambda e, S=S, lhsT=lhsT, rhs=rhs, xi=xi, nx=len(ex): e.matmul(
                out=S[:, 0:nh * 128], lhsT=lhsT, rhs=rhs, start=False, stop=(xi == nx - 1), skip_group_check=True),
                reads=reads, writes=[b_S], inc=(xi == len(ex) - 1))
        Pt, b_P = R.nextP()
        if bias_ap is None:
            P.op("act", lambda e, S=S, Pt=Pt: e.activation(out=Pt[:, 0:nh * 128], in_=S[:, 0:nh * 128], func=AF.Exp, scale=scale),
                 reads=[b_S], writes=[b_P])
        else:
            P.op("act", lambda e, S=S, Pt=Pt: e.activation(out=Pt[:, 0:nh * 128], in_=S[:, 0:nh * 128], func=AF.Exp, scale=scale,
                                                           bias=bias_ap), reads=[b_S, b_bias], writes=[b_P])
        return Pt, b_P

    def emit_pv(ii, kt, Pt, b_P):
        for hh in range(nh):
            rhs, reads = v_fn(kt, hh)
            P.op("pe", lambda e, Pt=Pt, hh=hh, rhs=rhs, ii=ii: e.matmul(
                out=O[:, hh, 0:nv], lhsT=Pt[:, hh * 128:(hh + 1) * 128], rhs=rhs, start=(ii == 0 and hh == 0),
                stop=(ii == n - 1 and hh == nh - 1), skip_group_check=True),
                reads=[b_P] + reads, writes=[b_O], inc=(hh == nh - 1 and post_fn is None))
        if post_fn is not None:
            post_fn(kt, ii, n, Pt, b_P)

    LA = max(1, min(len(R.S), len(R.Pt)) - 1)
    queue = []
    for ii, kt in enumerate(kts):
        cur = emit_qk(kt)
        queue.append((ii, kt, cur[0], cur[1]))
        if len(queue) > LA:
            emit_pv(*queue.pop(0))
    while queue:
        emit_pv(*queue.pop(0))


def moba_static(NT, STRIDE, j):
    LC = 11 * 128 + 127
    y = np.arange(LC)
    d = y - 127 - 384 + 128 * j
    oh_rel = onehot_table(d, "rel")
    ohsel = np.zeros((32, 32, 128), np.float32)
    for n in range(32):
        ohsel[n, n, :] = 1.0
    negvalid = np.zeros((NT, 32), np.float32)
    own1h = np.zeros((NT, 32), np.float32)
    for lt in range(NT):
        own = (STRIDE * lt + j) // 2
        negvalid[lt, own:] = -1e30
        own1h[lt, own] = 1.0
    return dict(oh_rel=oh_rel, ohsel=ohsel.reshape(32, 4096).astype(NPBF), negvalid=negvalid.reshape(1, -1),
                own1h=own1h.reshape(1, -1))


def gelu_tanh_ops(P, u, b_u, t, b_t, out_bf, b_out, width):
    P.op("dve", lambda e: e.tensor_tensor(out=t[:, 0:width], in0=u[:, 0:width], in1=u[:, 0:width], op=ALU.mult),
         reads=[b_u], writes=[b_t])
    P.op("dve", lambda e: e.tensor_scalar(out=t[:, 0:width], in0=t[:, 0:width], scalar1=0.044715, scalar2=1.0,
                                          op0=ALU.mult, op1=ALU.add), reads=[b_t], writes=[b_t])
    P.op("dve", lambda e: e.tensor_tensor(out=t[:, 0:width], in0=t[:, 0:width], in1=u[:, 0:width], op=ALU.mult),
         reads=[b_t, b_u], writes=[b_t])
    P.op("act", lambda e: e.activation(out=t[:, 0:width], in_=t[:, 0:width], func=AF.Sigmoid, scale=1.5957691216057308),
         reads=[b_t], writes=[b_t])
    P.op("dve", lambda e: e.tensor_tensor(out=out_bf, in0=t[:, 0:width], in1=u[:, 0:width], op=ALU.mult),
         reads=[b_t, b_u], writes=[b_out])


def attn0_static(NT, STRIDE, j, NKT=64):
    st = moba_static(NT, STRIDE, j)
    LW = 8 * 128 + 127
    y = np.arange(LW)
    st["oh_win"] = onehot_table(y - 127 - 384 + 128 * j, "abs", win=512)
    NRC = -(-2853 // (128 * STRIDE))
    LCM = 128 * STRIDE * (NRC - 1) + 127 + 16 * 127 + 1
    y = np.arange(LCM)
    st["oh_cmp"] = onehot_table(y + 128 * j - 2063, "abs")
    n = np.arange(512)
    cs = n * 16
    ce = cs + 31
    m = np.arange(128) * 64
    ov = ((cs[:, None] <= m[None, :] + 63) & (ce[:, None] >= m[None, :])).astype(np.float32)
    ov[511] = 0
    st["overlap"] = ov.reshape(4, 128, 128).transpose(1, 0, 2).reshape(128, 512).astype(NPBF)
    E = np.zeros((128, NKT * 128), np.float32)
    keys = np.arange(NKT * 128)
    E[keys // 64, keys] = 1.0
    st["E"] = E.astype(NPBF)
    OFF = 2 * STRIDE * (NT - 1)
    width = OFF + 128
    Fw = np.zeros((128, width), np.float32)
    q = np.arange(128)[:, None]
    xx = np.arange(width)[None, :]
    rel = xx - OFF - 2 * j
    hq = (q >= 64).astype(np.int64)
    Fw[np.broadcast_to(rel > hq, Fw.shape)] = -1e30
    Fw[np.broadcast_to((rel == hq) | (rel == hq - 1), Fw.shape)] = 1e30
    st["fwide"] = Fw
    st["cnt_win"] = pad_counts(NT, STRIDE, j, 512)
    return st


def n_aff(STRIDE, win):
    return -(-(win // 128) // STRIDE)


def pad_counts(NT, STRIDE, j, win):
    na = n_aff(STRIDE, win)
    cnt = np.zeros((32, na * 128), np.float32)
    for a in range(na):
        for q in range(128):
            t = 128 * (STRIDE * a + j) + q
            if t + 1 <= win - 1:
                d = np.arange(t + 1, win)
                bb = rel_bucket_np(d)
                cnt[:, a * 128 + q] = np.bincount(bb, minlength=32)
    return cnt


def build_attn0(NT=16, STRIDE=4, NKT=64, do_nsa=True, do_moba=True):
    nc = bass.Bass("TRN2", target_bir_lowering=False)
    NTOK = NT * 128
    NK = NKT * 128
    LC = 11 * 128 + 127
    LW = 8 * 128 + 127
    NRC = -(-2853 // (128 * STRIDE))
    LCM = 128 * STRIDE * (NRC - 1) + 127 + 16 * 127 + 1
    OFFW = 2 * STRIDE * (NT - 1)
    dt = lambda n, s, d, k: nc.dram_tensor(n, s, d, kind=k).ap()
    qT_d = dt("qT", [128, 16, NTOK], BF16, "ExternalInput")
    gates_d = dt("gates", [NTOK, 24], F32, "ExternalInput")
    kbT = dt("kbT", [128, 4, NK], BF16, "ExternalInput")
    ksT = dt("ksT", [128, 2, NK], BF16, "ExternalInput")
    kwT = dt("kwT", [128, 2, NK], BF16, "ExternalInput")
    kc2 = dt("kc2", [128, 4, NK // 2], BF16, "ExternalInput")
    vtok = dt("vtok", [NK, 768], BF16, "ExternalInput")
    tab33_d = dt("tab33", [33, 8], F32, "ExternalInput")
    oh_rel = dt("oh_rel", [33, LC], F32, "ExternalInput")
    oh_win = dt("oh_win", [33, LW], F32, "ExternalInput")
    oh_cmp = dt("oh_cmp", [33, LCM], F32, "ExternalInput")
    ohsel_d = dt("ohsel", [32, 32 * 128], BF16, "ExternalInput")
    negvalid_d = dt("negvalid", [1, NT * 32], F32, "ExternalInput")
    own1h_d = dt("own1h", [1, NT * 32], F32, "ExternalInput")
    overlap_d = dt("overlap", [128, 512], BF16, "ExternalInput")
    E_d = dt("E", [128, NK], BF16, "ExternalInput")
    fwide_d = dt("fwide", [128, OFFW + 128], F32, "ExternalInput")
    NAFF = n_aff(STRIDE, 512)
    cnt_win_d = dt("cnt_win", [32, NAFF * 128], F32, "ExternalInput")
    posc_d = dt("posc", [128, 2, 16], F32, "ExternalInput")
    w1_d = dt("cw1", [2, 2048, 256], F32, "ExternalInput")
    w2k_d = dt("cw2k", [256, 128], F32, "ExternalInput")
    w2v_d = dt("cw2v", [256, 64], F32, "ExternalInput")
    ident = dt("ident", [128, 128], F32, "ExternalInput")
    o_out = dt("o_attn", [NTOK, 1024], BF16, "ExternalOutput")
    crel_scr = dt("crel_scr", [8, LC], BF16, "Internal")
    cwin_scr = dt("cwin_scr", [8, LW], BF16, "Internal")
    ccmp_scr = dt("ccmp_scr", [8, LCM], BF16, "Internal")

    P = Prog(nc)
    C = Consts(P, nc, ident[:, :])
    R = AttnRes(P, nS=3, nP=3)
    ps_misc = P.psum("ps_misc", [128, 512], F32); b_ps_misc = Buf()
    ps_trf = ps_misc[:].bitcast(BF16).rearrange("p (k n) -> p k n", n=128); b_ps_tr = b_ps_misc
    Ops = [(P.psum(f"O{i}", [128, 4, 128], F32), Buf()) for i in range(3)]
    IMP = P.psum("IMP", [128, 4, 128], F32); b_IMP = Buf()
    tab33 = P.sbuf("tab33", [33, 8], F32); b_tab33 = Buf()
    P.dma("sp", lambda e: e.dma_start(out=tab33[:], in_=tab33_d[:, :]), writes=[b_tab33])
    b_scr_rel = build_ctab(P, nc, C, tab33, b_tab33, oh_rel, LC, "crel", crel_scr, ps_misc, b_ps_misc)
    b31 = P.sbuf("b31", [128, 8], F32); b_b31 = Buf()
    P.dma("sp", lambda e: e.dma_start(out=b31[:], in_=tab33_d[31:32, :].to_broadcast([128, 8])), writes=[b_b31])
    P.op("dve", lambda e: e.tensor_scalar(out=b31[:], in0=b31[:], scalar1=8.0, scalar2=None, op0=ALU.mult),
         reads=[b_b31], writes=[b_b31])
    QT = P.sbuf("QT", [128, 8, NTOK], BF16); b_QT = Buf()
    KV = P.sbuf("KV", [128, 33280], BF16); b_KV = Buf()
    o_sb = P.sbuf("o_sb", [128, NT, 1024], BF16); b_osb = Buf()
    W = P.sbuf("W", [128, 11, 4, 128], BF16); b_W = Buf()
    rz = P.sbuf("rz", [128, 4, 1], F32); b_rz = Buf()
    outs = []
    Esb = P.sbuf("Esb", [128, NK], BF16); b_E = Buf()
    u32 = P.sbuf("u32", [128, 512], F32); b_u32 = Buf()
    t32 = P.sbuf("t32", [128, 512], F32); b_t32 = Buf()
    Wwin = P.sbuf("Wwin", [128, 8, 4, 128], BF16); b_Wwin = Buf()
    Wc = P.sbuf("Wc", [128, NRC, 4, 128], BF16); b_Wc = Buf()

    if do_nsa:
        b_scr_win = build_ctab(P, nc, C, tab33, b_tab33, oh_win, LW, "cwin", cwin_scr, ps_misc, b_ps_misc)
        b_scr_cmp = build_ctab(P, nc, C, tab33, b_tab33, oh_cmp, LCM, "ccmp", ccmp_scr, ps_misc, b_ps_misc)
        P.dma("sp", lambda e: e.dma_start(out=Esb[:], in_=E_d[:, :]), writes=[b_E])
        ovl = P.sbuf("ovl", [128, 4, 128], BF16); b_ovl = Buf()
        P.dma("sp", lambda e: e.dma_start(out=ovl[:].rearrange("p a m -> p (a m)"), in_=overlap_d[:, :]), writes=[b_ovl])
        fw = P.sbuf("fw", [128, OFFW + 128], F32); b_fw = Buf()
        P.dma("sp", lambda e: e.dma_start(out=fw[:], in_=fwide_d[:, :]), writes=[b_fw])
        exptab = P.sbuf("exptab", [32, 8], F32); b_exptab = Buf()
        P.op("act", lambda e: e.activation(out=exptab[:], in_=tab33[0:32, :], func=AF.Exp), reads=[b_tab33], writes=[b_exptab])
        cntw = P.sbuf("cntw", [32, NAFF * 128], F32); b_cntw = Buf()
        P.dma("sp", lambda e: e.dma_start(out=cntw[:], in_=cnt_win_d[:, :]), writes=[b_cntw])
        zpad = P.sbuf("zpad", [128, NAFF, 8], F32); b_zpad = Buf()
        for a in range(NAFF):
            P.op("pe", lambda e, a=a: e.matmul(out=ps_misc[:, 0:8], lhsT=cntw[:, a * 128:(a + 1) * 128], rhs=exptab[:, :],
                                               start=True, stop=True), reads=[b_cntw, b_exptab], writes=[b_ps_misc])
            P.op("act", lambda e, a=a: e.copy(out=zpad[:, a, :], in_=ps_misc[:, 0:8]), reads=[b_ps_misc], writes=[b_zpad])
        gts = P.sbuf("gts", [128, NT, 24], F32); b_gts = Buf()
        P.dma("sp", lambda e: e.dma_start(out=gts[:], in_=gates_d.rearrange("(t p) n -> p t n", p=128)), writes=[b_gts])
        P.dma("sp", lambda e: e.dma_start(out=QT[:], in_=qT_d[:, 0:8, :]), writes=[b_QT])
        posc = P.sbuf("posc", [128, 2, 16], F32); b_posc = Buf()
        poscb = P.sbuf("poscb", [128, 2, 16], BF16); b_poscb = Buf()
        P.dma("sp", lambda e: e.dma_start(out=posc[:], in_=posc_d[:, :, :]), writes=[b_posc])
        P.op("dve", lambda e: e.tensor_copy(out=poscb[:], in_=posc[:]), reads=[b_posc], writes=[b_poscb])
        w2k = P.sbuf("w2k", [128, 2, 128], BF16); b_w2k = Buf()
        w2v = P.sbuf("w2v", [128, 2, 64], BF16); b_w2v = Buf()
        P.dma("pool", lambda e: e.dma_start(out=w2k[:], in_=w2k_d.rearrange("(k p) n -> p k n", p=128)), writes=[b_w2k])
        P.dma("pool", lambda e: e.dma_start(out=w2v[:], in_=w2v_d.rearrange("(k p) n -> p k n", p=128)), writes=[b_w2v])
        w1 = KV[:, 8192:8192 + 4096].rearrange("p (k n) -> p k n", n=256); b_w1 = Buf()
        kc2sb = KV[:, 0:NK // 2]; b_kc2 = Buf()
        bias1 = P.sbuf("bias1", [128, 2], F32); b_bias1 = Buf()
        GT = P.sbuf("GT", [128, 2, 512], BF16); b_GT = Buf()
        P.op("pool", lambda e: e.memset(GT[:], 0.0), writes=[b_GT])
        KcT = P.sbuf("KcT", [128, 2, 512], BF16); b_KcT = Buf()
        Vc = P.sbuf("Vc", [128, 2, 4, 65], BF16); b_Vc = Buf()
        P.op("pool", lambda e: e.memset(KcT[:], 0.0), writes=[b_KcT])
        P.op("pool", lambda e: e.memset(Vc[:], 1.0), writes=[b_Vc])
        ncmp = NK // 16 - 1
        for kvt in range(2):
            for k in range(16):
                P.dma("pool", lambda e, k=k, kvt=kvt: e.dma_start(out=w1[:, k, :], in_=w1_d[kvt, k * 128:(k + 1) * 128, :]),
                      writes=[b_w1])
            for hc in range(2):
                for k in range(16):
                    P.op("pe", lambda e, hc=hc, k=k, kvt=kvt: e.matmul(
                        out=ps_misc[:, 0:1], lhsT=w1[:, k, hc * 128:(hc + 1) * 128], rhs=poscb[:, kvt, k:k + 1],
                        start=(k == 0), stop=(k == 15)), reads=[b_w1, b_poscb], writes=[b_ps_misc], inc=(k == 15))
                P.op("act", lambda e, hc=hc: e.copy(out=bias1[:, hc:hc + 1], in_=ps_misc[:, 0:1]),
                     reads=[b_ps_misc], writes=[b_bias1])
            for kv in range(2):
                X = kvt * 2 + kv
                P.dma("sp", lambda e, X=X: e.dma_start(out=kc2sb, in_=kc2[:, X, :]), writes=[b_kc2])
                for hc in range(2):
                    for k in range(16):
                        P.op("pe", lambda e, hc=hc, k=k: e.matmul(
                            out=ps_misc[:, 0:ncmp], lhsT=w1[:, k, hc * 128:(hc + 1) * 128],
                            rhs=kc2sb[:, k:k + 8 * (ncmp - 1) + 1:8], start=(k == 0), stop=(k == 15)),
                            reads=[b_w1, b_kc2], writes=[b_ps_misc], inc=(k == 15))
                    P.op("act", lambda e, hc=hc: e.activation(out=u32[:, 0:ncmp], in_=ps_misc[:, 0:ncmp], func=AF.Identity,
                                                              bias=bias1[:, hc:hc + 1]),
                         reads=[b_ps_misc, b_bias1], writes=[b_u32])
                    gelu_tanh_ops(P, u32, b_u32, t32, b_t32, GT[:, hc, 0:ncmp], b_GT, ncmp)
                if kvt == 0:
                    for hc in range(2):
                        P.op("pe", lambda e, hc=hc: e.matmul(out=ps_misc[:, 0:512], lhsT=w2k[:, hc, :], rhs=GT[:, hc, :],
                                                             start=(hc == 0), stop=(hc == 1)),
                             reads=[b_w2k, b_GT], writes=[b_ps_misc], inc=(hc == 1))
                    P.op("act", lambda e, kv=kv: e.copy(out=KcT[:, kv, :], in_=ps_misc[:, 0:512]),
                         reads=[b_ps_misc], writes=[b_KcT])
                else:
                    for nc_ in range(4):
                        for hc in range(2):
                            P.op("pe", lambda e, hc=hc, nc_=nc_: e.matmul(
                                out=ps_misc[:, nc_ * 64:(nc_ + 1) * 64], lhsT=GT[:, hc, nc_ * 128:(nc_ + 1) * 128],
                                rhs=w2v[:, hc, :], start=(hc == 0 and nc_ == 0), stop=(hc == 1 and nc_ == 3),
                                skip_group_check=True),
                                reads=[b_w2v, b_GT], writes=[b_ps_misc], inc=(hc == 1 and nc_ == 3))
                    P.op("act", lambda e, kv=kv: e.copy(out=Vc[:, kv, :, 0:64],
                                                        in_=ps_misc[:, 0:256].rearrange("p (a d) -> p a d", d=64)),
                         reads=[b_ps_misc], writes=[b_Vc])
        KsT = KV[:, 0:NK]
        KwT = KV[:, NK:2 * NK]
        Vs = KV[:, 2 * NK:2 * NK + NKT * 65].rearrange("p (k d) -> p k d", d=65)
        Vw = KV[:, 2 * NK + NKT * 65:2 * NK + 2 * NKT * 65].rearrange("p (k d) -> p k d", d=65)
        imp = P.sbuf("imp", [128, 128], F32); b_imp = Buf()
        sc2 = P.sbuf("sc2", [128, 128], F32); b_sc2 = Buf()
        m8 = P.sbuf("m8", [128, 2, 8], F32); b_m8 = Buf()
        negm4 = P.sbuf("negm4", [128, 4, 128], BF16); b_negm4 = Buf()
        nT4 = [(P.sbuf(f"nT4_{i}", [128, 4, 128], BF16), Buf()) for i in range(2)]
        b31row = P.sbuf("b31row", [1, 2, 4, 128], BF16); b_b31row = Buf()
        for kv in range(2):
            P.op("dve", lambda e, kv=kv: e.tensor_copy(
                out=b31row[0:1, kv, :, :], in_=b31[0:1, 4 * kv:4 * kv + 4].unsqueeze(2).to_broadcast([1, 4, 128])),
                reads=[b_b31], writes=[b_b31row])
        ones_b = P.sbuf("ones_b", [1, 128], BF16); b_ones_b = Buf()
        P.op("dve", lambda e: e.memset(ones_b[:], 1.0), writes=[b_ones_b])
        rzg = P.sbuf("rzg", [128, 4, 1], F32); b_rzg = Buf()
        oacc = P.sbuf("oacc", [128, 4, 64], F32); b_oacc = Buf()
        otmp = P.sbuf("otmp", [128, 4, 64], F32); b_otmp = Buf()
        for kv in range(2):
            P.dma("sp", lambda e, kv=kv: e.dma_start(out=KsT, in_=ksT[:, kv, :]), writes=[b_KV, b_w1, b_kc2])
            P.dma("sp", lambda e, kv=kv: e.dma_start(out=KwT, in_=kwT[:, kv, :]), writes=[b_KV])
            P.op("pool", lambda e: e.memset(Vs[:, :, 64:65], 1.0), writes=[b_KV])
            P.op("pool", lambda e: e.memset(Vw[:, :, 64:65], 1.0), writes=[b_KV])
            for k0 in range(0, NKT, 8):
                P.dma("sp", lambda e, kv=kv, k0=k0: e.dma_start(
                    out=Vs[:, k0:k0 + 8, 0:64], in_=vtok[k0 * 128:(k0 + 8) * 128, kv * 64:(kv + 1) * 64].rearrange(
                        "(kt p) d -> p kt d", p=128)), writes=[b_KV])
                P.dma("sp", lambda e, kv=kv, k0=k0: e.dma_start(
                    out=Vw[:, k0:k0 + 8, 0:64], in_=vtok[k0 * 128:(k0 + 8) * 128, 128 + kv * 64:128 + (kv + 1) * 64].rearrange(
                        "(kt p) d -> p kt d", p=128)), writes=[b_KV])
            toeplitz_load(P, W, b_W, crel_scr, b_scr_rel, LC, 4 * kv, 11)
            toeplitz_load(P, Wwin, b_Wwin, cwin_scr, b_scr_win, LW, 4 * kv, 8)
            toeplitz_load(P, Wc, b_Wc, ccmp_scr, b_scr_cmp, LCM, 4 * kv, NRC, rstride=128 * STRIDE, pstride=16)
            for lt in range(NT):
                Oc, b_Oc = Ops[0]; Os, b_Os = Ops[1]; Ow, b_Ow = Ops[2]

                def qk_gen(Ksrc, bK, lt=lt, kv=kv):
                    def qk_fn(kt):
                        return [(hh * 128, (hh + 1) * 128,
                                 [(Ksrc(kt), QT[:, 4 * kv + hh, lt * 128:(lt + 1) * 128])], [bK, b_QT])
                                for hh in range(4)]
                    return qk_fn
                ncs = list(range(0, min(4, (STRIDE * lt + STRIDE - 1) // 16 + 1)))

                def extra_c(nc_, lt=lt, kv=kv):
                    r = (STRIDE * lt - 16 * nc_) // STRIDE
                    if r < NRC:
                        return [(C.antib[:], Wc[:, r, :, :].rearrange("p h q -> p (h q)"), [C.b_antib, b_Wc])]
                    return [(ones_b[0:1, :], b31row[0:1, kv, :, :].rearrange("p h q -> p (h q)"), [b_ones_b, b_b31row])]

                def post_c(nc_, ii, n, Pt, b_P):
                    for g in range(4):
                        P.op("pe", lambda e, g=g, nc_=nc_, ii=ii, n=n, Pt=Pt: e.matmul(
                            out=IMP[:, g, :], lhsT=Pt[:, g * 128:(g + 1) * 128], rhs=ovl[:, nc_, :],
                            start=(ii == 0 and g == 0), stop=(ii == n - 1 and g == 3), skip_group_check=True),
                            reads=[b_P, b_ovl], writes=[b_IMP], inc=(g == 3))
                attn_steps(P, R, ncs, qk_gen(lambda nc_, kv=kv: KcT[:, kv, nc_ * 128:(nc_ + 1) * 128], b_KcT), extra_c,
                           lambda nc_, hh, kv=kv: (Vc[:, kv, nc_, :], [b_Vc]), Oc, b_Oc, 0.125, post_fn=post_c)
                P.op("dve", lambda e, Oc=Oc: e.tensor_scalar(out=rz[:], in0=Oc[:, :, 64:65], scalar1=1e-30, scalar2=None,
                                                             op0=ALU.max), reads=[b_Oc], writes=[b_rz])
                P.op("dve", lambda e: e.reciprocal(out=rz[:], in_=rz[:]), reads=[b_rz], writes=[b_rz])
                P.op("dve", lambda e: e.tensor_scalar(out=imp[:], in0=IMP[:, 0, :], scalar1=rz[:, 0, :], scalar2=None,
                                                      op0=ALU.mult), reads=[b_IMP, b_rz], writes=[b_imp])
                for g in range(1, 4):
                    P.op("dve", lambda e, g=g: e.scalar_tensor_tensor(out=imp[:], in0=IMP[:, g, :], scalar=rz[:, g, :],
                                                                      in1=imp[:], op0=ALU.mult, op1=ALU.add),
                         reads=[b_IMP, b_rz, b_imp], writes=[b_imp])
                f0 = OFFW - 2 * STRIDE * lt
                P.op("dve", lambda e, f0=f0: e.tensor_tensor(out=imp[:], in0=imp[:], in1=fw[:, f0:f0 + 128], op=ALU.add),
                     reads=[b_imp, b_fw], writes=[b_imp])
                P.op("dve", lambda e: e.memset(imp[:, 0:1], 1e30), reads=[b_imp], writes=[b_imp])
                P.op("dve", lambda e: e.max(out=m8[:, 0, :], in_=imp[:]), reads=[b_imp], writes=[b_m8])
                P.op("dve", lambda e: e.match_replace(out=sc2[:], in_to_replace=m8[:, 0, :], in_values=imp[:],
                                                      imm_value=-3.0e38), reads=[b_imp, b_m8], writes=[b_sc2])
                P.op("dve", lambda e: e.max(out=m8[:, 1, :], in_=sc2[:]), reads=[b_sc2], writes=[b_m8])
                P.op("dve", lambda e: e.tensor_scalar(out=sc2[:], in0=imp[:], scalar1=m8[:, 1, 7:8], scalar2=None,
                                                      op0=ALU.is_ge), reads=[b_imp, b_m8], writes=[b_sc2])
                P.op("dve", lambda e: e.tensor_scalar(out=sc2[:], in0=sc2[:], scalar1=-1.0, scalar2=-NEGM,
                                                      op0=ALU.add, op1=ALU.mult), reads=[b_sc2], writes=[b_sc2])
                for hh in range(4):
                    P.op("dve", lambda e, hh=hh, kv=kv: e.tensor_scalar(
                        out=negm4[:, hh, :], in0=sc2[:], scalar1=b31[:, 4 * kv + hh:4 * kv + hh + 1], scalar2=None,
                        op0=ALU.add), reads=[b_sc2, b_b31], writes=[b_negm4], inc=(hh == 3))
                for hh in range(4):
                    P.op("pe", lambda e, hh=hh: e.transpose(out=ps_trf[:, hh, :], in_=negm4[:, hh, :], identity=C.idb[:]),
                         reads=[b_negm4, C.b_idb], writes=[b_ps_tr], inc=(hh == 3))
                nT, b_nT = nT4[lt % 2]
                P.op("act", lambda e, nT=nT: e.copy(out=nT[:], in_=ps_trf[:, 0:4, :]), reads=[b_ps_tr], writes=[b_nT])
                kts = list(range(0, min(STRIDE * lt + STRIDE, NKT)))

                def extra_s(kt, lt=lt, nT=nT, b_nT=b_nT):
                    ex = [(Esb[:, kt * 128:(kt + 1) * 128], nT[:].rearrange("m h q -> m (h q)"), [b_E, b_nT])]
                    dl = STRIDE * lt - kt
                    if dl <= 7:
                        ex.append((C.antib[:], W[:, dl + 3, :, :].rearrange("p h q -> p (h q)"), [C.b_antib, b_W]))
                    return ex
                attn_steps(P, R, kts, qk_gen(lambda kt: KsT[:, kt * 128:(kt + 1) * 128], b_KV), extra_s,
                           lambda kt, hh: (Vs[:, kt, :], [b_KV]), Os, b_Os, 0.125)
                ktw = list(range(max(0, STRIDE * lt - 4), min(STRIDE * lt + STRIDE, NKT)))

                def extra_w(kt, lt=lt):
                    dl = STRIDE * lt - kt
                    return [(C.antib[:], Wwin[:, dl + 3, :, :].rearrange("p h q -> p (h q)"), [C.b_antib, b_Wwin])]
                attn_steps(P, R, ktw, qk_gen(lambda kt: KwT[:, kt * 128:(kt + 1) * 128], b_KV), extra_w,
                           lambda kt, hh: (Vw[:, kt, :], [b_KV]), Ow, b_Ow, 0.125)
                for br, (O, b_O) in enumerate([(Oc, b_Oc), (Os, b_Os), (Ow, b_Ow)]):
                    if br == 2 and lt < NAFF:
                        P.op("dve", lambda e, O=O, lt=lt, kv=kv: e.tensor_tensor(
                            out=rz[:], in0=O[:, :, 64:65], in1=zpad[:, lt, 4 * kv:4 * kv + 4].unsqueeze(2), op=ALU.add),
                            reads=[b_O, b_zpad], writes=[b_rz])
                    else:
                        P.op("dve", lambda e, O=O: e.tensor_scalar(out=rz[:], in0=O[:, :, 64:65], scalar1=1e-30, scalar2=None,
                                                                   op0=ALU.max), reads=[b_O], writes=[b_rz])
                    P.op("dve", lambda e: e.reciprocal(out=rz[:], in_=rz[:]), reads=[b_rz], writes=[b_rz])
                    gsl = gts[:, lt, 12 * kv:12 * kv + 12].rearrange("p (h b) -> p h b", b=3)[:, :, br:br + 1]
                    P.op("dve", lambda e, gsl=gsl: e.tensor_tensor(out=rzg[:], in0=rz[:], in1=gsl, op=ALU.mult),
                         reads=[b_rz, b_gts], writes=[b_rzg])
                    if br == 0:
                        P.op("dve", lambda e, O=O: e.tensor_tensor(out=oacc[:], in0=O[:, :, 0:64],
                                                                   in1=rzg[:].to_broadcast([128, 4, 64]), op=ALU.mult),
                             reads=[b_O, b_rzg], writes=[b_oacc])
                    else:
                        P.op("dve", lambda e, O=O: e.tensor_tensor(out=otmp[:], in0=O[:, :, 0:64],
                                                                   in1=rzg[:].to_broadcast([128, 4, 64]), op=ALU.mult),
                             reads=[b_O, b_rzg], writes=[b_otmp])
                        if br == 1:
                            P.op("pool", lambda e: e.tensor_tensor(out=oacc[:], in0=oacc[:], in1=otmp[:], op=ALU.add),
                                 reads=[b_oacc, b_otmp], writes=[b_oacc])
                        else:
                            P.op("pool", lambda e, lt=lt, kv=kv: e.tensor_tensor(
                                out=o_sb[:, lt, kv * 256:(kv + 1) * 256].rearrange("p (h d) -> p h d", d=64),
                                in0=oacc[:], in1=otmp[:], op=ALU.add), reads=[b_oacc, b_otmp], writes=[b_osb])

    if do_moba:
        ohsel = Esb[0:32, 0:32 * 128]; b_ohsel = b_E
        P.dma("sp", lambda e: e.dma_start(out=ohsel, in_=ohsel_d[:, :]), writes=[b_ohsel])
        assert NT * 32 <= 512
        negvalid = u32[:, 0:NT * 32].rearrange("p (a n) -> p a n", n=32); b_nv = b_u32
        own1h = t32[:, 0:NT * 32].rearrange("p (a n) -> p a n", n=32); b_own = b_t32
        P.dma("sp", lambda e: e.dma_start(out=u32[:, 0:NT * 32],
                                          in_=negvalid_d[0:1, :].to_broadcast([128, NT * 32])), writes=[b_nv])
        P.dma("sp", lambda e: e.dma_start(out=t32[:, 0:NT * 32],
                                          in_=own1h_d[0:1, :].to_broadcast([128, NT * 32])), writes=[b_own])
        P.dma("sp", lambda e: e.dma_start(out=QT[:], in_=qT_d[:, 8:16, :]), writes=[b_QT])
        KT = KV[:, 0:2 * NK].rearrange("p (c k) -> p c k", c=2)
        V = KV[:, 2 * NK:2 * NK + NKT * 4 * 65].rearrange("p (k h d) -> p k h d", h=4, d=65)
        kmT = P.sbuf("kmT", [128, 2, 32], BF16); b_kmT = Buf()
        kms = P.sbuf("kms", [128, 32], F32); b_kms = Buf()
        gate = P.sbuf("gate", [128, 4, 32], F32); b_gate = Buf()
        mx8 = P.sbuf("mx8", [128, 4, 8], F32); b_mx8 = Buf()
        sel = P.sbuf("sel", [128, 4, 32], F32); b_sel = Buf()
        negm = P.sbuf("negm", [128, 4, 32], BF16); b_negm = Buf()
        negmT = [(Wwin[0:32, i, :, :], b_Wwin) for i in range(2)]
        for g in range(2):
            for cc in range(2):
                P.dma("sp", lambda e, cc=cc, g=g: e.dma_start(out=KT[:, cc, :], in_=kbT[:, 2 * g + cc, :]), writes=[b_KV])
            P.op("pool", lambda e: e.memset(V[:, :, :, 64:65], 1.0), writes=[b_KV])
            for hh in range(4):
                for k0 in range(0, NKT, 8):
                    P.dma("sp", lambda e, hh=hh, g=g, k0=k0: e.dma_start(
                        out=V[:, k0:k0 + 8, hh, 0:64],
                        in_=vtok[k0 * 128:(k0 + 8) * 128, 256 + (4 * g + hh) * 64:256 + (4 * g + hh + 1) * 64].rearrange(
                            "(kt p) d -> p kt d", p=128)), writes=[b_KV])
            toeplitz_load(P, W, b_W, crel_scr, b_scr_rel, LC, 4 * g, 11)
            for cc in range(2):
                P.op("dve", lambda e, cc=cc: e.tensor_reduce(out=kms[:], in_=KT[:, cc, :].rearrange("p (n k) -> p n k", k=256),
                                                             axis=AX.X, op=ALU.add), reads=[b_KV], writes=[b_kms])
                P.op("dve", lambda e, cc=cc: e.tensor_scalar(out=kmT[:, cc, :], in0=kms[:], scalar1=1.0 / 256, scalar2=None,
                                                             op0=ALU.mult), reads=[b_kms], writes=[b_kmT])
            for lt in range(NT):
                for hh in range(4):
                    cc = hh // 2
                    P.op("pe", lambda e, hh=hh, cc=cc, lt=lt, g=g: e.matmul(
                        out=ps_misc[:, hh * 32:(hh + 1) * 32], lhsT=QT[:, 4 * g + hh, lt * 128:(lt + 1) * 128],
                        rhs=kmT[:, cc, :], start=(hh == 0), stop=(hh == 3), skip_group_check=True),
                        reads=[b_QT, b_kmT], writes=[b_ps_misc], inc=(hh == 3))
                P.op("dve", lambda e, lt=lt: e.tensor_tensor(
                    out=gate[:], in0=ps_misc[:, 0:128].rearrange("p (h n) -> p h n", n=32),
                    in1=negvalid[:, lt:lt + 1, :].to_broadcast([128, 4, 32]), op=ALU.add),
                    reads=[b_ps_misc, b_nv], writes=[b_gate])
                for hh in range(4):
                    P.op("dve", lambda e, hh=hh: e.max(out=mx8[:, hh, :], in_=gate[:, hh, :]),
                         reads=[b_gate], writes=[b_mx8], inc=(hh == 3))
                P.op("dve", lambda e: e.tensor_tensor(out=sel[:], in0=gate[:], in1=mx8[:, :, 2:3].to_broadcast([128, 4, 32]),
                                                      op=ALU.is_ge), reads=[b_gate, b_mx8], writes=[b_sel])
                P.op("dve", lambda e, lt=lt: e.tensor_tensor(out=sel[:], in0=sel[:],
                                                             in1=own1h[:, lt:lt + 1, :].to_broadcast([128, 4, 32]),
                                                             op=ALU.max), reads=[b_sel, b_own], writes=[b_sel])
                P.op("dve", lambda e: e.tensor_scalar(out=sel[:], in0=sel[:], scalar1=-1.0, scalar2=-NEGM,
                                                      op0=ALU.add, op1=ALU.mult), reads=[b_sel], writes=[b_sel])
                P.op("dve", lambda e, g=g: e.tensor_tensor(
                    out=negm[:], in0=sel[:], in1=b31[:, 4 * g:4 * g + 4].unsqueeze(2).to_broadcast([128, 4, 32]),
                    op=ALU.add), reads=[b_sel, b_b31], writes=[b_negm])
                for hh in range(4):
                    P.op("pe", lambda e, hh=hh: e.transpose(out=ps_trf[0:32, hh, :], in_=negm[:, hh, :], identity=C.idb[:]),
                         reads=[b_negm, C.b_idb], writes=[b_ps_tr], inc=(hh == 3))
                nT, b_nT = negmT[lt % 2]
                P.op("act", lambda e, nT=nT: e.copy(out=nT, in_=ps_trf[0:32, 0:4, :]), reads=[b_ps_tr], writes=[b_nT])
                O, b_O = Ops[lt % 2]
                kts = list(range(0, min(STRIDE * lt + STRIDE, NKT)))

                def qk_fn(kt, lt=lt, g=g):
                    return [(hh * 128, (hh + 1) * 128,
                             [(KT[:, hh // 2, kt * 128:(kt + 1) * 128], QT[:, 4 * g + hh, lt * 128:(lt + 1) * 128])],
                             [b_KV, b_QT]) for hh in range(4)]

                def extra_fn(kt, lt=lt, nT=nT, b_nT=b_nT):
                    ex = [(ohsel[:, (kt // 2) * 128:(kt // 2 + 1) * 128], nT.rearrange("n h q -> n (h q)"),
                           [b_ohsel, b_nT])]
                    dl = STRIDE * lt - kt
                    if dl <= 7:
                        ex.append((C.antib[:], W[:, dl + 3, :, :].rearrange("p h q -> p (h q)"), [C.b_antib, b_W]))
                    return ex
                attn_steps(P, R, kts, qk_fn, extra_fn, lambda kt, hh: (V[:, kt, hh, :], [b_KV]), O, b_O, 0.125)
                P.op("dve", lambda e, O=O: e.tensor_scalar(out=rz[:], in0=O[:, :, 64:65], scalar1=1e-30, scalar2=None,
                                                           op0=ALU.max), reads=[b_O], writes=[b_rz])
                P.op("dve", lambda e: e.reciprocal(out=rz[:], in_=rz[:]), reads=[b_rz], writes=[b_rz])
                P.op("dve", lambda e, O=O, lt=lt, g=g: e.tensor_tensor(
                    out=o_sb[:, lt, 512 + g * 256:512 + (g + 1) * 256].rearrange("p (h d) -> p h d", d=64),
                    in0=O[:, :, 0:64], in1=rz[:].to_broadcast([128, 4, 64]), op=ALU.mult),
                    reads=[b_O, b_rz], writes=[b_osb])
    for t in range(NT):
        outs.append(P.dma("sp", lambda e, t=t: e.dma_start(out=o_out[t * 128:(t + 1) * 128, :], in_=o_sb[:, t, :]),
                          reads=[b_osb]))
    P.wait_all("sp", outs)
    P.finish()
    return nc


def build_post(NT=16, final=False, NEXP=16):
    nc = bass.Bass("TRN2", target_bir_lowering=False)
    NTOK = NT * 128
    dt = lambda n, s, d, k: nc.dram_tensor(n, s, d, kind=k).ap()
    o_attn = dt("o_attn", [NTOK, 1024], BF16, "ExternalInput")
    x_d = dt("x", [NTOK, D], F32, "ExternalInput")
    mod_d = dt("mod", [6, D], F32, "ExternalInput")
    w_out_d = dt("w_out", [D, D], F32, "ExternalInput")
    g_ffn_d = dt("g_ffn", [1, D], F32, "ExternalInput")
    g_fin_d = dt("g_fin", [1, D], F32, "ExternalInput")
    rw_d = dt("router_w", [D, 16], F32, "ExternalInput")
    rb_d = dt("router_b", [1, 16], F32, "ExternalInput")
    wg_d = dt("wg", [16, D, 512], F32, "ExternalInput")
    wu_d = dt("wu", [16, D, 512], F32, "ExternalInput")
    wd_d = dt("wd", [16, 512, D], F32, "ExternalInput")
    oh16_d = dt("oh16", [16, 16 * 128], F32, "ExternalInput")
    ident = dt("ident", [128, 128], F32, "ExternalInput")
    x_out = dt("x_out", [NTOK, D], F32, "ExternalOutput")

    P = Prog(nc)
    C = Consts(P, nc, ident[:, :])
    ps_a = [(P.psum(f"pa{i}", [128, 512], F32), Buf()) for i in range(4)]
    ps_y = [(P.psum(f"py{i}", [128, 512], F32), Buf()) for i in range(2)]
    ps_w = (P.psum("pw", [128, 512], F32), Buf())
    ps_tr = P.psum("ptr", [128, 8, 128], BF16); b_ps_tr = Buf()
    x_sb = P.sbuf("x_sb", [128, NT, D], F32); b_x = [Buf() for _ in range(NT)]
    hfT = P.sbuf("hfT", [128, 8, NTOK], BF16); b_hfT = Buf()
    WB = [P.sbuf(f"WB{i}", [128, 12288], BF16) for i in range(2)]; b_WB = [Buf(), Buf()]
    bc = P.sbuf("bc", [128, 4, D], F32); b_bc = Buf()
    scr_b = P.sbuf("scr_b", [128, 2048], BF16)
    nsc = norm_scratch(P, "p", hb=scr_b[:, 1024:2048])
    gf = nsc["h32"]; b_gf = nsc["b"][3]
    P.dma("sp", lambda e: e.dma_start(out=bc[:, 0, :], in_=mod_d[2:3, :].to_broadcast([128, D])), writes=[b_bc])
    P.dma("sp", lambda e: e.dma_start(out=bc[:, 1, :], in_=mod_d[4:5, :].to_broadcast([128, D])), writes=[b_bc])
    P.dma("sp", lambda e: e.dma_start(out=bc[:, 2, :], in_=mod_d[3:4, :].to_broadcast([128, D])), writes=[b_bc])
    P.dma("sp", lambda e: e.dma_start(out=bc[:, 3, :], in_=mod_d[5:6, :].to_broadcast([128, D])), writes=[b_bc])
    P.dma("sp", lambda e: e.dma_start(out=gf[:], in_=g_ffn_d[0:1, :].to_broadcast([128, D])), writes=[b_gf])
    P.op("dve", lambda e: e.scalar_tensor_tensor(out=bc[:, 1, :], in0=bc[:, 1, :], scalar=1.0, in1=gf[:],
                                                 op0=ALU.add, op1=ALU.mult), reads=[b_bc, b_gf], writes=[b_bc])
    wo = WB[1][:, 0:8192].rearrange("p (k n) -> p k n", n=1024)
    wov = w_out_d.rearrange("(k p) n -> p k n", p=128)
    for k in range(8):
        P.dma("pool", lambda e, k=k: e.dma_start(out=wo[:, k, :], in_=wov[:, k, :]), writes=[b_WB[1]])
    for k in range(8):
        P.op("dve", lambda e, k=k: e.tensor_tensor(out=wo[:, k, :], in0=wo[:, k, :], in1=bc[:, 0, :], op=ALU.mult),
             reads=[b_WB[1], b_bc], writes=[b_WB[1]])
    rw = P.sbuf("rw", [128, 8, 16], F32); b_rw = Buf()
    P.dma("sp", lambda e: e.dma_start(out=rw[:], in_=rw_d.rearrange("(k p) n -> p k n", p=128)), writes=[b_rw])
    rb = P.sbuf("rb", [128, 16], F32); b_rb = Buf()
    P.dma("sp", lambda e: e.dma_start(out=rb[:], in_=rb_d[0:1, :].to_broadcast([128, 16])), writes=[b_rb])
    oh16 = P.sbuf("oh16", [16, 16 * 128], F32); b_oh16 = Buf()
    P.dma("sp", lambda e: e.dma_start(out=oh16[:], in_=oh16_d[:, :]), writes=[b_oh16])
    wT = P.sbuf("wT", [16, NTOK], F32); b_wT = Buf()
    ob = [scr_b[:, 0:1024]] * 2; b_ob = [Buf()] * 2
    oT = P.sbuf("oT", [128, 8, 128], BF16); b_oT = Buf()
    h32T = nsc["sq"][:].rearrange("p (k n) -> p k n", n=128); b_h32T = nsc["b"][0]
    r_aff = P.sbuf("r_aff", [128, 16], F32); b_aff = Buf()
    r_b = P.sbuf("r_b", [128, 4, 4], F32); b_rbias = Buf()
    r_t = P.sbuf("r_t", [128, 8, 4], F32); b_rt = Buf()
    r_w = P.sbuf("r_w", [128, 4, 4], F32); b_rw2 = Buf()
    r_s = P.sbuf("r_s", [128, 2], F32); b_rs = Buf()
    def stage_pa(t):
        o_t = ob[t % 2]; bo = b_ob[t % 2]
        P.dma("sp", lambda e, o_t=o_t, t=t: e.dma_start(out=o_t[:], in_=o_attn[t * 128:(t + 1) * 128, :]), writes=[bo])
        P.dma("sp", lambda e, t=t: e.dma_start(out=x_sb[:, t, :], in_=x_d[t * 128:(t + 1) * 128, :]), writes=[b_x[t]])
        for k in range(8):
            P.op("pe", lambda e, k=k, o_t=o_t: e.transpose(out=ps_tr[:, k, :], in_=o_t[:, k * 128:(k + 1) * 128],
                                                           identity=C.idb[:]),
                 reads=[bo, C.b_idb], writes=[b_ps_tr], inc=(k == 7))
        P.op("act", lambda e: e.copy(out=oT[:], in_=ps_tr[:]), reads=[b_ps_tr], writes=[b_oT])
        for half in range(2):
            py, b_py = ps_y[half]
            for k in range(8):
                P.op("pe", lambda e, k=k, half=half, py=py: e.matmul(out=py[:], lhsT=oT[:, k, :],
                                                                   rhs=wo[:, k, half * 512:(half + 1) * 512],
                                                                   start=(k == 0), stop=(k == 7)),
                     reads=[b_oT, b_WB[1]], writes=[b_py], inc=(k == 7))
            P.op("dve", lambda e, half=half, py=py, t=t: e.tensor_tensor(
                out=x_sb[:, t, half * 512:(half + 1) * 512], in0=py[:], in1=x_sb[:, t, half * 512:(half + 1) * 512],
                op=ALU.add), reads=[b_py, b_x[t]], writes=[b_x[t]])
    def stage_pb(t):
        emit_norm_tile(P, C, x_sb[:, t, :], b_x[t], bc[:, 1, :], b_bc, bc[:, 2, :], b_bc,
                       hfT[:, :, t * 128:(t + 1) * 128], b_hfT, nsc, ps_tr, b_ps_tr)
        h32 = nsc["h32"]; b_h32 = nsc["b"][3]
        P.op("dve", lambda e: e.tensor_tensor(out=h32[:], in0=h32[:], in1=bc[:, 2, :], op=ALU.add),
             reads=[b_h32, b_bc], writes=[b_h32])
        for hf_ in range(2):
            pa, b_pa = ps_a[hf_]
            for k in range(4):
                kk = hf_ * 4 + k
                P.op("pe", lambda e, k=k, kk=kk, pa=pa: e.transpose(out=pa[:, k * 128:(k + 1) * 128],
                                                                  in_=h32[:, kk * 128:(kk + 1) * 128], identity=C.idf[:]),
                     reads=[b_h32, C.b_idf], writes=[b_pa], inc=(k == 3))
            P.op("act", lambda e, hf_=hf_, pa=pa: e.copy(out=h32T[:, hf_ * 4:(hf_ + 1) * 4, :],
                                                         in_=pa[:].rearrange("p (k n) -> p k n", n=128)),
                 reads=[b_pa], writes=[b_h32T])
        pw, b_pw = ps_w
        for k in range(8):
            P.op("pe", lambda e, k=k: e.matmul(out=pw[:, 0:16], lhsT=h32T[:, k, :], rhs=rw[:, k, :],
                                               start=(k == 0), stop=(k == 7)),
                 reads=[b_h32T, b_rw], writes=[b_pw], inc=(k == 7))
        P.op("act", lambda e: e.activation(out=r_aff[:], in_=pw[:, 0:16], func=AF.Sigmoid), reads=[b_pw], writes=[b_aff])
        r_bf = r_b[:].rearrange("p g e -> p (g e)")
        P.op("dve", lambda e: e.tensor_tensor(out=r_bf, in0=r_aff[:], in1=rb[:], op=ALU.add),
             reads=[b_aff, b_rb], writes=[b_rbias])
        a_, b_, c_, d_ = (r_b[:, :, i] for i in range(4))
        T = lambda i: r_t[:, i, :]
        seq = [(T(0), a_, b_, ALU.max), (T(1), a_, b_, ALU.min), (T(2), c_, d_, ALU.max), (T(3), c_, d_, ALU.min),
               (T(4), T(0), T(2), ALU.max), (T(5), T(0), T(2), ALU.min), (T(6), T(1), T(3), ALU.max),
               (T(7), T(5), T(6), ALU.max),
               (T(0), T(4), T(7), ALU.add)]
        for (o_, i0, i1, op_) in seq:
            P.op("dve", lambda e, o_=o_, i0=i0, i1=i1, op_=op_: e.tensor_tensor(out=o_, in0=i0, in1=i1, op=op_),
                 reads=[b_rbias, b_rt], writes=[b_rt])
        P.op("dve", lambda e: e.tensor_reduce(out=r_s[:, 0:1], in_=r_t[:, 0, :], axis=AX.X, op=ALU.max),
             reads=[b_rt], writes=[b_rs])
        P.op("dve", lambda e: e.tensor_scalar(out=r_t[:, 1, :], in0=r_t[:, 0, :], scalar1=r_s[:, 0:1], scalar2=None,
                                              op0=ALU.is_ge), reads=[b_rt, b_rs], writes=[b_rt])
        P.op("dve", lambda e: e.tensor_tensor(out=r_w[:], in0=r_b[:], in1=r_t[:, 7, :].unsqueeze(2).to_broadcast([128, 4, 4]),
                                              op=ALU.is_ge), reads=[b_rbias, b_rt], writes=[b_rw2])
        P.op("dve", lambda e: e.tensor_tensor(out=r_w[:], in0=r_w[:], in1=r_t[:, 1, :].unsqueeze(2).to_broadcast([128, 4, 4]),
                                              op=ALU.mult), reads=[b_rw2, b_rt], writes=[b_rw2])
        r_wf = r_w[:].rearrange("p g e -> p (g e)")
        P.op("dve", lambda e: e.tensor_tensor(out=r_wf, in0=r_wf, in1=r_aff[:], op=ALU.mult),
             reads=[b_rw2, b_aff], writes=[b_rw2])
        P.op("dve", lambda e: e.tensor_reduce(out=r_s[:, 1:2], in_=r_wf, axis=AX.X, op=ALU.add),
             reads=[b_rw2], writes=[b_rs])
        P.op("dve", lambda e: e.reciprocal(out=r_s[:, 1:2], in_=r_s[:, 1:2]), reads=[b_rs], writes=[b_rs])
        P.op("dve", lambda e: e.tensor_scalar(out=r_wf, in0=r_wf, scalar1=r_s[:, 1:2], scalar2=None, op0=ALU.mult),
             reads=[b_rw2, b_rs], writes=[b_rw2])
        pa, b_pa = ps_a[2]
        P.op("pe", lambda e, pa=pa: e.transpose(out=pa[0:16, 0:128], in_=r_wf, identity=C.idf[:]),
             reads=[b_rw2, C.b_idf], writes=[b_pa])
        P.op("act", lambda e, pa=pa, t=t: e.copy(out=wT[:, t * 128:(t + 1) * 128], in_=pa[0:16, 0:128]),
             reads=[b_pa], writes=[b_wT])

    for t in range(NT + 1):
        if t < NT:
            stage_pa(t)
        if t >= 1:
            stage_pb(t - 1)

    wbc = [P.sbuf("wbc0", [128, 512], F32)] * 2; b_wbc = [Buf()] * 2
    sg = [P.sbuf(f"sg{i}", [128, 512], BF16) for i in range(2)]; b_sg = [Buf(), Buf()]
    uw = [P.sbuf(f"uw{i}", [128, 512], BF16) for i in range(2)]; b_uw = [Buf(), Buf()]
    hid = [P.sbuf("hid0", [128, 4, 512], BF16), scr_b[:, :].rearrange("p (f n) -> p f n", n=512)]
    b_hid = [Buf(), Buf()]
    hid1_first = [True]
    NTG = NT // 4
    pai = [0]

    def stage_a(ex, tg, it, wg, wu, bW):
        wb_ = wbc[it % 2]; bwb = b_wbc[it % 2]
        hd = hid[it % 2]; bhd = b_hid[it % 2]
        pw, b_pw = ps_w
        P.op("pe", lambda e: e.matmul(out=pw[:], lhsT=oh16[:, ex * 128:(ex + 1) * 128],
                                      rhs=wT[:, tg * 512:(tg + 1) * 512], start=True, stop=True),
             reads=[b_oh16, b_wT], writes=[b_pw])
        P.op("act", lambda e: e.copy(out=wb_[:], in_=pw[:]), reads=[b_pw], writes=[bwb])
        for fc in range(4):
            pg, b_pg = ps_a[pai[0] % 4]; pai[0] += 1
            pu, b_pu = ps_a[pai[0] % 4]; pai[0] += 1
            for k in range(8):
                P.op("pe", lambda e, k=k, fc=fc, pg=pg: e.matmul(
                    out=pg[:], lhsT=wg[:, k, fc * 128:(fc + 1) * 128], rhs=hfT[:, k, tg * 512:(tg + 1) * 512],
                    start=(k == 0), stop=(k == 7)), reads=[bW, b_hfT], writes=[b_pg], inc=(k == 7))
            for k in range(8):
                P.op("pe", lambda e, k=k, fc=fc, pu=pu: e.matmul(
                    out=pu[:], lhsT=wu[:, k, fc * 128:(fc + 1) * 128], rhs=hfT[:, k, tg * 512:(tg + 1) * 512],
                    start=(k == 0), stop=(k == 7)), reads=[bW, b_hfT], writes=[b_pu], inc=(k == 7))
            s_ = sg[fc % 2]; bs_ = b_sg[fc % 2]
            u_ = uw[fc % 2]; bu_ = b_uw[fc % 2]
            P.op("act", lambda e, pg=pg, s_=s_: e.activation(out=s_[:], in_=pg[:], func=AF.Silu), reads=[b_pg], writes=[bs_])
            P.op("dve", lambda e, pu=pu, u_=u_: e.tensor_tensor(out=u_[:], in0=pu[:], in1=wb_[:], op=ALU.mult),
                 reads=[b_pu, bwb], writes=[bu_])
            wr = [bhd]
            if it % 2 == 1 and hid1_first[0]:
                wr = [bhd, b_ob[0], nsc["b"][4]]
                hid1_first[0] = False
            P.op("dve", lambda e, fc=fc, s_=s_, u_=u_: e.tensor_tensor(out=hd[:, fc, :], in0=s_[:], in1=u_[:], op=ALU.mult),
                 reads=[bs_, bu_], writes=wr)

    def stage_b(ex, tg, it, wd, bW):
        hd = hid[it % 2]; bhd = b_hid[it % 2]
        for tt in range(4):
            t = tg * 4 + tt
            for half in range(2):
                py, b_py = ps_y[half]
                for fc in range(4):
                    P.op("pe", lambda e, fc=fc, tt=tt, half=half, py=py: e.matmul(
                        out=py[:], lhsT=hd[:, fc, tt * 128:(tt + 1) * 128], rhs=wd[:, fc, half * 512:(half + 1) * 512],
                        start=(fc == 0), stop=(fc == 3)), reads=[bhd, bW], writes=[b_py], inc=(fc == 3))
                P.op("dve", lambda e, half=half, py=py, t=t: e.tensor_tensor(
                    out=x_sb[:, t, half * 512:(half + 1) * 512], in0=py[:], in1=x_sb[:, t, half * 512:(half + 1) * 512],
                    op=ALU.add), reads=[b_py, b_x[t]], writes=[b_x[t]])

    pend = None
    it = 0
    for ex in range(NEXP):
        Wb = WB[ex % 2]; bW = b_WB[ex % 2]
        wg = Wb[:, 0:4096].rearrange("p (k n) -> p k n", n=512)
        wu = Wb[:, 4096:8192].rearrange("p (k n) -> p k n", n=512)
        wd = Wb[:, 8192:12288].rearrange("p (k n) -> p k n", n=1024)
        wgv = wg_d[ex].rearrange("(k p) n -> p k n", p=128)
        wuv = wu_d[ex].rearrange("(k p) n -> p k n", p=128)
        wdv = wd_d[ex].rearrange("(k p) n -> p k n", p=128)
        for k in range(8):
            P.dma("pool", lambda e, k=k, wg=wg, wgv=wgv: e.dma_start(out=wg[:, k, :], in_=wgv[:, k, :]), writes=[bW])
            P.dma("pool", lambda e, k=k, wu=wu, wuv=wuv: e.dma_start(out=wu[:, k, :], in_=wuv[:, k, :]), writes=[bW])
        for k in range(4):
            P.dma("pool", lambda e, k=k, wd=wd, wdv=wdv: e.dma_start(out=wd[:, k, :], in_=wdv[:, k, :]), writes=[bW])
        for k in range(4):
            P.op("pool", lambda e, k=k, wd=wd: e.tensor_tensor(out=wd[:, k, :], in0=wd[:, k, :], in1=bc[:, 3, :], op=ALU.mult),
                 reads=[bW, b_bc], writes=[bW])
        for tg in range(NTG):
            stage_a(ex, tg, it, wg, wu, bW)
            if pend is not None:
                stage_b(*pend)
            pend = (ex, tg, it, wd, bW)
            it += 1
    if pend is not None:
        stage_b(*pend)
    outs = []
    if final:
        gfin = bc[:, 0, :]
        b_gf = b_bc
        P.dma("sp", lambda e: e.dma_start(out=gfin, in_=g_fin_d[0:1, :].to_broadcast([128, D])), writes=[b_gf])
        sq, ss, rstd = nsc["sq"], nsc["ss"], nsc["rstd"]
        b_sq, b_ss, b_rstd = nsc["b"][0:3]
        for t in range(NT):
            P.op("act", lambda e, t=t: e.activation(out=sq[:], in_=x_sb[:, t, :], func=AF.Square, accum_out=ss[:]),
                 reads=[b_x[t]], writes=[b_sq, b_ss])
            P.op("dve", lambda e: e.tensor_scalar(out=rstd[:], in0=ss[:], scalar1=1.0 / D, scalar2=1e-6,
                                                  op0=ALU.mult, op1=ALU.add), reads=[b_ss], writes=[b_rstd])
            P.op("act", lambda e: e.activation(out=rstd[:], in_=rstd[:], func=AF.Sqrt), reads=[b_rstd], writes=[b_rstd])
            P.op("dve", lambda e: e.reciprocal(out=rstd[:], in_=rstd[:]), reads=[b_rstd], writes=[b_rstd])
            P.op("dve", lambda e, t=t: e.scalar_tensor_tensor(out=x_sb[:, t, :], in0=x_sb[:, t, :], scalar=rstd[:, 0:1],
                                                              in1=gfin, op0=ALU.mult, op1=ALU.mult),
                 reads=[b_x[t], b_rstd, b_gf], writes=[b_x[t]])
    for t in range(NT):
        outs.append(P.dma("sp", lambda e, t=t: e.dma_start(out=x_out[t * 128:(t + 1) * 128, :], in_=x_sb[:, t, :]),
                          reads=[b_x[t]]))
    P.wait_all("sp", outs)
    P.finish()
    return nc


def oh16_static():
    oh = np.zeros((16, 16, 128), np.float32)
    for e in range(16):
        oh[e, e, :] = 1.0
    return oh.reshape(16, 2048)


OD = dict(c_q=(0, 256), c_kv=(256, 384), k_rope=(384, 448), q_d=(448, 960), k_d=(960, 1088), v_d=(1088, 1216))
NU1 = 10


def host_w_in_odd(w, wq_up, wkv_up):
    sl = lambda n: w[:, OD[n][0]:OD[n][1]]
    units = []
    for h in range(8):
        u = np.zeros((1024, 128), np.float32)
        u[:, (h % 2) * 64:(h % 2 + 1) * 64] = sl("q_d")[:, h * 64:(h + 1) * 64]
        units.append(u)
    for kv in range(2):
        c = sl("k_d")[:, kv * 64:(kv + 1) * 64]
        units.append(np.concatenate([c, c], axis=1))
    WF = np.concatenate(units, axis=1)
    WT = np.concatenate([sl("c_q"), sl("c_kv"), sl("k_rope"), sl("v_d")], axis=1)
    wq = wq_up.reshape(256, 4, 192)
    wq_nope = np.ascontiguousarray(wq[:, :, 0:128].reshape(256, 512))
    wq_rope = np.ascontiguousarray(wq[:, :, 128:192].reshape(256, 256))
    wkv = wkv_up.reshape(128, 4, 256)
    wk_nope = np.ascontiguousarray(wkv[:, :, 0:128].reshape(128, 512))
    wv = np.ascontiguousarray(wkv[:, :, 128:256].reshape(128, 512))
    return dict(wf=np.ascontiguousarray(WF), wt=np.ascontiguousarray(WT), wq_nope=wq_nope, wq_rope=wq_rope,
                wk_nope=wk_nope, wv=wv)


def rope_static(positions):
    inv = (10000.0 ** (-np.arange(0, 64, 2, dtype=np.float32) / 64)).astype(np.float32)
    ang = positions.astype(np.float32)[:, None] * inv[None, :]
    return np.cos(ang).astype(np.float32), np.sin(ang).astype(np.float32)


def build_L1odd(NT=16):
    nc = bass.Bass("TRN2", target_bir_lowering=False)
    NTOK = NT * 128
    dt = lambda n, s, d, k: nc.dram_tensor(n, s, d, kind=k).ap()
    x = dt("x", [NTOK, D], F32, "ExternalInput")
    c_cols = dt("c_cols", [128, 8], F32, "ExternalInput")
    ada_w = dt("ada_w", [D, 6 * D], F32, "ExternalInput")
    ada_b = dt("ada_b", [1, 6 * D], F32, "ExternalInput")
    g_mix = dt("g_mix", [1, D], F32, "ExternalInput")
    wf_d = dt("wf", [D, NU1 * 128], F32, "ExternalInput")
    wt_d = dt("wt", [D, 576], F32, "ExternalInput")
    wqn_d = dt("wq_nope", [256, 512], F32, "ExternalInput")
    wqr_d = dt("wq_rope", [256, 256], F32, "ExternalInput")
    wkn_d = dt("wk_nope", [128, 512], F32, "ExternalInput")
    wv_d = dt("wv", [128, 512], F32, "ExternalInput")
    qn_g = dt("q_norm", [1, 256], F32, "ExternalInput")
    kvn_g = dt("kv_norm", [1, 128], F32, "ExternalInput")
    cos_d = dt("cos", [NTOK, 32], F32, "ExternalInput")
    sin_d = dt("sin", [NTOK, 32], F32, "ExternalInput")
    ident = dt("ident", [128, 128], F32, "ExternalInput")
    o_fm = dt("o_fm", [128, NU1, NTOK], BF16, "ExternalOutput")
    o_qn = dt("o_qn", [128, 4, NTOK], BF16, "ExternalOutput")
    o_qr = dt("o_qr", [64, 4, NTOK], BF16, "ExternalOutput")
    o_kn = dt("o_kn", [128, 4, NTOK], BF16, "ExternalOutput")
    o_kr = dt("o_kr", [64, NTOK], BF16, "ExternalOutput")
    o_vtok = dt("o_vtok", [NTOK, 640], BF16, "ExternalOutput")
    o_mod = dt("o_mod", [6, D], F32, "ExternalOutput")

    P = Prog(nc)
    C = Consts(P, nc, ident[:, :])
    ps_row = P.psum("ps_row", [128, 512], F32); b_ps_row = Buf()
    ps_bc = P.psum("ps_bc", [128, 512], F32); b_ps_bc = Buf()
    ps_tr = P.psum("ps_tr", [128, 8, 128], BF16); b_ps_tr = Buf()
    ps_mm = [P.psum(f"ps_mm{i}", [128, 512], F32) for i in range(4)]
    b_ps_mm = [Buf() for _ in range(4)]
    mod, b_mod = emit_adaln(P, nc, C, c_cols[:, :], ada_w, ada_b[:, :], "1", ps_row, b_ps_row, ps_bc, b_ps_bc)
    outs = []
    outs.append(P.dma("sp", lambda e: e.dma_start(out=o_mod[:, :], in_=mod[0:1, :, :]), reads=[b_mod]))
    gm = P.sbuf("gm", [128, 1024], F32); b_gm = Buf()
    A = P.sbuf("A_m", [128, 1024], F32); b_A = Buf()
    P.dma("sp", lambda e: e.dma_start(out=gm[:], in_=g_mix[0:1, :].to_broadcast([128, 1024])), writes=[b_gm])
    P.op("dve", lambda e: e.scalar_tensor_tensor(out=A[:], in0=mod[:, 1, :], scalar=1.0, in1=gm[:],
                                                 op0=ALU.add, op1=ALU.mult), reads=[b_mod, b_gm], writes=[b_A])
    Bt = mod[:, 0, :]
    wf, b_wf = load_w_bf16(P, nc, "wf_sb", wf_d, NU1 * 128)
    wt, b_wt = load_w_bf16(P, nc, "wt_sb", wt_d, 576)
    wqn, b_wqn = load_w_bf16(P, nc, "wqn_sb", wqn_d, 512, rows=256)
    wqr, b_wqr = load_w_bf16(P, nc, "wqr_sb", wqr_d, 256, rows=256)
    wkn, b_wkn = load_w_bf16(P, nc, "wkn_sb", wkn_d, 512, rows=128)
    wv, b_wv = load_w_bf16(P, nc, "wv_sb", wv_d, 512, rows=128)
    qng = P.sbuf("qng", [128, 256], F32); b_qng = Buf()
    kvng = P.sbuf("kvng", [128, 128], F32); b_kvng = Buf()
    P.dma("sp", lambda e: e.dma_start(out=qng[:], in_=qn_g[0:1, :].to_broadcast([128, 256])), writes=[b_qng])
    P.dma("sp", lambda e: e.dma_start(out=kvng[:], in_=kvn_g[0:1, :].to_broadcast([128, 128])), writes=[b_kvng])
    cs = P.sbuf("cs", [128, NT, 2, 32], F32); b_cs = Buf()
    P.dma("sp", lambda e: e.dma_start(out=cs[:, :, 0, :], in_=cos_d.rearrange("(t p) n -> p t n", p=128)), writes=[b_cs])
    P.dma("sp", lambda e: e.dma_start(out=cs[:, :, 1, :], in_=sin_d.rearrange("(t p) n -> p t n", p=128)), writes=[b_cs])
    xt = [P.sbuf(f"xt{i}", [128, 1024], F32) for i in range(2)]
    b_xt = [Buf(), Buf()]
    hT = [P.sbuf(f"hT{i}", [128, 8, 512], BF16) for i in range(2)]
    b_hT = [Buf(), Buf()]
    nsc = norm_scratch(P, "a")
    stg = [P.sbuf(f"stg{i}", [128, 512], BF16) for i in range(4)]
    b_stg = [Buf() for _ in range(4)]
    cq = P.sbuf("cq", [128, 384], F32); b_cq = Buf()
    cqn = P.sbuf("cqn", [128, 384], BF16); b_cqn = Buf()
    cT = P.sbuf("cT", [128, 3, 128], BF16); b_cT = Buf()
    mss = P.sbuf("mss", [128, 4], F32); b_mss = Buf()
    junk = P.sbuf("junk", [128, 256], F32); b_junk = Buf()
    rp = P.sbuf("rp", [128, 5, 64], F32); b_rp = Buf()
    rt = P.sbuf("rt", [128, 4, 5, 32], F32); b_rt = Buf()
    rpb = P.sbuf("rpb", [128, 5, 64], BF16); b_rpb = Buf()
    rT = P.sbuf("rT", [64, 5, 128], BF16); b_rT = Buf()
    rr = RR()
    si = 0
    mi = 0

    def next_ps():
        nonlocal mi
        r = (ps_mm[mi % 4], b_ps_mm[mi % 4]); mi += 1
        return r

    def next_stg():
        nonlocal si
        r = (stg[si % 4], b_stg[si % 4]); si += 1
        return r
    for tg in range(NT // 4):
        h = hT[tg % 2]; bh = b_hT[tg % 2]
        for tt in range(4):
            t = tg * 4 + tt
            xb = xt[t % 2]; bx = b_xt[t % 2]
            P.dma("sp", lambda e, xb=xb, t=t: e.dma_start(out=xb[:], in_=x[t * 128:(t + 1) * 128, :]), writes=[bx])
            emit_norm_tile(P, C, xb[:], bx, A[:], b_A, Bt, b_mod, h[:, :, tt * 128:(tt + 1) * 128], bh,
                           nsc, ps_tr, b_ps_tr)
        for u in range(NU1):
            ps, bps = next_ps()
            for k in range(8):
                P.op("pe", lambda e, ps=ps, u=u, k=k, h=h: e.matmul(out=ps[:], lhsT=wf[:, k, u * 128:(u + 1) * 128],
                                                                  rhs=h[:, k, :], start=(k == 0), stop=(k == 7)),
                     reads=[b_wf, bh], writes=[bps], inc=(k == 7))
            st, bst = next_stg()
            evac(P, rr.next(), st[:], ps[:], [bps], [bst])
            outs.append(P.dma("sp", lambda e, st=st, u=u, tg=tg: e.dma_start(
                out=o_fm[:, u, tg * 512:(tg + 1) * 512], in_=st[:]), reads=[bst]))
        for tt in range(4):
            t = tg * 4 + tt
            tsl = slice(tt * 128, (tt + 1) * 128)
            ps, bps = next_ps()
            for k in range(8):
                P.op("pe", lambda e, ps=ps, k=k, h=h, tsl=tsl: e.matmul(out=ps[:, 0:384], lhsT=h[:, k, tsl], rhs=wt[:, k, 0:384],
                                                                      start=(k == 0), stop=(k == 7)),
                     reads=[b_wt, bh], writes=[bps], inc=(k == 7))
            P.op("act", lambda e, ps=ps: e.copy(out=cq[:], in_=ps[:, 0:384]), reads=[bps], writes=[b_cq])
            psB, bpsB = next_ps()
            for k in range(8):
                P.op("pe", lambda e, psB=psB, k=k, h=h, tsl=tsl: e.matmul(out=psB[:, 0:192], lhsT=h[:, k, tsl], rhs=wt[:, k, 384:576],
                                                                        start=(k == 0), stop=(k == 7)),
                     reads=[b_wt, bh], writes=[bpsB], inc=(k == 7))
            st, bst = next_stg()
            P.op("act", lambda e, st=st, psB=psB: e.copy(out=st[:, 0:128], in_=psB[:, 64:192]), reads=[bpsB], writes=[bst])
            outs.append(P.dma("sp", lambda e, st=st, t=t: e.dma_start(out=o_vtok[t * 128:(t + 1) * 128, 512:640], in_=st[:, 0:128]),
                              reads=[bst]))
            P.op("act", lambda e, psB=psB: e.copy(out=rp[:, 4, :], in_=psB[:, 0:64]), reads=[bpsB], writes=[b_rp])
            P.op("act", lambda e: e.activation(out=junk[:, 0:256], in_=cq[:, 0:256], func=AF.Square, accum_out=mss[:, 0:1]),
                 reads=[b_cq], writes=[b_junk, b_mss])
            P.op("act", lambda e: e.activation(out=junk[:, 0:128], in_=cq[:, 256:384], func=AF.Square, accum_out=mss[:, 1:2]),
                 reads=[b_cq], writes=[b_junk, b_mss])
            P.op("dve", lambda e: e.tensor_scalar(out=mss[:, 2:3], in0=mss[:, 0:1], scalar1=1.0 / 256, scalar2=1e-6,
                                                  op0=ALU.mult, op1=ALU.add), reads=[b_mss], writes=[b_mss])
            P.op("dve", lambda e: e.tensor_scalar(out=mss[:, 3:4], in0=mss[:, 1:2], scalar1=1.0 / 128, scalar2=1e-6,
                                                  op0=ALU.mult, op1=ALU.add), reads=[b_mss], writes=[b_mss])
            P.op("act", lambda e: e.activation(out=mss[:, 2:4], in_=mss[:, 2:4], func=AF.Sqrt), reads=[b_mss], writes=[b_mss])
            P.op("dve", lambda e: e.reciprocal(out=mss[:, 2:4], in_=mss[:, 2:4]), reads=[b_mss], writes=[b_mss])
            P.op("dve", lambda e: e.scalar_tensor_tensor(out=cqn[:, 0:256], in0=cq[:, 0:256], scalar=mss[:, 2:3], in1=qng[:],
                                                         op0=ALU.mult, op1=ALU.mult), reads=[b_cq, b_mss, b_qng], writes=[b_cqn])
            P.op("dve", lambda e: e.scalar_tensor_tensor(out=cqn[:, 256:384], in0=cq[:, 256:384], scalar=mss[:, 3:4], in1=kvng[:],
                                                         op0=ALU.mult, op1=ALU.mult), reads=[b_cq, b_mss, b_kvng], writes=[b_cqn])
            for k in range(3):
                P.op("pe", lambda e, k=k: e.transpose(out=ps_tr[:, k, :], in_=cqn[:, k * 128:(k + 1) * 128], identity=C.idb[:]),
                     reads=[b_cqn, C.b_idb], writes=[b_ps_tr], inc=(k == 2))
            P.op("act", lambda e: e.copy(out=cT[:], in_=ps_tr[:, 0:3, :]), reads=[b_ps_tr], writes=[b_cT])
            ps, bps = next_ps()
            for hh in range(4):
                for k in range(2):
                    P.op("pe", lambda e, ps=ps, hh=hh, k=k: e.matmul(
                        out=ps[:, hh * 128:(hh + 1) * 128], lhsT=wqn[:, k, hh * 128:(hh + 1) * 128], rhs=cT[:, k, :],
                        start=(hh == 0 and k == 0), stop=(hh == 3 and k == 1), skip_group_check=True),
                        reads=[b_wqn, b_cT], writes=[bps], inc=(hh == 3 and k == 1))
            st, bst = next_stg()
            evac(P, rr.next(), st[:], ps[:], [bps], [bst])
            outs.append(P.dma("sp", lambda e, st=st, t=t: e.dma_start(
                out=o_qn[:, :, t * 128:(t + 1) * 128], in_=st[:].rearrange("p (h q) -> p h q", q=128)), reads=[bst]))
            ps, bps = next_ps()
            for hh in range(4):
                P.op("pe", lambda e, ps=ps, hh=hh: e.matmul(
                    out=ps[:, hh * 128:(hh + 1) * 128], lhsT=wkn[:, 0, hh * 128:(hh + 1) * 128], rhs=cT[:, 2, :],
                    start=(hh == 0), stop=(hh == 3), skip_group_check=True),
                    reads=[b_wkn, b_cT], writes=[bps], inc=(hh == 3))
            st, bst = next_stg()
            evac(P, rr.next(), st[:], ps[:], [bps], [bst])
            outs.append(P.dma("sp", lambda e, st=st, t=t: e.dma_start(
                out=o_kn[:, :, t * 128:(t + 1) * 128], in_=st[:].rearrange("p (h q) -> p h q", q=128)), reads=[bst]))
            ps, bps = next_ps()
            P.op("pe", lambda e, ps=ps: e.matmul(out=ps[:], lhsT=cT[:, 2, :], rhs=wv[:, 0, :], start=True, stop=True),
                 reads=[b_wv, b_cT], writes=[bps])
            st, bst = next_stg()
            evac(P, rr.next(), st[:], ps[:], [bps], [bst])
            outs.append(P.dma("sp", lambda e, st=st, t=t: e.dma_start(out=o_vtok[t * 128:(t + 1) * 128, 0:512], in_=st[:]),
                              reads=[bst]))
            ps, bps = next_ps()
            for k in range(2):
                P.op("pe", lambda e, ps=ps, k=k: e.matmul(out=ps[:, 0:256], lhsT=cT[:, k, :], rhs=wqr[:, k, :],
                                                          start=(k == 0), stop=(k == 1)),
                     reads=[b_wqr, b_cT], writes=[bps], inc=(k == 1))
            P.op("act", lambda e, ps=ps: e.copy(out=rp[:, 0:4, :], in_=ps[:, 0:256].rearrange("p (h d) -> p h d", d=64)),
                 reads=[bps], writes=[b_rp])
            cosb = cs[:, t, 0, :].unsqueeze(1).to_broadcast([128, 5, 32])
            sinb = cs[:, t, 1, :].unsqueeze(1).to_broadcast([128, 5, 32])
            x1 = rp[:, :, 0:32]; x2 = rp[:, :, 32:64]
            for i_, (a_, b_) in enumerate([(x1, cosb), (x2, sinb), (x1, sinb), (x2, cosb)]):
                P.op("dve", lambda e, i_=i_, a_=a_, b_=b_: e.tensor_tensor(out=rt[:, i_, :, :], in0=a_, in1=b_, op=ALU.mult),
                     reads=[b_rp, b_cs], writes=[b_rt])
            P.op("dve", lambda e: e.tensor_tensor(out=rpb[:, :, 0:32], in0=rt[:, 0, :, :], in1=rt[:, 1, :, :], op=ALU.subtract),
                 reads=[b_rt], writes=[b_rpb])
            P.op("dve", lambda e: e.tensor_tensor(out=rpb[:, :, 32:64], in0=rt[:, 2, :, :], in1=rt[:, 3, :, :], op=ALU.add),
                 reads=[b_rt], writes=[b_rpb])
            for v_ in range(5):
                P.op("pe", lambda e, v_=v_: e.transpose(out=ps_tr[0:64, v_, :], in_=rpb[:, v_, :], identity=C.idb[:]),
                     reads=[b_rpb, C.b_idb], writes=[b_ps_tr], inc=(v_ == 4))
            P.op("act", lambda e: e.copy(out=rT[:], in_=ps_tr[0:64, 0:5, :]), reads=[b_ps_tr], writes=[b_rT])
            outs.append(P.dma("sp", lambda e, t=t: e.dma_start(out=o_qr[:, :, t * 128:(t + 1) * 128], in_=rT[:, 0:4, :]),
                              reads=[b_rT]))
            outs.append(P.dma("sp", lambda e, t=t: e.dma_start(out=o_kr[:, t * 128:(t + 1) * 128], in_=rT[:, 4, :]),
                              reads=[b_rT]))
    P.wait_all("sp", outs)
    P.finish()
    return nc


def attn1_static(NT, STRIDE, j):
    S_ = STRIDE
    st = {}
    pp = np.arange(128)[:, None, None]
    r = np.arange(S_)[None, :, None]
    x = np.arange(128)[None, None, :]
    d = 128 * (r - (S_ - 1)) + 128 * j + x - (127 - pp)
    m = np.where(d >= 0, 0.0, NEGM).astype(np.float32)
    st["wm"] = np.ascontiguousarray(np.stack([m, m], axis=2)).astype(NPBF)
    L = 128 * S_ + 127 + 128
    y = np.arange(L)
    st["oh_swa"] = onehot_table(y - 127 - 128 * (S_ - 1) + 128 * j, "abs", win=128)
    st["cnt_swa"] = pad_counts(NT, STRIDE, j, 128)
    return st


def build_attn1(NT=16, STRIDE=4, NKT=64):
    nc = bass.Bass("TRN2", target_bir_lowering=False)
    NTOK = NT * 128
    NK = NKT * 128
    S_ = STRIDE
    LS = 128 * S_ + 127 + 128
    NAFF = n_aff(STRIDE, 128)
    dt = lambda n, s, d, k: nc.dram_tensor(n, s, d, kind=k).ap()
    qn_d = dt("qn", [128, 4, NTOK], BF16, "ExternalInput")
    qr_d = dt("qr", [64, 4, NTOK], BF16, "ExternalInput")
    qd_d = dt("qd", [128, 8, NTOK], BF16, "ExternalInput")
    kn_d = dt("kn", [128, 4, NK], BF16, "ExternalInput")
    kr_d = dt("kr", [64, NK], BF16, "ExternalInput")
    kd_d = dt("kd", [128, 2, NK], BF16, "ExternalInput")
    vtok = dt("vtok", [NK, 640], BF16, "ExternalInput")
    tab33_d = dt("tab33", [33, 8], F32, "ExternalInput")
    oh_swa = dt("oh_swa", [33, LS], F32, "ExternalInput")
    cnt_swa_d = dt("cnt_swa", [32, NAFF * 128], F32, "ExternalInput")
    sinks_d = dt("sinks", [1, 8], F32, "ExternalInput")
    wm_d = dt("wm", [128, S_, 2, 128], BF16, "ExternalInput")
    ident = dt("ident", [128, 128], F32, "ExternalInput")
    o_out = dt("o_attn", [NTOK, 1024], BF16, "ExternalOutput")
    cswa_scr = dt("cswa_scr", [8, LS], BF16, "Internal")

    P = Prog(nc)
    C = Consts(P, nc, ident[:, :])
    R = AttnRes(P, nS=3, nP=4)
    ps_misc = P.psum("ps_misc", [128, 512], F32); b_ps_misc = Buf()
    Ops = [(P.psum(f"O{i}", [128, 512], F32), Buf()) for i in range(2)]
    tab33 = P.sbuf("tab33", [33, 8], F32); b_tab33 = Buf()
    P.dma("sp", lambda e: e.dma_start(out=tab33[:], in_=tab33_d[:, :]), writes=[b_tab33])
    b_scr = build_ctab(P, nc, C, tab33, b_tab33, oh_swa, LS, "cswa", cswa_scr, ps_misc, b_ps_misc)
    exptab = P.sbuf("exptab", [32, 8], F32); b_exptab = Buf()
    P.op("act", lambda e: e.activation(out=exptab[:], in_=tab33[0:32, :], func=AF.Exp), reads=[b_tab33], writes=[b_exptab])
    cnts = P.sbuf("cnts", [32, NAFF * 128], F32); b_cnts = Buf()
    P.dma("sp", lambda e: e.dma_start(out=cnts[:], in_=cnt_swa_d[:, :]), writes=[b_cnts])
    zpad = P.sbuf("zpad", [128, NAFF, 8], F32); b_zpad = Buf()
    for a in range(NAFF):
        P.op("pe", lambda e, a=a: e.matmul(out=ps_misc[:, 0:8], lhsT=cnts[:, a * 128:(a + 1) * 128], rhs=exptab[:, :],
                                           start=True, stop=True), reads=[b_cnts, b_exptab], writes=[b_ps_misc])
        P.op("act", lambda e, a=a: e.copy(out=zpad[:, a, :], in_=ps_misc[:, 0:8]), reads=[b_ps_misc], writes=[b_zpad])
    esink = P.sbuf("esink", [128, 8], F32); b_esink = Buf()
    P.dma("sp", lambda e: e.dma_start(out=esink[:], in_=sinks_d[0:1, :].to_broadcast([128, 8])), writes=[b_esink])
    P.op("act", lambda e: e.activation(out=esink[:], in_=esink[:], func=AF.Exp), reads=[b_esink], writes=[b_esink])
    wm = P.sbuf("wm", [128, S_, 2, 128], BF16); b_wm = Buf()
    P.dma("sp", lambda e: e.dma_start(out=wm[:], in_=wm_d[:, :, :, :]), writes=[b_wm])
    Wswa = P.sbuf("Wswa", [128, S_ + 1, 4, 128], BF16); b_Wswa = Buf()
    QT = P.sbuf("QT", [128, 8, NTOK], BF16); b_QT = Buf()
    KV = P.sbuf("KV", [128, 33280], BF16); b_KV = Buf()
    KrT = P.sbuf("KrT", [64, NK], BF16); b_KrT = Buf()
    o_sb = P.sbuf("o_sb", [128, NT, 1024], BF16); b_osb = Buf()
    rz = P.sbuf("rz", [128, 4, 1], F32); b_rz = Buf()
    P.dma("sp", lambda e: e.dma_start(out=QT[:, 0:4, :], in_=qn_d[:, :, :]), writes=[b_QT])
    P.dma("sp", lambda e: e.dma_start(out=QT[0:64, 4:8, :], in_=qr_d[:, :, :]), writes=[b_QT])
    P.dma("sp", lambda e: e.dma_start(out=KrT[:], in_=kr_d[:, :]), writes=[b_KrT])
    KnT = KV[:, 0:2 * NK].rearrange("p (c k) -> p c k", c=2)
    Vm = KV[:, 2 * NK:2 * NK + NKT * 2 * 129].rearrange("p (k h d) -> p k h d", h=2, d=129)
    sc_mla = float(192 ** -0.5)
    for pp in range(2):
        for hh in range(2):
            P.dma("sp", lambda e, hh=hh, pp=pp: e.dma_start(out=KnT[:, hh, :], in_=kn_d[:, 2 * pp + hh, :]), writes=[b_KV])
        P.op("pool", lambda e: e.memset(Vm[:, :, :, 128:129], 1.0), writes=[b_KV])
        for hh in range(2):
            for k0 in range(0, NKT, 8):
                P.dma("sp", lambda e, hh=hh, pp=pp, k0=k0: e.dma_start(
                    out=Vm[:, k0:k0 + 8, hh, 0:128],
                    in_=vtok[k0 * 128:(k0 + 8) * 128, (2 * pp + hh) * 128:(2 * pp + hh + 1) * 128].rearrange(
                        "(kt p) d -> p kt d", p=128)), writes=[b_KV])
        for lt in range(NT):
            O, b_O = Ops[lt % 2]
            Ov = O[:].rearrange("p (h d) -> p h d", d=256)
            kts = list(range(0, min(S_ * lt + S_, NKT)))

            def qk_fn(kt, lt=lt, pp=pp):
                return [(hh * 128, (hh + 1) * 128,
                         [(KnT[:, hh, kt * 128:(kt + 1) * 128], QT[:, 2 * pp + hh, lt * 128:(lt + 1) * 128]),
                          (KrT[:, kt * 128:(kt + 1) * 128], QT[0:64, 4 + 2 * pp + hh, lt * 128:(lt + 1) * 128])],
                         [b_KV, b_QT, b_KrT]) for hh in range(2)]

            def extra_fn(kt, lt=lt):
                dl = S_ * lt - kt
                if dl <= 0:
                    return [(C.antib[:], wm[:, dl + S_ - 1, :, :].rearrange("p h q -> p (h q)"), [C.b_antib, b_wm])]
                return []
            attn_steps(P, R, kts, qk_fn, extra_fn, lambda kt, hh: (Vm[:, kt, hh, :], [b_KV]), Ov, b_O, sc_mla,
                       nv=129, nh=2)
            P.op("dve", lambda e, Ov=Ov: e.tensor_scalar(out=rz[:, 0:2, :], in0=Ov[:, :, 128:129], scalar1=1e-30, scalar2=None,
                                                         op0=ALU.max), reads=[b_O], writes=[b_rz])
            P.op("dve", lambda e: e.reciprocal(out=rz[:, 0:2, :], in_=rz[:, 0:2, :]), reads=[b_rz], writes=[b_rz])
            P.op("dve", lambda e, Ov=Ov, lt=lt, pp=pp: e.tensor_tensor(
                out=o_sb[:, lt, pp * 256:(pp + 1) * 256].rearrange("p (h d) -> p h d", d=128), in0=Ov[:, :, 0:128],
                in1=rz[:, 0:2, :].to_broadcast([128, 2, 128]), op=ALU.mult), reads=[b_O, b_rz], writes=[b_osb])
    P.dma("sp", lambda e: e.dma_start(out=QT[:], in_=qd_d[:, :, :]), writes=[b_QT])
    KdT = KV[:, 0:NK]
    Vd = KV[:, NK:NK + NKT * 65].rearrange("p (k d) -> p k d", d=65)
    for kv in range(2):
        P.dma("sp", lambda e, kv=kv: e.dma_start(out=KdT, in_=kd_d[:, kv, :]), writes=[b_KV])
        P.op("pool", lambda e: e.memset(Vd[:, :, 64:65], 1.0), writes=[b_KV])
        for k0 in range(0, NKT, 8):
            P.dma("sp", lambda e, kv=kv, k0=k0: e.dma_start(
                out=Vd[:, k0:k0 + 8, 0:64], in_=vtok[k0 * 128:(k0 + 8) * 128, 512 + kv * 64:512 + (kv + 1) * 64].rearrange(
                    "(kt p) d -> p kt d", p=128)), writes=[b_KV])
        toeplitz_load(P, Wswa, b_Wswa, cswa_scr, b_scr, LS, 4 * kv, S_ + 1)
        for lt in range(NT):
            O, b_O = Ops[lt % 2]
            Ov = O[:].rearrange("p (h d) -> p h d", d=128)
            kts = list(range(max(0, S_ * lt - 1), min(S_ * lt + S_, NKT)))

            def qk_fn(kt, lt=lt, kv=kv):
                return [(hh * 128, (hh + 1) * 128,
                         [(KdT[:, kt * 128:(kt + 1) * 128], QT[:, 4 * kv + hh, lt * 128:(lt + 1) * 128])],
                         [b_KV, b_QT]) for hh in range(4)]

            def extra_fn(kt, lt=lt):
                dl = S_ * lt - kt
                return [(C.antib[:], Wswa[:, dl + S_ - 1, :, :].rearrange("p h q -> p (h q)"), [C.b_antib, b_Wswa])]
            attn_steps(P, R, kts, qk_fn, extra_fn, lambda kt, hh: (Vd[:, kt, :], [b_KV]), Ov, b_O, 0.125)
            P.op("dve", lambda e, Ov=Ov, kv=kv: e.tensor_tensor(out=rz[:], in0=Ov[:, :, 64:65],
                                                                in1=esink[:, 4 * kv:4 * kv + 4].unsqueeze(2), op=ALU.add),
                 reads=[b_O, b_esink], writes=[b_rz])
            if lt < NAFF:
                P.op("dve", lambda e, lt=lt, kv=kv: e.tensor_tensor(out=rz[:], in0=rz[:],
                                                                    in1=zpad[:, lt, 4 * kv:4 * kv + 4].unsqueeze(2), op=ALU.add),
                     reads=[b_rz, b_zpad], writes=[b_rz])
            P.op("dve", lambda e: e.reciprocal(out=rz[:], in_=rz[:]), reads=[b_rz], writes=[b_rz])
            P.op("dve", lambda e, Ov=Ov, lt=lt, kv=kv: e.tensor_tensor(
                out=o_sb[:, lt, 512 + kv * 256:512 + (kv + 1) * 256].rearrange("p (h d) -> p h d", d=64),
                in0=Ov[:, :, 0:64], in1=rz[:].to_broadcast([128, 4, 64]), op=ALU.mult),
                reads=[b_O, b_rz], writes=[b_osb])
    outs = []
    for t in range(NT):
        outs.append(P.dma("sp", lambda e, t=t: e.dma_start(out=o_out[t * 128:(t + 1) * 128, :], in_=o_sb[:, t, :]),
                          reads=[b_osb]))
    P.wait_all("sp", outs)
    P.finish()
    return nc


from concourse.bass_utils import run_bass_kernel_spmd

_NT = 16
_STRIDE = 4
_PROGS = {}


def _prog(name, fn):
    if name not in _PROGS:
        _PROGS[name] = fn()
    return _PROGS[name]


def _own(a, j):
    sh = a.shape
    return np.ascontiguousarray(a.reshape((16, 4, 128) + sh[1:])[:, j].reshape((2048,) + sh[1:]))


def _gather_last(parts, blk=128):
    sh = parts[0].shape
    out = np.zeros(sh[:-1] + (64 * blk,), parts[0].dtype)
    o5 = out.reshape(sh[:-1] + (16, 4, blk))
    for r in range(4):
        o5[..., r, :] = parts[r].reshape(sh[:-1] + (16, blk))
    return out


def _gather_rows(parts):
    sh = parts[0].shape
    out = np.zeros((8192,) + sh[1:], parts[0].dtype)
    o5 = out.reshape((16, 4, 128) + sh[1:])
    for r in range(4):
        o5[:, r] = parts[r].reshape((16, 128) + sh[1:])
    return out


def _run(nc, ins):
    res = run_bass_kernel_spmd(nc, ins, core_ids=list(range(8)))
    return res.results


def kernel(x, c, rel_table, router_w, router_b, final_norm, norm_mix, norm_ffn, ada_w, ada_b,
           moe_w_gate, moe_w_up, moe_w_down, ev_w_in, ev_w_out, nsa_pos_k, nsa_pos_v,
           nsa_ck_w1, nsa_ck_w2, nsa_cv_w1, nsa_cv_w2, od_w_in, od_w_out, mla_q_norm,
           mla_kv_norm, mla_w_q_up, mla_w_kv_up, swa_sinks):
    f32 = lambda a: np.ascontiguousarray(np.asarray(a, dtype=np.float32))
    x = f32(x); c = f32(c)
    ident = np.eye(128, dtype=np.float32)
    tab33 = np.concatenate([f32(rel_table), np.ones((1, 8), np.float32)], 0)
    NT, ST = _NT, _STRIDE
    cores = [(cc // 4, cc % 4) for cc in range(8)]
    c_cols = [np.ascontiguousarray(c[b].reshape(8, 128).T) for b in range(2)]

    WF, WP, WT = host_w_in_even(f32(ev_w_in[0]))
    ins = [dict(x=_own(x[b], j), c_cols=c_cols[b], ada_w=f32(ada_w[0]), ada_b=f32(ada_b[0])[None],
                g_mix=f32(norm_mix[0])[None], wf=WF, wp=WP, wt=WT, ident=ident) for (b, j) in cores]
    r1 = _run(_prog("L1", lambda: build_L1(NT)), ins)
    posc = np.ascontiguousarray(np.stack([f32(nsa_pos_k[0]).reshape(16, 128).T, f32(nsa_pos_v[0]).reshape(16, 128).T], 1))
    cw1 = np.ascontiguousarray(np.stack([f32(nsa_ck_w1[0]), f32(nsa_cv_w1[0])], 0))
    cw2k = np.ascontiguousarray(np.concatenate([f32(nsa_ck_w2[0]), f32(nsa_ck_w2[0])], 1))
    G = {}
    for b in range(2):
        fm = [np.asarray(r1[4 * b + r]['o_fm']) for r in range(4)]
        G[b] = dict(kbT=_gather_last([f[:, 16:20] for f in fm]), ksT=_gather_last([f[:, 20:22] for f in fm]),
                    kwT=_gather_last([f[:, 22:24] for f in fm]),
                    kc2=_gather_last([np.asarray(r1[4 * b + r]['o_kc2']) for r in range(4)], blk=64),
                    vtok=_gather_rows([np.asarray(r1[4 * b + r]['o_vtok']) for r in range(4)]))
    ins = []
    for cc, (b, j) in enumerate(cores):
        st = attn0_static(NT, ST, j)
        ins.append(dict(qT=np.ascontiguousarray(np.asarray(r1[cc]['o_fm'])[:, 0:16]), gates=np.asarray(r1[cc]['o_gates']),
                        tab33=tab33, posc=posc, cw1=cw1, cw2k=cw2k, cw2v=f32(nsa_cv_w2[0]), ident=ident, **G[b], **st))
    ra = _run(_prog("A0", lambda: build_attn0(NT, ST, 64)), ins)
    oh16 = oh16_static()
    ins = [dict(o_attn=np.asarray(ra[cc]['o_attn']), x=_own(x[b], j), mod=np.asarray(r1[cc]['o_mod']),
                w_out=f32(ev_w_out[0]), g_ffn=f32(norm_ffn[0])[None], g_fin=f32(final_norm)[None],
                router_w=f32(router_w), router_b=f32(router_b)[None], wg=f32(moe_w_gate[0]), wu=f32(moe_w_up[0]),
                wd=f32(moe_w_down[0]), oh16=oh16, ident=ident) for cc, (b, j) in enumerate(cores)]
    rp0 = _run(_prog("P0", lambda: build_post(NT, final=False)), ins)
    hw = host_w_in_odd(f32(od_w_in[0]), f32(mla_w_q_up[0]), f32(mla_w_kv_up[0]))
    ins = []
    for cc, (b, j) in enumerate(cores):
        pos = (np.arange(64).reshape(16, 4)[:, j][:, None] * 128 + np.arange(128)[None, :]).reshape(-1)
        cos, sin = rope_static(pos)
        ins.append(dict(x=np.asarray(rp0[cc]['x_out']), c_cols=c_cols[b], ada_w=f32(ada_w[1]), ada_b=f32(ada_b[1])[None],
                        g_mix=f32(norm_mix[1])[None], q_norm=f32(mla_q_norm), kv_norm=f32(mla_kv_norm), cos=cos, sin=sin,
                        ident=ident, **hw))
    r2 = _run(_prog("L1o", lambda: build_L1odd(NT)), ins)
    G = {}
    for b in range(2):
        G[b] = dict(kn=_gather_last([np.asarray(r2[4 * b + r]['o_kn']) for r in range(4)]),
                    kr=_gather_last([np.asarray(r2[4 * b + r]['o_kr']) for r in range(4)]),
                    kd=_gather_last([np.asarray(r2[4 * b + r]['o_fm'])[:, 8:10] for r in range(4)]),
                    vtok=_gather_rows([np.asarray(r2[4 * b + r]['o_vtok']) for r in range(4)]))
    ins = []
    for cc, (b, j) in enumerate(cores):
        st = attn1_static(NT, ST, j)
        ins.append(dict(qn=np.asarray(r2[cc]['o_qn']), qr=np.asarray(r2[cc]['o_qr']),
                        qd=np.ascontiguousarray(np.asarray(r2[cc]['o_fm'])[:, 0:8]), tab33=tab33, sinks=f32(swa_sinks),
                        ident=ident, **G[b], **st))
    rb = _run(_prog("A1", lambda: build_attn1(NT, ST, 64)), ins)
    ins = [dict(o_attn=np.asarray(rb[cc]['o_attn']), x=np.asarray(rp0[cc]['x_out']), mod=np.asarray(r2[cc]['o_mod']),
                w_out=f32(od_w_out[0]), g_ffn=f32(norm_ffn[1])[None], g_fin=f32(final_norm)[None],
                router_w=f32(router_w), router_b=f32(router_b)[None], wg=f32(moe_w_gate[1]), wu=f32(moe_w_up[1]),
                wd=f32(moe_w_down[1]), oh16=oh16, ident=ident) for cc, (b, j) in enumerate(cores)]
    rp1 = _run(_prog("P1", lambda: build_post(NT, final=True)), ins)
    out = np.zeros((2, 8192, 1024), np.float32)
    o6 = out.reshape(2, 16, 4, 128, 1024)
    for cc, (b, j) in enumerate(cores):
        o6[b, :, j] = np.asarray(rp1[cc]['x_out']).reshape(16, 128, 1024)
    return out
```
